# Optimizing a Trainium2 kernel written in Bass

```python
import math
import jax
import jax.numpy as jnp
from jax import lax
import numpy as np

D_MODEL = 1024
BATCH = 8
SEQ = 4096
DEPTH = 4

GRID_W = 64
CTX_LEN = 256
EPS = 1e-6

DA_HEADS = 8
DA_HEAD_DIM = D_MODEL // (2 * DA_HEADS)
DA_V_DIM = 2 * DA_HEAD_DIM
DA_QK_W = DA_HEADS * 2 * DA_HEAD_DIM
DA_V_W = DA_HEADS * DA_V_DIM
Q_BLOCK = 128
ROPE_BASE = 10000.0

LRU_W = D_MODEL
LRU_BLOCKS = 8
LRU_BW = LRU_W // LRU_BLOCKS
LRU_CONV = 4
LRU_CONV_LEFT = 2
LRU_C = 8.0

HY_W = D_MODEL
HY_ORDER = 2
HY_SHORT = 3
HY_BANDS = 16
HY_EMB = 1 + 2 * HY_BANDS
HY_HID = 64
HY_DECAY_MIN = -math.log(1e-2) / 1.5
HY_DECAY_MAX = -math.log(1e-2) / 0.3

N_BRANCH = 3
BRANCH_W = D_MODEL

C_K = 0
C_V = C_K + DA_QK_W
C_LX = C_V + DA_V_W
C_Q = C_LX + LRU_W
C_LY = C_Q + DA_QK_W
C_HY = C_LY + LRU_W
C_G = C_HY + 3 * HY_W
C_END = C_G + N_BRANCH * D_MODEL

D_FF = 256 * ((8 * D_MODEL // 3 + 255) // 256)
N_EXPERTS = 8
TOP_K = 2
MOE_BLOCK = 256
N_DENSE = (DEPTH + 1) // 2
N_MOE = DEPTH // 2

kernel_name = 'hybrid_diffusion_trunk'


def rmsnorm(x, g):
    xf = x.astype(jnp.float32)
    y = xf * lax.rsqrt(jnp.mean(xf * xf, axis=-1, keepdims=True) + EPS)
    return (y * g.astype(jnp.float32)).astype(x.dtype)


def modulate(h, shift, scale):
    return h * (1.0 + scale) + shift


def dwconv(x, w, b, left):
    k, ch = w.shape
    y = lax.conv_general_dilated(x, w.astype(x.dtype)[:, None, :], window_strides=(1,),
                                 padding=[(left, k - 1 - left)],
                                 dimension_numbers=('NWC', 'WIO', 'NWC'), feature_group_count=ch)
    return y + b.astype(x.dtype)


def axial_rope(length):
    rows = length // GRID_W
    row = jnp.repeat(jnp.arange(rows, dtype=jnp.float32), GRID_W)
    col = jnp.tile(jnp.arange(GRID_W, dtype=jnp.float32), rows)
    n_freq = DA_HEAD_DIM // 4
    inv = ROPE_BASE ** (-jnp.arange(n_freq, dtype=jnp.float32) / n_freq)
    ar = row[:, None] * inv
    ac = col[:, None] * inv
    ang = jnp.concatenate([ar, ar, ac, ac], axis=-1)
    return jnp.cos(ang), jnp.sin(ang)


def apply_axial_rope(t, cos, sin):
    ts = t.reshape(t.shape[:-1] + (2, 2, DA_HEAD_DIM // 4))
    rot = jnp.stack([-ts[..., 1, :], ts[..., 0, :]], axis=-2).reshape(t.shape)
    return t * cos[None, :, None, None, :] + rot * sin[None, :, None, None, :]


def split_qk(t):
    return t.reshape(t.shape[:2] + (DA_HEADS, 2, DA_HEAD_DIM))


def split_v(t):
    return t.reshape(t.shape[:2] + (DA_HEADS, DA_V_DIM))


def diff_attn_core(q, k, v, lam):
    s = jnp.einsum('bqhcd,bkhcd->bhcqk', q, k, preferred_element_type=jnp.float32) * (DA_HEAD_DIM ** -0.5)
    p = jax.nn.softmax(s, axis=-1)
    w = p[:, :, 0] - lam * p[:, :, 1]
    return jnp.einsum('bhqk,bkhe->bqhe', w.astype(v.dtype), v)


def head_out(o, g, lam_init):
    o = rmsnorm(o, g) * (1.0 - lam_init)
    return o.reshape(o.shape[:2] + (DA_V_W,))


def diff_attention(p_l, p_c, cos, sin, lam_vecs, subln_g, lam_init, ctx_out):
    bsz, length = p_l.shape[:2]
    dt = p_l.dtype
    lv = lam_vecs.astype(jnp.float32)
    lam = jnp.exp(jnp.sum(lv[0] * lv[1])) - jnp.exp(jnp.sum(lv[2] * lv[3])) + lam_init
    q_l = apply_axial_rope(split_qk(p_l[..., C_Q:C_LY]), cos, sin).astype(dt)
    k_l = apply_axial_rope(split_qk(p_l[..., C_K:C_V]), cos, sin).astype(dt)
    k_c = split_qk(p_c[..., C_K:C_V])
    v_c = split_v(p_c[..., C_V:C_LX])
    k_all = jnp.concatenate([k_l, k_c], axis=1)
    v_all = jnp.concatenate([split_v(p_l[..., C_V:C_LX]), v_c], axis=1)
    nb = length // Q_BLOCK
    q_blocks = jnp.swapaxes(q_l.reshape((bsz, nb, Q_BLOCK) + q_l.shape[2:]), 0, 1)
    o_l = lax.map(lambda qb: diff_attn_core(qb, k_all, v_all, lam), q_blocks)
    o_l = jnp.swapaxes(o_l, 0, 1).reshape(bsz, length, DA_HEADS, DA_V_DIM)
    out_l = head_out(o_l, subln_g, lam_init)
    out_c = None
    if ctx_out:
        q_c = split_qk(p_c[..., C_Q:C_LY])
        out_c = head_out(diff_attn_core(q_c, k_c, v_c, lam), subln_g, lam_init)
    return out_l, out_c


def block_diag(x, w, b):
    xb = x.reshape(x.shape[:-1] + (LRU_BLOCKS, LRU_BW))
    return jnp.einsum('blni,nio->blno', xb, w).reshape(x.shape) + b


def linear_scan(a, b, h0, reverse):
    if reverse:
        b = b.at[:, -1].add(a[:, -1] * h0)
    else:
        b = b.at[:, 0].add(a[:, 0] * h0)

    def combine(e1, e2):
        a1, b1 = e1
        a2, b2 = e2
        return a1 * a2, a2 * b1 + b2

    _, h = lax.associative_scan(combine, (a, b), axis=1, reverse=reverse)
    return h


def rglru_scan(xc, wa, ba, wi, bi, lam, h0, reverse):
    r = jax.nn.sigmoid(block_diag(xc, wa, ba).astype(jnp.float32))
    i = jax.nn.sigmoid(block_diag(xc, wi, bi).astype(jnp.float32))
    log_a = -LRU_C * r * jax.nn.softplus(-lam.astype(jnp.float32))
    a = jnp.exp(log_a)
    b = jnp.sqrt(-jnp.expm1(2.0 * log_a)) * (i * xc.astype(jnp.float32))
    return linear_scan(a, b, h0, reverse)


def rglru_block(p_l, p_c, conv_w, conv_b, wa, ba, wi, bi, lam, ctx_out):
    xl = dwconv(p_l[..., C_LX:C_Q], conv_w, conv_b, LRU_CONV_LEFT)
    xcx = dwconv(p_c[..., C_LX:C_Q], conv_w, conv_b, LRU_CONV_LEFT)
    h0 = jnp.zeros((p_l.shape[0], LRU_W), jnp.float32)
    hc_f = rglru_scan(xcx, wa[0], ba[0], wi[0], bi[0], lam[0], h0, False)
    hc_b = rglru_scan(xcx, wa[1], ba[1], wi[1], bi[1], lam[1], h0, True)
    hl_f = rglru_scan(xl, wa[0], ba[0], wi[0], bi[0], lam[0], hc_f[:, -1], False)
    hl_b = rglru_scan(xl, wa[1], ba[1], wi[1], bi[1], lam[1], hc_b[:, 0], True)
    out_l = jax.nn.gelu(p_l[..., C_LY:C_HY]) * (hl_f + hl_b).astype(p_l.dtype)
    out_c = None
    if ctx_out:
        out_c = jax.nn.gelu(p_c[..., C_LY:C_HY]) * (hc_f + hc_b).astype(p_c.dtype)
    return out_l, out_c


def hyena_filters(length, w1, b1, w2, b2, freq, w3, decay):
    f32 = jnp.float32
    t = jnp.linspace(0.0, 1.0, length, dtype=f32)[:, None]
    w = 2.0 * math.pi * jnp.arange(length, dtype=f32)[:, None] / length
    bands = jnp.linspace(1e-4, HY_BANDS - 1, HY_BANDS, dtype=f32)
    z = jnp.concatenate([t, jnp.cos(bands * w), -jnp.sin(bands * w)], axis=-1)
    fr = freq.astype(f32)
    hdn = jnp.sin(fr * (z @ w1.astype(f32) + b1.astype(f32)))
    hdn = jnp.sin(fr * (hdn @ w2.astype(f32) + b2.astype(f32)))
    filt = (hdn @ w3.astype(f32)).reshape(length, HY_ORDER, 2, HY_W)
    window = jnp.exp(-t[:, :, None] * jnp.abs(decay.astype(f32)))
    filt = filt * window[:, :, None, :]
    return filt / (jnp.sum(jnp.abs(filt), axis=(0, 2), keepdims=True) + EPS)


def bidir_long_conv(u, h_fwd, h_bwd, skip):
    length = u.shape[1]
    hc = jnp.concatenate([h_fwd[:1] + h_bwd[:1], h_fwd[1:], jnp.zeros_like(h_fwd[:1]), h_bwd[:0:-1]], axis=0)
    y = jnp.fft.irfft(jnp.fft.rfft(u, n=2 * length, axis=1) * jnp.fft.rfft(hc, axis=0)[None],
                      n=2 * length, axis=1)[:, :length]
    return y + skip * u


def hyena_seq(p, conv_w, conv_b, w1, b1, w2, b2, freq, w3, decay, skip):
    u = dwconv(p, conv_w, conv_b, 1).astype(jnp.float32)
    v, x1, x2 = jnp.split(u, 3, axis=-1)
    filt = hyena_filters(u.shape[1], w1, b1, w2, b2, freq, w3, decay)
    sk = skip.astype(jnp.float32)
    z = x1 * bidir_long_conv(v, filt[:, 0, 0], filt[:, 0, 1], sk[0])
    return x2 * bidir_long_conv(z, filt[:, 1, 0], filt[:, 1, 1], sk[1])


def merge_branches(p, branches, b_gate, w_br, w_out):
    gates = jax.nn.sigmoid(p[..., C_G:C_END] + b_gate)
    m = None
    for k in range(N_BRANCH):
        term = gates[..., k * D_MODEL:(k + 1) * D_MODEL] * (branches[k].astype(p.dtype) @ w_br[k])
        m = term if m is None else m + term
    return m @ w_out


def swiglu(x, w_gate, w_up, w_down):
    return (jax.nn.silu(x @ w_gate) * (x @ w_up)) @ w_down


def moe_swiglu(x, w_router, w_gate, w_up, w_down):
    shp = x.shape
    xt = x.reshape(-1, shp[-1])
    n_tok = xt.shape[0]
    n_asg = n_tok * TOP_K
    logits = jnp.einsum('td,de->te', xt, w_router, preferred_element_type=jnp.float32)
    top_v, top_i = lax.top_k(logits, TOP_K)
    top_w = jax.nn.softmax(top_v, axis=-1)
    e_flat = top_i.reshape(-1)
    tok_flat = jnp.repeat(jnp.arange(n_tok, dtype=jnp.int32), TOP_K)
    w_flat = top_w.reshape(-1)
    order = jnp.argsort(e_flat)
    e_sorted = e_flat[order]
    counts = jnp.bincount(e_flat, length=N_EXPERTS)
    padded = ((counts + MOE_BLOCK - 1) // MOE_BLOCK) * MOE_BLOCK
    pad_end = jnp.cumsum(padded)
    pad_start = pad_end - padded
    start = jnp.cumsum(counts) - counts
    dest = pad_start[e_sorted] + jnp.arange(n_asg, dtype=jnp.int32) - start[e_sorted]
    n_blocks = -(-n_asg // MOE_BLOCK) + N_EXPERTS
    n_slots = n_blocks * MOE_BLOCK
    slot_tok = jnp.zeros((n_slots,), jnp.int32).at[dest].set(tok_flat[order])
    slot_w = jnp.zeros((n_slots,), jnp.float32).at[dest].set(w_flat[order])
    block_e = jnp.minimum(jnp.searchsorted(pad_end, jnp.arange(n_blocks) * MOE_BLOCK, side='right'), N_EXPERTS - 1)
    xs = xt[slot_tok].reshape(n_blocks, MOE_BLOCK, shp[-1])

    def expert_block(args):
        xb, e = args
        return (jax.nn.silu(xb @ w_gate[e]) * (xb @ w_up[e])) @ w_down[e]

    ys = lax.map(expert_block, (xs, block_e)).reshape(n_slots, shp[-1])
    out = jnp.zeros_like(xt).at[slot_tok].add(ys * slot_w[:, None].astype(ys.dtype))
    return out.reshape(shp)


def setup_inputs(seed: int = 0) -> dict:
    key = jax.random.key(seed)
    keys = iter(jax.random.split(key, 48))
    f32 = jnp.float32
    D = D_MODEL

    def nrm(shape, std):
        return jax.random.normal(next(keys), shape, f32) * std

    def gain(shape):
        return 1.0 + nrm(shape, 0.05)

    u = jax.random.uniform(next(keys), (DEPTH, 2, LRU_W), f32, 0.9, 0.999)
    a_base = u ** (1.0 / LRU_C)
    lru_lambda = jnp.log(a_base) - jnp.log1p(-a_base)
    hy_decay = jnp.broadcast_to(jnp.linspace(HY_DECAY_MIN, HY_DECAY_MAX, HY_W, dtype=f32),
                                (DEPTH, HY_ORDER, HY_W)) + nrm((DEPTH, HY_ORDER, HY_W), 0.1)
    return {
        'x': nrm((BATCH, SEQ, D), 1.0),
        'c': nrm((BATCH, D), 1.0),
        'ctx': nrm((BATCH, CTX_LEN, D), 1.0),
        'c_ctx': nrm((D,), 1.0),
        'w_mod': nrm((DEPTH, D, 6 * D), 0.5 * D ** -0.5),
        'b_mod': nrm((DEPTH, 6 * D), 0.02),
        'g_mix': gain((DEPTH, D)),
        'g_ffn': gain((DEPTH, D)),
        'w_in': nrm((DEPTH, D, C_END), D ** -0.5),
        'b_gate': nrm((DEPTH, N_BRANCH * D), 0.02),
        'w_br': nrm((DEPTH, N_BRANCH, BRANCH_W, D), BRANCH_W ** -0.5),
        'w_out': nrm((DEPTH, D, D), D ** -0.5),
        'da_lambda': nrm((DEPTH, 4, DA_HEAD_DIM), 0.1),
        'da_subln_g': gain((DEPTH, DA_V_DIM)),
        'lru_conv_w': nrm((DEPTH, LRU_CONV, LRU_W), LRU_CONV ** -0.5),
        'lru_conv_b': nrm((DEPTH, LRU_W), 0.02),
        'lru_wa': nrm((DEPTH, 2, LRU_BLOCKS, LRU_BW, LRU_BW), LRU_BW ** -0.5),
        'lru_ba': nrm((DEPTH, 2, LRU_W), 0.02),
        'lru_wi': nrm((DEPTH, 2, LRU_BLOCKS, LRU_BW, LRU_BW), LRU_BW ** -0.5),
        'lru_bi': nrm((DEPTH, 2, LRU_W), 0.02),
        'lru_lambda': lru_lambda,
        'hy_conv_w': nrm((DEPTH, HY_SHORT, 3 * HY_W), HY_SHORT ** -0.5),
        'hy_conv_b': nrm((DEPTH, 3 * HY_W), 0.02),
        'hy_f_w1': nrm((DEPTH, HY_EMB, HY_HID), HY_EMB ** -0.5),
        'hy_f_b1': nrm((DEPTH, HY_HID), 0.02),
        'hy_f_w2': nrm((DEPTH, HY_HID, HY_HID), HY_HID ** -0.5),
        'hy_f_b2': nrm((DEPTH, HY_HID), 0.02),
        'hy_f_freq': gain((DEPTH, HY_HID)),
        'hy_f_w3': nrm((DEPTH, HY_HID, HY_ORDER * 2 * HY_W), HY_HID ** -0.5),
        'hy_decay': hy_decay,
        'hy_skip': nrm((DEPTH, HY_ORDER, HY_W), 1.0),
        'ffn_w_gate': nrm((N_DENSE, D, D_FF), D ** -0.5),
        'ffn_w_up': nrm((N_DENSE, D, D_FF), D ** -0.5),
        'ffn_w_down': nrm((N_DENSE, D_FF, D), D_FF ** -0.5),
        'moe_router': nrm((N_MOE, D, N_EXPERTS), D ** -0.5),
        'moe_w_gate': nrm((N_MOE, N_EXPERTS, D, D_FF), D ** -0.5),
        'moe_w_up': nrm((N_MOE, N_EXPERTS, D, D_FF), D ** -0.5),
        'moe_w_down': nrm((N_MOE, N_EXPERTS, D_FF, D), D_FF ** -0.5),
        'g_final': gain((D,)),
    }


def reference(x, c, ctx, c_ctx, w_mod, b_mod, g_mix, g_ffn, w_in, b_gate, w_br, w_out,
              da_lambda, da_subln_g, lru_conv_w, lru_conv_b, lru_wa, lru_ba, lru_wi, lru_bi,
              lru_lambda, hy_conv_w, hy_conv_b, hy_f_w1, hy_f_b1, hy_f_w2, hy_f_b2, hy_f_freq,
              hy_f_w3, hy_decay, hy_skip, ffn_w_gate, ffn_w_up, ffn_w_down, moe_router,
              moe_w_gate, moe_w_up, moe_w_down, g_final):
    bsz, length, dm = x.shape
    n_ctx = ctx.shape[1]
    cos, sin = axial_rope(length)
    silu_c = jax.nn.silu(c)
    silu_cc = jax.nn.silu(c_ctx)
    xc = ctx
    for li in range(DEPTH):
        last = li == DEPTH - 1
        ctx_out = not last
        n_mod_c = 2 if last else 6
        lam_init = 0.8 - 0.6 * math.exp(-0.3 * li)
        mod_l = jnp.split((silu_c @ w_mod[li] + b_mod[li])[:, None, :], 6, axis=-1)
        mod_c = jnp.split(silu_cc @ w_mod[li][:, :n_mod_c * dm] + b_mod[li][:n_mod_c * dm], n_mod_c)

        h_l = modulate(rmsnorm(x, g_mix[li]), mod_l[0], mod_l[1])
        h_c = modulate(rmsnorm(xc, g_mix[li]), mod_c[0], mod_c[1])
        p_l = h_l @ w_in[li]
        p_c = h_c @ w_in[li][:, :(C_END if ctx_out else C_Q)]

        a_l, a_c = diff_attention(p_l, p_c, cos, sin, da_lambda[li], da_subln_g[li], lam_init, ctx_out)
        r_l, r_c = rglru_block(p_l, p_c, lru_conv_w[li], lru_conv_b[li], lru_wa[li], lru_ba[li],
                               lru_wi[li], lru_bi[li], lru_lambda[li], ctx_out)
        hy_args = (hy_conv_w[li], hy_conv_b[li], hy_f_w1[li], hy_f_b1[li], hy_f_w2[li], hy_f_b2[li],
                   hy_f_freq[li], hy_f_w3[li], hy_decay[li], hy_skip[li])
        y_l = hyena_seq(p_l[..., C_HY:C_G], *hy_args)
        x = x + mod_l[2] * merge_branches(p_l, (a_l, r_l, y_l), b_gate[li], w_br[li], w_out[li])
        if ctx_out:
            y_c = hyena_seq(p_c[..., C_HY:C_G], *hy_args)
            xc = xc + mod_c[2] * merge_branches(p_c, (a_c, r_c, y_c), b_gate[li], w_br[li], w_out[li])

        f_l = modulate(rmsnorm(x, g_ffn[li]), mod_l[3], mod_l[4])
        if ctx_out:
            f_c = modulate(rmsnorm(xc, g_ffn[li]), mod_c[3], mod_c[4])
            tokens = jnp.concatenate([f_c, f_l], axis=1)
        else:
            tokens = f_l
        if li % 2 == 0:
            j = li // 2
            y = swiglu(tokens, ffn_w_gate[j], ffn_w_up[j], ffn_w_down[j])
        else:
            j = li // 2
            y = moe_swiglu(tokens, moe_router[j], moe_w_gate[j], moe_w_up[j], moe_w_down[j])
        x = x + mod_l[5] * y[:, -length:]
        if ctx_out:
            xc = xc + mod_c[5] * y[:, :n_ctx]
    return rmsnorm(x, g_final)
```

```python
import math
from contextlib import ExitStack
import numpy as np
import concourse.bass as bass
import concourse.mybir as mybir
from concourse.bass_utils import run_bass_kernel_spmd

F32 = mybir.dt.float32
BF16 = mybir.dt.bfloat16
ALU = mybir.AluOpType
AF = mybir.ActivationFunctionType
AX = mybir.AxisListType

D = 1024
L = 4096
NCX = 256
T = L + NCX
KC = 8
DFF = 2816
FC = DFF // 128
NE = 8
C_K, C_V, C_LX, C_Q, C_LY, C_HY, C_G, C_END = 0, 1024, 2048, 3072, 4096, 5120, 8192, 11264
EPS = 1e-6
TT = [(0, 256)] + [(256 + 512 * i, 512) for i in range(8)]
MAGIC = 12582912.0
PI = math.pi

ENGS = ("pe", "act", "dve", "pool", "sp")
SAME_ENGINE_SYNC = True
NDMASEM = 6
NPESEM = 6
KEEP_WARM = True


class Reg:
    __slots__ = ("w", "r", "name", "multi")

    def __init__(self, name="", multi=False):
        self.w = {}
        self.r = {}
        self.name = name
        self.multi = multi


class Prog:
    def __init__(self, nc):
        self.nc = nc
        self.q = {e: [] for e in ENGS}
        self.sems = {}
        self.cnt = {}
        self.key_eng = {}
        self.keys = {}
        for e in ENGS:
            nk = NPESEM if e == "pe" else 1
            self.keys[e] = []
            for i in range(nk):
                k = e if i == 0 else "%s%d" % (e, i)
                self.sems[k] = nc.alloc_semaphore(name="s_" + k)
                self.cnt[k] = 0
                self.key_eng[k] = e
                self.keys[e].append(k)
        self.cur = {e: 0 for e in ENGS}
        self.dma_next = {}
        for e in ("sp", "act", "pool"):
            for i in range(NDMASEM):
                k = "d_%s%d" % (e, i)
                self.sems[k] = nc.alloc_semaphore(name=k)
                self.cnt[k] = 0
            self.dma_next[e] = 0
        self.seen = {e: {} for e in ENGS}
        self.n_ins = 0
        self.n_wait = 0

    def _need(self, eng, tok, waits):
        k, v = tok
        if self.key_eng.get(k) == eng and (eng == "pe" or not SAME_ENGINE_SYNC):
            return
        if self.seen[eng].get(k, 0) >= v:
            return
        waits[k] = max(waits.get(k, 0), v)

    def _deps(self, eng, reads, writes):
        waits = {}
        for r in reads:
            for k, v in r.w.items():
                self._need(eng, (k, v), waits)
        for w in writes:
            if not w.multi:
                for k, v in w.w.items():
                    self._need(eng, (k, v), waits)
            for k, v in w.r.items():
                self._need(eng, (k, v), waits)
        for k, v in waits.items():
            self.seen[eng][k] = v
        return waits

    def _commit(self, tok, reads, writes):
        k, v = tok
        for r in reads:
            r.r[k] = max(r.r.get(k, 0), v)
        for w in writes:
            if w.multi:
                w.w[k] = max(w.w.get(k, 0), v)
            else:
                w.w = {k: v}
            w.r = {}

    def op(self, eng, fn, reads=(), writes=()):
        waits = self._deps(eng, reads, writes)
        key = self.keys[eng][self.cur[eng]]
        self.cnt[key] += 1
        tok = (key, self.cnt[key])
        self.q[eng].append((waits, fn, key, 1))
        self._commit(tok, reads, writes)
        self.n_ins += 1
        self.n_wait += len(waits)
        return tok

    def dma(self, eng, fn, reads=(), writes=()):
        i = self.dma_next[eng]
        self.dma_next[eng] = (i + 1) % NDMASEM
        k = "d_%s%d" % (eng, i)
        waits = self._deps(eng, reads, writes)
        prev = self.cnt[k]
        if prev > 0 and self.seen[eng].get(k, 0) < prev:
            waits[k] = max(waits.get(k, 0), prev)
            self.seen[eng][k] = prev
        self.cnt[k] += 16
        tok = (k, self.cnt[k])
        self.q[eng].append((waits, fn, k, 16))
        self._commit(tok, reads, writes)
        self.n_ins += 1
        self.n_wait += len(waits)
        return tok

    def barrier(self):
        for e in ENGS:
            waits = {}
            for k, v in self.cnt.items():
                if v > 0 and self.seen[e].get(k, 0) < v:
                    waits[k] = v
                    self.seen[e][k] = v
            if waits:
                self.q[e].append((waits, None, None, 0))
        self.cur["pe"] = (self.cur["pe"] + 1) % len(self.keys["pe"])

    def finish(self):
        self.flush()

    def flush(self):
        self.barrier()
        sems = self.sems
        q = self.q
        self.q = {e: [] for e in ENGS}

        def emit(e, lst):
            for waits, fn, k, inc in lst:
                for wk, wv in waits.items():
                    e.wait_ge(sems[wk], wv)
                if fn is not None:
                    fn(e).then_inc(sems[k], inc)

        with self.nc.Block() as block:
            @block.tensor
            def _(e):
                emit(e, q["pe"])

            @block.scalar
            def _(e):
                emit(e, q["act"])

            @block.vector
            def _(e):
                emit(e, q["dve"])

            @block.gpsimd
            def _(e):
                emit(e, q["pool"])

            @block.sync
            def _(e):
                emit(e, q["sp"])


def bcast_rows(a, n):
    return bass.AP(a.tensor, a.offset, [[0, 128], [1, n]])


_uid = [0]


def uname(s):
    _uid[0] += 1
    return "%s_%d" % (s, _uid[0])


class Stage:
    def __init__(self, B):
        self.B = B
        self.es = ExitStack()

    def sb(self, name, shape, dt):
        t = self.es.enter_context(self.B.nc.sbuf_tensor(uname(name), list(shape), dt))
        return t, Reg(name)

    def ps(self, name, shape, dt=F32):
        t = self.es.enter_context(self.B.nc.psum_tensor(uname(name), list(shape), dt))
        return t, Reg(name)

    def pool(self, name, n, shape, dt, psum=False):
        return RPool([(self.ps if psum else self.sb)(name, shape, dt) for _ in range(n)])

    def close(self):
        self.B.P.barrier()
        self.es.close()

    def __enter__(self):
        return self

    def __exit__(self, *a):
        if a[0] is None:
            self.close()
        return False


class RPool:
    def __init__(self, tiles):
        self.tiles = tiles
        self.i = 0

    def get(self):
        t = self.tiles[self.i]
        self.i = (self.i + 1) % len(self.tiles)
        return t


def _cols(v):
    v = np.asarray(v, np.float32).reshape(-1, 128)
    return np.ascontiguousarray(v.T)


VP = {}
_o = 0
for _n, _w in [("b_mod", 48), ("g_mix", 8), ("g_ffn", 8), ("b_gate", 24), ("lru_cw", 32), ("lru_cb", 8),
               ("lru_ba", 16), ("lru_bi", 16), ("lru_lam", 16), ("hy_cw", 72), ("hy_cb", 24), ("subln", 1),
               ("c", 8), ("c_ctx", 8), ("g_final", 8), ("hy_b1", 1), ("hy_b2", 1), ("hy_freq", 1),
               ("bands", 1), ("phase", 1), ("ropeinv", 1), ("pidx", 1), ("ident", 128), ("rmat", 128)]:
    VP[_n] = (_o, _w)
    _o += _w
NV = _o


def make_vp(inp, li, b):
    vp = np.zeros((128, NV), np.float32)

    def put(n, a):
        o, w = VP[n]
        a = np.asarray(a, np.float32)
        assert a.shape[1] == w, (n, a.shape, w)
        vp[:a.shape[0], o:o + w] = a

    put("b_mod", _cols(inp["b_mod"][li]))
    put("g_mix", _cols(inp["g_mix"][li]))
    put("g_ffn", _cols(inp["g_ffn"][li]))
    put("b_gate", _cols(inp["b_gate"][li]))
    put("lru_cw", _cols(inp["lru_conv_w"][li].reshape(-1)))
    put("lru_cb", _cols(inp["lru_conv_b"][li]))
    put("lru_ba", _cols(inp["lru_ba"][li].reshape(-1)))
    put("lru_bi", _cols(inp["lru_bi"][li].reshape(-1)))
    put("lru_lam", _cols(inp["lru_lambda"][li].reshape(-1)))
    put("hy_cw", _cols(inp["hy_conv_w"][li].reshape(-1)))
    put("hy_cb", _cols(inp["hy_conv_b"][li]))
    put("subln", _cols(inp["da_subln_g"][li]))
    put("c", _cols(inp["c"][b]))
    put("c_ctx", _cols(inp["c_ctx"]))
    put("g_final", _cols(inp["g_final"]))
    put("hy_b1", inp["hy_f_b1"][li].reshape(64, 1))
    put("hy_b2", inp["hy_f_b2"][li].reshape(64, 1))
    put("hy_freq", inp["hy_f_freq"][li].reshape(64, 1))
    bands = np.linspace(1e-4, 15.0, 16, dtype=np.float32)
    bcol = np.zeros((33, 1), np.float32)
    bcol[1:17, 0] = bands
    bcol[17:33, 0] = bands
    pcol = np.zeros((33, 1), np.float32)
    pcol[1:17, 0] = np.float32(PI / 2)
    pcol[17:33, 0] = np.float32(PI)
    put("bands", bcol)
    put("phase", pcol)
    inv = (10000.0 ** (-(np.arange(128) % 16).astype(np.float32) / 16.0)).astype(np.float32)
    put("ropeinv", inv.reshape(128, 1))
    put("pidx", np.arange(128, dtype=np.float32).reshape(128, 1))
    put("ident", np.eye(128, dtype=np.float32))
    rm = np.zeros((128, 128), np.float32)
    for m in range(128):
        if m % 32 < 16:
            rm[m + 16, m] = -1.0
        else:
            rm[m - 16, m] = 1.0
    put("rmat", rm)
    return vp


class Builder:
    LAYER_INPUTS = [("vp", [128, NV]), ("w_mod", [D, 6 * D]), ("w_in", [D, C_END]), ("w_br", [3, D, D]), ("w_out", [D, D]),
                    ("lru_wa", [2, 8, 128, 128]), ("lru_wi", [2, 8, 128, 128]), ("hy_w1", [33, 64]), ("hy_w2", [64, 64]),
                    ("hy_w3", [64, 4096]), ("hy_decay", [1, 2048]), ("hy_skip", [1, 2048]), ("da_lam", [1, 256])]

    def __init__(self, layers=(0, 1, 2, 3), debug=False):
        self.layers = list(layers)
        self.debug = debug
        nc = bass.Bass("TRN2", target_bir_lowering=False)
        self.nc = nc
        self.P = Prog(nc)
        self.dr = {}
        self.drreg = {}
        self.dft_done = set()
        I = "ExternalInput"
        self.dram("x_ext", [128, KC, T], F32, I)
        for li in self.layers:
            sfx = "_L%d" % li
            for nm, shp in self.LAYER_INPUTS:
                self.dram(nm + sfx, shp, F32, I)
            ne = NE if li % 2 == 1 else 1
            self.dram("f_wg" + sfx, [ne, D, DFF], F32, I)
            self.dram("f_wu" + sfx, [ne, D, DFF], F32, I)
            self.dram("f_wd" + sfx, [ne, DFF, D], F32, I)
            if li % 2 == 1:
                self.dram("router" + sfx, [D, NE], F32, I)
        dk = "Internal"
        self.dram("pt", [88, 128, T], BF16, dk)
        self.dram("at", [8, 128, T], BF16, dk)
        self.dram("rt", [8, 128, T], BF16, dk)
        self.dram("yt", [8, 128, T], BF16, dk)
        dk2 = "ExternalOutput" if debug else "Internal"
        self.dram("xt_mid", [128, KC, T], F32, dk2)
        self.dram("xr0", [128, KC, T], F32, dk2)
        self.dram("xr1", [128, KC, T], F32, dk2)
        self.dram("y_out", [128, KC, L], F32, "ExternalOutput")

    def set_layer(self, idx):
        li = self.layers[idx]
        self.li = li
        self.moe = (li % 2 == 1)
        self.ne = NE if self.moe else 1
        self.lam_init = 0.8 - 0.6 * math.exp(-0.3 * li)
        sfx = "_L%d" % li
        names = [n for n, _ in self.LAYER_INPUTS] + ["f_wg", "f_wu", "f_wd"] + (["router"] if self.moe else [])
        for n in names:
            self.dr[n] = self.dr[n + sfx]
            self.drreg[n] = self.drreg[n + sfx]
        xin = "x_ext" if idx == 0 else "xr%d" % ((idx - 1) % 2)
        xout = "xr%d" % (idx % 2)
        for alias, real in (("xt", xin), ("xt_out", xout)):
            self.dr[alias] = self.dr[real]
            self.drreg[alias] = self.drreg[real]

    def dram(self, name, shape, dt, kind):
        if name in self.dr:
            return self.dr[name]
        self.dr[name] = self.nc.dram_tensor(name, list(shape), dt, kind=kind).ap()
        self.drreg[name] = Reg(name, multi=True)
        return self.dr[name]

    def load_consts(self, S):
        P, nc = self.P, self.nc
        self.vp, self.rvp = S.sb("vp", [128, NV], F32)
        vp, rvp = self.vp, self.rvp
        P.dma("sp", lambda e: e.dma_start(out=vp[:], in_=self.dr["vp"]), reads=[self.drreg["vp"]], writes=[rvp])
        self.ones_bf, self.rones = S.sb("ones", [128, 128], BF16)
        P.op("pool", lambda e: e.memset(self.ones_bf[:], 1.0), writes=[self.rones])
        self.epsc, self.repsc = S.sb("epsc", [128, 4], F32)
        P.op("pool", lambda e: e.memset(self.epsc[:, 0:1], EPS), writes=[self.repsc])
        P.op("pool", lambda e: e.memset(self.epsc[:, 1:2], 1.0), writes=[self.repsc])
        P.op("pool", lambda e: e.memset(self.epsc[:, 2:3], 0.0), writes=[self.repsc])
        self.ident_bf, self.rident_bf = S.sb("identbf", [128, 128], BF16)
        o = VP["ident"][0]
        P.op("dve", lambda e: e.tensor_copy(out=self.ident_bf[:], in_=vp[:, o:o + 128]), reads=[rvp], writes=[self.rident_bf])
        self.rmat_bf, self.rrmat = S.sb("rmatbf", [128, 128], BF16)
        o2 = VP["rmat"][0]
        P.op("dve", lambda e: e.tensor_copy(out=self.rmat_bf[:], in_=vp[:, o2:o2 + 128]), reads=[rvp], writes=[self.rrmat])

    def vcol(self, name, j=0, w=1):
        o = VP[name][0]
        return self.vp[:, o + j:o + j + w]

    def mod_vectors(self, S):
        P = self.P
        self.modv, self.rmodv = S.sb("modv", [128, 48, 2], F32)
        self.gsm, self.rgsm = S.sb("gsm", [128, 8, 2], F32)
        self.gsf, self.rgsf = S.sb("gsf", [128, 8, 2], F32)
        modv, rmodv = self.modv, self.rmodv
        with Stage(self) as S2:
            cs, rcs = S2.sb("cs", [128, 8, 2], F32)
            oc_, occ = VP["c"][0], VP["c_ctx"][0]
            vp = self.vp
            P.op("act", lambda e: e.activation(out=cs[:, :, 0], in_=vp[:, oc_:oc_ + 8], func=AF.Silu), reads=[self.rvp], writes=[rcs])
            P.op("act", lambda e: e.activation(out=cs[:, :, 1], in_=vp[:, occ:occ + 8], func=AF.Silu), reads=[self.rvp], writes=[rcs])
            wpool = S2.pool("wmod", 2, [128, 8, 768], F32)
            pspool = S2.pool("psmod", 2, [128, 2], F32, psum=True)
            wsrc = self.dr["w_mod"].rearrange("(k p) n -> p k n", p=128)
            ob = VP["b_mod"][0]
            for og in range(8):
                wt, rwt = wpool.get()
                P.dma("sp", (lambda wt=wt, og=og: lambda e: e.dma_start(out=wt[:], in_=wsrc[:, :, og * 768:(og + 1) * 768]))(),
                      reads=[self.drreg["w_mod"]], writes=[rwt])
                for oc in range(6):
                    ps, rps = pspool.get()
                    for k in range(8):
                        P.op("pe", (lambda ps=ps, wt=wt, k=k, oc=oc: lambda e: e.matmul(ps[:], lhsT=wt[:, k, oc * 128:(oc + 1) * 128], rhs=cs[:, k, :], start=(k == 0), stop=(k == 7)))(),
                             reads=[rwt, rcs], writes=[rps])
                    j = og * 6 + oc
                    P.op("dve", (lambda ps=ps, j=j: lambda e: e.tensor_scalar(out=modv[:, j, :], in0=ps[:], scalar1=vp[:, ob + j:ob + j + 1], scalar2=None, op0=ALU.add))(),
                         reads=[rps, self.rvp], writes=[rmodv])
            for (gs, rgs, gname, j0) in ((self.gsm, self.rgsm, "g_mix", 8), (self.gsf, self.rgsf, "g_ffn", 32)):
                og_ = VP[gname][0]
                for w in range(2):
                    P.op("dve", (lambda gs=gs, j0=j0, w=w: lambda e: e.tensor_scalar(out=gs[:, :, w], in0=modv[:, j0:j0 + 8, w], scalar1=1.0, scalar2=None, op0=ALU.add))(),
                         reads=[rmodv], writes=[rgs])
                    P.op("dve", (lambda gs=gs, og_=og_, w=w: lambda e: e.tensor_tensor(out=gs[:, :, w], in0=gs[:, :, w], in1=vp[:, og_:og_ + 8], op=ALU.mult))(),
                         reads=[rgs, self.rvp], writes=[rgs])

    def norm_mod(self, S, src, tiles, gs, rgs, shift_j, out, rout, col0, shift=None, router=None):
        P = self.P
        xin_p = S.pool("nm_x", 1, [128, 8, 512], F32)
        sq_p = S.pool("nm_sq", 1, [128, 8, 512], BF16)
        rs_p = S.pool("nm_rs", 2, [128, 512], F32)
        tmp_p = S.pool("nm_t", 2, [128, 512], F32)
        ps_p = S.pool("nm_ps", 2, [128, 512], F32, psum=True)
        modv, rmodv = (self.modv, self.rmodv) if shift is None else shift
        if router is not None:
            f32_all = S.sb("nm_f32", [128, 8, 512], F32)
            f32, rf32 = f32_all
        for (n0, nsz) in tiles:
            w = 1 if n0 < NCX else 0
            xin, rxin = xin_p.get()
            P.dma("sp", (lambda xin=xin, n0=n0, nsz=nsz: lambda e: e.dma_start(out=xin[:, :, :nsz], in_=self.dr[src][:, :, n0:n0 + nsz]))(),
                  reads=[self.drreg[src]], writes=[rxin])
            sq, rsq = sq_p.get()
            P.op("act", (lambda sq=sq, xin=xin, nsz=nsz: lambda e: e.activation(out=sq[:, :, :nsz], in_=xin[:, :, :nsz], func=AF.Square))(),
                 reads=[rxin], writes=[rsq])
            ps, rps = ps_p.get()
            for k in range(8):
                P.op("pe", (lambda ps=ps, sq=sq, k=k, nsz=nsz: lambda e: e.matmul(ps[:, :nsz], lhsT=self.ones_bf[:], rhs=sq[:, k, :nsz], start=(k == 0), stop=(k == 7)))(),
                     reads=[rsq, self.rones], writes=[rps])
            rs, rrs = rs_p.get()
            P.op("act", (lambda rs=rs, ps=ps, nsz=nsz: lambda e: e.activation(out=rs[:, :nsz], in_=ps[:, :nsz], func=AF.Sqrt, bias=self.epsc[:, 0:1], scale=1.0 / D))(),
                 reads=[rps, self.repsc], writes=[rrs])
            P.op("dve", (lambda rs=rs, nsz=nsz: lambda e: e.reciprocal(out=rs[:, :nsz], in_=rs[:, :nsz]))(), reads=[rrs], writes=[rrs])
            for k in range(8):
                tmp, rtmp = tmp_p.get()
                P.op("dve", (lambda tmp=tmp, xin=xin, rs=rs, k=k, nsz=nsz: lambda e: e.tensor_tensor(out=tmp[:, :nsz], in0=xin[:, k, :nsz], in1=rs[:, :nsz], op=ALU.mult))(),
                     reads=[rxin, rrs], writes=[rtmp])
                P.op("act", (lambda tmp=tmp, k=k, nsz=nsz, n0=n0, w=w: lambda e: e.activation(out=out[:, k, n0 - col0:n0 - col0 + nsz], in_=tmp[:, :nsz], func=AF.Identity,
                                                                                         bias=modv[:, shift_j * 8 + k, w:w + 1], scale=gs[:, k, w:w + 1]))(),
                     reads=[rtmp, rmodv, rgs], writes=[rout])
                if router is not None:
                    f32, rf32 = f32_all
                    P.op("act", (lambda tmp=tmp, k=k, nsz=nsz, w=w: lambda e: e.activation(out=f32[:, k, :nsz], in_=tmp[:, :nsz], func=AF.Identity,
                                                                                bias=modv[:, shift_j * 8 + k, w:w + 1], scale=gs[:, k, w:w + 1]))(),
                         reads=[rtmp, rmodv, rgs], writes=[rf32])
            if router is not None:
                wr, rwr, psr, rpsr = router
                for sub in range(nsz // 128):
                    for k in range(8):
                        P.op("pe", (lambda sub=sub, k=k: lambda e: e.matmul(psr[:, sub, :], lhsT=f32[:, k, sub * 128:(sub + 1) * 128], rhs=wr[:, k, :], start=(k == 0), stop=(k == 7)))(),
                             reads=[rf32, rwr], writes=[rpsr])
            if router is not None:
                router_done = getattr(self, "_router_done")
                router_done(n0, nsz)

    def gemm(self, S, wsrc, rw, n_k, m_total, xT, rxT, tiles, col0, evac, mg=512, skip_chunks=()):
        P = self.P
        wp = S.pool("g_w", 2, [128, n_k, mg], BF16)
        psp = S.pool("g_ps", 4, [128, 512], F32, psum=True)
        for g0 in range(0, m_total, mg):
            gsz = min(mg, m_total - g0)
            chunks = [c for c in range(g0 // 128, (g0 + gsz) // 128) if c not in skip_chunks]
            if not chunks:
                continue
            wt, rwt = wp.get()
            P.dma("pool", (lambda wt=wt, g0=g0, gsz=gsz: lambda e: e.dma_start(out=wt[:, :, :gsz], in_=wsrc[:, :, g0:g0 + gsz]))(),
                  reads=[rw], writes=[rwt])
            for ti, (n0, nsz) in enumerate(tiles):
                for mc in chunks:
                    ps, rps = psp.get()
                    mo = mc * 128 - g0
                    for k in range(n_k):
                        P.op("pe", (lambda ps=ps, wt=wt, k=k, mo=mo, n0=n0, nsz=nsz: lambda e: e.matmul(ps[:, :nsz], lhsT=wt[:, k, mo:mo + 128], rhs=xT[:, k, n0 - col0:n0 - col0 + nsz], start=(k == 0), stop=(k == n_k - 1)))(),
                             reads=[rwt, rxT], writes=[rps])
                    evac(mc, ti, n0, nsz, ps, rps)

    def stage_inproj(self):
        P = self.P
        with Stage(self) as S:
            hT, rhT = S.sb("hT", [128, 8, T], BF16)
            with Stage(self) as S2:
                self.norm_mod(S2, "xt", TT, self.gsm, self.rgsm, 0, hT, rhT, 0)
            op_ = S.pool("ip_o", 4, [128, 512], BF16)
            cnt = [0]

            def evac(mc, ti, n0, nsz, ps, rps):
                ot, rot = op_.get()
                if cnt[0] % 2 == 0:
                    P.op("act", lambda e: e.copy(out=ot[:, :nsz], in_=ps[:, :nsz]), reads=[rps], writes=[rot])
                else:
                    P.op("dve", lambda e: e.tensor_copy(out=ot[:, :nsz], in_=ps[:, :nsz]), reads=[rps], writes=[rot])
                cnt[0] += 1
                P.dma("sp", lambda e: e.dma_start(out=self.dr["pt"][mc, :, n0:n0 + nsz], in_=ot[:, :nsz]), reads=[rot], writes=[self.drreg["pt"]])

            wsrc = self.dr["w_in"].rearrange("(k p) n -> p k n", p=128)
            self.gemm(S, wsrc, self.drreg["w_in"], 8, C_END, hT, rhT, TT, 0, evac)

    def range_sin(self, S, out, rout, src, rsrc, shape, shift=0.0, whole=True):
        P = self.P
        t1, rt1 = S.sb("rs_t1", shape, F32)
        t2, rt2 = S.sb("rs_t2", shape, F32)
        inv2pi = 1.0 / (2 * PI)
        if shift != 0.0:
            t0, rt0 = S.sb("rs_t0", shape, F32)
            P.op("dve", (lambda s0=src: lambda e: e.tensor_scalar(out=t0[:], in0=s0[:], scalar1=shift, scalar2=None, op0=ALU.add))(), reads=[rsrc], writes=[rt0])
            src, rsrc = t0, rt0
        P.op("dve", lambda e: e.tensor_scalar(out=t1[:], in0=src[:], scalar1=inv2pi, scalar2=MAGIC, op0=ALU.mult, op1=ALU.add), reads=[rsrc], writes=[rt1])
        P.op("dve", lambda e: e.tensor_scalar(out=t1[:], in0=t1[:], scalar1=-MAGIC, scalar2=None, op0=ALU.add), reads=[rt1], writes=[rt1])
        P.op("dve", lambda e: e.scalar_tensor_tensor(out=t2[:], in0=t1[:], scalar=-2 * PI, in1=src[:], op0=ALU.mult, op1=ALU.add), reads=[rt1, rsrc], writes=[rt2])
        P.op("dve", lambda e: e.tensor_scalar(out=t2[:], in0=t2[:], scalar1=PI, scalar2=-PI, op0=ALU.min, op1=ALU.max), reads=[rt2], writes=[rt2])
        P.op("act", lambda e: e.activation(out=out[:], in_=t2[:], func=AF.Sin), reads=[rt2], writes=[rout])

    def rope_tables(self, S):
        P = self.P
        self.cosT, self.rcosT = S.sb("cosT", [128, L], F32)
        self.sinT, self.rsinT = S.sb("sinT", [128, L], F32)
        with Stage(self) as S2:
            pos, rpos = S2.sb("pos", [128, L], F32)
            for p0 in (0, 32, 64, 96):
                pat = [[1, 64], [0, 64]] if (p0 % 64) == 0 else [[0, 64], [1, 64]]
                P.op("pool", (lambda p0=p0, pat=pat: lambda e: e.iota(pos[p0:p0 + 32, :].rearrange("p (a b) -> p a b", a=64), pattern=pat, base=0, channel_multiplier=0, allow_small_or_imprecise_dtypes=True))(), writes=[rpos])
            P.op("dve", lambda e: e.tensor_scalar(out=pos[:], in0=pos[:], scalar1=self.vcol("ropeinv"), scalar2=None, op0=ALU.mult), reads=[rpos, self.rvp], writes=[rpos])
            self.range_sin(S2, self.sinT, self.rsinT, pos, rpos, [128, L], 0.0)
            self.range_sin(S2, self.cosT, self.rcosT, pos, rpos, [128, L], PI / 2)

    def stage_attn(self):
        P = self.P
        pt, rpt = self.dr["pt"], self.drreg["pt"]
        with Stage(self) as S:
            neglam, rneglam = S.sb("neglam", [128, 1], F32)
            gsub, rgsub = S.sb("gsub", [128, 1], F32)
            lamrow, rlamrow = S.sb("lamrow", [1, 256], F32)
            lamw, rlamw = S.sb("lamw", [1, 8], F32)
            onesf, ronesf = S.sb("onesf", [1, 128], F32)
            ps_m, rps_m = S.ps("ps_m", [128, 512], F32)
            P.op("pool", lambda e: e.memset(onesf[:], 1.0), writes=[ronesf])
            P.dma("sp", lambda e: e.dma_start(out=lamrow[:], in_=self.dr["da_lam"]), reads=[self.drreg["da_lam"]], writes=[rlamrow])
            P.op("dve", lambda e: e.tensor_tensor(out=lamrow[:, 0:64], in0=lamrow[:, 0:64], in1=lamrow[:, 64:128], op=ALU.mult), reads=[rlamrow], writes=[rlamrow])
            P.op("dve", lambda e: e.tensor_tensor(out=lamrow[:, 128:192], in0=lamrow[:, 128:192], in1=lamrow[:, 192:256], op=ALU.mult), reads=[rlamrow], writes=[rlamrow])
            P.op("dve", lambda e: e.reduce_sum(out=lamw[:, 0:1], in_=lamrow[:, 0:64], axis=AX.X), reads=[rlamrow], writes=[rlamw])
            P.op("dve", lambda e: e.reduce_sum(out=lamw[:, 1:2], in_=lamrow[:, 128:192], axis=AX.X), reads=[rlamrow], writes=[rlamw])
            P.op("act", lambda e: e.activation(out=lamw[:, 2:4], in_=lamw[:, 0:2], func=AF.Exp), reads=[rlamw], writes=[rlamw])
            P.op("dve", lambda e: e.tensor_tensor(out=lamw[:, 4:5], in0=lamw[:, 3:4], in1=lamw[:, 2:3], op=ALU.subtract), reads=[rlamw], writes=[rlamw])
            P.op("dve", lambda e: e.tensor_scalar(out=lamw[:, 5:6], in0=lamw[:, 4:5], scalar1=-self.lam_init, scalar2=None, op0=ALU.add), reads=[rlamw], writes=[rlamw])
            P.op("pe", lambda e: e.matmul(ps_m[:, 0:1], lhsT=onesf[:], rhs=lamw[:, 5:6], start=True, stop=True), reads=[ronesf, rlamw], writes=[rps_m])
            P.op("dve", lambda e: e.tensor_copy(out=neglam[:], in_=ps_m[:, 0:1]), reads=[rps_m], writes=[rneglam])
            P.op("dve", lambda e: e.tensor_scalar(out=gsub[:], in0=self.vcol("subln"), scalar1=1.0 - self.lam_init, scalar2=None, op0=ALU.mult), reads=[self.rvp], writes=[rgsub])

            KT, rKT = S.sb("KT", [128, T], BF16)
            QT, rQT = S.sb("QT", [128, T], BF16)
            VT, rVT = S.sb("VT", [128, T], BF16)
            KR, rKR = S.sb("KR", [128, T], BF16)
            QR, rQR = S.sb("QR", [128, T], BF16)
            Vh, rVh = S.sb("Vh", [128, 34, 128], BF16)
            ps_tr, rps_tr = S.ps("ps_tr", [128, 512], BF16)
            ps_s = S.pool("ps_s", 3, [128, 512], F32, psum=True)
            ps_dum, rps_dum = S.ps("ps_dum", [128, 512], F32)
            ps_o = S.pool("ps_o", 1, [128, 512], F32, psum=True)
            ps_r = S.pool("ps_r", 1, [128, 512], F32, psum=True)
            ptp = S.pool("ptile", 5, [128, 512], BF16)
            t1p = S.pool("at1", 2, [128, 512], F32)
            t2p = S.pool("at2", 2, [128, 512], F32)
            onp = S.pool("on", 4, [128, 512], F32)
            rcp = S.pool("rc", 2, [128, 512], F32)
            o_p = S.pool("o", 2, [128, 512], F32)
            sqp = S.pool("asq", 2, [128, 512], BF16)
            aop = S.pool("aout", 2, [128, 512], BF16)
            for h in range(8):
                P.dma("sp", (lambda h=h: lambda e: e.dma_start(out=KT[:], in_=pt[C_K // 128 + h]))(), reads=[rpt], writes=[rKT])
                P.dma("sp", (lambda h=h: lambda e: e.dma_start(out=QT[:], in_=pt[C_Q // 128 + h]))(), reads=[rpt], writes=[rQT])
                P.dma("sp", (lambda h=h: lambda e: e.dma_start(out=VT[:], in_=pt[C_V // 128 + h]))(), reads=[rpt], writes=[rVT])
                P.op("pool", lambda e: e.tensor_copy(out=KR[:, 0:NCX], in_=KT[:, 0:NCX]), reads=[rKT], writes=[rKR])
                P.op("pool", lambda e: e.tensor_copy(out=QR[:, 0:NCX], in_=QT[:, 0:NCX]), reads=[rQT], writes=[rQR])
                for (src, rsrc, dst, rdst) in ((KT, rKT, KR, rKR), (QT, rQT, QR, rQR)):
                    for (n0, nsz) in TT[1:]:
                        P.op("pe", (lambda src=src, n0=n0: lambda e: e.matmul(ps_m[:], lhsT=self.rmat_bf[:], rhs=src[:, n0:n0 + 512], start=True, stop=True))(), reads=[self.rrmat, rsrc], writes=[rps_m])
                        t1, rt1 = t1p.get()
                        t2, rt2 = t2p.get()
                        P.op("dve", (lambda t1=t1, src=src, n0=n0: lambda e: e.tensor_tensor(out=t1[:], in0=src[:, n0:n0 + 512], in1=self.cosT[:, n0 - NCX:n0 - NCX + 512], op=ALU.mult))(), reads=[rsrc, self.rcosT], writes=[rt1])
                        P.op("dve", (lambda t2=t2, n0=n0: lambda e: e.tensor_tensor(out=t2[:], in0=ps_m[:], in1=self.sinT[:, n0 - NCX:n0 - NCX + 512], op=ALU.mult))(), reads=[rps_m, self.rsinT], writes=[rt2])
                        P.op("dve", (lambda t1=t1, t2=t2, dst=dst, n0=n0: lambda e: e.tensor_tensor(out=dst[:, n0:n0 + 512], in0=t1[:], in1=t2[:], op=ALU.add))(), reads=[rt1, rt2], writes=[rdst])
                for b0 in range(0, 34, 4):
                    nb = min(4, 34 - b0)
                    for j in range(nb):
                        P.op("pe", (lambda b0=b0, j=j: lambda e: e.transpose(ps_tr[:, j * 128:(j + 1) * 128], VT[:, (b0 + j) * 128:(b0 + j + 1) * 128], self.ident_bf[:]))(), reads=[rVT, self.rident_bf], writes=[rps_tr])
                    P.op("act", (lambda b0=b0, nb=nb: lambda e: e.copy(out=Vh[:, b0:b0 + nb, :], in_=ps_tr[:, :nb * 128].rearrange("p (a b) -> p a b", b=128)))(), reads=[rps_tr], writes=[rVh])
                for (q0, qsz) in TT:
                    kbs = range(0, 2) if q0 < NCX else range(0, 34)
                    ons = []
                    for c in range(2):
                        pso, rpso = ps_o.get()
                        psr, rpsr = ps_r.get()
                        c0 = c * 64
                        nk = len(kbs)
                        pend = []

                        def emit_pv(item, pso=pso, rpso=rpso, psr=psr, rpsr=rpsr, nk=nk, qsz=qsz):
                            ik, kb, ptl, rptl = item
                            P.op("pe", (lambda: lambda e: e.matmul(pso[:, :qsz], lhsT=Vh[:, kb, :], rhs=ptl[:, :qsz], start=(ik == 0), stop=(ik == nk - 1)))(), reads=[rVh, rptl], writes=[rpso])
                            P.op("pe", (lambda: lambda e: e.matmul(psr[:, :qsz], lhsT=self.ones_bf[:], rhs=ptl[:, :qsz], start=(ik == 0), stop=(ik == nk - 1)))(), reads=[self.rones, rptl], writes=[rpsr])
                            if KEEP_WARM:
                                P.op("pe", (lambda: lambda e: e.matmul(ps_dum[:, :qsz], lhsT=self.ones_bf[:], rhs=ptl[:, :qsz], start=True, stop=True))(), reads=[self.rones, rptl], writes=[rps_dum])

                        for ik, kb in enumerate(kbs):
                            pss, rpss = ps_s.get()
                            P.op("pe", (lambda pss=pss, kb=kb, c0=c0, q0=q0, qsz=qsz: lambda e: e.matmul(pss[:, :qsz], lhsT=KR[c0:c0 + 64, kb * 128:(kb + 1) * 128], rhs=QR[c0:c0 + 64, q0:q0 + qsz], start=True, stop=True))(), reads=[rKR, rQR], writes=[rpss])
                            ptl, rptl = ptp.get()
                            P.op("act", (lambda ptl=ptl, pss=pss, qsz=qsz: lambda e: e.activation(out=ptl[:, :qsz], in_=pss[:, :qsz], func=AF.Exp, scale=0.125))(), reads=[rpss], writes=[rptl])
                            pend.append((ik, kb, ptl, rptl))
                            if len(pend) > 2:
                                emit_pv(pend.pop(0))
                        while pend:
                            emit_pv(pend.pop(0))
                        rc, rrc = rcp.get()
                        P.op("dve", (lambda rc=rc, psr=psr, qsz=qsz: lambda e: e.reciprocal(out=rc[:, :qsz], in_=psr[:, :qsz]))(), reads=[rpsr], writes=[rrc])
                        on, ron = onp.get()
                        P.op("dve", (lambda on=on, pso=pso, rc=rc, qsz=qsz: lambda e: e.tensor_tensor(out=on[:, :qsz], in0=pso[:, :qsz], in1=rc[:, :qsz], op=ALU.mult))(), reads=[rpso, rrc], writes=[ron])
                        ons.append((on, ron))
                    (on0, ron0), (on1, ron1) = ons
                    o, ro = o_p.get()
                    P.op("dve", (lambda o=o, on0=on0, on1=on1, qsz=qsz: lambda e: e.scalar_tensor_tensor(out=o[:, :qsz], in0=on1[:, :qsz], scalar=neglam[:, 0:1], in1=on0[:, :qsz], op0=ALU.mult, op1=ALU.add))(), reads=[ron0, ron1, rneglam], writes=[ro])
                    sq, rsq = sqp.get()
                    P.op("act", (lambda sq=sq, o=o, qsz=qsz: lambda e: e.activation(out=sq[:, :qsz], in_=o[:, :qsz], func=AF.Square))(), reads=[ro], writes=[rsq])
                    P.op("pe", (lambda sq=sq, qsz=qsz: lambda e: e.matmul(ps_m[:, :qsz], lhsT=self.ones_bf[:], rhs=sq[:, :qsz], start=True, stop=True))(), reads=[self.rones, rsq], writes=[rps_m])
                    rc, rrc = rcp.get()
                    P.op("act", (lambda rc=rc, qsz=qsz: lambda e: e.activation(out=rc[:, :qsz], in_=ps_m[:, :qsz], func=AF.Sqrt, bias=self.epsc[:, 0:1], scale=1.0 / 128))(), reads=[rps_m, self.repsc], writes=[rrc])
                    P.op("dve", (lambda rc=rc, qsz=qsz: lambda e: e.reciprocal(out=rc[:, :qsz], in_=rc[:, :qsz]))(), reads=[rrc], writes=[rrc])
                    P.op("dve", (lambda o=o, rc=rc, qsz=qsz: lambda e: e.tensor_tensor(out=o[:, :qsz], in0=o[:, :qsz], in1=rc[:, :qsz], op=ALU.mult))(), reads=[ro, rrc], writes=[ro])
                    ao, rao = aop.get()
                    P.op("act", (lambda ao=ao, o=o, qsz=qsz: lambda e: e.activation(out=ao[:, :qsz], in_=o[:, :qsz], func=AF.Copy, scale=gsub[:, 0:1]))(), reads=[ro, rgsub], writes=[rao])
                    P.dma("sp", (lambda ao=ao, h=h, q0=q0, qsz=qsz: lambda e: e.dma_start(out=self.dr["at"][h, :, q0:q0 + qsz], in_=ao[:, :qsz]))(), reads=[rao], writes=[self.drreg["at"]])

    def stage_lru(self):
        P = self.P
        pt, rpt = self.dr["pt"], self.drreg["pt"]
        SEG = [(0, NCX), (NCX, L)]
        with Stage(self) as S:
            xin, rxin = S.sb("lx", [128, T], BF16)
            ly, rly = S.sb("ly", [128, T], BF16)
            xc, rxc = S.sb("xc", [128, T], F32)
            xcb, rxcb = S.sb("xcb", [128, T], BF16)
            ra, rra = S.sb("ra", [128, T], F32)
            ib, rib = S.sb("ib", [128, T], F32)
            tmp, rtmp = S.sb("ltmp", [128, T], F32)
            hf, rhf = S.sb("hf", [128, T], F32)
            hb, rhb = S.sb("hb", [128, T], F32)
            ob, rob = S.sb("lout", [128, T], BF16)
            wab, rwab = S.sb("wab", [128, 2, 128], BF16)
            wib, rwib = S.sb("wib", [128, 2, 128], BF16)
            cl, rcl = S.sb("cl", [128, 4], F32)
            psp = S.pool("lps", 4, [128, 512], F32, psum=True)

            def rev(t, c0, n):
                a = t[:, c0:c0 + n]
                return bass.AP(a.tensor, a.offset + (n - 1), [[a.ap[0][0], 128], [-1, n]])

            for n in range(8):
                P.dma("sp", (lambda n=n: lambda e: e.dma_start(out=xin[:], in_=pt[C_LX // 128 + n]))(), reads=[rpt], writes=[rxin])
                P.dma("sp", (lambda n=n: lambda e: e.dma_start(out=ly[:], in_=pt[C_LY // 128 + n]))(), reads=[rpt], writes=[rly])
                P.dma("pool", (lambda n=n: lambda e: e.dma_start(out=wab[:], in_=self.dr["lru_wa"][:, n].rearrange("d i o -> i d o")))(), reads=[self.drreg["lru_wa"]], writes=[rwab])
                P.dma("pool", (lambda n=n: lambda e: e.dma_start(out=wib[:], in_=self.dr["lru_wi"][:, n].rearrange("d i o -> i d o")))(), reads=[self.drreg["lru_wi"]], writes=[rwib])
                w = lambda j, n=n: self.vcol("lru_cw", j * 8 + n)
                bcol = self.vcol("lru_cb", n)
                for (s0, ls) in SEG:
                    P.op("dve", (lambda s0=s0, ls=ls, n=n: lambda e: e.tensor_scalar(out=xc[:, s0:s0 + ls], in0=xin[:, s0:s0 + ls], scalar1=self.vcol("lru_cw", 2 * 8 + n), scalar2=self.vcol("lru_cb", n), op0=ALU.mult, op1=ALU.add))(),
                         reads=[rxin, self.rvp], writes=[rxc])
                    for j in (0, 1, 3):
                        d = j - 2
                        lo, hi = max(0, -d), ls - max(0, d)
                        P.op("dve", (lambda s0=s0, lo=lo, hi=hi, d=d, j=j, n=n: lambda e: e.scalar_tensor_tensor(out=xc[:, s0 + lo:s0 + hi], in0=xin[:, s0 + lo + d:s0 + hi + d], scalar=self.vcol("lru_cw", j * 8 + n), in1=xc[:, s0 + lo:s0 + hi], op0=ALU.mult, op1=ALU.add))(),
                             reads=[rxin, rxc, self.rvp], writes=[rxc])
                P.op("pool", lambda e: e.tensor_copy(out=xcb[:], in_=xc[:]), reads=[rxc], writes=[rxcb])
                for d in range(2):
                    hh, rhh = (hf, rhf) if d == 0 else (hb, rhb)
                    lamc = self.vcol("lru_lam", d * 8 + n)
                    P.op("act", (lambda lamc=lamc: lambda e: e.activation(out=cl[:, 0:1], in_=lamc, func=AF.Exp, scale=-1.0))(), reads=[self.rvp], writes=[rcl])
                    P.op("act", lambda e: e.activation(out=cl[:, 1:2], in_=cl[:, 0:1], func=AF.Ln, bias=self.epsc[:, 1:2], scale=1.0), reads=[rcl, self.repsc], writes=[rcl])
                    P.op("dve", lambda e: e.tensor_scalar(out=cl[:, 2:3], in0=cl[:, 1:2], scalar1=-8.0, scalar2=None, op0=ALU.mult), reads=[rcl], writes=[rcl])
                    for (n0, nsz) in TT:
                        ps, rps = psp.get()
                        P.op("pe", (lambda ps=ps, d=d, n0=n0, nsz=nsz: lambda e: e.matmul(ps[:, :nsz], lhsT=wab[:, d, :], rhs=xcb[:, n0:n0 + nsz], start=True, stop=True))(), reads=[rwab, rxcb], writes=[rps])
                        P.op("act", (lambda ps=ps, d=d, n=n, n0=n0, nsz=nsz: lambda e: e.activation(out=ra[:, n0:n0 + nsz], in_=ps[:, :nsz], func=AF.Sigmoid, bias=self.vcol("lru_ba", d * 8 + n), scale=1.0))(), reads=[rps, self.rvp], writes=[rra])
                        ps2, rps2 = psp.get()
                        P.op("pe", (lambda ps2=ps2, d=d, n0=n0, nsz=nsz: lambda e: e.matmul(ps2[:, :nsz], lhsT=wib[:, d, :], rhs=xcb[:, n0:n0 + nsz], start=True, stop=True))(), reads=[rwib, rxcb], writes=[rps2])
                        P.op("act", (lambda ps2=ps2, d=d, n=n, n0=n0, nsz=nsz: lambda e: e.activation(out=ib[:, n0:n0 + nsz], in_=ps2[:, :nsz], func=AF.Sigmoid, bias=self.vcol("lru_bi", d * 8 + n), scale=1.0))(), reads=[rps2, self.rvp], writes=[rib])
                    P.op("act", lambda e: e.activation(out=ra[:], in_=ra[:], func=AF.Exp, scale=cl[:, 2:3]), reads=[rra, rcl], writes=[rra])
                    P.op("pool", lambda e: e.tensor_tensor(out=tmp[:], in0=ra[:], in1=ra[:], op=ALU.mult), reads=[rra], writes=[rtmp])
                    P.op("act", lambda e: e.activation(out=tmp[:], in_=tmp[:], func=AF.Sqrt, bias=self.epsc[:, 1:2], scale=-1.0), reads=[rtmp, self.repsc], writes=[rtmp])
                    P.op("dve", lambda e: e.tensor_tensor(out=ib[:], in0=ib[:], in1=xc[:], op=ALU.mult), reads=[rib, rxc], writes=[rib])
                    P.op("dve", lambda e: e.tensor_tensor(out=ib[:], in0=ib[:], in1=tmp[:], op=ALU.mult), reads=[rib, rtmp], writes=[rib])
                    if d == 0:
                        P.op("dve", lambda e: e.tensor_tensor_scan(out=hf[:, 0:NCX], data0=ra[:, 0:NCX], data1=ib[:, 0:NCX], initial=0.0, op0=ALU.mult, op1=ALU.add), reads=[rra, rib], writes=[rhf])
                        P.op("dve", lambda e: e.tensor_tensor_scan(out=hf[:, NCX:T], data0=ra[:, NCX:T], data1=ib[:, NCX:T], initial=hf[:, NCX - 1:NCX], op0=ALU.mult, op1=ALU.add), reads=[rra, rib, rhf], writes=[rhf])
                    else:
                        P.op("dve", lambda e: e.tensor_tensor_scan(out=rev(hb, 0, NCX), data0=rev(ra, 0, NCX), data1=rev(ib, 0, NCX), initial=0.0, op0=ALU.mult, op1=ALU.add), reads=[rra, rib], writes=[rhb])
                        P.op("dve", lambda e: e.tensor_tensor_scan(out=rev(hb, NCX, L), data0=rev(ra, NCX, L), data1=rev(ib, NCX, L), initial=hb[:, 0:1], op0=ALU.mult, op1=ALU.add), reads=[rra, rib, rhb], writes=[rhb])
                P.op("pool", lambda e: e.tensor_tensor(out=hf[:], in0=hf[:], in1=hb[:], op=ALU.add), reads=[rhf, rhb], writes=[rhf])
                P.op("act", lambda e: e.activation(out=tmp[:], in_=ly[:], func=AF.Square), reads=[rly], writes=[rtmp])
                P.op("dve", lambda e: e.tensor_scalar(out=tmp[:], in0=tmp[:], scalar1=0.044715, scalar2=1.0, op0=ALU.mult, op1=ALU.add), reads=[rtmp], writes=[rtmp])
                P.op("dve", lambda e: e.tensor_tensor(out=tmp[:], in0=tmp[:], in1=ly[:], op=ALU.mult), reads=[rtmp, rly], writes=[rtmp])
                P.op("act", lambda e: e.activation(out=tmp[:], in_=tmp[:], func=AF.Sigmoid, scale=1.5957691216057308), reads=[rtmp], writes=[rtmp])
                P.op("dve", lambda e: e.tensor_tensor(out=tmp[:], in0=tmp[:], in1=ly[:], op=ALU.mult), reads=[rtmp, rly], writes=[rtmp])
                P.op("dve", lambda e: e.tensor_tensor(out=ob[:], in0=tmp[:], in1=hf[:], op=ALU.mult), reads=[rtmp, rhf], writes=[rob])
                P.dma("sp", (lambda n=n: lambda e: e.dma_start(out=self.dr["rt"][n], in_=ob[:]))(), reads=[rob], writes=[self.drreg["rt"]])

    def stage_merge(self):
        P = self.P
        with Stage(self) as S:
            wbr, rwbr = S.sb("wbr", [128, 3, 8, 1024], BF16)
            wo, rwo = S.sb("wo", [128, 8, 1024], BF16)
            for k in range(3):
                P.dma("pool", (lambda k=k: lambda e: e.dma_start(out=wbr[:, k], in_=self.dr["w_br"][k].rearrange("(c p) n -> p c n", p=128)))(), reads=[self.drreg["w_br"]], writes=[rwbr])
            P.dma("pool", lambda e: e.dma_start(out=wo[:], in_=self.dr["w_out"].rearrange("(c p) n -> p c n", p=128)), reads=[self.drreg["w_out"]], writes=[rwo])
            brp = [S.pool("br%d" % k, 1, [128, 8, 512], BF16) for k in range(3)]
            gp = S.pool("mg", 2, [128, 24, 512], BF16)
            xp = S.pool("mx", 2, [128, 8, 512], F32)
            mtp = S.pool("mt", 2, [128, 8, 512], BF16)
            macc = S.pool("macc", 2, [128, 512], F32)
            mtmp = S.pool("mtmp", 2, [128, 512], F32)
            psp = S.pool("mps", 4, [128, 512], F32, psum=True)
            names = ["at", "rt", "yt"]
            for (n0, nsz) in TT:
                w = 1 if n0 < NCX else 0
                brs = []
                for k in range(3):
                    bt, rbt = brp[k].get()
                    P.dma("sp", (lambda bt=bt, k=k, n0=n0, nsz=nsz: lambda e: e.dma_start(out=bt[:, :, :nsz], in_=self.dr[names[k]][:, :, n0:n0 + nsz].rearrange("c p n -> p c n")))(), reads=[self.drreg[names[k]]], writes=[rbt])
                    brs.append((bt, rbt))
                g, rg = gp.get()
                P.dma("sp", (lambda g=g, n0=n0, nsz=nsz: lambda e: e.dma_start(out=g[:, :, :nsz], in_=self.dr["pt"][C_G // 128:C_END // 128, :, n0:n0 + nsz].rearrange("c p n -> p c n")))(), reads=[self.drreg["pt"]], writes=[rg])
                xt_, rxt_ = xp.get()
                P.dma("sp", (lambda xt_=xt_, n0=n0, nsz=nsz: lambda e: e.dma_start(out=xt_[:, :, :nsz], in_=self.dr["xt"][:, :, n0:n0 + nsz]))(), reads=[self.drreg["xt"]], writes=[rxt_])
                sg, rsg = g, rg
                for c in range(24):
                    P.op("act", (lambda sg=sg, g=g, c=c, nsz=nsz: lambda e: e.activation(out=sg[:, c, :nsz], in_=g[:, c, :nsz], func=AF.Sigmoid, bias=self.vcol("b_gate", c), scale=1.0))(), reads=[rg, self.rvp], writes=[rsg])
                mt, rmt = mtp.get()
                for oc in range(8):
                    acc, racc = macc.get()
                    for k in range(3):
                        ps, rps = psp.get()
                        bt, rbt = brs[k]
                        for kc in range(8):
                            P.op("pe", (lambda ps=ps, bt=bt, k=k, kc=kc, oc=oc, nsz=nsz: lambda e: e.matmul(ps[:, :nsz], lhsT=wbr[:, k, kc, oc * 128:(oc + 1) * 128], rhs=bt[:, kc, :nsz], start=(kc == 0), stop=(kc == 7)))(), reads=[rwbr, rbt], writes=[rps])
                        if k == 0:
                            P.op("dve", (lambda acc=acc, ps=ps, sg=sg, oc=oc, nsz=nsz: lambda e: e.tensor_tensor(out=acc[:, :nsz], in0=ps[:, :nsz], in1=sg[:, oc, :nsz], op=ALU.mult))(), reads=[rps, rsg], writes=[racc])
                        else:
                            t_, rt_ = mtmp.get()
                            P.op("dve", (lambda t_=t_, ps=ps, sg=sg, k=k, oc=oc, nsz=nsz: lambda e: e.tensor_tensor(out=t_[:, :nsz], in0=ps[:, :nsz], in1=sg[:, k * 8 + oc, :nsz], op=ALU.mult))(), reads=[rps, rsg], writes=[rt_])
                            if k == 1:
                                P.op("pool", (lambda acc=acc, t_=t_, nsz=nsz: lambda e: e.tensor_tensor(out=acc[:, :nsz], in0=acc[:, :nsz], in1=t_[:, :nsz], op=ALU.add))(), reads=[racc, rt_], writes=[racc])
                            else:
                                P.op("pool", (lambda acc=acc, t_=t_, mt=mt, oc=oc, nsz=nsz: lambda e: e.tensor_tensor(out=mt[:, oc, :nsz], in0=acc[:, :nsz], in1=t_[:, :nsz], op=ALU.add))(), reads=[racc, rt_], writes=[rmt])
                xo, rxo = xt_, rxt_
                for oc in range(8):
                    ps, rps = psp.get()
                    for kc in range(8):
                        P.op("pe", (lambda ps=ps, mt=mt, kc=kc, oc=oc, nsz=nsz: lambda e: e.matmul(ps[:, :nsz], lhsT=wo[:, kc, oc * 128:(oc + 1) * 128], rhs=mt[:, kc, :nsz], start=(kc == 0), stop=(kc == 7)))(), reads=[rwo, rmt], writes=[rps])
                    P.op("dve", (lambda xo=xo, ps=ps, xt_=xt_, oc=oc, nsz=nsz, w=w: lambda e: e.scalar_tensor_tensor(out=xo[:, oc, :nsz], in0=ps[:, :nsz], scalar=self.modv[:, 16 + oc, w:w + 1], in1=xt_[:, oc, :nsz], op0=ALU.mult, op1=ALU.add))(), reads=[rps, rxt_, self.rmodv], writes=[rxo])
                P.dma("sp", (lambda xo=xo, n0=n0, nsz=nsz: lambda e: e.dma_start(out=self.dr["xt_mid"][:, :, n0:n0 + nsz], in_=xo[:, :, :nsz]))(), reads=[rxo], writes=[self.drreg["xt_mid"]])

    def stage_ffn(self):
        P = self.P
        moe = self.moe
        STS = [TT[0:3], TT[3:6], TT[6:9]]
        FG = [(0, 4), (4, 4), (8, 4), (12, 4), (16, 4), (20, 2)]
        if moe:
            self.dram("gate_s", [NE, T], F32, "Internal")
        with Stage(self) as S:
            fT, rfT = S.sb("fT", [128, 8, 1536], BF16)
            yacc, ryacc = S.sb("yacc", [128, 8, 1536], F32)
            wgp = S.pool("wg", 2, [128, 8, 512], BF16)
            wup = S.pool("wu", 2, [128, 8, 512], BF16)
            wdp = S.pool("wd", 2, [128, 4, 1024], BF16)
            if moe:
                wr, rwr = S.sb("wr", [128, 8, NE], F32)
                P.dma("sp", lambda e: e.dma_start(out=wr[:], in_=self.dr["router"].rearrange("(k p) n -> p k n", p=128)), reads=[self.drreg["router"]], writes=[rwr])
                gbc, rgbc = S.sb("gbc", [128, 1536], F32)
                identf = self.vp[:, VP["ident"][0]:VP["ident"][0] + 128]
            for st in STS:
                c0 = st[0][0]
                c1 = st[-1][0] + st[-1][1]
                with Stage(self) as S2:
                    router = None
                    if moe:
                        psr, rpsr = S2.ps("psr", [128, 4, NE], F32)
                        ps_t, rps_t = S2.ps("ps_t", [NE, 512], F32)
                        lg, rlg = S2.sb("lg", [128, 4, NE], F32)
                        l2, rl2 = S2.sb("l2", [128, 4, NE], F32)
                        m1, rm1 = S2.sb("m1", [128, 4], F32)
                        m2, rm2 = S2.sb("m2", [128, 4], F32)
                        gT, rgT = S2.sb("gT", [NE, 512], F32)
                        router = (wr, rwr, psr, rpsr)

                        def router_done(n0, nsz):
                            ns = nsz // 128
                            bc = lambda t: t[:, :ns].unsqueeze(2).to_broadcast([128, ns, NE])
                            P.op("dve", lambda e: e.tensor_copy(out=lg[:, :ns], in_=psr[:, :ns]), reads=[rpsr], writes=[rlg])
                            P.op("dve", lambda e: e.tensor_reduce(out=m1[:, :ns], in_=lg[:, :ns], axis=AX.X, op=ALU.max), reads=[rlg], writes=[rm1])
                            P.op("dve", lambda e: e.tensor_tensor(out=l2[:, :ns], in0=lg[:, :ns], in1=bc(m1), op=ALU.is_equal), reads=[rlg, rm1], writes=[rl2])
                            P.op("dve", lambda e: e.scalar_tensor_tensor(out=l2[:, :ns], in0=l2[:, :ns], scalar=-1e30, in1=lg[:, :ns], op0=ALU.mult, op1=ALU.add), reads=[rl2, rlg], writes=[rl2])
                            P.op("dve", lambda e: e.tensor_reduce(out=m2[:, :ns], in_=l2[:, :ns], axis=AX.X, op=ALU.max), reads=[rl2], writes=[rm2])
                            P.op("dve", lambda e: e.tensor_tensor(out=l2[:, :ns], in0=lg[:, :ns], in1=bc(m2), op=ALU.is_ge), reads=[rlg, rm2], writes=[rl2])
                            P.op("dve", lambda e: e.tensor_tensor(out=lg[:, :ns], in0=lg[:, :ns], in1=bc(m1), op=ALU.subtract), reads=[rlg, rm1], writes=[rlg])
                            P.op("act", lambda e: e.activation(out=lg[:, :ns], in_=lg[:, :ns], func=AF.Exp), reads=[rlg], writes=[rlg])
                            P.op("dve", lambda e: e.tensor_tensor(out=lg[:, :ns], in0=lg[:, :ns], in1=l2[:, :ns], op=ALU.mult), reads=[rlg, rl2], writes=[rlg])
                            P.op("dve", lambda e: e.tensor_reduce(out=m1[:, :ns], in_=lg[:, :ns], axis=AX.X, op=ALU.add), reads=[rlg], writes=[rm1])
                            P.op("dve", lambda e: e.reciprocal(out=m1[:, :ns], in_=m1[:, :ns]), reads=[rm1], writes=[rm1])
                            P.op("dve", lambda e: e.tensor_tensor(out=lg[:, :ns], in0=lg[:, :ns], in1=bc(m1), op=ALU.mult), reads=[rlg, rm1], writes=[rlg])
                            for sub in range(ns):
                                P.op("pe", (lambda sub=sub: lambda e: e.transpose(ps_t[:, sub * 128:(sub + 1) * 128], lg[:, sub, :], identf))(), reads=[rlg, self.rvp], writes=[rps_t])
                            P.op("dve", lambda e: e.tensor_copy(out=gT[:, :nsz], in_=ps_t[:, :nsz]), reads=[rps_t], writes=[rgT])
                            P.dma("sp", lambda e: e.dma_start(out=self.dr["gate_s"][:, n0:n0 + nsz], in_=gT[:, :nsz]), reads=[rgT], writes=[self.drreg["gate_s"]])

                        self._router_done = router_done
                    self.norm_mod(S2, "xt_mid", st, self.gsf, self.rgsf, 3, fT, rfT, c0, router=router)
                with Stage(self) as S3:
                    psg = S3.pool("psg", 2, [128, 512], F32, psum=True)
                    psu = S3.pool("psu", 2, [128, 512], F32, psum=True)
                    psd = S3.pool("psd", 3, [128, 512], F32, psum=True)
                    sgp = S3.pool("fsg", 2, [128, 512], F32)
                    actp = S3.pool("fact", 2, [128, 4, 512], BF16)
                    first = True
                    for ex in range(self.ne):
                        if moe:
                            P.dma("sp", (lambda ex=ex, c0=c0, c1=c1: lambda e: e.dma_start(out=gbc[:, :c1 - c0], in_=bcast_rows(self.dr["gate_s"][ex:ex + 1, c0:c1], c1 - c0)))(), reads=[self.drreg["gate_s"]], writes=[rgbc])
                        for (g0, gn) in FG:
                            wg, rwg = wgp.get()
                            wu, rwu = wup.get()
                            wd, rwd = wdp.get()
                            P.dma("pool", (lambda wg=wg, ex=ex, g0=g0, gn=gn: lambda e: e.dma_start(out=wg[:, :, :gn * 128], in_=self.dr["f_wg"][ex].rearrange("(k p) n -> p k n", p=128)[:, :, g0 * 128:(g0 + gn) * 128]))(), reads=[self.drreg["f_wg"]], writes=[rwg])
                            P.dma("pool", (lambda wu=wu, ex=ex, g0=g0, gn=gn: lambda e: e.dma_start(out=wu[:, :, :gn * 128], in_=self.dr["f_wu"][ex].rearrange("(k p) n -> p k n", p=128)[:, :, g0 * 128:(g0 + gn) * 128]))(), reads=[self.drreg["f_wu"]], writes=[rwu])
                            P.dma("pool", (lambda wd=wd, ex=ex, g0=g0, gn=gn: lambda e: e.dma_start(out=wd[:, :gn, :], in_=self.dr["f_wd"][ex].rearrange("(c p) n -> p c n", p=128)[:, g0:g0 + gn, :]))(), reads=[self.drreg["f_wd"]], writes=[rwd])
                            for (n0, nsz) in st:
                                o0 = n0 - c0
                                act, ract = actp.get()
                                for c in range(gn):
                                    pg, rpg = psg.get()
                                    pu, rpu = psu.get()
                                    for k in range(8):
                                        P.op("pe", (lambda pg=pg, wg=wg, k=k, c=c, o0=o0, nsz=nsz: lambda e: e.matmul(pg[:, :nsz], lhsT=wg[:, k, c * 128:(c + 1) * 128], rhs=fT[:, k, o0:o0 + nsz], start=(k == 0), stop=(k == 7)))(), reads=[rwg, rfT], writes=[rpg])
                                    for k in range(8):
                                        P.op("pe", (lambda pu=pu, wu=wu, k=k, c=c, o0=o0, nsz=nsz: lambda e: e.matmul(pu[:, :nsz], lhsT=wu[:, k, c * 128:(c + 1) * 128], rhs=fT[:, k, o0:o0 + nsz], start=(k == 0), stop=(k == 7)))(), reads=[rwu, rfT], writes=[rpu])
                                    sg, rsg = sgp.get()
                                    P.op("act", (lambda sg=sg, pg=pg, nsz=nsz: lambda e: e.activation(out=sg[:, :nsz], in_=pg[:, :nsz], func=AF.Silu))(), reads=[rpg], writes=[rsg])
                                    if moe:
                                        P.op("pool", (lambda sg=sg, o0=o0, nsz=nsz: lambda e: e.tensor_tensor(out=sg[:, :nsz], in0=sg[:, :nsz], in1=gbc[:, o0:o0 + nsz], op=ALU.mult))(), reads=[rsg, rgbc], writes=[rsg])
                                    P.op("dve", (lambda act=act, sg=sg, pu=pu, c=c, nsz=nsz: lambda e: e.tensor_tensor(out=act[:, c, :nsz], in0=pu[:, :nsz], in1=sg[:, :nsz], op=ALU.mult))(), reads=[rpu, rsg], writes=[ract])
                                for oc in range(8):
                                    pd, rpd = psd.get()
                                    for c in range(gn):
                                        P.op("pe", (lambda pd=pd, wd=wd, act=act, c=c, oc=oc, nsz=nsz, gn=gn: lambda e: e.matmul(pd[:, :nsz], lhsT=wd[:, c, oc * 128:(oc + 1) * 128], rhs=act[:, c, :nsz], start=(c == 0), stop=(c == gn - 1)))(), reads=[rwd, ract], writes=[rpd])
                                    if first:
                                        P.op("act", (lambda pd=pd, oc=oc, o0=o0, nsz=nsz: lambda e: e.copy(out=yacc[:, oc, o0:o0 + nsz], in_=pd[:, :nsz]))(), reads=[rpd], writes=[ryacc])
                                    else:
                                        P.op("dve", (lambda pd=pd, oc=oc, o0=o0, nsz=nsz: lambda e: e.tensor_tensor(out=yacc[:, oc, o0:o0 + nsz], in0=yacc[:, oc, o0:o0 + nsz], in1=pd[:, :nsz], op=ALU.add))(), reads=[rpd, ryacc], writes=[ryacc])
                            first = False
                    xp = S3.pool("fx", 2, [128, 8, 512], F32)
                    for (n0, nsz) in st:
                        o0 = n0 - c0
                        w = 1 if n0 < NCX else 0
                        xt_, rxt_ = xp.get()
                        P.dma("sp", (lambda xt_=xt_, n0=n0, nsz=nsz: lambda e: e.dma_start(out=xt_[:, :, :nsz], in_=self.dr["xt_mid"][:, :, n0:n0 + nsz]))(), reads=[self.drreg["xt_mid"]], writes=[rxt_])
                        for oc in range(8):
                            P.op("dve", (lambda xt_=xt_, oc=oc, o0=o0, nsz=nsz, w=w: lambda e: e.scalar_tensor_tensor(out=xt_[:, oc, :nsz], in0=yacc[:, oc, o0:o0 + nsz], scalar=self.modv[:, 40 + oc, w:w + 1], in1=xt_[:, oc, :nsz], op0=ALU.mult, op1=ALU.add))(), reads=[ryacc, rxt_, self.rmodv], writes=[rxt_])
                        P.dma("sp", (lambda xt_=xt_, n0=n0, nsz=nsz: lambda e: e.dma_start(out=self.dr["xt_out"][:, :, n0:n0 + nsz], in_=xt_[:, :, :nsz]))(), reads=[rxt_], writes=[self.drreg["xt_out"]])

    def stage_final(self):
        P = self.P
        with Stage(self) as S:
            gfin, rgfin = S.sb("gfin", [128, 8, 2], F32)
            zsh, rzsh = S.sb("zsh", [128, 8, 2], F32)
            o = VP["g_final"][0]
            for w in range(2):
                P.op("dve", (lambda w=w: lambda e: e.tensor_copy(out=gfin[:, :, w], in_=self.vp[:, o:o + 8]))(), reads=[self.rvp], writes=[rgfin])
            P.op("pool", lambda e: e.memset(zsh[:], 0.0), writes=[rzsh])
            for (n0, nsz) in TT[1:]:
                with Stage(self) as S2:
                    ot, rot = S2.sb("fin_o", [128, 8, 512], F32)
                    self.norm_mod(S2, "xt_out", [(n0, nsz)], gfin, rgfin, 0, ot, rot, n0, shift=(zsh, rzsh))
                    P.dma("sp", (lambda ot=ot, n0=n0: lambda e: e.dma_start(out=self.dr["y_out"][:, :, n0 - NCX:n0 - NCX + 512], in_=ot[:]))(), reads=[rot], writes=[self.drreg["y_out"]])

    def mod_reduce(self, S, t, rt, scr, rscr, M, shape_ap=None):
        P = self.P
        P.op("dve", lambda e: e.tensor_scalar(out=scr, in0=t, scalar1=1.0 / M, scalar2=MAGIC, op0=ALU.mult, op1=ALU.add), reads=[rt], writes=[rscr])
        P.op("dve", lambda e: e.tensor_scalar(out=scr, in0=scr, scalar1=-MAGIC, scalar2=None, op0=ALU.add), reads=[rscr], writes=[rscr])
        P.op("dve", lambda e: e.scalar_tensor_tensor(out=t, in0=scr, scalar=-float(M), in1=t, op0=ALU.mult, op1=ALU.add), reads=[rscr, rt], writes=[rt])

    def hy_dft_gen(self, Ls):
        P = self.P
        nt = Ls // 128
        N = 2 * Ls
        nm = "L%d" % Ls
        for k in ("CF", "SF", "CI", "SI"):
            self.dram(k + nm, [nt, 128, nt, 128], BF16, "Internal")
        with Stage(self) as S:
            q, rq = S.sb("q", [128, Ls], F32)
            x1, rx1 = S.sb("x1", [128, Ls], F32)
            scr, rscr = S.sb("scr", [128, Ls], F32)
            m, rm = S.sb("m", [128, Ls], F32)
            m2, rm2 = S.sb("m2", [128, Ls], F32)
            pc2, rpc2 = S.sb("pc2", [128, 1], F32)
            outp = S.pool("dfto", 2, [128, Ls], BF16)
            P.op("dve", lambda e: e.tensor_scalar(out=pc2[:], in0=self.vcol("pidx"), scalar1=2.0, scalar2=1.0, op0=ALU.mult, op1=ALU.add), reads=[self.rvp], writes=[rpc2])
            ptr = S.pool("dftptr", 2, [128, 512], BF16, psum=True)
            sI_p = S.pool("dftsI", 2, [128, nt, 128], BF16)
            P.op("pool", lambda e: e.iota(q[:], pattern=[[2, Ls]], base=1, channel_multiplier=0, allow_small_or_imprecise_dtypes=True), writes=[rq])
            M1, mult1, pcol = (2 * N) // 128, 128.0, self.vcol("pidx")
            for a in range(nt):
                P.op("dve", (lambda a=a: lambda e: e.tensor_scalar(out=x1[:], in0=q[:], scalar1=float(a), scalar2=None, op0=ALU.mult))(), reads=[rq], writes=[rx1])
                self.mod_reduce(S, x1[:], rx1, scr[:], rscr, M1)
                P.op("dve", lambda e: e.tensor_scalar(out=x1[:], in0=x1[:], scalar1=mult1, scalar2=None, op0=ALU.mult), reads=[rx1], writes=[rx1])
                P.op("dve", lambda e: e.scalar_tensor_tensor(out=m[:], in0=q[:], scalar=pcol, in1=x1[:], op0=ALU.mult, op1=ALU.add), reads=[rq, rx1, self.rvp], writes=[rm])
                P.op("pool", lambda e: e.tensor_scalar(out=m2[:], in0=m[:], scalar1=float(N // 2), scalar2=None, op0=ALU.add), reads=[rm], writes=[rm2])
                self.mod_reduce(S, m[:], rm, scr[:], rscr, 2 * N)
                self.mod_reduce(S, m2[:], rm2, scr[:], rscr, 2 * N)
                for (src, rsrc, cs) in ((m2, rm2, "C"), (m, rm, "S")):
                    ot, rot = outp.get()
                    P.op("act", (lambda ot=ot, src=src: lambda e: e.activation(out=ot[:], in_=src[:], func=AF.Sin, scale=3.1415925 / N))(), reads=[rsrc], writes=[rot])
                    P.dma("sp", (lambda ot=ot, cs=cs, a=a: lambda e: e.dma_start(out=self.dr[cs + "F" + nm][:, :, a, :].rearrange("c p j -> p c j"), in_=ot[:].rearrange("p (c j) -> p c j", j=128)))(), reads=[rot], writes=[self.drreg[cs + "F" + nm]])
                    sI, rsI = sI_p.get()
                    for c0 in range(0, nt, 4):
                        nb = min(4, nt - c0)
                        pt_, rpt_ = ptr.get()
                        for j in range(nb):
                            P.op("pe", (lambda pt_=pt_, ot=ot, c0=c0, j=j: lambda e: e.transpose(pt_[:, j * 128:(j + 1) * 128], ot[:, (c0 + j) * 128:(c0 + j + 1) * 128], self.ident_bf[:]))(), reads=[rot, self.rident_bf], writes=[rpt_])
                        P.op("act", (lambda pt_=pt_, sI=sI, c0=c0, nb=nb: lambda e: e.activation(out=sI[:, c0:c0 + nb, :], in_=pt_[:, :nb * 128].rearrange("p (a b) -> p a b", b=128), func=AF.Copy, scale=2.0 / N))(), reads=[rpt_], writes=[rsI])
                    P.dma("sp", (lambda sI=sI, cs=cs, a=a: lambda e: e.dma_start(out=self.dr[cs + "I" + nm][a], in_=sI[:]))(), reads=[rsI], writes=[self.drreg[cs + "I" + nm]])

    def hy_prep(self):
        P = self.P
        self.dram("utm", [3, 34, 128, 1024], BF16, "Internal")
        self.dram("z1", [34, 128, 1024], BF16, "Internal")
        self.dram("ytm", [34, 128, 1024], BF16, "Internal")
        SEG = [(0, NCX), (NCX, L)]
        with Stage(self) as S:
            xin_p = S.pool("hx", 2, [128, T], BF16)
            u, ru = S.sb("hu", [128, T], F32)
            ub, rub = S.sb("hub", [128, T], BF16)
            stg_p = S.pool("hstg", 2, [128, 34, 128], BF16)
            ptr = S.pool("hptr", 2, [128, 512], BF16, psum=True)
            for ch in range(24):
                xin, rxin = xin_p.get()
                P.dma("sp", (lambda xin=xin, ch=ch: lambda e: e.dma_start(out=xin[:], in_=self.dr["pt"][C_HY // 128 + ch]))(), reads=[self.drreg["pt"]], writes=[rxin])
                for (s0, ls) in SEG:
                    P.op("dve", (lambda xin=xin, s0=s0, ls=ls, ch=ch: lambda e: e.tensor_scalar(out=u[:, s0:s0 + ls], in0=xin[:, s0:s0 + ls], scalar1=self.vcol("hy_cw", 1 * 24 + ch), scalar2=self.vcol("hy_cb", ch), op0=ALU.mult, op1=ALU.add))(), reads=[rxin, self.rvp], writes=[ru])
                    for j in (0, 2):
                        d = j - 1
                        lo, hi = max(0, -d), ls - max(0, d)
                        eng = "dve"
                        P.op(eng, (lambda xin=xin, s0=s0, lo=lo, hi=hi, d=d, j=j, ch=ch: lambda e: e.scalar_tensor_tensor(out=u[:, s0 + lo:s0 + hi], in0=xin[:, s0 + lo + d:s0 + hi + d], scalar=self.vcol("hy_cw", j * 24 + ch), in1=u[:, s0 + lo:s0 + hi], op0=ALU.mult, op1=ALU.add))(), reads=[rxin, ru, self.rvp], writes=[ru])
                P.op("act", lambda e: e.copy(out=ub[:], in_=u[:]), reads=[ru], writes=[rub])
                stg, rstg = stg_p.get()
                for b0 in range(0, 34, 4):
                    nb = min(4, 34 - b0)
                    pt_, rpt_ = ptr.get()
                    for j in range(nb):
                        P.op("pe", (lambda pt_=pt_, b0=b0, j=j: lambda e: e.transpose(pt_[:, j * 128:(j + 1) * 128], ub[:, (b0 + j) * 128:(b0 + j + 1) * 128], self.ident_bf[:]))(), reads=[rub, self.rident_bf], writes=[rpt_])
                    eng = "act" if (b0 // 4) % 2 == 0 else "dve"
                    if eng == "act":
                        P.op("act", (lambda pt_=pt_, stg=stg, b0=b0, nb=nb: lambda e: e.copy(out=stg[:, b0:b0 + nb, :], in_=pt_[:, :nb * 128].rearrange("p (a b) -> p a b", b=128)))(), reads=[rpt_], writes=[rstg])
                    else:
                        P.op("dve", (lambda pt_=pt_, stg=stg, b0=b0, nb=nb: lambda e: e.tensor_copy(out=stg[:, b0:b0 + nb, :], in_=pt_[:, :nb * 128].rearrange("p (a b) -> p a b", b=128)))(), reads=[rpt_], writes=[rstg])
                wsel, cc = ch // 8, ch % 8
                P.dma("sp", (lambda stg=stg, wsel=wsel, cc=cc: lambda e: e.dma_start(out=self.dr["utm"][wsel][:, :, cc * 128:(cc + 1) * 128].rearrange("b p c -> p b c"), in_=stg[:]))(), reads=[rstg], writes=[self.drreg["utm"]])

    def hy_filters(self, Ls):
        P = self.P
        nt = Ls // 128
        nm = "L%d" % Ls
        self.dram("HS" + nm, [nt, 128, 2048], BF16, "Internal")
        self.dram("HD" + nm, [nt, 128, 2048], BF16, "Internal")
        self.dram("RN" + nm, [128, 2048], F32, "Internal")
        with Stage(self) as S:
            w1, rw1 = S.sb("hw1", [33, 64], F32)
            w2, rw2 = S.sb("hw2", [64, 64], F32)
            w3, rw3 = S.sb("hw3", [64, 4096], F32)
            dec, rdec = S.sb("hdec", [1, 2048], F32)
            P.dma("sp", lambda e: e.dma_start(out=w1[:], in_=self.dr["hy_w1"]), reads=[self.drreg["hy_w1"]], writes=[rw1])
            P.dma("sp", lambda e: e.dma_start(out=w2[:], in_=self.dr["hy_w2"]), reads=[self.drreg["hy_w2"]], writes=[rw2])
            P.dma("sp", lambda e: e.dma_start(out=w3[:], in_=self.dr["hy_w3"]), reads=[self.drreg["hy_w3"]], writes=[rw3])
            P.dma("sp", lambda e: e.dma_start(out=dec[:], in_=self.dr["hy_decay"]), reads=[self.drreg["hy_decay"]], writes=[rdec])
            P.op("act", lambda e: e.activation(out=dec[:], in_=dec[:], func=AF.Abs), reads=[rdec], writes=[rdec])
            nv, rnv = S.sb("hnv", [64, Ls], F32)
            z, rz = S.sb("hz", [64, Ls], F32)
            h1, rh1 = S.sb("hh1", [64, Ls], F32)
            h2, rh2 = S.sb("hh2", [64, Ls], F32)
            tv, rtv = S.sb("htv", [1, Ls], F32)
            P.op("pool", lambda e: e.iota(nv[:], pattern=[[1, Ls]], base=0, channel_multiplier=0, allow_small_or_imprecise_dtypes=True), writes=[rnv])
            P.op("dve", lambda e: e.tensor_scalar(out=h1[0:33, :], in0=nv[0:33, :], scalar1=2 * PI / Ls, scalar2=self.vp[0:33, VP["bands"][0]:VP["bands"][0] + 1], op0=ALU.mult, op1=ALU.mult), reads=[rnv, self.rvp], writes=[rh1])
            P.op("dve", lambda e: e.tensor_scalar(out=h1[0:33, :], in0=h1[0:33, :], scalar1=self.vp[0:33, VP["phase"][0]:VP["phase"][0] + 1], scalar2=None, op0=ALU.add), reads=[rh1, self.rvp], writes=[rh1])
            with Stage(self) as S2:
                self.range_sin(S2, z[0:33, :], rz, h1[0:33, :], rh1, [33, Ls], 0.0, whole=False)
            P.op("dve", lambda e: e.tensor_scalar(out=z[0:1, :], in0=nv[0:1, :], scalar1=1.0 / (Ls - 1), scalar2=None, op0=ALU.mult), reads=[rnv, rz], writes=[rz])
            P.op("dve", lambda e: e.tensor_scalar(out=tv[:], in0=nv[0:1, :], scalar1=1.0 / (Ls - 1), scalar2=None, op0=ALU.mult), reads=[rnv], writes=[rtv])
            psp = S.pool("hfps", 2, [128, 512], F32, psum=True)
            for (wt, rwt, kk, src, rsrc, dst, rdst, bname) in ((w1, rw1, 33, z, rz, h1, rh1, "hy_b1"), (w2, rw2, 64, h1, rh1, h2, rh2, "hy_b2")):
                pre, rpre = S.sb("hpre", [64, Ls], F32)
                for c0 in range(0, Ls, 512):
                    csz = min(512, Ls - c0)
                    ps, rps = psp.get()
                    P.op("pe", (lambda ps=ps, wt=wt, kk=kk, src=src, c0=c0, csz=csz: lambda e: e.matmul(ps[0:64, :csz], lhsT=wt[0:kk, :], rhs=src[0:kk, c0:c0 + csz], start=True, stop=True))(), reads=[rwt, rsrc], writes=[rps])
                    P.op("dve", (lambda ps=ps, pre=pre, c0=c0, bname=bname, csz=csz: lambda e: e.tensor_scalar(out=pre[:, c0:c0 + csz], in0=ps[0:64, :csz], scalar1=self.vp[0:64, VP[bname][0]:VP[bname][0] + 1], scalar2=self.vp[0:64, VP["hy_freq"][0]:VP["hy_freq"][0] + 1], op0=ALU.add, op1=ALU.mult))(), reads=[rps, self.rvp], writes=[rpre])
                with Stage(self) as S2:
                    self.range_sin(S2, dst[:, :], rdst, pre[:, :], rpre, [64, Ls], 0.0, whole=False)
            psn = [S.ps("hpsn", [128, 512], F32) for _ in range(4)]
            psw, rpsw = S.ps("hpsw", [128, 512], F32)
            winp = S.pool("hwin", 2, [128, 512], F32)
            fp_ = S.pool("hf", 4, [128, 512], F32)
            abp = S.pool("hab", 2, [128, 512], BF16)
            hsp = S.pool("hhs", 2, [128, 512], BF16)
            hdp = S.pool("hhd", 2, [128, 512], BF16)
            for a in range(nt):
                for o in range(2):
                    for ct in range(2):
                        P.op("pe", (lambda a=a, o=o, ct=ct: lambda e: e.matmul(psw[:], lhsT=tv[0:1, a * 128:(a + 1) * 128], rhs=dec[0:1, o * 1024 + ct * 512:o * 1024 + ct * 512 + 512], start=True, stop=True))(), reads=[rtv, rdec], writes=[rpsw])
                        win, rwin = winp.get()
                        P.op("act", (lambda win=win: lambda e: e.activation(out=win[:], in_=psw[:], func=AF.Exp, scale=-1.0))(), reads=[rpsw], writes=[rwin])
                        fs = []
                        for d in range(2):
                            ps, rps = psp.get()
                            col = o * 2048 + d * 1024 + ct * 512
                            P.op("pe", (lambda ps=ps, a=a, col=col: lambda e: e.matmul(ps[:], lhsT=h2[:, a * 128:(a + 1) * 128], rhs=w3[:, col:col + 512], start=True, stop=True))(), reads=[rh2, rw3], writes=[rps])
                            f, rf = fp_.get()
                            P.op("dve", (lambda f=f, ps=ps, win=win: lambda e: e.tensor_tensor(out=f[:], in0=ps[:], in1=win[:], op=ALU.mult))(), reads=[rps, rwin], writes=[rf])
                            ab, rab = abp.get()
                            P.op("dve", (lambda ab=ab, f=f: lambda e: e.scalar_tensor_tensor(out=ab[:], in0=f[:], scalar=-1.0, in1=f[:], op0=ALU.mult, op1=ALU.max))(), reads=[rf], writes=[rab])
                            pn, rpn = psn[o * 2 + ct]
                            P.op("pe", (lambda pn=pn, ab=ab, a=a, d=d: lambda e: e.matmul(pn[:], lhsT=self.ones_bf[:], rhs=ab[:], start=(a == 0 and d == 0), stop=(a == nt - 1 and d == 1)))(), reads=[self.rones, rab], writes=[rpn])
                            fs.append((f, rf))
                        (f0, rf0), (f1, rf1) = fs
                        hs, rhs_ = hsp.get()
                        hd, rhd = hdp.get()
                        P.op("pool", (lambda hs=hs, f0=f0, f1=f1: lambda e: e.tensor_tensor(out=hs[:], in0=f0[:], in1=f1[:], op=ALU.add))(), reads=[rf0, rf1], writes=[rhs_])
                        P.op("pool", (lambda hd=hd, f0=f0, f1=f1: lambda e: e.tensor_tensor(out=hd[:], in0=f0[:], in1=f1[:], op=ALU.subtract))(), reads=[rf0, rf1], writes=[rhd])
                        c2 = o * 1024 + ct * 512
                        P.dma("sp", (lambda hs=hs, a=a, c2=c2: lambda e: e.dma_start(out=self.dr["HS" + nm][a, :, c2:c2 + 512], in_=hs[:]))(), reads=[rhs_], writes=[self.drreg["HS" + nm]])
                        P.dma("sp", (lambda hd=hd, a=a, c2=c2: lambda e: e.dma_start(out=self.dr["HD" + nm][a, :, c2:c2 + 512], in_=hd[:]))(), reads=[rhd], writes=[self.drreg["HD" + nm]])
            rn, rrn = S.sb("hrn", [128, 2048], F32)
            for i in range(4):
                pn, rpn = psn[i]
                P.op("dve", (lambda pn=pn, i=i: lambda e: e.tensor_scalar(out=rn[:, i * 512:(i + 1) * 512], in0=pn[:], scalar1=EPS, scalar2=None, op0=ALU.add))(), reads=[rpn], writes=[rrn])
            P.op("dve", lambda e: e.reciprocal(out=rn[:], in_=rn[:]), reads=[rrn], writes=[rrn])
            P.dma("sp", lambda e: e.dma_start(out=self.dr["RN" + nm], in_=rn[:]), reads=[rrn], writes=[self.drreg["RN" + nm]])

    def hy_gemm(self, S, wnames, nm, xs, n_k, evac, nchunks):
        P = self.P
        wps = [S.pool("hgw%d" % i, 2, [128, n_k, 128], BF16) for i in range(len(wnames))]
        pps = [S.pool("hgp%d" % i, 2, [128, 512], F32, psum=True) for i in range(len(set(g for (_, g) in wnames)))]
        def load(oc):
            wts = []
            for i, (wn, g) in enumerate(wnames):
                wt, rwt = wps[i].get()
                P.dma("sp" if i % 2 == 0 else "act", (lambda wt=wt, wn=wn, oc=oc: lambda e: e.dma_start(out=wt[:], in_=self.dr[wn + nm][oc]))(), reads=[self.drreg[wn + nm]], writes=[rwt])
                wts.append((wt, rwt))
            return wts

        nxt = load(0)
        for oc in range(nchunks):
            wts = nxt
            if oc + 1 < nchunks:
                nxt = load(oc + 1)
            outs = {}
            groups = sorted(set(g for (_, g) in wnames))
            for g in groups:
                outs[g] = pps[g].get()
            cntg = {g: 0 for g in groups}
            totg = {g: sum(1 for (_, gg) in wnames if gg == g) * n_k for g in groups}
            for i, (wn, g) in enumerate(wnames):
                wt, rwt = wts[i]
                x, rx = xs[i]
                ps, rps = outs[g]
                for a in range(n_k):
                    first = cntg[g] == 0
                    cntg[g] += 1
                    last = cntg[g] == totg[g]
                    P.op("pe", (lambda ps=ps, wt=wt, x=x, a=a, first=first, last=last: lambda e: e.matmul(ps[:], lhsT=wt[:, a, :], rhs=x[:, a, :], start=first, stop=last))(), reads=[rwt, rx], writes=[rps])
            evac(oc, outs)

    def hy_spectra(self, Ls):
        P = self.P
        nt = Ls // 128
        nm = "L%d" % Ls
        self.dram("GR" + nm, [nt, 128, 2048], BF16, "Internal")
        self.dram("GQ" + nm, [nt, 128, 2048], BF16, "Internal")
        with Stage(self) as S:
            rn, rrn = S.sb("srn", [128, 2048], F32)
            P.dma("sp", lambda e: e.dma_start(out=rn[:], in_=self.dr["RN" + nm]), reads=[self.drreg["RN" + nm]], writes=[rrn])
            hs, rhs_ = S.sb("shs", [128, nt, 512], BF16)
            hd, rhd = S.sb("shd", [128, nt, 512], BF16)
            gop = S.pool("sgo", 4, [128, 512], BF16)
            for ct in range(4):
                P.dma("sp", (lambda ct=ct: lambda e: e.dma_start(out=hs[:], in_=self.dr["HS" + nm][:, :, ct * 512:(ct + 1) * 512].rearrange("a p c -> p a c")))(), reads=[self.drreg["HS" + nm]], writes=[rhs_])
                P.dma("sp", (lambda ct=ct: lambda e: e.dma_start(out=hd[:], in_=self.dr["HD" + nm][:, :, ct * 512:(ct + 1) * 512].rearrange("a p c -> p a c")))(), reads=[self.drreg["HD" + nm]], writes=[rhd])

                def evac(fc, outs, ct=ct):
                    for g, name in ((0, "GR"), (1, "GQ")):
                        ps, rps = outs[g]
                        go, rgo = gop.get()
                        P.op("dve", (lambda go=go, ps=ps: lambda e: e.tensor_tensor(out=go[:], in0=ps[:], in1=rn[:, ct * 512:(ct + 1) * 512], op=ALU.mult))(), reads=[rps, rrn], writes=[rgo])
                        P.dma("sp", (lambda go=go, name=name, fc=fc: lambda e: e.dma_start(out=self.dr[name + nm][fc, :, ct * 512:(ct + 1) * 512], in_=go[:]))(), reads=[rgo], writes=[self.drreg[name + nm]])

                with Stage(self) as S2:
                    self.hy_gemm(S2, [("CF", 0), ("SF", 1)], nm, [(hs, rhs_), (hd, rhd)], nt, evac, nt)

    def hy_conv(self, Ls, blk0):
        P = self.P
        nt = Ls // 128
        nm = "L%d" % Ls
        with Stage(self) as S:
            u, ru = S.sb("cu", [128, nt, 512], BF16)
            Yr, rYr = S.sb("cYr", [128, nt, 512], BF16)
            Yq, rYq = S.sb("cYq", [128, nt, 512], BF16)
            skb, rskb = S.sb("cskb", [128, 512], F32)
            grp = S.pool("cgr", 2, [128, 512], BF16)
            gqp = S.pool("cgq", 2, [128, 512], BF16)
            ap_ = S.pool("cA", 2, [128, 512], F32)
            bp_ = S.pool("cB", 2, [128, 512], F32)
            t1p = S.pool("ct1", 2, [128, 512], F32)
            t2p = S.pool("ct2", 2, [128, 512], F32)
            xgp = S.pool("cxg", 2, [128, 512], BF16)
            zop = S.pool("czo", 2, [128, 512], BF16)
            for o in range(2):
                for ct in range(2):
                    cs = slice(ct * 512, (ct + 1) * 512)
                    src = self.dr["utm"][0] if o == 0 else self.dr["z1"]
                    rsrc = self.drreg["utm"] if o == 0 else self.drreg["z1"]
                    P.dma("sp", (lambda src=src, cs=cs: lambda e: e.dma_start(out=u[:], in_=src[blk0:blk0 + nt, :, cs].rearrange("a p c -> p a c")))(), reads=[rsrc], writes=[ru])
                    sk0 = o * 1024 + ct * 512
                    P.dma("sp", (lambda sk0=sk0: lambda e: e.dma_start(out=skb[:], in_=bcast_rows(self.dr["hy_skip"][0:1, sk0:sk0 + 512], 512)))(), reads=[self.drreg["hy_skip"]], writes=[rskb])

                    def evac_f(fc, outs, o=o, ct=ct):
                        pa, rpa = outs[0]
                        pb, rpb = outs[1]
                        gr, rgr = grp.get()
                        gq, rgq = gqp.get()
                        gc0 = o * 1024 + ct * 512
                        P.dma("sp", (lambda gr=gr, fc=fc, gc0=gc0: lambda e: e.dma_start(out=gr[:], in_=self.dr["GR" + nm][fc, :, gc0:gc0 + 512]))(), reads=[self.drreg["GR" + nm]], writes=[rgr])
                        P.dma("sp", (lambda gq=gq, fc=fc, gc0=gc0: lambda e: e.dma_start(out=gq[:], in_=self.dr["GQ" + nm][fc, :, gc0:gc0 + 512]))(), reads=[self.drreg["GQ" + nm]], writes=[rgq])
                        A, rA = ap_.get()
                        Bq, rBq = bp_.get()
                        P.op("act", (lambda A=A, pa=pa: lambda e: e.copy(out=A[:], in_=pa[:]))(), reads=[rpa], writes=[rA])
                        P.op("act", (lambda Bq=Bq, pb=pb: lambda e: e.copy(out=Bq[:], in_=pb[:]))(), reads=[rpb], writes=[rBq])
                        t1, rt1 = t1p.get()
                        t2, rt2 = t2p.get()
                        P.op("dve", (lambda t1=t1, A=A, gr=gr: lambda e: e.tensor_tensor(out=t1[:], in0=A[:], in1=gr[:], op=ALU.mult))(), reads=[rA, rgr], writes=[rt1])
                        P.op("pool", (lambda t2=t2, Bq=Bq, gq=gq: lambda e: e.tensor_tensor(out=t2[:], in0=Bq[:], in1=gq[:], op=ALU.mult))(), reads=[rBq, rgq], writes=[rt2])
                        P.op("dve", (lambda t1=t1, t2=t2, fc=fc: lambda e: e.tensor_tensor(out=Yr[:, fc, :], in0=t1[:], in1=t2[:], op=ALU.subtract))(), reads=[rt1, rt2], writes=[rYr])
                        t3, rt3 = t1p.get()
                        t4, rt4 = t2p.get()
                        P.op("dve", (lambda t3=t3, A=A, gq=gq: lambda e: e.tensor_tensor(out=t3[:], in0=A[:], in1=gq[:], op=ALU.mult))(), reads=[rA, rgq], writes=[rt3])
                        P.op("pool", (lambda t4=t4, Bq=Bq, gr=gr: lambda e: e.tensor_tensor(out=t4[:], in0=Bq[:], in1=gr[:], op=ALU.mult))(), reads=[rBq, rgr], writes=[rt4])
                        P.op("pool", (lambda t3=t3, t4=t4, fc=fc: lambda e: e.tensor_tensor(out=Yq[:, fc, :], in0=t3[:], in1=t4[:], op=ALU.add))(), reads=[rt3, rt4], writes=[rYq])

                    with Stage(self) as S2:
                        self.hy_gemm(S2, [("CF", 0), ("SF", 1)], nm, [(u, ru), (u, ru)], nt, evac_f, nt)

                    def evac_i(tc, outs, o=o, ct=ct, cs=cs):
                        py, rpy = outs[0]
                        xg, rxg = xgp.get()
                        P.dma("sp", (lambda xg=xg, tc=tc: lambda e: e.dma_start(out=xg[:], in_=self.dr["utm"][1 + o][blk0 + tc, :, cs]))(), reads=[self.drreg["utm"]], writes=[rxg])
                        t1, rt1 = t1p.get()
                        P.op("pool", (lambda t1=t1, tc=tc: lambda e: e.tensor_tensor(out=t1[:], in0=u[:, tc, :], in1=skb[:], op=ALU.mult))(), reads=[ru, rskb], writes=[rt1])
                        P.op("dve", (lambda t1=t1, py=py: lambda e: e.tensor_tensor(out=t1[:], in0=py[:], in1=t1[:], op=ALU.add))(), reads=[rpy, rt1], writes=[rt1])
                        zo, rzo = zop.get()
                        P.op("dve", (lambda zo=zo, t1=t1, xg=xg: lambda e: e.tensor_tensor(out=zo[:], in0=t1[:], in1=xg[:], op=ALU.mult))(), reads=[rt1, rxg], writes=[rzo])
                        dst = "z1" if o == 0 else "ytm"
                        P.dma("sp", (lambda zo=zo, dst=dst, tc=tc: lambda e: e.dma_start(out=self.dr[dst][blk0 + tc, :, cs], in_=zo[:]))(), reads=[rzo], writes=[self.drreg[dst]])

                    with Stage(self) as S2:
                        self.hy_gemm(S2, [("CI", 0), ("SI", 0)], nm, [(Yr, rYr), (Yq, rYq)], nt, evac_i, nt)

    def hy_out(self):
        P = self.P
        with Stage(self) as S:
            yin_p = S.pool("yin", 2, [128, 1024], BF16)
            ptr = S.pool("yptr", 2, [128, 1024], BF16, psum=True)
            stg_p = S.pool("ystg", 2, [128, 8, 128], BF16)
            for blk in range(34):
                yin, ryin = yin_p.get()
                P.dma("sp", (lambda yin=yin, blk=blk: lambda e: e.dma_start(out=yin[:], in_=self.dr["ytm"][blk]))(), reads=[self.drreg["ytm"]], writes=[ryin])
                pt_, rpt_ = ptr.get()
                for c in range(8):
                    P.op("pe", (lambda pt_=pt_, yin=yin, c=c: lambda e: e.transpose(pt_[:, c * 128:(c + 1) * 128], yin[:, c * 128:(c + 1) * 128], self.ident_bf[:]))(), reads=[ryin, self.rident_bf], writes=[rpt_])
                stg, rstg = stg_p.get()
                P.op("act" if blk % 2 == 0 else "dve", (lambda pt_=pt_, stg=stg, blk=blk: (lambda e: e.copy(out=stg[:], in_=pt_[:].rearrange("p (a b) -> p a b", b=128))) if blk % 2 == 0 else (lambda e: e.tensor_copy(out=stg[:], in_=pt_[:].rearrange("p (a b) -> p a b", b=128))))(), reads=[rpt_], writes=[rstg])
                P.dma("sp", (lambda stg=stg, blk=blk: lambda e: e.dma_start(out=self.dr["yt"][:, :, blk * 128:(blk + 1) * 128].rearrange("c p n -> p c n"), in_=stg[:]))(), reads=[rstg], writes=[self.drreg["yt"]])

    def stage_hyena(self):
        self.hy_prep()
        for Ls, blk0 in ((NCX, 0), (L, 2)):
            if Ls not in self.dft_done:
                self.hy_dft_gen(Ls)
                self.dft_done.add(Ls)
            self.hy_filters(Ls)
            self.hy_spectra(Ls)
            self.hy_conv(Ls, blk0)
        self.hy_out()

    def build(self, stages=None):
        on = lambda s_: stages is None or s_ in stages
        for idx in range(len(self.layers)):
            self.set_layer(idx)
            with Stage(self) as S0:
                self.load_consts(S0)
                self.mod_vectors(S0)
                if on("inproj"):
                    self.stage_inproj()
                if on("attn"):
                    with Stage(self) as SA:
                        self.rope_tables(SA)
                        self.stage_attn()
                if on("lru"):
                    self.stage_lru()
                if on("hyena"):
                    self.stage_hyena()
                if on("merge"):
                    self.stage_merge()
                if on("ffn"):
                    self.stage_ffn()
                if idx == len(self.layers) - 1 and on("final"):
                    self.stage_final()
                self.P.flush()
        return self.nc


def make_xt(inp, b):
    tok = np.concatenate([inp["ctx"][b], inp["x"][b]], axis=0)
    return np.ascontiguousarray(tok.T.reshape(KC, 128, T).transpose(1, 0, 2))


def layer_inputs(inp, li, b):
    j = li // 2
    m = {"vp": make_vp(inp, li, b), "w_mod": inp["w_mod"][li], "w_in": inp["w_in"][li],
         "w_br": inp["w_br"][li], "w_out": inp["w_out"][li], "lru_wa": inp["lru_wa"][li], "lru_wi": inp["lru_wi"][li],
         "hy_w1": inp["hy_f_w1"][li], "hy_w2": inp["hy_f_w2"][li], "hy_w3": inp["hy_f_w3"][li],
         "hy_decay": inp["hy_decay"][li].reshape(1, 2048), "hy_skip": inp["hy_skip"][li].reshape(1, 2048),
         "da_lam": inp["da_lambda"][li].reshape(1, 256)}
    if li % 2 == 0:
        m["f_wg"] = inp["ffn_w_gate"][j][None]
        m["f_wu"] = inp["ffn_w_up"][j][None]
        m["f_wd"] = inp["ffn_w_down"][j][None]
    else:
        m["f_wg"] = inp["moe_w_gate"][j]
        m["f_wu"] = inp["moe_w_up"][j]
        m["f_wd"] = inp["moe_w_down"][j]
        m["router"] = inp["moe_router"][j]
    return {k + "_L%d" % li: np.ascontiguousarray(np.asarray(v, np.float32)) for k, v in m.items()}


def kernel(**inputs):
    inp = {k: np.asarray(v) for k, v in inputs.items()}
    nb = inp["x"].shape[0]
    layers = (0, 1, 2, 3)
    bld = Builder(layers)
    nc = bld.build()
    shared = {}
    in_maps = []
    for b in range(nb):
        m = {"x_ext": make_xt(inp, b)}
        for li in layers:
            li_in = layer_inputs(inp, li, b)
            for k, v in li_in.items():
                if k.startswith("vp"):
                    m[k] = v
                else:
                    m[k] = shared.setdefault(k, v)
        in_maps.append(m)
    res = run_bass_kernel_spmd(nc, in_maps, core_ids=list(range(nb)))
    out = np.empty((nb, L, D), np.float32)
    for b in range(nb):
        y = np.asarray(res.results[b]["y_out"], np.float32)
        out[b] = y.transpose(1, 0, 2).reshape(D, L).T
    return out
```

```python
import math
from contextlib import ExitStack
import numpy as np
import concourse.bass as bass
import concourse.mybir as mybir
from concourse.bass_utils import run_bass_kernel_spmd

F32 = mybir.dt.float32
BF16 = mybir.dt.bfloat16
ALU = mybir.AluOpType
AF = mybir.ActivationFunctionType
AX = mybir.AxisListType

D = 1024
L = 4096
NCX = 256
T = L + NCX
KC = 8
DFF = 2816
FC = DFF // 128
NE = 8
C_K, C_V, C_LX, C_Q, C_LY, C_HY, C_G, C_END = 0, 1024, 2048, 3072, 4096, 5120, 8192, 11264
EPS = 1e-6
TT = [(0, 256)] + [(256 + 512 * i, 512) for i in range(8)]
MAGIC = 12582912.0
PI = math.pi

ENGS = ("pe", "act", "dve", "pool", "sp")
SAME_ENGINE_SYNC = True
NDMASEM = 6
NPESEM = 6
KEEP_WARM = False


class Reg:
    __slots__ = ("w", "r", "name", "multi")

    def __init__(self, name="", multi=False):
        self.w = {}
        self.r = {}
        self.name = name
        self.multi = multi


class Prog:
    def __init__(self, nc):
        self.nc = nc
        self.q = {e: [] for e in ENGS}
        self.sems = {}
        self.cnt = {}
        self.key_eng = {}
        self.keys = {}
        for e in ENGS:
            nk = NPESEM if e == "pe" else 1
            self.keys[e] = []
            for i in range(nk):
                k = e if i == 0 else "%s%d" % (e, i)
                self.sems[k] = nc.alloc_semaphore(name="s_" + k)
                self.cnt[k] = 0
                self.key_eng[k] = e
                self.keys[e].append(k)
        self.cur = {e: 0 for e in ENGS}
        self.dma_next = {}
        for e in ("sp", "act", "pool"):
            for i in range(NDMASEM):
                k = "d_%s%d" % (e, i)
                self.sems[k] = nc.alloc_semaphore(name=k)
                self.cnt[k] = 0
            self.dma_next[e] = 0
        self.seen = {e: {} for e in ENGS}
        self.n_ins = 0
        self.n_wait = 0

    def _need(self, eng, tok, waits):
        k, v = tok
        if self.key_eng.get(k) == eng and (eng == "pe" or not SAME_ENGINE_SYNC):
            return
        if self.seen[eng].get(k, 0) >= v:
            return
        waits[k] = max(waits.get(k, 0), v)

    def _deps(self, eng, reads, writes):
        waits = {}
        for r in reads:
            for k, v in r.w.items():
                self._need(eng, (k, v), waits)
        for w in writes:
            if not w.multi:
                for k, v in w.w.items():
                    self._need(eng, (k, v), waits)
            for k, v in w.r.items():
                self._need(eng, (k, v), waits)
        for k, v in waits.items():
            self.seen[eng][k] = v
        return waits

    def _commit(self, tok, reads, writes):
        k, v = tok
        for r in reads:
            r.r[k] = max(r.r.get(k, 0), v)
        for w in writes:
            if w.multi:
                w.w[k] = max(w.w.get(k, 0), v)
            else:
                w.w = {k: v}
            w.r = {}

    def op(self, eng, fn, reads=(), writes=()):
        waits = self._deps(eng, reads, writes)
        key = self.keys[eng][self.cur[eng]]
        self.cnt[key] += 1
        tok = (key, self.cnt[key])
        self.q[eng].append((waits, fn, key, 1))
        self._commit(tok, reads, writes)
        self.n_ins += 1
        self.n_wait += len(waits)
        return tok

    def dma(self, eng, fn, reads=(), writes=()):
        i = self.dma_next[eng]
        self.dma_next[eng] = (i + 1) % NDMASEM
        k = "d_%s%d" % (eng, i)
        waits = self._deps(eng, reads, writes)
        prev = self.cnt[k]
        if prev > 0 and self.seen[eng].get(k, 0) < prev:
            waits[k] = max(waits.get(k, 0), prev)
            self.seen[eng][k] = prev
        self.cnt[k] += 16
        tok = (k, self.cnt[k])
        self.q[eng].append((waits, fn, k, 16))
        self._commit(tok, reads, writes)
        self.n_ins += 1
        self.n_wait += len(waits)
        return tok

    def barrier(self):
        for e in ENGS:
            waits = {}
            for k, v in self.cnt.items():
                if v > 0 and self.seen[e].get(k, 0) < v:
                    waits[k] = v
                    self.seen[e][k] = v
            if waits:
                self.q[e].append((waits, None, None, 0))
        self.cur["pe"] = (self.cur["pe"] + 1) % len(self.keys["pe"])

    def finish(self):
        self.flush()

    def flush(self):
        self.barrier()
        sems = self.sems
        q = self.q
        self.q = {e: [] for e in ENGS}

        def emit(e, lst):
            for waits, fn, k, inc in lst:
                for wk, wv in waits.items():
                    e.wait_ge(sems[wk], wv)
                if fn is not None:
                    fn(e).then_inc(sems[k], inc)

        with self.nc.Block() as block:
            @block.tensor
            def _(e):
                emit(e, q["pe"])

            @block.scalar
            def _(e):
                emit(e, q["act"])

            @block.vector
            def _(e):
                emit(e, q["dve"])

            @block.gpsimd
            def _(e):
                emit(e, q["pool"])

            @block.sync
            def _(e):
                emit(e, q["sp"])


def bcast_rows(a, n):
    return bass.AP(a.tensor, a.offset, [[0, 128], [1, n]])


_uid = [0]


def uname(s):
    _uid[0] += 1
    return "%s_%d" % (s, _uid[0])


class Stage:
    def __init__(self, B):
        self.B = B
        self.es = ExitStack()

    def sb(self, name, shape, dt):
        t = self.es.enter_context(self.B.nc.sbuf_tensor(uname(name), list(shape), dt))
        return t, Reg(name)

    def ps(self, name, shape, dt=F32):
        t = self.es.enter_context(self.B.nc.psum_tensor(uname(name), list(shape), dt))
        return t, Reg(name)

    def pool(self, name, n, shape, dt, psum=False):
        return RPool([(self.ps if psum else self.sb)(name, shape, dt) for _ in range(n)])

    def close(self):
        self.B.P.barrier()
        self.es.close()

    def __enter__(self):
        return self

    def __exit__(self, *a):
        if a[0] is None:
            self.close()
        return False


class RPool:
    def __init__(self, tiles):
        self.tiles = tiles
        self.i = 0

    def get(self):
        t = self.tiles[self.i]
        self.i = (self.i + 1) % len(self.tiles)
        return t


def _cols(v):
    v = np.asarray(v, np.float32).reshape(-1, 128)
    return np.ascontiguousarray(v.T)


VP = {}
_o = 0
for _n, _w in [("b_mod", 48), ("g_mix", 8), ("g_ffn", 8), ("b_gate", 24), ("lru_cw", 32), ("lru_cb", 8),
               ("lru_ba", 16), ("lru_bi", 16), ("lru_lam", 16), ("hy_cw", 72), ("hy_cb", 24), ("subln", 1),
               ("c", 8), ("c_ctx", 8), ("g_final", 8), ("hy_b1", 1), ("hy_b2", 1), ("hy_freq", 1),
               ("bands", 1), ("phase", 1), ("ropeinv", 1), ("pidx", 1), ("ident", 128), ("rmat", 128)]:
    VP[_n] = (_o, _w)
    _o += _w
NV = _o


def make_vp(inp, li, b):
    vp = np.zeros((128, NV), np.float32)

    def put(n, a):
        o, w = VP[n]
        a = np.asarray(a, np.float32)
        assert a.shape[1] == w, (n, a.shape, w)
        vp[:a.shape[0], o:o + w] = a

    put("b_mod", _cols(inp["b_mod"][li]))
    put("g_mix", _cols(inp["g_mix"][li]))
    put("g_ffn", _cols(inp["g_ffn"][li]))
    put("b_gate", _cols(inp["b_gate"][li]))
    put("lru_cw", _cols(inp["lru_conv_w"][li].reshape(-1)))
    put("lru_cb", _cols(inp["lru_conv_b"][li]))
    put("lru_ba", _cols(inp["lru_ba"][li].reshape(-1)))
    put("lru_bi", _cols(inp["lru_bi"][li].reshape(-1)))
    put("lru_lam", _cols(inp["lru_lambda"][li].reshape(-1)))
    put("hy_cw", _cols(inp["hy_conv_w"][li].reshape(-1)))
    put("hy_cb", _cols(inp["hy_conv_b"][li]))
    put("subln", _cols(inp["da_subln_g"][li]))
    put("c", _cols(inp["c"][b]))
    put("c_ctx", _cols(inp["c_ctx"]))
    put("g_final", _cols(inp["g_final"]))
    put("hy_b1", inp["hy_f_b1"][li].reshape(64, 1))
    put("hy_b2", inp["hy_f_b2"][li].reshape(64, 1))
    put("hy_freq", inp["hy_f_freq"][li].reshape(64, 1))
    bands = np.linspace(1e-4, 15.0, 16, dtype=np.float32)
    bcol = np.zeros((33, 1), np.float32)
    bcol[1:17, 0] = bands
    bcol[17:33, 0] = bands
    pcol = np.zeros((33, 1), np.float32)
    pcol[1:17, 0] = np.float32(PI / 2)
    pcol[17:33, 0] = np.float32(PI)
    put("bands", bcol)
    put("phase", pcol)
    inv = (10000.0 ** (-(np.arange(128) % 16).astype(np.float32) / 16.0)).astype(np.float32)
    put("ropeinv", inv.reshape(128, 1))
    put("pidx", np.arange(128, dtype=np.float32).reshape(128, 1))
    put("ident", np.eye(128, dtype=np.float32))
    rm = np.zeros((128, 128), np.float32)
    for m in range(128):
        if m % 32 < 16:
            rm[m + 16, m] = -1.0
        else:
            rm[m - 16, m] = 1.0
    put("rmat", rm)
    return vp


class Builder:
    LAYER_INPUTS = [("vp", [128, NV]), ("w_mod", [D, 6 * D]), ("w_in", [D, C_END]), ("w_br", [3, D, D]), ("w_out", [D, D]),
                    ("lru_wa", [2, 8, 128, 128]), ("lru_wi", [2, 8, 128, 128]), ("hy_w1", [33, 64]), ("hy_w2", [64, 64]),
                    ("hy_w3", [64, 4096]), ("hy_decay", [1, 2048]), ("hy_skip", [1, 2048]), ("da_lam", [1, 256])]

    def __init__(self, layers=(0, 1, 2, 3), debug=False):
        self.layers = list(layers)
        self.debug = debug
        nc = bass.Bass("TRN2", target_bir_lowering=False)
        self.nc = nc
        self.P = Prog(nc)
        self.dr = {}
        self.drreg = {}
        self.dft_done = set()
        I = "ExternalInput"
        self.dram("x_ext", [128, KC, T], F32, I)
        for li in self.layers:
            sfx = "_L%d" % li
            for nm, shp in self.LAYER_INPUTS:
                self.dram(nm + sfx, shp, F32, I)
            ne = NE if li % 2 == 1 else 1
            self.dram("f_wg" + sfx, [ne, D, DFF], F32, I)
            self.dram("f_wu" + sfx, [ne, D, DFF], F32, I)
            self.dram("f_wd" + sfx, [ne, DFF, D], F32, I)
            if li % 2 == 1:
                self.dram("router" + sfx, [D, NE], F32, I)
        dk = "Internal"
        self.dram("pt", [88, 128, T], BF16, dk)
        self.dram("at", [8, 128, T], BF16, dk)
        self.dram("rt", [8, 128, T], BF16, dk)
        self.dram("yt", [8, 128, T], BF16, dk)
        dk2 = "ExternalOutput" if debug else "Internal"
        self.dram("xt_mid", [128, KC, T], F32, dk2)
        self.dram("xr0", [128, KC, T], F32, dk2)
        self.dram("xr1", [128, KC, T], F32, dk2)
        self.dram("y_out", [128, KC, L], F32, "ExternalOutput")

    def set_layer(self, idx):
        li = self.layers[idx]
        self.li = li
        self.moe = (li % 2 == 1)
        self.ne = NE if self.moe else 1
        self.lam_init = 0.8 - 0.6 * math.exp(-0.3 * li)
        sfx = "_L%d" % li
        names = [n for n, _ in self.LAYER_INPUTS] + ["f_wg", "f_wu", "f_wd"] + (["router"] if self.moe else [])
        for n in names:
            self.dr[n] = self.dr[n + sfx]
            self.drreg[n] = self.drreg[n + sfx]
        xin = "x_ext" if idx == 0 else "xr%d" % ((idx - 1) % 2)
        xout = "xr%d" % (idx % 2)
        for alias, real in (("xt", xin), ("xt_out", xout)):
            self.dr[alias] = self.dr[real]
            self.drreg[alias] = self.drreg[real]

    def dram(self, name, shape, dt, kind):
        if name in self.dr:
            return self.dr[name]
        self.dr[name] = self.nc.dram_tensor(name, list(shape), dt, kind=kind).ap()
        self.drreg[name] = Reg(name, multi=True)
        return self.dr[name]

    def load_consts(self, S):
        P, nc = self.P, self.nc
        self.vp, self.rvp = S.sb("vp", [128, NV], F32)
        vp, rvp = self.vp, self.rvp
        P.dma("sp", lambda e: e.dma_start(out=vp[:], in_=self.dr["vp"]), reads=[self.drreg["vp"]], writes=[rvp])
        self.ones_bf, self.rones = S.sb("ones", [128, 128], BF16)
        P.op("pool", lambda e: e.memset(self.ones_bf[:], 1.0), writes=[self.rones])
        self.epsc, self.repsc = S.sb("epsc", [128, 4], F32)
        P.op("pool", lambda e: e.memset(self.epsc[:, 0:1], EPS), writes=[self.repsc])
        P.op("pool", lambda e: e.memset(self.epsc[:, 1:2], 1.0), writes=[self.repsc])
        P.op("pool", lambda e: e.memset(self.epsc[:, 2:3], 0.0), writes=[self.repsc])
        self.ident_bf, self.rident_bf = S.sb("identbf", [128, 128], BF16)
        o = VP["ident"][0]
        P.op("dve", lambda e: e.tensor_copy(out=self.ident_bf[:], in_=vp[:, o:o + 128]), reads=[rvp], writes=[self.rident_bf])
        self.rmat_bf, self.rrmat = S.sb("rmatbf", [128, 128], BF16)
        o2 = VP["rmat"][0]
        P.op("dve", lambda e: e.tensor_copy(out=self.rmat_bf[:], in_=vp[:, o2:o2 + 128]), reads=[rvp], writes=[self.rrmat])

    def vcol(self, name, j=0, w=1):
        o = VP[name][0]
        return self.vp[:, o + j:o + j + w]

    def mod_vectors(self, S):
        P = self.P
        self.modv, self.rmodv = S.sb("modv", [128, 48, 2], F32)
        self.gsm, self.rgsm = S.sb("gsm", [128, 8, 2], F32)
        self.gsf, self.rgsf = S.sb("gsf", [128, 8, 2], F32)
        modv, rmodv = self.modv, self.rmodv
        with Stage(self) as S2:
            cs, rcs = S2.sb("cs", [128, 8, 2], F32)
            oc_, occ = VP["c"][0], VP["c_ctx"][0]
            vp = self.vp
            P.op("act", lambda e: e.activation(out=cs[:, :, 0], in_=vp[:, oc_:oc_ + 8], func=AF.Silu), reads=[self.rvp], writes=[rcs])
            P.op("act", lambda e: e.activation(out=cs[:, :, 1], in_=vp[:, occ:occ + 8], func=AF.Silu), reads=[self.rvp], writes=[rcs])
            wpool = S2.pool("wmod", 2, [128, 8, 768], F32)
            pspool = S2.pool("psmod", 2, [128, 2], F32, psum=True)
            wsrc = self.dr["w_mod"].rearrange("(k p) n -> p k n", p=128)
            ob = VP["b_mod"][0]
            for og in range(8):
                wt, rwt = wpool.get()
                P.dma("sp", (lambda wt=wt, og=og: lambda e: e.dma_start(out=wt[:], in_=wsrc[:, :, og * 768:(og + 1) * 768]))(),
                      reads=[self.drreg["w_mod"]], writes=[rwt])
                for oc in range(6):
                    ps, rps = pspool.get()
                    for k in range(8):
                        P.op("pe", (lambda ps=ps, wt=wt, k=k, oc=oc: lambda e: e.matmul(ps[:], lhsT=wt[:, k, oc * 128:(oc + 1) * 128], rhs=cs[:, k, :], start=(k == 0), stop=(k == 7)))(),
                             reads=[rwt, rcs], writes=[rps])
                    j = og * 6 + oc
                    P.op("dve", (lambda ps=ps, j=j: lambda e: e.tensor_scalar(out=modv[:, j, :], in0=ps[:], scalar1=vp[:, ob + j:ob + j + 1], scalar2=None, op0=ALU.add))(),
                         reads=[rps, self.rvp], writes=[rmodv])
            for (gs, rgs, gname, j0) in ((self.gsm, self.rgsm, "g_mix", 8), (self.gsf, self.rgsf, "g_ffn", 32)):
                og_ = VP[gname][0]
                for w in range(2):
                    P.op("dve", (lambda gs=gs, j0=j0, w=w: lambda e: e.tensor_scalar(out=gs[:, :, w], in0=modv[:, j0:j0 + 8, w], scalar1=1.0, scalar2=None, op0=ALU.add))(),
                         reads=[rmodv], writes=[rgs])
                    P.op("dve", (lambda gs=gs, og_=og_, w=w: lambda e: e.tensor_tensor(out=gs[:, :, w], in0=gs[:, :, w], in1=vp[:, og_:og_ + 8], op=ALU.mult))(),
                         reads=[rgs, self.rvp], writes=[rgs])

    def norm_mod(self, S, src, tiles, gs, rgs, shift_j, out, rout, col0, shift=None, router=None):
        P = self.P
        xin_p = S.pool("nm_x", 1, [128, 8, 512], F32)
        sq_p = S.pool("nm_sq", 1, [128, 8, 512], BF16)
        rs_p = S.pool("nm_rs", 2, [128, 512], F32)
        tmp_p = S.pool("nm_t", 2, [128, 512], F32)
        ps_p = S.pool("nm_ps", 2, [128, 512], F32, psum=True)
        modv, rmodv = (self.modv, self.rmodv) if shift is None else shift
        if router is not None:
            f32_all = S.sb("nm_f32", [128, 8, 512], F32)
            f32, rf32 = f32_all
        for (n0, nsz) in tiles:
            w = 1 if n0 < NCX else 0
            xin, rxin = xin_p.get()
            P.dma("sp", (lambda xin=xin, n0=n0, nsz=nsz: lambda e: e.dma_start(out=xin[:, :, :nsz], in_=self.dr[src][:, :, n0:n0 + nsz]))(),
                  reads=[self.drreg[src]], writes=[rxin])
            sq, rsq = sq_p.get()
            P.op("act", (lambda sq=sq, xin=xin, nsz=nsz: lambda e: e.activation(out=sq[:, :, :nsz], in_=xin[:, :, :nsz], func=AF.Square))(),
                 reads=[rxin], writes=[rsq])
            ps, rps = ps_p.get()
            for k in range(8):
                P.op("pe", (lambda ps=ps, sq=sq, k=k, nsz=nsz: lambda e: e.matmul(ps[:, :nsz], lhsT=self.ones_bf[:], rhs=sq[:, k, :nsz], start=(k == 0), stop=(k == 7)))(),
                     reads=[rsq, self.rones], writes=[rps])
            rs, rrs = rs_p.get()
            P.op("act", (lambda rs=rs, ps=ps, nsz=nsz: lambda e: e.activation(out=rs[:, :nsz], in_=ps[:, :nsz], func=AF.Sqrt, bias=self.epsc[:, 0:1], scale=1.0 / D))(),
                 reads=[rps, self.repsc], writes=[rrs])
            P.op("dve", (lambda rs=rs, nsz=nsz: lambda e: e.reciprocal(out=rs[:, :nsz], in_=rs[:, :nsz]))(), reads=[rrs], writes=[rrs])
            for k in range(8):
                tmp, rtmp = tmp_p.get()
                P.op("dve", (lambda tmp=tmp, xin=xin, rs=rs, k=k, nsz=nsz: lambda e: e.tensor_tensor(out=tmp[:, :nsz], in0=xin[:, k, :nsz], in1=rs[:, :nsz], op=ALU.mult))(),
                     reads=[rxin, rrs], writes=[rtmp])
                P.op("act", (lambda tmp=tmp, k=k, nsz=nsz, n0=n0, w=w: lambda e: e.activation(out=out[:, k, n0 - col0:n0 - col0 + nsz], in_=tmp[:, :nsz], func=AF.Identity,
                                                                                         bias=modv[:, shift_j * 8 + k, w:w + 1], scale=gs[:, k, w:w + 1]))(),
                     reads=[rtmp, rmodv, rgs], writes=[rout])
                if router is not None:
                    f32, rf32 = f32_all
                    P.op("act", (lambda tmp=tmp, k=k, nsz=nsz, w=w: lambda e: e.activation(out=f32[:, k, :nsz], in_=tmp[:, :nsz], func=AF.Identity,
                                                                                bias=modv[:, shift_j * 8 + k, w:w + 1], scale=gs[:, k, w:w + 1]))(),
                         reads=[rtmp, rmodv, rgs], writes=[rf32])
            if router is not None:
                wr, rwr, psr, rpsr = router
                for sub in range(nsz // 128):
                    for k in range(8):
                        P.op("pe", (lambda sub=sub, k=k: lambda e: e.matmul(psr[:, sub, :], lhsT=f32[:, k, sub * 128:(sub + 1) * 128], rhs=wr[:, k, :], start=(k == 0), stop=(k == 7)))(),
                             reads=[rf32, rwr], writes=[rpsr])
            if router is not None:
                router_done = getattr(self, "_router_done")
                router_done(n0, nsz)

    def gemm(self, S, wsrc, rw, n_k, m_total, xT, rxT, tiles, col0, evac, mg=512, skip_chunks=()):
        P = self.P
        wp = S.pool("g_w", 2, [128, n_k, mg], BF16)
        psp = S.pool("g_ps", 4, [128, 512], F32, psum=True)
        for g0 in range(0, m_total, mg):
            gsz = min(mg, m_total - g0)
            chunks = [c for c in range(g0 // 128, (g0 + gsz) // 128) if c not in skip_chunks]
            if not chunks:
                continue
            wt, rwt = wp.get()
            P.dma("pool", (lambda wt=wt, g0=g0, gsz=gsz: lambda e: e.dma_start(out=wt[:, :, :gsz], in_=wsrc[:, :, g0:g0 + gsz]))(),
                  reads=[rw], writes=[rwt])
            for ti, (n0, nsz) in enumerate(tiles):
                for mc in chunks:
                    ps, rps = psp.get()
                    mo = mc * 128 - g0
                    for k in range(n_k):
                        P.op("pe", (lambda ps=ps, wt=wt, k=k, mo=mo, n0=n0, nsz=nsz: lambda e: e.matmul(ps[:, :nsz], lhsT=wt[:, k, mo:mo + 128], rhs=xT[:, k, n0 - col0:n0 - col0 + nsz], start=(k == 0), stop=(k == n_k - 1)))(),
                             reads=[rwt, rxT], writes=[rps])
                    evac(mc, ti, n0, nsz, ps, rps)

    def stage_inproj(self):
        P = self.P
        with Stage(self) as S:
            hT, rhT = S.sb("hT", [128, 8, T], BF16)
            with Stage(self) as S2:
                self.norm_mod(S2, "xt", TT, self.gsm, self.rgsm, 0, hT, rhT, 0)
            op_ = S.pool("ip_o", 4, [128, 512], BF16)
            cnt = [0]

            def evac(mc, ti, n0, nsz, ps, rps):
                ot, rot = op_.get()
                if cnt[0] % 2 == 0:
                    P.op("act", lambda e: e.copy(out=ot[:, :nsz], in_=ps[:, :nsz]), reads=[rps], writes=[rot])
                else:
                    P.op("dve", lambda e: e.tensor_copy(out=ot[:, :nsz], in_=ps[:, :nsz]), reads=[rps], writes=[rot])
                cnt[0] += 1
                P.dma("sp", lambda e: e.dma_start(out=self.dr["pt"][mc, :, n0:n0 + nsz], in_=ot[:, :nsz]), reads=[rot], writes=[self.drreg["pt"]])

            wsrc = self.dr["w_in"].rearrange("(k p) n -> p k n", p=128)
            self.gemm(S, wsrc, self.drreg["w_in"], 8, C_END, hT, rhT, TT, 0, evac)

    def range_sin(self, S, out, rout, src, rsrc, shape, shift=0.0, whole=True):
        P = self.P
        t1, rt1 = S.sb("rs_t1", shape, F32)
        t2, rt2 = S.sb("rs_t2", shape, F32)
        inv2pi = 1.0 / (2 * PI)
        if shift != 0.0:
            t0, rt0 = S.sb("rs_t0", shape, F32)
            P.op("dve", (lambda s0=src: lambda e: e.tensor_scalar(out=t0[:], in0=s0[:], scalar1=shift, scalar2=None, op0=ALU.add))(), reads=[rsrc], writes=[rt0])
            src, rsrc = t0, rt0
        P.op("dve", lambda e: e.tensor_scalar(out=t1[:], in0=src[:], scalar1=inv2pi, scalar2=MAGIC, op0=ALU.mult, op1=ALU.add), reads=[rsrc], writes=[rt1])
        P.op("dve", lambda e: e.tensor_scalar(out=t1[:], in0=t1[:], scalar1=-MAGIC, scalar2=None, op0=ALU.add), reads=[rt1], writes=[rt1])
        P.op("dve", lambda e: e.scalar_tensor_tensor(out=t2[:], in0=t1[:], scalar=-2 * PI, in1=src[:], op0=ALU.mult, op1=ALU.add), reads=[rt1, rsrc], writes=[rt2])
        P.op("dve", lambda e: e.tensor_scalar(out=t2[:], in0=t2[:], scalar1=PI, scalar2=-PI, op0=ALU.min, op1=ALU.max), reads=[rt2], writes=[rt2])
        P.op("act", lambda e: e.activation(out=out[:], in_=t2[:], func=AF.Sin), reads=[rt2], writes=[rout])

    def rope_tables(self, S):
        P = self.P
        self.cosT, self.rcosT = S.sb("cosT", [128, L], F32)
        self.sinT, self.rsinT = S.sb("sinT", [128, L], F32)
        with Stage(self) as S2:
            pos, rpos = S2.sb("pos", [128, L], F32)
            for p0 in (0, 32, 64, 96):
                pat = [[1, 64], [0, 64]] if (p0 % 64) == 0 else [[0, 64], [1, 64]]
                P.op("pool", (lambda p0=p0, pat=pat: lambda e: e.iota(pos[p0:p0 + 32, :].rearrange("p (a b) -> p a b", a=64), pattern=pat, base=0, channel_multiplier=0, allow_small_or_imprecise_dtypes=True))(), writes=[rpos])
            P.op("dve", lambda e: e.tensor_scalar(out=pos[:], in0=pos[:], scalar1=self.vcol("ropeinv"), scalar2=None, op0=ALU.mult), reads=[rpos, self.rvp], writes=[rpos])
            self.range_sin(S2, self.sinT, self.rsinT, pos, rpos, [128, L], 0.0)
            self.range_sin(S2, self.cosT, self.rcosT, pos, rpos, [128, L], PI / 2)

    def stage_attn(self):
        P = self.P
        pt, rpt = self.dr["pt"], self.drreg["pt"]
        with Stage(self) as S:
            neglam, rneglam = S.sb("neglam", [128, 1], F32)
            gsub, rgsub = S.sb("gsub", [128, 1], F32)
            lamrow, rlamrow = S.sb("lamrow", [1, 256], F32)
            lamw, rlamw = S.sb("lamw", [1, 8], F32)
            onesf, ronesf = S.sb("onesf", [1, 128], F32)
            ps_m, rps_m = S.ps("ps_m", [128, 512], F32)
            P.op("pool", lambda e: e.memset(onesf[:], 1.0), writes=[ronesf])
            P.dma("sp", lambda e: e.dma_start(out=lamrow[:], in_=self.dr["da_lam"]), reads=[self.drreg["da_lam"]], writes=[rlamrow])
            P.op("dve", lambda e: e.tensor_tensor(out=lamrow[:, 0:64], in0=lamrow[:, 0:64], in1=lamrow[:, 64:128], op=ALU.mult), reads=[rlamrow], writes=[rlamrow])
            P.op("dve", lambda e: e.tensor_tensor(out=lamrow[:, 128:192], in0=lamrow[:, 128:192], in1=lamrow[:, 192:256], op=ALU.mult), reads=[rlamrow], writes=[rlamrow])
            P.op("dve", lambda e: e.reduce_sum(out=lamw[:, 0:1], in_=lamrow[:, 0:64], axis=AX.X), reads=[rlamrow], writes=[rlamw])
            P.op("dve", lambda e: e.reduce_sum(out=lamw[:, 1:2], in_=lamrow[:, 128:192], axis=AX.X), reads=[rlamrow], writes=[rlamw])
            P.op("act", lambda e: e.activation(out=lamw[:, 2:4], in_=lamw[:, 0:2], func=AF.Exp), reads=[rlamw], writes=[rlamw])
            P.op("dve", lambda e: e.tensor_tensor(out=lamw[:, 4:5], in0=lamw[:, 3:4], in1=lamw[:, 2:3], op=ALU.subtract), reads=[rlamw], writes=[rlamw])
            P.op("dve", lambda e: e.tensor_scalar(out=lamw[:, 5:6], in0=lamw[:, 4:5], scalar1=-self.lam_init, scalar2=None, op0=ALU.add), reads=[rlamw], writes=[rlamw])
            P.op("pe", lambda e: e.matmul(ps_m[:, 0:1], lhsT=onesf[:], rhs=lamw[:, 5:6], start=True, stop=True), reads=[ronesf, rlamw], writes=[rps_m])
            P.op("dve", lambda e: e.tensor_copy(out=neglam[:], in_=ps_m[:, 0:1]), reads=[rps_m], writes=[rneglam])
            P.op("dve", lambda e: e.tensor_scalar(out=gsub[:], in0=self.vcol("subln"), scalar1=1.0 - self.lam_init, scalar2=None, op0=ALU.mult), reads=[self.rvp], writes=[rgsub])

            KT, rKT = S.sb("KT", [128, T], BF16)
            QT, rQT = S.sb("QT", [128, T], BF16)
            VT, rVT = S.sb("VT", [128, T], BF16)
            KR, rKR = S.sb("KR", [128, T], BF16)
            QR, rQR = S.sb("QR", [128, T], BF16)
            Vh, rVh = S.sb("Vh", [128, 34, 128], BF16)
            Qc = [S.sb("Qc%d" % c, [128, T], BF16) for c in range(2)]
            for c in range(2):
                P.op("pool", (lambda c=c: lambda e: e.memset(Qc[c][0][:], 0.0))(), writes=[Qc[c][1]])
            ps_tr, rps_tr = S.ps("ps_tr", [128, 512], BF16)
            ps_s = S.pool("ps_s", 3, [128, 512], F32, psum=True)
            ps_dum, rps_dum = S.ps("ps_dum", [128, 512], F32)
            ps_o = S.pool("ps_o", 1, [128, 512], F32, psum=True)
            ps_r = S.pool("ps_r", 1, [128, 512], F32, psum=True)
            ptp = S.pool("ptile", 5, [128, 512], BF16)
            t1p = S.pool("at1", 2, [128, 512], F32)
            t2p = S.pool("at2", 2, [128, 512], F32)
            onp = S.pool("on", 4, [128, 512], F32)
            rcp = S.pool("rc", 2, [128, 512], F32)
            o_p = S.pool("o", 2, [128, 512], F32)
            sqp = S.pool("asq", 2, [128, 512], BF16)
            aop = S.pool("aout", 2, [128, 512], BF16)
            for h in range(8):
                P.dma("sp", (lambda h=h: lambda e: e.dma_start(out=KT[:], in_=pt[C_K // 128 + h]))(), reads=[rpt], writes=[rKT])
                P.dma("sp", (lambda h=h: lambda e: e.dma_start(out=QT[:], in_=pt[C_Q // 128 + h]))(), reads=[rpt], writes=[rQT])
                P.dma("sp", (lambda h=h: lambda e: e.dma_start(out=VT[:], in_=pt[C_V // 128 + h]))(), reads=[rpt], writes=[rVT])
                P.op("pool", lambda e: e.tensor_copy(out=KR[:, 0:NCX], in_=KT[:, 0:NCX]), reads=[rKT], writes=[rKR])
                P.op("pool", lambda e: e.tensor_copy(out=QR[:, 0:NCX], in_=QT[:, 0:NCX]), reads=[rQT], writes=[rQR])
                for (src, rsrc, dst, rdst) in ((KT, rKT, KR, rKR), (QT, rQT, QR, rQR)):
                    for (n0, nsz) in TT[1:]:
                        P.op("pe", (lambda src=src, n0=n0: lambda e: e.matmul(ps_m[:], lhsT=self.rmat_bf[:], rhs=src[:, n0:n0 + 512], start=True, stop=True))(), reads=[self.rrmat, rsrc], writes=[rps_m])
                        t1, rt1 = t1p.get()
                        t2, rt2 = t2p.get()
                        P.op("dve", (lambda t1=t1, src=src, n0=n0: lambda e: e.tensor_tensor(out=t1[:], in0=src[:, n0:n0 + 512], in1=self.cosT[:, n0 - NCX:n0 - NCX + 512], op=ALU.mult))(), reads=[rsrc, self.rcosT], writes=[rt1])
                        P.op("dve", (lambda t2=t2, n0=n0: lambda e: e.tensor_tensor(out=t2[:], in0=ps_m[:], in1=self.sinT[:, n0 - NCX:n0 - NCX + 512], op=ALU.mult))(), reads=[rps_m, self.rsinT], writes=[rt2])
                        P.op("dve", (lambda t1=t1, t2=t2, dst=dst, n0=n0: lambda e: e.tensor_tensor(out=dst[:, n0:n0 + 512], in0=t1[:], in1=t2[:], op=ALU.add))(), reads=[rt1, rt2], writes=[rdst])
                for c in range(2):
                    P.op("pool", (lambda c=c: lambda e: e.tensor_copy(out=Qc[c][0][c * 64:(c + 1) * 64, :], in_=QR[c * 64:(c + 1) * 64, :]))(), reads=[rQR], writes=[Qc[c][1]])
                for b0 in range(0, 34, 4):
                    nb = min(4, 34 - b0)
                    for j in range(nb):
                        P.op("pe", (lambda b0=b0, j=j: lambda e: e.transpose(ps_tr[:, j * 128:(j + 1) * 128], VT[:, (b0 + j) * 128:(b0 + j + 1) * 128], self.ident_bf[:]))(), reads=[rVT, self.rident_bf], writes=[rps_tr])
                    P.op("act", (lambda b0=b0, nb=nb: lambda e: e.copy(out=Vh[:, b0:b0 + nb, :], in_=ps_tr[:, :nb * 128].rearrange("p (a b) -> p a b", b=128)))(), reads=[rps_tr], writes=[rVh])
                for (q0, qsz) in TT:
                    kbs = range(0, 2) if q0 < NCX else range(0, 34)
                    ons = []
                    for c in range(2):
                        pso, rpso = ps_o.get()
                        psr, rpsr = ps_r.get()
                        c0 = c * 64
                        nk = len(kbs)
                        pend = []

                        def emit_pv(item, pso=pso, rpso=rpso, psr=psr, rpsr=rpsr, nk=nk, qsz=qsz):
                            ik, kb, ptl, rptl = item
                            P.op("pe", (lambda: lambda e: e.matmul(pso[:, :qsz], lhsT=Vh[:, kb, :], rhs=ptl[:, :qsz], start=(ik == 0), stop=(ik == nk - 1)))(), reads=[rVh, rptl], writes=[rpso])
                            P.op("pe", (lambda: lambda e: e.matmul(psr[:, :qsz], lhsT=self.ones_bf[:], rhs=ptl[:, :qsz], start=(ik == 0), stop=(ik == nk - 1)))(), reads=[self.rones, rptl], writes=[rpsr])
                            if KEEP_WARM:
                                P.op("pe", (lambda: lambda e: e.matmul(ps_dum[:, :qsz], lhsT=self.ones_bf[:], rhs=ptl[:, :qsz], start=True, stop=True))(), reads=[self.rones, rptl], writes=[rps_dum])

                        for ik, kb in enumerate(kbs):
                            pss, rpss = ps_s.get()
                            P.op("pe", (lambda pss=pss, kb=kb, c=c, q0=q0, qsz=qsz: lambda e: e.matmul(pss[:, :qsz], lhsT=KR[:, kb * 128:(kb + 1) * 128], rhs=Qc[c][0][:, q0:q0 + qsz], start=True, stop=True))(), reads=[rKR, Qc[c][1]], writes=[rpss])
                            ptl, rptl = ptp.get()
                            P.op("act", (lambda ptl=ptl, pss=pss, qsz=qsz: lambda e: e.activation(out=ptl[:, :qsz], in_=pss[:, :qsz], func=AF.Exp, scale=0.125))(), reads=[rpss], writes=[rptl])
                            pend.append((ik, kb, ptl, rptl))
                            if len(pend) > 2:
                                emit_pv(pend.pop(0))
                        while pend:
                            emit_pv(pend.pop(0))
                        rc, rrc = rcp.get()
                        P.op("dve", (lambda rc=rc, psr=psr, qsz=qsz: lambda e: e.reciprocal(out=rc[:, :qsz], in_=psr[:, :qsz]))(), reads=[rpsr], writes=[rrc])
                        on, ron = onp.get()
                        P.op("dve", (lambda on=on, pso=pso, rc=rc, qsz=qsz: lambda e: e.tensor_tensor(out=on[:, :qsz], in0=pso[:, :qsz], in1=rc[:, :qsz], op=ALU.mult))(), reads=[rpso, rrc], writes=[ron])
                        ons.append((on, ron))
                    (on0, ron0), (on1, ron1) = ons
                    o, ro = o_p.get()
                    P.op("dve", (lambda o=o, on0=on0, on1=on1, qsz=qsz: lambda e: e.scalar_tensor_tensor(out=o[:, :qsz], in0=on1[:, :qsz], scalar=neglam[:, 0:1], in1=on0[:, :qsz], op0=ALU.mult, op1=ALU.add))(), reads=[ron0, ron1, rneglam], writes=[ro])
                    sq, rsq = sqp.get()
                    P.op("act", (lambda sq=sq, o=o, qsz=qsz: lambda e: e.activation(out=sq[:, :qsz], in_=o[:, :qsz], func=AF.Square))(), reads=[ro], writes=[rsq])
                    P.op("pe", (lambda sq=sq, qsz=qsz: lambda e: e.matmul(ps_m[:, :qsz], lhsT=self.ones_bf[:], rhs=sq[:, :qsz], start=True, stop=True))(), reads=[self.rones, rsq], writes=[rps_m])
                    rc, rrc = rcp.get()
                    P.op("act", (lambda rc=rc, qsz=qsz: lambda e: e.activation(out=rc[:, :qsz], in_=ps_m[:, :qsz], func=AF.Sqrt, bias=self.epsc[:, 0:1], scale=1.0 / 128))(), reads=[rps_m, self.repsc], writes=[rrc])
                    P.op("dve", (lambda rc=rc, qsz=qsz: lambda e: e.reciprocal(out=rc[:, :qsz], in_=rc[:, :qsz]))(), reads=[rrc], writes=[rrc])
                    P.op("dve", (lambda o=o, rc=rc, qsz=qsz: lambda e: e.tensor_tensor(out=o[:, :qsz], in0=o[:, :qsz], in1=rc[:, :qsz], op=ALU.mult))(), reads=[ro, rrc], writes=[ro])
                    ao, rao = aop.get()
                    P.op("act", (lambda ao=ao, o=o, qsz=qsz: lambda e: e.activation(out=ao[:, :qsz], in_=o[:, :qsz], func=AF.Copy, scale=gsub[:, 0:1]))(), reads=[ro, rgsub], writes=[rao])
                    P.dma("sp", (lambda ao=ao, h=h, q0=q0, qsz=qsz: lambda e: e.dma_start(out=self.dr["at"][h, :, q0:q0 + qsz], in_=ao[:, :qsz]))(), reads=[rao], writes=[self.drreg["at"]])

    def stage_lru(self):
        P = self.P
        pt, rpt = self.dr["pt"], self.drreg["pt"]
        SEG = [(0, NCX), (NCX, L)]
        with Stage(self) as S:
            xin, rxin = S.sb("lx", [128, T], BF16)
            ly, rly = S.sb("ly", [128, T], BF16)
            xc, rxc = S.sb("xc", [128, T], F32)
            xcb, rxcb = S.sb("xcb", [128, T], BF16)
            ra, rra = S.sb("ra", [128, T], F32)
            ib, rib = S.sb("ib", [128, T], F32)
            tmp, rtmp = S.sb("ltmp", [128, T], F32)
            hf, rhf = S.sb("hf", [128, T], F32)
            hb, rhb = S.sb("hb", [128, T], F32)
            ob, rob = S.sb("lout", [128, T], BF16)
            wab, rwab = S.sb("wab", [128, 2, 128], BF16)
            wib, rwib = S.sb("wib", [128, 2, 128], BF16)
            cl, rcl = S.sb("cl", [128, 4], F32)
            psp = S.pool("lps", 4, [128, 512], F32, psum=True)

            def rev(t, c0, n):
                a = t[:, c0:c0 + n]
                return bass.AP(a.tensor, a.offset + (n - 1), [[a.ap[0][0], 128], [-1, n]])

            for n in range(8):
                P.dma("sp", (lambda n=n: lambda e: e.dma_start(out=xin[:], in_=pt[C_LX // 128 + n]))(), reads=[rpt], writes=[rxin])
                P.dma("sp", (lambda n=n: lambda e: e.dma_start(out=ly[:], in_=pt[C_LY // 128 + n]))(), reads=[rpt], writes=[rly])
                P.dma("pool", (lambda n=n: lambda e: e.dma_start(out=wab[:], in_=self.dr["lru_wa"][:, n].rearrange("d i o -> i d o")))(), reads=[self.drreg["lru_wa"]], writes=[rwab])
                P.dma("pool", (lambda n=n: lambda e: e.dma_start(out=wib[:], in_=self.dr["lru_wi"][:, n].rearrange("d i o -> i d o")))(), reads=[self.drreg["lru_wi"]], writes=[rwib])
                w = lambda j, n=n: self.vcol("lru_cw", j * 8 + n)
                bcol = self.vcol("lru_cb", n)
                for (s0, ls) in SEG:
                    P.op("dve", (lambda s0=s0, ls=ls, n=n: lambda e: e.tensor_scalar(out=xc[:, s0:s0 + ls], in0=xin[:, s0:s0 + ls], scalar1=self.vcol("lru_cw", 2 * 8 + n), scalar2=self.vcol("lru_cb", n), op0=ALU.mult, op1=ALU.add))(),
                         reads=[rxin, self.rvp], writes=[rxc])
                    for j in (0, 1, 3):
                        d = j - 2
                        lo, hi = max(0, -d), ls - max(0, d)
                        P.op("dve", (lambda s0=s0, lo=lo, hi=hi, d=d, j=j, n=n: lambda e: e.scalar_tensor_tensor(out=xc[:, s0 + lo:s0 + hi], in0=xin[:, s0 + lo + d:s0 + hi + d], scalar=self.vcol("lru_cw", j * 8 + n), in1=xc[:, s0 + lo:s0 + hi], op0=ALU.mult, op1=ALU.add))(),
                             reads=[rxin, rxc, self.rvp], writes=[rxc])
                P.op("pool", lambda e: e.tensor_copy(out=xcb[:], in_=xc[:]), reads=[rxc], writes=[rxcb])
                for d in range(2):
                    hh, rhh = (hf, rhf) if d == 0 else (hb, rhb)
                    lamc = self.vcol("lru_lam", d * 8 + n)
                    P.op("act", (lambda lamc=lamc: lambda e: e.activation(out=cl[:, 0:1], in_=lamc, func=AF.Exp, scale=-1.0))(), reads=[self.rvp], writes=[rcl])
                    P.op("act", lambda e: e.activation(out=cl[:, 1:2], in_=cl[:, 0:1], func=AF.Ln, bias=self.epsc[:, 1:2], scale=1.0), reads=[rcl, self.repsc], writes=[rcl])
                    P.op("dve", lambda e: e.tensor_scalar(out=cl[:, 2:3], in0=cl[:, 1:2], scalar1=-8.0, scalar2=None, op0=ALU.mult), reads=[rcl], writes=[rcl])
                    for (n0, nsz) in TT:
                        ps, rps = psp.get()
                        P.op("pe", (lambda ps=ps, d=d, n0=n0, nsz=nsz: lambda e: e.matmul(ps[:, :nsz], lhsT=wab[:, d, :], rhs=xcb[:, n0:n0 + nsz], start=True, stop=True))(), reads=[rwab, rxcb], writes=[rps])
                        P.op("act", (lambda ps=ps, d=d, n=n, n0=n0, nsz=nsz: lambda e: e.activation(out=ra[:, n0:n0 + nsz], in_=ps[:, :nsz], func=AF.Sigmoid, bias=self.vcol("lru_ba", d * 8 + n), scale=1.0))(), reads=[rps, self.rvp], writes=[rra])
                        ps2, rps2 = psp.get()
                        P.op("pe", (lambda ps2=ps2, d=d, n0=n0, nsz=nsz: lambda e: e.matmul(ps2[:, :nsz], lhsT=wib[:, d, :], rhs=xcb[:, n0:n0 + nsz], start=True, stop=True))(), reads=[rwib, rxcb], writes=[rps2])
                        P.op("act", (lambda ps2=ps2, d=d, n=n, n0=n0, nsz=nsz: lambda e: e.activation(out=ib[:, n0:n0 + nsz], in_=ps2[:, :nsz], func=AF.Sigmoid, bias=self.vcol("lru_bi", d * 8 + n), scale=1.0))(), reads=[rps2, self.rvp], writes=[rib])
                    P.op("act", lambda e: e.activation(out=ra[:], in_=ra[:], func=AF.Exp, scale=cl[:, 2:3]), reads=[rra, rcl], writes=[rra])
                    P.op("pool", lambda e: e.tensor_tensor(out=tmp[:], in0=ra[:], in1=ra[:], op=ALU.mult), reads=[rra], writes=[rtmp])
                    P.op("act", lambda e: e.activation(out=tmp[:], in_=tmp[:], func=AF.Sqrt, bias=self.epsc[:, 1:2], scale=-1.0), reads=[rtmp, self.repsc], writes=[rtmp])
                    P.op("dve", lambda e: e.tensor_tensor(out=ib[:], in0=ib[:], in1=xc[:], op=ALU.mult), reads=[rib, rxc], writes=[rib])
                    P.op("dve", lambda e: e.tensor_tensor(out=ib[:], in0=ib[:], in1=tmp[:], op=ALU.mult), reads=[rib, rtmp], writes=[rib])
                    if d == 0:
                        P.op("dve", lambda e: e.tensor_tensor_scan(out=hf[:, 0:NCX], data0=ra[:, 0:NCX], data1=ib[:, 0:NCX], initial=0.0, op0=ALU.mult, op1=ALU.add), reads=[rra, rib], writes=[rhf])
                        P.op("dve", lambda e: e.tensor_tensor_scan(out=hf[:, NCX:T], data0=ra[:, NCX:T], data1=ib[:, NCX:T], initial=hf[:, NCX - 1:NCX], op0=ALU.mult, op1=ALU.add), reads=[rra, rib, rhf], writes=[rhf])
                    else:
                        P.op("dve", lambda e: e.tensor_tensor_scan(out=rev(hb, 0, NCX), data0=rev(ra, 0, NCX), data1=rev(ib, 0, NCX), initial=0.0, op0=ALU.mult, op1=ALU.add), reads=[rra, rib], writes=[rhb])
                        P.op("dve", lambda e: e.tensor_tensor_scan(out=rev(hb, NCX, L), data0=rev(ra, NCX, L), data1=rev(ib, NCX, L), initial=hb[:, 0:1], op0=ALU.mult, op1=ALU.add), reads=[rra, rib, rhb], writes=[rhb])
                P.op("pool", lambda e: e.tensor_tensor(out=hf[:], in0=hf[:], in1=hb[:], op=ALU.add), reads=[rhf, rhb], writes=[rhf])
                P.op("act", lambda e: e.activation(out=tmp[:], in_=ly[:], func=AF.Square), reads=[rly], writes=[rtmp])
                P.op("dve", lambda e: e.tensor_scalar(out=tmp[:], in0=tmp[:], scalar1=0.044715, scalar2=1.0, op0=ALU.mult, op1=ALU.add), reads=[rtmp], writes=[rtmp])
                P.op("dve", lambda e: e.tensor_tensor(out=tmp[:], in0=tmp[:], in1=ly[:], op=ALU.mult), reads=[rtmp, rly], writes=[rtmp])
                P.op("act", lambda e: e.activation(out=tmp[:], in_=tmp[:], func=AF.Sigmoid, scale=1.5957691216057308), reads=[rtmp], writes=[rtmp])
                P.op("dve", lambda e: e.tensor_tensor(out=tmp[:], in0=tmp[:], in1=ly[:], op=ALU.mult), reads=[rtmp, rly], writes=[rtmp])
                P.op("dve", lambda e: e.tensor_tensor(out=ob[:], in0=tmp[:], in1=hf[:], op=ALU.mult), reads=[rtmp, rhf], writes=[rob])
                P.dma("sp", (lambda n=n: lambda e: e.dma_start(out=self.dr["rt"][n], in_=ob[:]))(), reads=[rob], writes=[self.drreg["rt"]])

    def stage_merge(self):
        P = self.P
        with Stage(self) as S:
            wbr, rwbr = S.sb("wbr", [128, 3, 8, 1024], BF16)
            wo, rwo = S.sb("wo", [128, 8, 1024], BF16)
            for k in range(3):
                P.dma("pool", (lambda k=k: lambda e: e.dma_start(out=wbr[:, k], in_=self.dr["w_br"][k].rearrange("(c p) n -> p c n", p=128)))(), reads=[self.drreg["w_br"]], writes=[rwbr])
            P.dma("pool", lambda e: e.dma_start(out=wo[:], in_=self.dr["w_out"].rearrange("(c p) n -> p c n", p=128)), reads=[self.drreg["w_out"]], writes=[rwo])
            brp = [S.pool("br%d" % k, 1, [128, 8, 512], BF16) for k in range(3)]
            gp = S.pool("mg", 2, [128, 24, 512], BF16)
            xp = S.pool("mx", 2, [128, 8, 512], F32)
            mtp = S.pool("mt", 2, [128, 8, 512], BF16)
            macc = S.pool("macc", 2, [128, 512], F32)
            mtmp = S.pool("mtmp", 2, [128, 512], F32)
            psp = S.pool("mps", 4, [128, 512], F32, psum=True)
            names = ["at", "rt", "yt"]
            for (n0, nsz) in TT:
                w = 1 if n0 < NCX else 0
                brs = []
                for k in range(3):
                    bt, rbt = brp[k].get()
                    P.dma("sp", (lambda bt=bt, k=k, n0=n0, nsz=nsz: lambda e: e.dma_start(out=bt[:, :, :nsz], in_=self.dr[names[k]][:, :, n0:n0 + nsz].rearrange("c p n -> p c n")))(), reads=[self.drreg[names[k]]], writes=[rbt])
                    brs.append((bt, rbt))
                g, rg = gp.get()
                P.dma("sp", (lambda g=g, n0=n0, nsz=nsz: lambda e: e.dma_start(out=g[:, :, :nsz], in_=self.dr["pt"][C_G // 128:C_END // 128, :, n0:n0 + nsz].rearrange("c p n -> p c n")))(), reads=[self.drreg["pt"]], writes=[rg])
                xt_, rxt_ = xp.get()
                P.dma("sp", (lambda xt_=xt_, n0=n0, nsz=nsz: lambda e: e.dma_start(out=xt_[:, :, :nsz], in_=self.dr["xt"][:, :, n0:n0 + nsz]))(), reads=[self.drreg["xt"]], writes=[rxt_])
                sg, rsg = g, rg
                for c in range(24):
                    P.op("act", (lambda sg=sg, g=g, c=c, nsz=nsz: lambda e: e.activation(out=sg[:, c, :nsz], in_=g[:, c, :nsz], func=AF.Sigmoid, bias=self.vcol("b_gate", c), scale=1.0))(), reads=[rg, self.rvp], writes=[rsg])
                mt, rmt = mtp.get()
                for oc in range(8):
                    acc, racc = macc.get()
                    for k in range(3):
                        ps, rps = psp.get()
                        bt, rbt = brs[k]
                        for kc in range(8):
                            P.op("pe", (lambda ps=ps, bt=bt, k=k, kc=kc, oc=oc, nsz=nsz: lambda e: e.matmul(ps[:, :nsz], lhsT=wbr[:, k, kc, oc * 128:(oc + 1) * 128], rhs=bt[:, kc, :nsz], start=(kc == 0), stop=(kc == 7)))(), reads=[rwbr, rbt], writes=[rps])
                        if k == 0:
                            P.op("dve", (lambda acc=acc, ps=ps, sg=sg, oc=oc, nsz=nsz: lambda e: e.tensor_tensor(out=acc[:, :nsz], in0=ps[:, :nsz], in1=sg[:, oc, :nsz], op=ALU.mult))(), reads=[rps, rsg], writes=[racc])
                        else:
                            t_, rt_ = mtmp.get()
                            P.op("dve", (lambda t_=t_, ps=ps, sg=sg, k=k, oc=oc, nsz=nsz: lambda e: e.tensor_tensor(out=t_[:, :nsz], in0=ps[:, :nsz], in1=sg[:, k * 8 + oc, :nsz], op=ALU.mult))(), reads=[rps, rsg], writes=[rt_])
                            if k == 1:
                                P.op("pool", (lambda acc=acc, t_=t_, nsz=nsz: lambda e: e.tensor_tensor(out=acc[:, :nsz], in0=acc[:, :nsz], in1=t_[:, :nsz], op=ALU.add))(), reads=[racc, rt_], writes=[racc])
                            else:
                                P.op("pool", (lambda acc=acc, t_=t_, mt=mt, oc=oc, nsz=nsz: lambda e: e.tensor_tensor(out=mt[:, oc, :nsz], in0=acc[:, :nsz], in1=t_[:, :nsz], op=ALU.add))(), reads=[racc, rt_], writes=[rmt])
                xo, rxo = xt_, rxt_
                for oc in range(8):
                    ps, rps = psp.get()
                    for kc in range(8):
                        P.op("pe", (lambda ps=ps, mt=mt, kc=kc, oc=oc, nsz=nsz: lambda e: e.matmul(ps[:, :nsz], lhsT=wo[:, kc, oc * 128:(oc + 1) * 128], rhs=mt[:, kc, :nsz], start=(kc == 0), stop=(kc == 7)))(), reads=[rwo, rmt], writes=[rps])
                    P.op("dve", (lambda xo=xo, ps=ps, xt_=xt_, oc=oc, nsz=nsz, w=w: lambda e: e.scalar_tensor_tensor(out=xo[:, oc, :nsz], in0=ps[:, :nsz], scalar=self.modv[:, 16 + oc, w:w + 1], in1=xt_[:, oc, :nsz], op0=ALU.mult, op1=ALU.add))(), reads=[rps, rxt_, self.rmodv], writes=[rxo])
                P.dma("sp", (lambda xo=xo, n0=n0, nsz=nsz: lambda e: e.dma_start(out=self.dr["xt_mid"][:, :, n0:n0 + nsz], in_=xo[:, :, :nsz]))(), reads=[rxo], writes=[self.drreg["xt_mid"]])

    def stage_ffn(self):
        P = self.P
        moe = self.moe
        STS = [TT[0:3], TT[3:6], TT[6:9]]
        FG = [(0, 4), (4, 4), (8, 4), (12, 4), (16, 4), (20, 2)]
        if moe:
            self.dram("gate_s", [NE, T], F32, "Internal")
        with Stage(self) as S:
            fT, rfT = S.sb("fT", [128, 8, 1536], BF16)
            yacc, ryacc = S.sb("yacc", [128, 8, 1536], F32)
            wgp = S.pool("wg", 2, [128, 8, 512], BF16)
            wup = S.pool("wu", 2, [128, 8, 512], BF16)
            wdp = S.pool("wd", 2, [128, 4, 1024], BF16)
            if moe:
                wr, rwr = S.sb("wr", [128, 8, NE], F32)
                P.dma("sp", lambda e: e.dma_start(out=wr[:], in_=self.dr["router"].rearrange("(k p) n -> p k n", p=128)), reads=[self.drreg["router"]], writes=[rwr])
                gbc, rgbc = S.sb("gbc", [128, 1536], F32)
                identf = self.vp[:, VP["ident"][0]:VP["ident"][0] + 128]
            for st in STS:
                c0 = st[0][0]
                c1 = st[-1][0] + st[-1][1]
                with Stage(self) as S2:
                    router = None
                    if moe:
                        psr, rpsr = S2.ps("psr", [128, 4, NE], F32)
                        ps_t, rps_t = S2.ps("ps_t", [NE, 512], F32)
                        lg, rlg = S2.sb("lg", [128, 4, NE], F32)
                        l2, rl2 = S2.sb("l2", [128, 4, NE], F32)
                        m1, rm1 = S2.sb("m1", [128, 4], F32)
                        m2, rm2 = S2.sb("m2", [128, 4], F32)
                        gT, rgT = S2.sb("gT", [NE, 512], F32)
                        router = (wr, rwr, psr, rpsr)

                        def router_done(n0, nsz):
                            ns = nsz // 128
                            bc = lambda t: t[:, :ns].unsqueeze(2).to_broadcast([128, ns, NE])
                            P.op("dve", lambda e: e.tensor_copy(out=lg[:, :ns], in_=psr[:, :ns]), reads=[rpsr], writes=[rlg])
                            P.op("dve", lambda e: e.tensor_reduce(out=m1[:, :ns], in_=lg[:, :ns], axis=AX.X, op=ALU.max), reads=[rlg], writes=[rm1])
                            P.op("dve", lambda e: e.tensor_tensor(out=l2[:, :ns], in0=lg[:, :ns], in1=bc(m1), op=ALU.is_equal), reads=[rlg, rm1], writes=[rl2])
                            P.op("dve", lambda e: e.scalar_tensor_tensor(out=l2[:, :ns], in0=l2[:, :ns], scalar=-1e30, in1=lg[:, :ns], op0=ALU.mult, op1=ALU.add), reads=[rl2, rlg], writes=[rl2])
                            P.op("dve", lambda e: e.tensor_reduce(out=m2[:, :ns], in_=l2[:, :ns], axis=AX.X, op=ALU.max), reads=[rl2], writes=[rm2])
                            P.op("dve", lambda e: e.tensor_tensor(out=l2[:, :ns], in0=lg[:, :ns], in1=bc(m2), op=ALU.is_ge), reads=[rlg, rm2], writes=[rl2])
                            P.op("dve", lambda e: e.tensor_tensor(out=lg[:, :ns], in0=lg[:, :ns], in1=bc(m1), op=ALU.subtract), reads=[rlg, rm1], writes=[rlg])
                            P.op("act", lambda e: e.activation(out=lg[:, :ns], in_=lg[:, :ns], func=AF.Exp), reads=[rlg], writes=[rlg])
                            P.op("dve", lambda e: e.tensor_tensor(out=lg[:, :ns], in0=lg[:, :ns], in1=l2[:, :ns], op=ALU.mult), reads=[rlg, rl2], writes=[rlg])
                            P.op("dve", lambda e: e.tensor_reduce(out=m1[:, :ns], in_=lg[:, :ns], axis=AX.X, op=ALU.add), reads=[rlg], writes=[rm1])
                            P.op("dve", lambda e: e.reciprocal(out=m1[:, :ns], in_=m1[:, :ns]), reads=[rm1], writes=[rm1])
                            P.op("dve", lambda e: e.tensor_tensor(out=lg[:, :ns], in0=lg[:, :ns], in1=bc(m1), op=ALU.mult), reads=[rlg, rm1], writes=[rlg])
                            for sub in range(ns):
                                P.op("pe", (lambda sub=sub: lambda e: e.transpose(ps_t[:, sub * 128:(sub + 1) * 128], lg[:, sub, :], identf))(), reads=[rlg, self.rvp], writes=[rps_t])
                            P.op("dve", lambda e: e.tensor_copy(out=gT[:, :nsz], in_=ps_t[:, :nsz]), reads=[rps_t], writes=[rgT])
                            P.dma("sp", lambda e: e.dma_start(out=self.dr["gate_s"][:, n0:n0 + nsz], in_=gT[:, :nsz]), reads=[rgT], writes=[self.drreg["gate_s"]])

                        self._router_done = router_done
                    self.norm_mod(S2, "xt_mid", st, self.gsf, self.rgsf, 3, fT, rfT, c0, router=router)
                with Stage(self) as S3:
                    psg = S3.pool("psg", 2, [128, 512], F32, psum=True)
                    psu = S3.pool("psu", 2, [128, 512], F32, psum=True)
                    psd = S3.pool("psd", 3, [128, 512], F32, psum=True)
                    sgp = S3.pool("fsg", 2, [128, 512], F32)
                    actp = S3.pool("fact", 2, [128, 4, 512], BF16)
                    first = True
                    for ex in range(self.ne):
                        if moe:
                            P.dma("sp", (lambda ex=ex, c0=c0, c1=c1: lambda e: e.dma_start(out=gbc[:, :c1 - c0], in_=bcast_rows(self.dr["gate_s"][ex:ex + 1, c0:c1], c1 - c0)))(), reads=[self.drreg["gate_s"]], writes=[rgbc])
                        for (g0, gn) in FG:
                            wg, rwg = wgp.get()
                            wu, rwu = wup.get()
                            wd, rwd = wdp.get()
                            P.dma("pool", (lambda wg=wg, ex=ex, g0=g0, gn=gn: lambda e: e.dma_start(out=wg[:, :, :gn * 128], in_=self.dr["f_wg"][ex].rearrange("(k p) n -> p k n", p=128)[:, :, g0 * 128:(g0 + gn) * 128]))(), reads=[self.drreg["f_wg"]], writes=[rwg])
                            P.dma("pool", (lambda wu=wu, ex=ex, g0=g0, gn=gn: lambda e: e.dma_start(out=wu[:, :, :gn * 128], in_=self.dr["f_wu"][ex].rearrange("(k p) n -> p k n", p=128)[:, :, g0 * 128:(g0 + gn) * 128]))(), reads=[self.drreg["f_wu"]], writes=[rwu])
                            P.dma("pool", (lambda wd=wd, ex=ex, g0=g0, gn=gn: lambda e: e.dma_start(out=wd[:, :gn, :], in_=self.dr["f_wd"][ex].rearrange("(c p) n -> p c n", p=128)[:, g0:g0 + gn, :]))(), reads=[self.drreg["f_wd"]], writes=[rwd])
                            for (n0, nsz) in st:
                                o0 = n0 - c0
                                act, ract = actp.get()
                                for c in range(gn):
                                    pg, rpg = psg.get()
                                    pu, rpu = psu.get()
                                    for k in range(8):
                                        P.op("pe", (lambda pg=pg, wg=wg, k=k, c=c, o0=o0, nsz=nsz: lambda e: e.matmul(pg[:, :nsz], lhsT=wg[:, k, c * 128:(c + 1) * 128], rhs=fT[:, k, o0:o0 + nsz], start=(k == 0), stop=(k == 7)))(), reads=[rwg, rfT], writes=[rpg])
                                    for k in range(8):
                                        P.op("pe", (lambda pu=pu, wu=wu, k=k, c=c, o0=o0, nsz=nsz: lambda e: e.matmul(pu[:, :nsz], lhsT=wu[:, k, c * 128:(c + 1) * 128], rhs=fT[:, k, o0:o0 + nsz], start=(k == 0), stop=(k == 7)))(), reads=[rwu, rfT], writes=[rpu])
                                    sg, rsg = sgp.get()
                                    P.op("act", (lambda sg=sg, pg=pg, nsz=nsz: lambda e: e.activation(out=sg[:, :nsz], in_=pg[:, :nsz], func=AF.Silu))(), reads=[rpg], writes=[rsg])
                                    if moe:
                                        P.op("pool", (lambda sg=sg, o0=o0, nsz=nsz: lambda e: e.tensor_tensor(out=sg[:, :nsz], in0=sg[:, :nsz], in1=gbc[:, o0:o0 + nsz], op=ALU.mult))(), reads=[rsg, rgbc], writes=[rsg])
                                    P.op("dve", (lambda act=act, sg=sg, pu=pu, c=c, nsz=nsz: lambda e: e.tensor_tensor(out=act[:, c, :nsz], in0=pu[:, :nsz], in1=sg[:, :nsz], op=ALU.mult))(), reads=[rpu, rsg], writes=[ract])
                                for oc in range(8):
                                    pd, rpd = psd.get()
                                    for c in range(gn):
                                        P.op("pe", (lambda pd=pd, wd=wd, act=act, c=c, oc=oc, nsz=nsz, gn=gn: lambda e: e.matmul(pd[:, :nsz], lhsT=wd[:, c, oc * 128:(oc + 1) * 128], rhs=act[:, c, :nsz], start=(c == 0), stop=(c == gn - 1)))(), reads=[rwd, ract], writes=[rpd])
                                    if first:
                                        P.op("act", (lambda pd=pd, oc=oc, o0=o0, nsz=nsz: lambda e: e.copy(out=yacc[:, oc, o0:o0 + nsz], in_=pd[:, :nsz]))(), reads=[rpd], writes=[ryacc])
                                    else:
                                        P.op("dve", (lambda pd=pd, oc=oc, o0=o0, nsz=nsz: lambda e: e.tensor_tensor(out=yacc[:, oc, o0:o0 + nsz], in0=yacc[:, oc, o0:o0 + nsz], in1=pd[:, :nsz], op=ALU.add))(), reads=[rpd, ryacc], writes=[ryacc])
                            first = False
                    xp = S3.pool("fx", 2, [128, 8, 512], F32)
                    for (n0, nsz) in st:
                        o0 = n0 - c0
                        w = 1 if n0 < NCX else 0
                        xt_, rxt_ = xp.get()
                        P.dma("sp", (lambda xt_=xt_, n0=n0, nsz=nsz: lambda e: e.dma_start(out=xt_[:, :, :nsz], in_=self.dr["xt_mid"][:, :, n0:n0 + nsz]))(), reads=[self.drreg["xt_mid"]], writes=[rxt_])
                        for oc in range(8):
                            P.op("dve", (lambda xt_=xt_, oc=oc, o0=o0, nsz=nsz, w=w: lambda e: e.scalar_tensor_tensor(out=xt_[:, oc, :nsz], in0=yacc[:, oc, o0:o0 + nsz], scalar=self.modv[:, 40 + oc, w:w + 1], in1=xt_[:, oc, :nsz], op0=ALU.mult, op1=ALU.add))(), reads=[ryacc, rxt_, self.rmodv], writes=[rxt_])
                        P.dma("sp", (lambda xt_=xt_, n0=n0, nsz=nsz: lambda e: e.dma_start(out=self.dr["xt_out"][:, :, n0:n0 + nsz], in_=xt_[:, :, :nsz]))(), reads=[rxt_], writes=[self.drreg["xt_out"]])

    def stage_final(self):
        P = self.P
        with Stage(self) as S:
            gfin, rgfin = S.sb("gfin", [128, 8, 2], F32)
            zsh, rzsh = S.sb("zsh", [128, 8, 2], F32)
            o = VP["g_final"][0]
            for w in range(2):
                P.op("dve", (lambda w=w: lambda e: e.tensor_copy(out=gfin[:, :, w], in_=self.vp[:, o:o + 8]))(), reads=[self.rvp], writes=[rgfin])
            P.op("pool", lambda e: e.memset(zsh[:], 0.0), writes=[rzsh])
            for (n0, nsz) in TT[1:]:
                with Stage(self) as S2:
                    ot, rot = S2.sb("fin_o", [128, 8, 512], F32)
                    self.norm_mod(S2, "xt_out", [(n0, nsz)], gfin, rgfin, 0, ot, rot, n0, shift=(zsh, rzsh))
                    P.dma("sp", (lambda ot=ot, n0=n0: lambda e: e.dma_start(out=self.dr["y_out"][:, :, n0 - NCX:n0 - NCX + 512], in_=ot[:]))(), reads=[rot], writes=[self.drreg["y_out"]])

    def mod_reduce(self, S, t, rt, scr, rscr, M, shape_ap=None):
        P = self.P
        P.op("dve", lambda e: e.tensor_scalar(out=scr, in0=t, scalar1=1.0 / M, scalar2=MAGIC, op0=ALU.mult, op1=ALU.add), reads=[rt], writes=[rscr])
        P.op("dve", lambda e: e.tensor_scalar(out=scr, in0=scr, scalar1=-MAGIC, scalar2=None, op0=ALU.add), reads=[rscr], writes=[rscr])
        P.op("dve", lambda e: e.scalar_tensor_tensor(out=t, in0=scr, scalar=-float(M), in1=t, op0=ALU.mult, op1=ALU.add), reads=[rscr, rt], writes=[rt])

    def hy_dft_gen(self, Ls):
        P = self.P
        nt = Ls // 128
        N = 2 * Ls
        nm = "L%d" % Ls
        for k in ("CF", "SF", "CI", "SI"):
            self.dram(k + nm, [nt, 128, nt, 128], BF16, "Internal")
        with Stage(self) as S:
            q, rq = S.sb("q", [128, Ls], F32)
            x1, rx1 = S.sb("x1", [128, Ls], F32)
            scr, rscr = S.sb("scr", [128, Ls], F32)
            m, rm = S.sb("m", [128, Ls], F32)
            m2, rm2 = S.sb("m2", [128, Ls], F32)
            pc2, rpc2 = S.sb("pc2", [128, 1], F32)
            outp = S.pool("dfto", 2, [128, Ls], BF16)
            P.op("dve", lambda e: e.tensor_scalar(out=pc2[:], in0=self.vcol("pidx"), scalar1=2.0, scalar2=1.0, op0=ALU.mult, op1=ALU.add), reads=[self.rvp], writes=[rpc2])
            ptr = S.pool("dftptr", 2, [128, 512], BF16, psum=True)
            sI_p = S.pool("dftsI", 2, [128, nt, 128], BF16)
            P.op("pool", lambda e: e.iota(q[:], pattern=[[2, Ls]], base=1, channel_multiplier=0, allow_small_or_imprecise_dtypes=True), writes=[rq])
            M1, mult1, pcol = (2 * N) // 128, 128.0, self.vcol("pidx")
            for a in range(nt):
                P.op("dve", (lambda a=a: lambda e: e.tensor_scalar(out=x1[:], in0=q[:], scalar1=float(a), scalar2=None, op0=ALU.mult))(), reads=[rq], writes=[rx1])
                self.mod_reduce(S, x1[:], rx1, scr[:], rscr, M1)
                P.op("dve", lambda e: e.tensor_scalar(out=x1[:], in0=x1[:], scalar1=mult1, scalar2=None, op0=ALU.mult), reads=[rx1], writes=[rx1])
                P.op("dve", lambda e: e.scalar_tensor_tensor(out=m[:], in0=q[:], scalar=pcol, in1=x1[:], op0=ALU.mult, op1=ALU.add), reads=[rq, rx1, self.rvp], writes=[rm])
                P.op("pool", lambda e: e.tensor_scalar(out=m2[:], in0=m[:], scalar1=float(N // 2), scalar2=None, op0=ALU.add), reads=[rm], writes=[rm2])
                self.mod_reduce(S, m[:], rm, scr[:], rscr, 2 * N)
                self.mod_reduce(S, m2[:], rm2, scr[:], rscr, 2 * N)
                for (src, rsrc, cs) in ((m2, rm2, "C"), (m, rm, "S")):
                    ot, rot = outp.get()
                    P.op("act", (lambda ot=ot, src=src: lambda e: e.activation(out=ot[:], in_=src[:], func=AF.Sin, scale=3.1415925 / N))(), reads=[rsrc], writes=[rot])
                    P.dma("sp", (lambda ot=ot, cs=cs, a=a: lambda e: e.dma_start(out=self.dr[cs + "F" + nm][:, :, a, :].rearrange("c p j -> p c j"), in_=ot[:].rearrange("p (c j) -> p c j", j=128)))(), reads=[rot], writes=[self.drreg[cs + "F" + nm]])
                    sI, rsI = sI_p.get()
                    for c0 in range(0, nt, 4):
                        nb = min(4, nt - c0)
                        pt_, rpt_ = ptr.get()
                        for j in range(nb):
                            P.op("pe", (lambda pt_=pt_, ot=ot, c0=c0, j=j: lambda e: e.transpose(pt_[:, j * 128:(j + 1) * 128], ot[:, (c0 + j) * 128:(c0 + j + 1) * 128], self.ident_bf[:]))(), reads=[rot, self.rident_bf], writes=[rpt_])
                        P.op("act", (lambda pt_=pt_, sI=sI, c0=c0, nb=nb: lambda e: e.activation(out=sI[:, c0:c0 + nb, :], in_=pt_[:, :nb * 128].rearrange("p (a b) -> p a b", b=128), func=AF.Copy, scale=2.0 / N))(), reads=[rpt_], writes=[rsI])
                    P.dma("sp", (lambda sI=sI, cs=cs, a=a: lambda e: e.dma_start(out=self.dr[cs + "I" + nm][a], in_=sI[:]))(), reads=[rsI], writes=[self.drreg[cs + "I" + nm]])

    def hy_prep(self):
        P = self.P
        self.dram("utm", [3, 34, 128, 1024], BF16, "Internal")
        self.dram("z1", [34, 128, 1024], BF16, "Internal")
        self.dram("ytm", [34, 128, 1024], BF16, "Internal")
        SEG = [(0, NCX), (NCX, L)]
        with Stage(self) as S:
            xin_p = S.pool("hx", 2, [128, T], BF16)
            u, ru = S.sb("hu", [128, T], F32)
            ub, rub = S.sb("hub", [128, T], BF16)
            stg_p = S.pool("hstg", 2, [128, 34, 128], BF16)
            ptr = S.pool("hptr", 2, [128, 512], BF16, psum=True)
            for ch in range(24):
                xin, rxin = xin_p.get()
                P.dma("sp", (lambda xin=xin, ch=ch: lambda e: e.dma_start(out=xin[:], in_=self.dr["pt"][C_HY // 128 + ch]))(), reads=[self.drreg["pt"]], writes=[rxin])
                for (s0, ls) in SEG:
                    P.op("dve", (lambda xin=xin, s0=s0, ls=ls, ch=ch: lambda e: e.tensor_scalar(out=u[:, s0:s0 + ls], in0=xin[:, s0:s0 + ls], scalar1=self.vcol("hy_cw", 1 * 24 + ch), scalar2=self.vcol("hy_cb", ch), op0=ALU.mult, op1=ALU.add))(), reads=[rxin, self.rvp], writes=[ru])
                    for j in (0, 2):
                        d = j - 1
                        lo, hi = max(0, -d), ls - max(0, d)
                        eng = "dve"
                        P.op(eng, (lambda xin=xin, s0=s0, lo=lo, hi=hi, d=d, j=j, ch=ch: lambda e: e.scalar_tensor_tensor(out=u[:, s0 + lo:s0 + hi], in0=xin[:, s0 + lo + d:s0 + hi + d], scalar=self.vcol("hy_cw", j * 24 + ch), in1=u[:, s0 + lo:s0 + hi], op0=ALU.mult, op1=ALU.add))(), reads=[rxin, ru, self.rvp], writes=[ru])
                P.op("act", lambda e: e.copy(out=ub[:], in_=u[:]), reads=[ru], writes=[rub])
                stg, rstg = stg_p.get()
                for b0 in range(0, 34, 4):
                    nb = min(4, 34 - b0)
                    pt_, rpt_ = ptr.get()
                    for j in range(nb):
                        P.op("pe", (lambda pt_=pt_, b0=b0, j=j: lambda e: e.transpose(pt_[:, j * 128:(j + 1) * 128], ub[:, (b0 + j) * 128:(b0 + j + 1) * 128], self.ident_bf[:]))(), reads=[rub, self.rident_bf], writes=[rpt_])
                    eng = "act" if (b0 // 4) % 2 == 0 else "dve"
                    if eng == "act":
                        P.op("act", (lambda pt_=pt_, stg=stg, b0=b0, nb=nb: lambda e: e.copy(out=stg[:, b0:b0 + nb, :], in_=pt_[:, :nb * 128].rearrange("p (a b) -> p a b", b=128)))(), reads=[rpt_], writes=[rstg])
                    else:
                        P.op("dve", (lambda pt_=pt_, stg=stg, b0=b0, nb=nb: lambda e: e.tensor_copy(out=stg[:, b0:b0 + nb, :], in_=pt_[:, :nb * 128].rearrange("p (a b) -> p a b", b=128)))(), reads=[rpt_], writes=[rstg])
                wsel, cc = ch // 8, ch % 8
                P.dma("sp", (lambda stg=stg, wsel=wsel, cc=cc: lambda e: e.dma_start(out=self.dr["utm"][wsel][:, :, cc * 128:(cc + 1) * 128].rearrange("b p c -> p b c"), in_=stg[:]))(), reads=[rstg], writes=[self.drreg["utm"]])

    def hy_filters(self, Ls):
        P = self.P
        nt = Ls // 128
        nm = "L%d" % Ls
        self.dram("HS" + nm, [nt, 128, 2048], BF16, "Internal")
        self.dram("HD" + nm, [nt, 128, 2048], BF16, "Internal")
        self.dram("RN" + nm, [128, 2048], F32, "Internal")
        with Stage(self) as S:
            w1, rw1 = S.sb("hw1", [33, 64], F32)
            w2, rw2 = S.sb("hw2", [64, 64], F32)
            w3, rw3 = S.sb("hw3", [64, 4096], F32)
            dec, rdec = S.sb("hdec", [1, 2048], F32)
            P.dma("sp", lambda e: e.dma_start(out=w1[:], in_=self.dr["hy_w1"]), reads=[self.drreg["hy_w1"]], writes=[rw1])
            P.dma("sp", lambda e: e.dma_start(out=w2[:], in_=self.dr["hy_w2"]), reads=[self.drreg["hy_w2"]], writes=[rw2])
            P.dma("sp", lambda e: e.dma_start(out=w3[:], in_=self.dr["hy_w3"]), reads=[self.drreg["hy_w3"]], writes=[rw3])
            P.dma("sp", lambda e: e.dma_start(out=dec[:], in_=self.dr["hy_decay"]), reads=[self.drreg["hy_decay"]], writes=[rdec])
            P.op("act", lambda e: e.activation(out=dec[:], in_=dec[:], func=AF.Abs), reads=[rdec], writes=[rdec])
            nv, rnv = S.sb("hnv", [64, Ls], F32)
            z, rz = S.sb("hz", [64, Ls], F32)
            h1, rh1 = S.sb("hh1", [64, Ls], F32)
            h2, rh2 = S.sb("hh2", [64, Ls], F32)
            tv, rtv = S.sb("htv", [1, Ls], F32)
            P.op("pool", lambda e: e.iota(nv[:], pattern=[[1, Ls]], base=0, channel_multiplier=0, allow_small_or_imprecise_dtypes=True), writes=[rnv])
            P.op("dve", lambda e: e.tensor_scalar(out=h1[0:33, :], in0=nv[0:33, :], scalar1=2 * PI / Ls, scalar2=self.vp[0:33, VP["bands"][0]:VP["bands"][0] + 1], op0=ALU.mult, op1=ALU.mult), reads=[rnv, self.rvp], writes=[rh1])
            P.op("dve", lambda e: e.tensor_scalar(out=h1[0:33, :], in0=h1[0:33, :], scalar1=self.vp[0:33, VP["phase"][0]:VP["phase"][0] + 1], scalar2=None, op0=ALU.add), reads=[rh1, self.rvp], writes=[rh1])
            with Stage(self) as S2:
                self.range_sin(S2, z[0:33, :], rz, h1[0:33, :], rh1, [33, Ls], 0.0, whole=False)
            P.op("dve", lambda e: e.tensor_scalar(out=z[0:1, :], in0=nv[0:1, :], scalar1=1.0 / (Ls - 1), scalar2=None, op0=ALU.mult), reads=[rnv, rz], writes=[rz])
            P.op("dve", lambda e: e.tensor_scalar(out=tv[:], in0=nv[0:1, :], scalar1=1.0 / (Ls - 1), scalar2=None, op0=ALU.mult), reads=[rnv], writes=[rtv])
            psp = S.pool("hfps", 2, [128, 512], F32, psum=True)
            for (wt, rwt, kk, src, rsrc, dst, rdst, bname) in ((w1, rw1, 33, z, rz, h1, rh1, "hy_b1"), (w2, rw2, 64, h1, rh1, h2, rh2, "hy_b2")):
                pre, rpre = S.sb("hpre", [64, Ls], F32)
                for c0 in range(0, Ls, 512):
                    csz = min(512, Ls - c0)
                    ps, rps = psp.get()
                    P.op("pe", (lambda ps=ps, wt=wt, kk=kk, src=src, c0=c0, csz=csz: lambda e: e.matmul(ps[0:64, :csz], lhsT=wt[0:kk, :], rhs=src[0:kk, c0:c0 + csz], start=True, stop=True))(), reads=[rwt, rsrc], writes=[rps])
                    P.op("dve", (lambda ps=ps, pre=pre, c0=c0, bname=bname, csz=csz: lambda e: e.tensor_scalar(out=pre[:, c0:c0 + csz], in0=ps[0:64, :csz], scalar1=self.vp[0:64, VP[bname][0]:VP[bname][0] + 1], scalar2=self.vp[0:64, VP["hy_freq"][0]:VP["hy_freq"][0] + 1], op0=ALU.add, op1=ALU.mult))(), reads=[rps, self.rvp], writes=[rpre])
                with Stage(self) as S2:
                    self.range_sin(S2, dst[:, :], rdst, pre[:, :], rpre, [64, Ls], 0.0, whole=False)
            psn = [S.ps("hpsn", [128, 512], F32) for _ in range(4)]
            psw, rpsw = S.ps("hpsw", [128, 512], F32)
            winp = S.pool("hwin", 2, [128, 512], F32)
            fp_ = S.pool("hf", 4, [128, 512], F32)
            abp = S.pool("hab", 2, [128, 512], BF16)
            hsp = S.pool("hhs", 2, [128, 512], BF16)
            hdp = S.pool("hhd", 2, [128, 512], BF16)
            for a in range(nt):
                for o in range(2):
                    for ct in range(2):
                        P.op("pe", (lambda a=a, o=o, ct=ct: lambda e: e.matmul(psw[:], lhsT=tv[0:1, a * 128:(a + 1) * 128], rhs=dec[0:1, o * 1024 + ct * 512:o * 1024 + ct * 512 + 512], start=True, stop=True))(), reads=[rtv, rdec], writes=[rpsw])
                        win, rwin = winp.get()
                        P.op("act", (lambda win=win: lambda e: e.activation(out=win[:], in_=psw[:], func=AF.Exp, scale=-1.0))(), reads=[rpsw], writes=[rwin])
                        fs = []
                        for d in range(2):
                            ps, rps = psp.get()
                            col = o * 2048 + d * 1024 + ct * 512
                            P.op("pe", (lambda ps=ps, a=a, col=col: lambda e: e.matmul(ps[:], lhsT=h2[:, a * 128:(a + 1) * 128], rhs=w3[:, col:col + 512], start=True, stop=True))(), reads=[rh2, rw3], writes=[rps])
                            f, rf = fp_.get()
                            P.op("dve", (lambda f=f, ps=ps, win=win: lambda e: e.tensor_tensor(out=f[:], in0=ps[:], in1=win[:], op=ALU.mult))(), reads=[rps, rwin], writes=[rf])
                            ab, rab = abp.get()
                            P.op("dve", (lambda ab=ab, f=f: lambda e: e.scalar_tensor_tensor(out=ab[:], in0=f[:], scalar=-1.0, in1=f[:], op0=ALU.mult, op1=ALU.max))(), reads=[rf], writes=[rab])
                            pn, rpn = psn[o * 2 + ct]
                            P.op("pe", (lambda pn=pn, ab=ab, a=a, d=d: lambda e: e.matmul(pn[:], lhsT=self.ones_bf[:], rhs=ab[:], start=(a == 0 and d == 0), stop=(a == nt - 1 and d == 1)))(), reads=[self.rones, rab], writes=[rpn])
                            fs.append((f, rf))
                        (f0, rf0), (f1, rf1) = fs
                        hs, rhs_ = hsp.get()
                        hd, rhd = hdp.get()
                        P.op("pool", (lambda hs=hs, f0=f0, f1=f1: lambda e: e.tensor_tensor(out=hs[:], in0=f0[:], in1=f1[:], op=ALU.add))(), reads=[rf0, rf1], writes=[rhs_])
                        P.op("pool", (lambda hd=hd, f0=f0, f1=f1: lambda e: e.tensor_tensor(out=hd[:], in0=f0[:], in1=f1[:], op=ALU.subtract))(), reads=[rf0, rf1], writes=[rhd])
                        c2 = o * 1024 + ct * 512
                        P.dma("sp", (lambda hs=hs, a=a, c2=c2: lambda e: e.dma_start(out=self.dr["HS" + nm][a, :, c2:c2 + 512], in_=hs[:]))(), reads=[rhs_], writes=[self.drreg["HS" + nm]])
                        P.dma("sp", (lambda hd=hd, a=a, c2=c2: lambda e: e.dma_start(out=self.dr["HD" + nm][a, :, c2:c2 + 512], in_=hd[:]))(), reads=[rhd], writes=[self.drreg["HD" + nm]])
            rn, rrn = S.sb("hrn", [128, 2048], F32)
            for i in range(4):
                pn, rpn = psn[i]
                P.op("dve", (lambda pn=pn, i=i: lambda e: e.tensor_scalar(out=rn[:, i * 512:(i + 1) * 512], in0=pn[:], scalar1=EPS, scalar2=None, op0=ALU.add))(), reads=[rpn], writes=[rrn])
            P.op("dve", lambda e: e.reciprocal(out=rn[:], in_=rn[:]), reads=[rrn], writes=[rrn])
            P.dma("sp", lambda e: e.dma_start(out=self.dr["RN" + nm], in_=rn[:]), reads=[rrn], writes=[self.drreg["RN" + nm]])

    def hy_gemm(self, S, wnames, nm, xs, n_k, evac, nchunks):
        P = self.P
        wps = [S.pool("hgw%d" % i, 2, [128, n_k, 128], BF16) for i in range(len(wnames))]
        pps = [S.pool("hgp%d" % i, 2, [128, 512], F32, psum=True) for i in range(len(set(g for (_, g) in wnames)))]
        def load(oc):
            wts = []
            for i, (wn, g) in enumerate(wnames):
                wt, rwt = wps[i].get()
                P.dma("sp" if i % 2 == 0 else "act", (lambda wt=wt, wn=wn, oc=oc: lambda e: e.dma_start(out=wt[:], in_=self.dr[wn + nm][oc]))(), reads=[self.drreg[wn + nm]], writes=[rwt])
                wts.append((wt, rwt))
            return wts

        nxt = load(0)
        for oc in range(nchunks):
            wts = nxt
            if oc + 1 < nchunks:
                nxt = load(oc + 1)
            outs = {}
            groups = sorted(set(g for (_, g) in wnames))
            for g in groups:
                outs[g] = pps[g].get()
            cntg = {g: 0 for g in groups}
            totg = {g: sum(1 for (_, gg) in wnames if gg == g) * n_k for g in groups}
            for i, (wn, g) in enumerate(wnames):
                wt, rwt = wts[i]
                x, rx = xs[i]
                ps, rps = outs[g]
                for a in range(n_k):
                    first = cntg[g] == 0
                    cntg[g] += 1
                    last = cntg[g] == totg[g]
                    P.op("pe", (lambda ps=ps, wt=wt, x=x, a=a, first=first, last=last: lambda e: e.matmul(ps[:], lhsT=wt[:, a, :], rhs=x[:, a, :], start=first, stop=last))(), reads=[rwt, rx], writes=[rps])
            evac(oc, outs)

    def hy_spectra(self, Ls):
        P = self.P
        nt = Ls // 128
        nm = "L%d" % Ls
        self.dram("GR" + nm, [nt, 128, 2048], BF16, "Internal")
        self.dram("GQ" + nm, [nt, 128, 2048], BF16, "Internal")
        with Stage(self) as S:
            rn, rrn = S.sb("srn", [128, 2048], F32)
            P.dma("sp", lambda e: e.dma_start(out=rn[:], in_=self.dr["RN" + nm]), reads=[self.drreg["RN" + nm]], writes=[rrn])
            hs, rhs_ = S.sb("shs", [128, nt, 512], BF16)
            hd, rhd = S.sb("shd", [128, nt, 512], BF16)
            gop = S.pool("sgo", 4, [128, 512], BF16)
            for ct in range(4):
                P.dma("sp", (lambda ct=ct: lambda e: e.dma_start(out=hs[:], in_=self.dr["HS" + nm][:, :, ct * 512:(ct + 1) * 512].rearrange("a p c -> p a c")))(), reads=[self.drreg["HS" + nm]], writes=[rhs_])
                P.dma("sp", (lambda ct=ct: lambda e: e.dma_start(out=hd[:], in_=self.dr["HD" + nm][:, :, ct * 512:(ct + 1) * 512].rearrange("a p c -> p a c")))(), reads=[self.drreg["HD" + nm]], writes=[rhd])

                def evac(fc, outs, ct=ct):
                    for g, name in ((0, "GR"), (1, "GQ")):
                        ps, rps = outs[g]
                        go, rgo = gop.get()
                        P.op("dve", (lambda go=go, ps=ps: lambda e: e.tensor_tensor(out=go[:], in0=ps[:], in1=rn[:, ct * 512:(ct + 1) * 512], op=ALU.mult))(), reads=[rps, rrn], writes=[rgo])
                        P.dma("sp", (lambda go=go, name=name, fc=fc: lambda e: e.dma_start(out=self.dr[name + nm][fc, :, ct * 512:(ct + 1) * 512], in_=go[:]))(), reads=[rgo], writes=[self.drreg[name + nm]])

                with Stage(self) as S2:
                    self.hy_gemm(S2, [("CF", 0), ("SF", 1)], nm, [(hs, rhs_), (hd, rhd)], nt, evac, nt)

    def hy_conv(self, Ls, blk0):
        P = self.P
        nt = Ls // 128
        nm = "L%d" % Ls
        with Stage(self) as S:
            u, ru = S.sb("cu", [128, nt, 512], BF16)
            Yr, rYr = S.sb("cYr", [128, nt, 512], BF16)
            Yq, rYq = S.sb("cYq", [128, nt, 512], BF16)
            skb, rskb = S.sb("cskb", [128, 512], F32)
            grp = S.pool("cgr", 2, [128, 512], BF16)
            gqp = S.pool("cgq", 2, [128, 512], BF16)
            ap_ = S.pool("cA", 2, [128, 512], F32)
            bp_ = S.pool("cB", 2, [128, 512], F32)
            t1p = S.pool("ct1", 2, [128, 512], F32)
            t2p = S.pool("ct2", 2, [128, 512], F32)
            xgp = S.pool("cxg", 2, [128, 512], BF16)
            zop = S.pool("czo", 2, [128, 512], BF16)
            for o in range(2):
                for ct in range(2):
                    cs = slice(ct * 512, (ct + 1) * 512)
                    src = self.dr["utm"][0] if o == 0 else self.dr["z1"]
                    rsrc = self.drreg["utm"] if o == 0 else self.drreg["z1"]
                    P.dma("sp", (lambda src=src, cs=cs: lambda e: e.dma_start(out=u[:], in_=src[blk0:blk0 + nt, :, cs].rearrange("a p c -> p a c")))(), reads=[rsrc], writes=[ru])
                    sk0 = o * 1024 + ct * 512
                    P.dma("sp", (lambda sk0=sk0: lambda e: e.dma_start(out=skb[:], in_=bcast_rows(self.dr["hy_skip"][0:1, sk0:sk0 + 512], 512)))(), reads=[self.drreg["hy_skip"]], writes=[rskb])

                    def evac_f(fc, outs, o=o, ct=ct):
                        pa, rpa = outs[0]
                        pb, rpb = outs[1]
                        gr, rgr = grp.get()
                        gq, rgq = gqp.get()
                        gc0 = o * 1024 + ct * 512
                        P.dma("sp", (lambda gr=gr, fc=fc, gc0=gc0: lambda e: e.dma_start(out=gr[:], in_=self.dr["GR" + nm][fc, :, gc0:gc0 + 512]))(), reads=[self.drreg["GR" + nm]], writes=[rgr])
                        P.dma("sp", (lambda gq=gq, fc=fc, gc0=gc0: lambda e: e.dma_start(out=gq[:], in_=self.dr["GQ" + nm][fc, :, gc0:gc0 + 512]))(), reads=[self.drreg["GQ" + nm]], writes=[rgq])
                        A, rA = ap_.get()
                        Bq, rBq = bp_.get()
                        P.op("act", (lambda A=A, pa=pa: lambda e: e.copy(out=A[:], in_=pa[:]))(), reads=[rpa], writes=[rA])
                        P.op("act", (lambda Bq=Bq, pb=pb: lambda e: e.copy(out=Bq[:], in_=pb[:]))(), reads=[rpb], writes=[rBq])
                        t1, rt1 = t1p.get()
                        t2, rt2 = t2p.get()
                        P.op("dve", (lambda t1=t1, A=A, gr=gr: lambda e: e.tensor_tensor(out=t1[:], in0=A[:], in1=gr[:], op=ALU.mult))(), reads=[rA, rgr], writes=[rt1])
                        P.op("pool", (lambda t2=t2, Bq=Bq, gq=gq: lambda e: e.tensor_tensor(out=t2[:], in0=Bq[:], in1=gq[:], op=ALU.mult))(), reads=[rBq, rgq], writes=[rt2])
                        P.op("dve", (lambda t1=t1, t2=t2, fc=fc: lambda e: e.tensor_tensor(out=Yr[:, fc, :], in0=t1[:], in1=t2[:], op=ALU.subtract))(), reads=[rt1, rt2], writes=[rYr])
                        t3, rt3 = t1p.get()
                        t4, rt4 = t2p.get()
                        P.op("dve", (lambda t3=t3, A=A, gq=gq: lambda e: e.tensor_tensor(out=t3[:], in0=A[:], in1=gq[:], op=ALU.mult))(), reads=[rA, rgq], writes=[rt3])
                        P.op("pool", (lambda t4=t4, Bq=Bq, gr=gr: lambda e: e.tensor_tensor(out=t4[:], in0=Bq[:], in1=gr[:], op=ALU.mult))(), reads=[rBq, rgr], writes=[rt4])
                        P.op("pool", (lambda t3=t3, t4=t4, fc=fc: lambda e: e.tensor_tensor(out=Yq[:, fc, :], in0=t3[:], in1=t4[:], op=ALU.add))(), reads=[rt3, rt4], writes=[rYq])

                    with Stage(self) as S2:
                        self.hy_gemm(S2, [("CF", 0), ("SF", 1)], nm, [(u, ru), (u, ru)], nt, evac_f, nt)

                    def evac_i(tc, outs, o=o, ct=ct, cs=cs):
                        py, rpy = outs[0]
                        xg, rxg = xgp.get()
                        P.dma("sp", (lambda xg=xg, tc=tc: lambda e: e.dma_start(out=xg[:], in_=self.dr["utm"][1 + o][blk0 + tc, :, cs]))(), reads=[self.drreg["utm"]], writes=[rxg])
                        t1, rt1 = t1p.get()
                        P.op("pool", (lambda t1=t1, tc=tc: lambda e: e.tensor_tensor(out=t1[:], in0=u[:, tc, :], in1=skb[:], op=ALU.mult))(), reads=[ru, rskb], writes=[rt1])
                        P.op("dve", (lambda t1=t1, py=py: lambda e: e.tensor_tensor(out=t1[:], in0=py[:], in1=t1[:], op=ALU.add))(), reads=[rpy, rt1], writes=[rt1])
                        zo, rzo = zop.get()
                        P.op("dve", (lambda zo=zo, t1=t1, xg=xg: lambda e: e.tensor_tensor(out=zo[:], in0=t1[:], in1=xg[:], op=ALU.mult))(), reads=[rt1, rxg], writes=[rzo])
                        dst = "z1" if o == 0 else "ytm"
                        P.dma("sp", (lambda zo=zo, dst=dst, tc=tc: lambda e: e.dma_start(out=self.dr[dst][blk0 + tc, :, cs], in_=zo[:]))(), reads=[rzo], writes=[self.drreg[dst]])

                    with Stage(self) as S2:
                        self.hy_gemm(S2, [("CI", 0), ("SI", 0)], nm, [(Yr, rYr), (Yq, rYq)], nt, evac_i, nt)

    def hy_out(self):
        P = self.P
        with Stage(self) as S:
            yin_p = S.pool("yin", 2, [128, 1024], BF16)
            ptr = S.pool("yptr", 2, [128, 1024], BF16, psum=True)
            stg_p = S.pool("ystg", 2, [128, 8, 128], BF16)
            for blk in range(34):
                yin, ryin = yin_p.get()
                P.dma("sp", (lambda yin=yin, blk=blk: lambda e: e.dma_start(out=yin[:], in_=self.dr["ytm"][blk]))(), reads=[self.drreg["ytm"]], writes=[ryin])
                pt_, rpt_ = ptr.get()
                for c in range(8):
                    P.op("pe", (lambda pt_=pt_, yin=yin, c=c: lambda e: e.transpose(pt_[:, c * 128:(c + 1) * 128], yin[:, c * 128:(c + 1) * 128], self.ident_bf[:]))(), reads=[ryin, self.rident_bf], writes=[rpt_])
                stg, rstg = stg_p.get()
                P.op("act" if blk % 2 == 0 else "dve", (lambda pt_=pt_, stg=stg, blk=blk: (lambda e: e.copy(out=stg[:], in_=pt_[:].rearrange("p (a b) -> p a b", b=128))) if blk % 2 == 0 else (lambda e: e.tensor_copy(out=stg[:], in_=pt_[:].rearrange("p (a b) -> p a b", b=128))))(), reads=[rpt_], writes=[rstg])
                P.dma("sp", (lambda stg=stg, blk=blk: lambda e: e.dma_start(out=self.dr["yt"][:, :, blk * 128:(blk + 1) * 128].rearrange("c p n -> p c n"), in_=stg[:]))(), reads=[rstg], writes=[self.drreg["yt"]])

    def stage_hyena(self):
        self.hy_prep()
        for Ls, blk0 in ((NCX, 0), (L, 2)):
            if Ls not in self.dft_done:
                self.hy_dft_gen(Ls)
                self.dft_done.add(Ls)
            self.hy_filters(Ls)
            self.hy_spectra(Ls)
            self.hy_conv(Ls, blk0)
        self.hy_out()

    def build(self, stages=None):
        on = lambda s_: stages is None or s_ in stages
        for idx in range(len(self.layers)):
            self.set_layer(idx)
            with Stage(self) as S0:
                self.load_consts(S0)
                self.mod_vectors(S0)
                if on("inproj"):
                    self.stage_inproj()
                if on("attn"):
                    with Stage(self) as SA:
                        self.rope_tables(SA)
                        self.stage_attn()
                if on("lru"):
                    self.stage_lru()
                if on("hyena"):
                    self.stage_hyena()
                if on("merge"):
                    self.stage_merge()
                if on("ffn"):
                    self.stage_ffn()
                if idx == len(self.layers) - 1 and on("final"):
                    self.stage_final()
                self.P.flush()
        return self.nc


def make_xt(inp, b):
    tok = np.concatenate([inp["ctx"][b], inp["x"][b]], axis=0)
    return np.ascontiguousarray(tok.T.reshape(KC, 128, T).transpose(1, 0, 2))


def layer_inputs(inp, li, b):
    j = li // 2
    m = {"vp": make_vp(inp, li, b), "w_mod": inp["w_mod"][li], "w_in": inp["w_in"][li],
         "w_br": inp["w_br"][li], "w_out": inp["w_out"][li], "lru_wa": inp["lru_wa"][li], "lru_wi": inp["lru_wi"][li],
         "hy_w1": inp["hy_f_w1"][li], "hy_w2": inp["hy_f_w2"][li], "hy_w3": inp["hy_f_w3"][li],
         "hy_decay": inp["hy_decay"][li].reshape(1, 2048), "hy_skip": inp["hy_skip"][li].reshape(1, 2048),
         "da_lam": inp["da_lambda"][li].reshape(1, 256)}
    if li % 2 == 0:
        m["f_wg"] = inp["ffn_w_gate"][j][None]
        m["f_wu"] = inp["ffn_w_up"][j][None]
        m["f_wd"] = inp["ffn_w_down"][j][None]
    else:
        m["f_wg"] = inp["moe_w_gate"][j]
        m["f_wu"] = inp["moe_w_up"][j]
        m["f_wd"] = inp["moe_w_down"][j]
        m["router"] = inp["moe_router"][j]
    return {k + "_L%d" % li: np.ascontiguousarray(np.asarray(v, np.float32)) for k, v in m.items()}


def kernel(**inputs):
    inp = {k: np.asarray(v) for k, v in inputs.items()}
    nb = inp["x"].shape[0]
    layers = (0, 1, 2, 3)
    bld = Builder(layers)
    nc = bld.build()
    shared = {}
    in_maps = []
    for b in range(nb):
        m = {"x_ext": make_xt(inp, b)}
        for li in layers:
            li_in = layer_inputs(inp, li, b)
            for k, v in li_in.items():
                if k.startswith("vp"):
                    m[k] = v
                else:
                    m[k] = shared.setdefault(k, v)
        in_maps.append(m)
    res = run_bass_kernel_spmd(nc, in_maps, core_ids=list(range(nb)))
    out = np.empty((nb, L, D), np.float32)
    for b in range(nb):
        y = np.asarray(res.results[b]["y_out"], np.float32)
        out[b] = y.transpose(1, 0, 2).reshape(D, L).T
    return out
```

```python
import math
from contextlib import ExitStack
import numpy as np
import concourse.bass as bass
import concourse.mybir as mybir
from concourse.bass_utils import run_bass_kernel_spmd

F32 = mybir.dt.float32
BF16 = mybir.dt.bfloat16
ALU = mybir.AluOpType
AF = mybir.ActivationFunctionType
AX = mybir.AxisListType

D = 1024
L = 4096
NCX = 256
T = L + NCX
KC = 8
DFF = 2816
FC = DFF // 128
NE = 8
C_K, C_V, C_LX, C_Q, C_LY, C_HY, C_G, C_END = 0, 1024, 2048, 3072, 4096, 5120, 8192, 11264
EPS = 1e-6
TT = [(0, 256)] + [(256 + 512 * i, 512) for i in range(8)]
MAGIC = 12582912.0
PI = math.pi

ENGS = ("pe", "act", "dve", "pool", "sp")
SAME_ENGINE_SYNC = True
NDMASEM = 6
NPESEM = 6
KEEP_WARM = False


class Reg:
    __slots__ = ("w", "r", "name", "multi")

    def __init__(self, name="", multi=False):
        self.w = {}
        self.r = {}
        self.name = name
        self.multi = multi


class Prog:
    def __init__(self, nc):
        self.nc = nc
        self.q = {e: [] for e in ENGS}
        self.sems = {}
        self.cnt = {}
        self.key_eng = {}
        self.keys = {}
        for e in ENGS:
            nk = NPESEM if e == "pe" else 1
            self.keys[e] = []
            for i in range(nk):
                k = e if i == 0 else "%s%d" % (e, i)
                self.sems[k] = nc.alloc_semaphore(name="s_" + k)
                self.cnt[k] = 0
                self.key_eng[k] = e
                self.keys[e].append(k)
        self.cur = {e: 0 for e in ENGS}
        self.dma_next = {}
        for e in ("sp", "act", "pool"):
            for i in range(NDMASEM):
                k = "d_%s%d" % (e, i)
                self.sems[k] = nc.alloc_semaphore(name=k)
                self.cnt[k] = 0
            self.dma_next[e] = 0
        self.seen = {e: {} for e in ENGS}
        self.n_ins = 0
        self.n_wait = 0

    def _need(self, eng, tok, waits):
        k, v = tok
        if self.key_eng.get(k) == eng and (eng == "pe" or not SAME_ENGINE_SYNC):
            return
        if self.seen[eng].get(k, 0) >= v:
            return
        waits[k] = max(waits.get(k, 0), v)

    def _deps(self, eng, reads, writes):
        waits = {}
        for r in reads:
            for k, v in r.w.items():
                self._need(eng, (k, v), waits)
        for w in writes:
            if not w.multi:
                for k, v in w.w.items():
                    self._need(eng, (k, v), waits)
            for k, v in w.r.items():
                self._need(eng, (k, v), waits)
        for k, v in waits.items():
            self.seen[eng][k] = v
        return waits

    def _commit(self, tok, reads, writes):
        k, v = tok
        for r in reads:
            r.r[k] = max(r.r.get(k, 0), v)
        for w in writes:
            if w.multi:
                w.w[k] = max(w.w.get(k, 0), v)
            else:
                w.w = {k: v}
            w.r = {}

    def op(self, eng, fn, reads=(), writes=()):
        waits = self._deps(eng, reads, writes)
        key = self.keys[eng][self.cur[eng]]
        self.cnt[key] += 1
        tok = (key, self.cnt[key])
        self.q[eng].append((waits, fn, key, 1))
        self._commit(tok, reads, writes)
        self.n_ins += 1
        self.n_wait += len(waits)
        return tok

    def dma(self, eng, fn, reads=(), writes=()):
        i = self.dma_next[eng]
        self.dma_next[eng] = (i + 1) % NDMASEM
        k = "d_%s%d" % (eng, i)
        waits = self._deps(eng, reads, writes)
        prev = self.cnt[k]
        if prev > 0 and self.seen[eng].get(k, 0) < prev:
            waits[k] = max(waits.get(k, 0), prev)
            self.seen[eng][k] = prev
        self.cnt[k] += 16
        tok = (k, self.cnt[k])
        self.q[eng].append((waits, fn, k, 16))
        self._commit(tok, reads, writes)
        self.n_ins += 1
        self.n_wait += len(waits)
        return tok

    def barrier(self):
        for e in ENGS:
            waits = {}
            for k, v in self.cnt.items():
                if v > 0 and self.seen[e].get(k, 0) < v:
                    waits[k] = v
                    self.seen[e][k] = v
            if waits:
                self.q[e].append((waits, None, None, 0))
        self.cur["pe"] = (self.cur["pe"] + 1) % len(self.keys["pe"])

    def finish(self):
        self.flush()

    def flush(self):
        self.barrier()
        sems = self.sems
        q = self.q
        self.q = {e: [] for e in ENGS}

        def emit(e, lst):
            for waits, fn, k, inc in lst:
                for wk, wv in waits.items():
                    e.wait_ge(sems[wk], wv)
                if fn is not None:
                    fn(e).then_inc(sems[k], inc)

        with self.nc.Block() as block:
            @block.tensor
            def _(e):
                emit(e, q["pe"])

            @block.scalar
            def _(e):
                emit(e, q["act"])

            @block.vector
            def _(e):
                emit(e, q["dve"])

            @block.gpsimd
            def _(e):
                emit(e, q["pool"])

            @block.sync
            def _(e):
                emit(e, q["sp"])


def bcast_rows(a, n):
    return bass.AP(a.tensor, a.offset, [[0, 128], [1, n]])


_uid = [0]


def uname(s):
    _uid[0] += 1
    return "%s_%d" % (s, _uid[0])


class Stage:
    def __init__(self, B):
        self.B = B
        self.es = ExitStack()

    def sb(self, name, shape, dt):
        t = self.es.enter_context(self.B.nc.sbuf_tensor(uname(name), list(shape), dt))
        return t, Reg(name)

    def ps(self, name, shape, dt=F32):
        t = self.es.enter_context(self.B.nc.psum_tensor(uname(name), list(shape), dt))
        return t, Reg(name)

    def pool(self, name, n, shape, dt, psum=False):
        return RPool([(self.ps if psum else self.sb)(name, shape, dt) for _ in range(n)])

    def close(self):
        self.B.P.barrier()
        self.es.close()

    def __enter__(self):
        return self

    def __exit__(self, *a):
        if a[0] is None:
            self.close()
        return False


class RPool:
    def __init__(self, tiles):
        self.tiles = tiles
        self.i = 0

    def get(self):
        t = self.tiles[self.i]
        self.i = (self.i + 1) % len(self.tiles)
        return t


def _cols(v):
    v = np.asarray(v, np.float32).reshape(-1, 128)
    return np.ascontiguousarray(v.T)


VP = {}
_o = 0
for _n, _w in [("b_mod", 48), ("g_mix", 8), ("g_ffn", 8), ("b_gate", 24), ("lru_cw", 32), ("lru_cb", 8),
               ("lru_ba", 16), ("lru_bi", 16), ("lru_lam", 16), ("hy_cw", 72), ("hy_cb", 24), ("subln", 1),
               ("c", 8), ("c_ctx", 8), ("g_final", 8), ("hy_b1", 1), ("hy_b2", 1), ("hy_freq", 1),
               ("bands", 1), ("phase", 1), ("ropeinv", 1), ("pidx", 1), ("ident", 128), ("rmat", 128)]:
    VP[_n] = (_o, _w)
    _o += _w
NV = _o


def make_vp(inp, li, b):
    vp = np.zeros((128, NV), np.float32)

    def put(n, a):
        o, w = VP[n]
        a = np.asarray(a, np.float32)
        assert a.shape[1] == w, (n, a.shape, w)
        vp[:a.shape[0], o:o + w] = a

    put("b_mod", _cols(inp["b_mod"][li]))
    put("g_mix", _cols(inp["g_mix"][li]))
    put("g_ffn", _cols(inp["g_ffn"][li]))
    put("b_gate", _cols(inp["b_gate"][li]))
    put("lru_cw", _cols(inp["lru_conv_w"][li].reshape(-1)))
    put("lru_cb", _cols(inp["lru_conv_b"][li]))
    put("lru_ba", _cols(inp["lru_ba"][li].reshape(-1)))
    put("lru_bi", _cols(inp["lru_bi"][li].reshape(-1)))
    put("lru_lam", _cols(inp["lru_lambda"][li].reshape(-1)))
    put("hy_cw", _cols(inp["hy_conv_w"][li].reshape(-1)))
    put("hy_cb", _cols(inp["hy_conv_b"][li]))
    put("subln", _cols(inp["da_subln_g"][li]))
    put("c", _cols(inp["c"][b]))
    put("c_ctx", _cols(inp["c_ctx"]))
    put("g_final", _cols(inp["g_final"]))
    put("hy_b1", inp["hy_f_b1"][li].reshape(64, 1))
    put("hy_b2", inp["hy_f_b2"][li].reshape(64, 1))
    put("hy_freq", inp["hy_f_freq"][li].reshape(64, 1))
    bands = np.linspace(1e-4, 15.0, 16, dtype=np.float32)
    bcol = np.zeros((33, 1), np.float32)
    bcol[1:17, 0] = bands
    bcol[17:33, 0] = bands
    pcol = np.zeros((33, 1), np.float32)
    pcol[1:17, 0] = np.float32(PI / 2)
    pcol[17:33, 0] = np.float32(PI)
    put("bands", bcol)
    put("phase", pcol)
    inv = (10000.0 ** (-(np.arange(128) % 16).astype(np.float32) / 16.0)).astype(np.float32)
    put("ropeinv", inv.reshape(128, 1))
    put("pidx", np.arange(128, dtype=np.float32).reshape(128, 1))
    put("ident", np.eye(128, dtype=np.float32))
    rm = np.zeros((128, 128), np.float32)
    for m in range(128):
        if m % 32 < 16:
            rm[m + 16, m] = -1.0
        else:
            rm[m - 16, m] = 1.0
    put("rmat", rm)
    return vp


class Builder:
    LAYER_INPUTS = [("vp", [128, NV]), ("w_mod", [D, 6 * D]), ("w_in", [D, C_END]), ("w_br", [3, D, D]), ("w_out", [D, D]),
                    ("lru_wa", [2, 8, 128, 128]), ("lru_wi", [2, 8, 128, 128]), ("hy_w1", [33, 64]), ("hy_w2", [64, 64]),
                    ("hy_w3", [64, 4096]), ("hy_decay", [1, 2048]), ("hy_skip", [1, 2048]), ("da_lam", [1, 256])]

    def __init__(self, layers=(0, 1, 2, 3), debug=False):
        self.layers = list(layers)
        self.debug = debug
        nc = bass.Bass("TRN2", target_bir_lowering=False)
        self.nc = nc
        self.P = Prog(nc)
        self.dr = {}
        self.drreg = {}
        self.dft_done = set()
        I = "ExternalInput"
        self.dram("x_ext", [128, KC, T], F32, I)
        for li in self.layers:
            sfx = "_L%d" % li
            for nm, shp in self.LAYER_INPUTS:
                self.dram(nm + sfx, shp, F32, I)
            ne = NE if li % 2 == 1 else 1
            self.dram("f_wg" + sfx, [ne, D, DFF], F32, I)
            self.dram("f_wu" + sfx, [ne, D, DFF], F32, I)
            self.dram("f_wd" + sfx, [ne, DFF, D], F32, I)
            if li % 2 == 1:
                self.dram("router" + sfx, [D, NE], F32, I)
        dk = "Internal"
        self.dram("pt", [88, 128, T], BF16, dk)
        self.dram("at", [8, 128, T], BF16, dk)
        self.dram("rt", [8, 128, T], BF16, dk)
        self.dram("yt", [8, 128, T], BF16, dk)
        dk2 = "ExternalOutput" if debug else "Internal"
        self.dram("xt_mid", [128, KC, T], F32, dk2)
        self.dram("xr0", [128, KC, T], F32, dk2)
        self.dram("xr1", [128, KC, T], F32, dk2)
        self.dram("y_out", [128, KC, L], F32, "ExternalOutput")

    def set_layer(self, idx):
        li = self.layers[idx]
        self.li = li
        self.moe = (li % 2 == 1)
        self.ne = NE if self.moe else 1
        self.lam_init = 0.8 - 0.6 * math.exp(-0.3 * li)
        sfx = "_L%d" % li
        names = [n for n, _ in self.LAYER_INPUTS] + ["f_wg", "f_wu", "f_wd"] + (["router"] if self.moe else [])
        for n in names:
            self.dr[n] = self.dr[n + sfx]
            self.drreg[n] = self.drreg[n + sfx]
        xin = "x_ext" if idx == 0 else "xr%d" % ((idx - 1) % 2)
        xout = "xr%d" % (idx % 2)
        for alias, real in (("xt", xin), ("xt_out", xout)):
            self.dr[alias] = self.dr[real]
            self.drreg[alias] = self.drreg[real]

    def dram(self, name, shape, dt, kind):
        if name in self.dr:
            return self.dr[name]
        self.dr[name] = self.nc.dram_tensor(name, list(shape), dt, kind=kind).ap()
        self.drreg[name] = Reg(name, multi=True)
        return self.dr[name]

    def load_consts(self, S):
        P, nc = self.P, self.nc
        self.vp, self.rvp = S.sb("vp", [128, NV], F32)
        vp, rvp = self.vp, self.rvp
        P.dma("sp", lambda e: e.dma_start(out=vp[:], in_=self.dr["vp"]), reads=[self.drreg["vp"]], writes=[rvp])
        self.ones_bf, self.rones = S.sb("ones", [128, 128], BF16)
        P.op("pool", lambda e: e.memset(self.ones_bf[:], 1.0), writes=[self.rones])
        self.epsc, self.repsc = S.sb("epsc", [128, 4], F32)
        P.op("pool", lambda e: e.memset(self.epsc[:, 0:1], EPS), writes=[self.repsc])
        P.op("pool", lambda e: e.memset(self.epsc[:, 1:2], 1.0), writes=[self.repsc])
        P.op("pool", lambda e: e.memset(self.epsc[:, 2:3], 0.0), writes=[self.repsc])
        self.ident_bf, self.rident_bf = S.sb("identbf", [128, 128], BF16)
        o = VP["ident"][0]
        P.op("dve", lambda e: e.tensor_copy(out=self.ident_bf[:], in_=vp[:, o:o + 128]), reads=[rvp], writes=[self.rident_bf])
        self.rmat_bf, self.rrmat = S.sb("rmatbf", [128, 128], BF16)
        o2 = VP["rmat"][0]
        P.op("dve", lambda e: e.tensor_copy(out=self.rmat_bf[:], in_=vp[:, o2:o2 + 128]), reads=[rvp], writes=[self.rrmat])

    def vcol(self, name, j=0, w=1):
        o = VP[name][0]
        return self.vp[:, o + j:o + j + w]

    def mod_vectors(self, S):
        P = self.P
        self.modv, self.rmodv = S.sb("modv", [128, 48, 2], F32)
        self.gsm, self.rgsm = S.sb("gsm", [128, 8, 2], F32)
        self.gsf, self.rgsf = S.sb("gsf", [128, 8, 2], F32)
        modv, rmodv = self.modv, self.rmodv
        with Stage(self) as S2:
            cs, rcs = S2.sb("cs", [128, 8, 2], F32)
            oc_, occ = VP["c"][0], VP["c_ctx"][0]
            vp = self.vp
            P.op("act", lambda e: e.activation(out=cs[:, :, 0], in_=vp[:, oc_:oc_ + 8], func=AF.Silu), reads=[self.rvp], writes=[rcs])
            P.op("act", lambda e: e.activation(out=cs[:, :, 1], in_=vp[:, occ:occ + 8], func=AF.Silu), reads=[self.rvp], writes=[rcs])
            wpool = S2.pool("wmod", 2, [128, 8, 768], F32)
            pspool = S2.pool("psmod", 2, [128, 2], F32, psum=True)
            wsrc = self.dr["w_mod"].rearrange("(k p) n -> p k n", p=128)
            ob = VP["b_mod"][0]
            for og in range(8):
                wt, rwt = wpool.get()
                P.dma("sp", (lambda wt=wt, og=og: lambda e: e.dma_start(out=wt[:], in_=wsrc[:, :, og * 768:(og + 1) * 768]))(),
                      reads=[self.drreg["w_mod"]], writes=[rwt])
                for oc in range(6):
                    ps, rps = pspool.get()
                    for k in range(8):
                        P.op("pe", (lambda ps=ps, wt=wt, k=k, oc=oc: lambda e: e.matmul(ps[:], lhsT=wt[:, k, oc * 128:(oc + 1) * 128], rhs=cs[:, k, :], start=(k == 0), stop=(k == 7)))(),
                             reads=[rwt, rcs], writes=[rps])
                    j = og * 6 + oc
                    P.op("dve", (lambda ps=ps, j=j: lambda e: e.tensor_scalar(out=modv[:, j, :], in0=ps[:], scalar1=vp[:, ob + j:ob + j + 1], scalar2=None, op0=ALU.add))(),
                         reads=[rps, self.rvp], writes=[rmodv])
            for (gs, rgs, gname, j0) in ((self.gsm, self.rgsm, "g_mix", 8), (self.gsf, self.rgsf, "g_ffn", 32)):
                og_ = VP[gname][0]
                for w in range(2):
                    P.op("dve", (lambda gs=gs, j0=j0, w=w: lambda e: e.tensor_scalar(out=gs[:, :, w], in0=modv[:, j0:j0 + 8, w], scalar1=1.0, scalar2=None, op0=ALU.add))(),
                         reads=[rmodv], writes=[rgs])
                    P.op("dve", (lambda gs=gs, og_=og_, w=w: lambda e: e.tensor_tensor(out=gs[:, :, w], in0=gs[:, :, w], in1=vp[:, og_:og_ + 8], op=ALU.mult))(),
                         reads=[rgs, self.rvp], writes=[rgs])

    def norm_mod(self, S, src, tiles, gs, rgs, shift_j, out, rout, col0, shift=None, router=None):
        P = self.P
        xin_p = S.pool("nm_x", 1, [128, 8, 512], F32)
        sq_p = S.pool("nm_sq", 1, [128, 8, 512], BF16)
        rs_p = S.pool("nm_rs", 2, [128, 512], F32)
        tmp_p = S.pool("nm_t", 2, [128, 512], F32)
        ps_p = S.pool("nm_ps", 2, [128, 512], F32, psum=True)
        modv, rmodv = (self.modv, self.rmodv) if shift is None else shift
        if router is not None:
            f32_all = S.sb("nm_f32", [128, 8, 512], F32)
            f32, rf32 = f32_all
        for (n0, nsz) in tiles:
            w = 1 if n0 < NCX else 0
            xin, rxin = xin_p.get()
            P.dma("sp", (lambda xin=xin, n0=n0, nsz=nsz: lambda e: e.dma_start(out=xin[:, :, :nsz], in_=self.dr[src][:, :, n0:n0 + nsz]))(),
                  reads=[self.drreg[src]], writes=[rxin])
            sq, rsq = sq_p.get()
            P.op("act", (lambda sq=sq, xin=xin, nsz=nsz: lambda e: e.activation(out=sq[:, :, :nsz], in_=xin[:, :, :nsz], func=AF.Square))(),
                 reads=[rxin], writes=[rsq])
            ps, rps = ps_p.get()
            for k in range(8):
                P.op("pe", (lambda ps=ps, sq=sq, k=k, nsz=nsz: lambda e: e.matmul(ps[:, :nsz], lhsT=self.ones_bf[:], rhs=sq[:, k, :nsz], start=(k == 0), stop=(k == 7)))(),
                     reads=[rsq, self.rones], writes=[rps])
            rs, rrs = rs_p.get()
            P.op("act", (lambda rs=rs, ps=ps, nsz=nsz: lambda e: e.activation(out=rs[:, :nsz], in_=ps[:, :nsz], func=AF.Sqrt, bias=self.epsc[:, 0:1], scale=1.0 / D))(),
                 reads=[rps, self.repsc], writes=[rrs])
            P.op("dve", (lambda rs=rs, nsz=nsz: lambda e: e.reciprocal(out=rs[:, :nsz], in_=rs[:, :nsz]))(), reads=[rrs], writes=[rrs])
            for k in range(8):
                tmp, rtmp = tmp_p.get()
                P.op("dve", (lambda tmp=tmp, xin=xin, rs=rs, k=k, nsz=nsz: lambda e: e.tensor_tensor(out=tmp[:, :nsz], in0=xin[:, k, :nsz], in1=rs[:, :nsz], op=ALU.mult))(),
                     reads=[rxin, rrs], writes=[rtmp])
                P.op("act", (lambda tmp=tmp, k=k, nsz=nsz, n0=n0, w=w: lambda e: e.activation(out=out[:, k, n0 - col0:n0 - col0 + nsz], in_=tmp[:, :nsz], func=AF.Identity,
                                                                                         bias=modv[:, shift_j * 8 + k, w:w + 1], scale=gs[:, k, w:w + 1]))(),
                     reads=[rtmp, rmodv, rgs], writes=[rout])
                if router is not None:
                    f32, rf32 = f32_all
                    P.op("act", (lambda tmp=tmp, k=k, nsz=nsz, w=w: lambda e: e.activation(out=f32[:, k, :nsz], in_=tmp[:, :nsz], func=AF.Identity,
                                                                                bias=modv[:, shift_j * 8 + k, w:w + 1], scale=gs[:, k, w:w + 1]))(),
                         reads=[rtmp, rmodv, rgs], writes=[rf32])
            if router is not None:
                wr, rwr, psr, rpsr = router
                for sub in range(nsz // 128):
                    for k in range(8):
                        P.op("pe", (lambda sub=sub, k=k: lambda e: e.matmul(psr[:, sub, :], lhsT=f32[:, k, sub * 128:(sub + 1) * 128], rhs=wr[:, k, :], start=(k == 0), stop=(k == 7)))(),
                             reads=[rf32, rwr], writes=[rpsr])
            if router is not None:
                router_done = getattr(self, "_router_done")
                router_done(n0, nsz)

    def gemm(self, S, wsrc, rw, n_k, m_total, xT, rxT, tiles, col0, evac, mg=512, skip_chunks=()):
        P = self.P
        wp = S.pool("g_w", 2, [128, n_k, mg], BF16)
        psp = S.pool("g_ps", 4, [128, 512], F32, psum=True)
        for g0 in range(0, m_total, mg):
            gsz = min(mg, m_total - g0)
            chunks = [c for c in range(g0 // 128, (g0 + gsz) // 128) if c not in skip_chunks]
            if not chunks:
                continue
            wt, rwt = wp.get()
            P.dma("pool", (lambda wt=wt, g0=g0, gsz=gsz: lambda e: e.dma_start(out=wt[:, :, :gsz], in_=wsrc[:, :, g0:g0 + gsz]))(),
                  reads=[rw], writes=[rwt])
            for ti, (n0, nsz) in enumerate(tiles):
                for mc in chunks:
                    ps, rps = psp.get()
                    mo = mc * 128 - g0
                    for k in range(n_k):
                        P.op("pe", (lambda ps=ps, wt=wt, k=k, mo=mo, n0=n0, nsz=nsz: lambda e: e.matmul(ps[:, :nsz], lhsT=wt[:, k, mo:mo + 128], rhs=xT[:, k, n0 - col0:n0 - col0 + nsz], start=(k == 0), stop=(k == n_k - 1)))(),
                             reads=[rwt, rxT], writes=[rps])
                    evac(mc, ti, n0, nsz, ps, rps)

    def stage_inproj(self):
        P = self.P
        with Stage(self) as S:
            hT, rhT = S.sb("hT", [128, 8, T], BF16)
            with Stage(self) as S2:
                self.norm_mod(S2, "xt", TT, self.gsm, self.rgsm, 0, hT, rhT, 0)
            op_ = S.pool("ip_o", 4, [128, 512], BF16)
            cnt = [0]

            def evac(mc, ti, n0, nsz, ps, rps):
                ot, rot = op_.get()
                if cnt[0] % 2 == 0:
                    P.op("act", lambda e: e.copy(out=ot[:, :nsz], in_=ps[:, :nsz]), reads=[rps], writes=[rot])
                else:
                    P.op("dve", lambda e: e.tensor_copy(out=ot[:, :nsz], in_=ps[:, :nsz]), reads=[rps], writes=[rot])
                cnt[0] += 1
                P.dma("sp", lambda e: e.dma_start(out=self.dr["pt"][mc, :, n0:n0 + nsz], in_=ot[:, :nsz]), reads=[rot], writes=[self.drreg["pt"]])

            wsrc = self.dr["w_in"].rearrange("(k p) n -> p k n", p=128)
            self.gemm(S, wsrc, self.drreg["w_in"], 8, C_END, hT, rhT, TT, 0, evac)

    def range_sin(self, S, out, rout, src, rsrc, shape, shift=0.0, whole=True):
        P = self.P
        t1, rt1 = S.sb("rs_t1", shape, F32)
        t2, rt2 = S.sb("rs_t2", shape, F32)
        inv2pi = 1.0 / (2 * PI)
        if shift != 0.0:
            t0, rt0 = S.sb("rs_t0", shape, F32)
            P.op("dve", (lambda s0=src: lambda e: e.tensor_scalar(out=t0[:], in0=s0[:], scalar1=shift, scalar2=None, op0=ALU.add))(), reads=[rsrc], writes=[rt0])
            src, rsrc = t0, rt0
        P.op("dve", lambda e: e.tensor_scalar(out=t1[:], in0=src[:], scalar1=inv2pi, scalar2=MAGIC, op0=ALU.mult, op1=ALU.add), reads=[rsrc], writes=[rt1])
        P.op("dve", lambda e: e.tensor_scalar(out=t1[:], in0=t1[:], scalar1=-MAGIC, scalar2=None, op0=ALU.add), reads=[rt1], writes=[rt1])
        P.op("dve", lambda e: e.scalar_tensor_tensor(out=t2[:], in0=t1[:], scalar=-2 * PI, in1=src[:], op0=ALU.mult, op1=ALU.add), reads=[rt1, rsrc], writes=[rt2])
        P.op("dve", lambda e: e.tensor_scalar(out=t2[:], in0=t2[:], scalar1=PI, scalar2=-PI, op0=ALU.min, op1=ALU.max), reads=[rt2], writes=[rt2])
        P.op("act", lambda e: e.activation(out=out[:], in_=t2[:], func=AF.Sin), reads=[rt2], writes=[rout])

    def rope_tables(self, S):
        P = self.P
        self.cosT, self.rcosT = S.sb("cosT", [128, L], F32)
        self.sinT, self.rsinT = S.sb("sinT", [128, L], F32)
        with Stage(self) as S2:
            pos, rpos = S2.sb("pos", [128, L], F32)
            for p0 in (0, 32, 64, 96):
                pat = [[1, 64], [0, 64]] if (p0 % 64) == 0 else [[0, 64], [1, 64]]
                P.op("pool", (lambda p0=p0, pat=pat: lambda e: e.iota(pos[p0:p0 + 32, :].rearrange("p (a b) -> p a b", a=64), pattern=pat, base=0, channel_multiplier=0, allow_small_or_imprecise_dtypes=True))(), writes=[rpos])
            P.op("dve", lambda e: e.tensor_scalar(out=pos[:], in0=pos[:], scalar1=self.vcol("ropeinv"), scalar2=None, op0=ALU.mult), reads=[rpos, self.rvp], writes=[rpos])
            self.range_sin(S2, self.sinT, self.rsinT, pos, rpos, [128, L], 0.0)
            self.range_sin(S2, self.cosT, self.rcosT, pos, rpos, [128, L], PI / 2)

    def stage_attn(self):
        P = self.P
        pt, rpt = self.dr["pt"], self.drreg["pt"]
        with Stage(self) as S:
            neglam, rneglam = S.sb("neglam", [128, 1], F32)
            gsub, rgsub = S.sb("gsub", [128, 1], F32)
            lamrow, rlamrow = S.sb("lamrow", [1, 256], F32)
            lamw, rlamw = S.sb("lamw", [1, 8], F32)
            onesf, ronesf = S.sb("onesf", [1, 128], F32)
            ps_m, rps_m = S.ps("ps_m", [128, 512], F32)
            P.op("pool", lambda e: e.memset(onesf[:], 1.0), writes=[ronesf])
            P.dma("sp", lambda e: e.dma_start(out=lamrow[:], in_=self.dr["da_lam"]), reads=[self.drreg["da_lam"]], writes=[rlamrow])
            P.op("dve", lambda e: e.tensor_tensor(out=lamrow[:, 0:64], in0=lamrow[:, 0:64], in1=lamrow[:, 64:128], op=ALU.mult), reads=[rlamrow], writes=[rlamrow])
            P.op("dve", lambda e: e.tensor_tensor(out=lamrow[:, 128:192], in0=lamrow[:, 128:192], in1=lamrow[:, 192:256], op=ALU.mult), reads=[rlamrow], writes=[rlamrow])
            P.op("dve", lambda e: e.reduce_sum(out=lamw[:, 0:1], in_=lamrow[:, 0:64], axis=AX.X), reads=[rlamrow], writes=[rlamw])
            P.op("dve", lambda e: e.reduce_sum(out=lamw[:, 1:2], in_=lamrow[:, 128:192], axis=AX.X), reads=[rlamrow], writes=[rlamw])
            P.op("act", lambda e: e.activation(out=lamw[:, 2:4], in_=lamw[:, 0:2], func=AF.Exp), reads=[rlamw], writes=[rlamw])
            P.op("dve", lambda e: e.tensor_tensor(out=lamw[:, 4:5], in0=lamw[:, 3:4], in1=lamw[:, 2:3], op=ALU.subtract), reads=[rlamw], writes=[rlamw])
            P.op("dve", lambda e: e.tensor_scalar(out=lamw[:, 5:6], in0=lamw[:, 4:5], scalar1=-self.lam_init, scalar2=None, op0=ALU.add), reads=[rlamw], writes=[rlamw])
            P.op("pe", lambda e: e.matmul(ps_m[:, 0:1], lhsT=onesf[:], rhs=lamw[:, 5:6], start=True, stop=True), reads=[ronesf, rlamw], writes=[rps_m])
            P.op("dve", lambda e: e.tensor_copy(out=neglam[:], in_=ps_m[:, 0:1]), reads=[rps_m], writes=[rneglam])
            P.op("dve", lambda e: e.tensor_scalar(out=gsub[:], in0=self.vcol("subln"), scalar1=1.0 - self.lam_init, scalar2=None, op0=ALU.mult), reads=[self.rvp], writes=[rgsub])

            KT, rKT = S.sb("KT", [128, T], BF16)
            QT, rQT = S.sb("QT", [128, T], BF16)
            VT, rVT = S.sb("VT", [128, T], BF16)
            KR, rKR = S.sb("KR", [128, T], BF16)
            QR, rQR = S.sb("QR", [128, T], BF16)
            Vh, rVh = S.sb("Vh", [128, 34, 128], BF16)
            Qc = [S.sb("Qc%d" % c, [128, T], BF16) for c in range(2)]
            for c in range(2):
                P.op("pool", (lambda c=c: lambda e: e.memset(Qc[c][0][:], 0.0))(), writes=[Qc[c][1]])
            ps_tr, rps_tr = S.ps("ps_tr", [128, 512], BF16)
            ps_s = S.pool("ps_s", 4, [128, 512], F32, psum=True)
            ps_dum, rps_dum = None, None
            ps_o = S.pool("ps_o", 1, [128, 512], F32, psum=True)
            ps_r = S.pool("ps_r", 1, [128, 512], F32, psum=True)
            ptp = S.pool("ptile", 6, [128, 512], BF16)
            t1p = S.pool("at1", 2, [128, 512], F32)
            t2p = S.pool("at2", 2, [128, 512], F32)
            onp = S.pool("on", 4, [128, 512], F32)
            rcp = S.pool("rc", 2, [128, 512], F32)
            o_p = S.pool("o", 2, [128, 512], F32)
            sqp = S.pool("asq", 2, [128, 512], BF16)
            aop = S.pool("aout", 2, [128, 512], BF16)
            for h in range(8):
                P.dma("sp", (lambda h=h: lambda e: e.dma_start(out=KT[:], in_=pt[C_K // 128 + h]))(), reads=[rpt], writes=[rKT])
                P.dma("sp", (lambda h=h: lambda e: e.dma_start(out=QT[:], in_=pt[C_Q // 128 + h]))(), reads=[rpt], writes=[rQT])
                P.dma("sp", (lambda h=h: lambda e: e.dma_start(out=VT[:], in_=pt[C_V // 128 + h]))(), reads=[rpt], writes=[rVT])
                P.op("pool", lambda e: e.tensor_copy(out=KR[:, 0:NCX], in_=KT[:, 0:NCX]), reads=[rKT], writes=[rKR])
                P.op("pool", lambda e: e.tensor_copy(out=QR[:, 0:NCX], in_=QT[:, 0:NCX]), reads=[rQT], writes=[rQR])
                for (src, rsrc, dst, rdst) in ((KT, rKT, KR, rKR), (QT, rQT, QR, rQR)):
                    for (n0, nsz) in TT[1:]:
                        P.op("pe", (lambda src=src, n0=n0: lambda e: e.matmul(ps_m[:], lhsT=self.rmat_bf[:], rhs=src[:, n0:n0 + 512], start=True, stop=True))(), reads=[self.rrmat, rsrc], writes=[rps_m])
                        t1, rt1 = t1p.get()
                        t2, rt2 = t2p.get()
                        P.op("dve", (lambda t1=t1, src=src, n0=n0: lambda e: e.tensor_tensor(out=t1[:], in0=src[:, n0:n0 + 512], in1=self.cosT[:, n0 - NCX:n0 - NCX + 512], op=ALU.mult))(), reads=[rsrc, self.rcosT], writes=[rt1])
                        P.op("dve", (lambda t2=t2, n0=n0: lambda e: e.tensor_tensor(out=t2[:], in0=ps_m[:], in1=self.sinT[:, n0 - NCX:n0 - NCX + 512], op=ALU.mult))(), reads=[rps_m, self.rsinT], writes=[rt2])
                        P.op("dve", (lambda t1=t1, t2=t2, dst=dst, n0=n0: lambda e: e.tensor_tensor(out=dst[:, n0:n0 + 512], in0=t1[:], in1=t2[:], op=ALU.add))(), reads=[rt1, rt2], writes=[rdst])
                for c in range(2):
                    P.op("pool", (lambda c=c: lambda e: e.tensor_copy(out=Qc[c][0][c * 64:(c + 1) * 64, :], in_=QR[c * 64:(c + 1) * 64, :]))(), reads=[rQR], writes=[Qc[c][1]])
                for b0 in range(0, 34, 4):
                    nb = min(4, 34 - b0)
                    for j in range(nb):
                        P.op("pe", (lambda b0=b0, j=j: lambda e: e.transpose(ps_tr[:, j * 128:(j + 1) * 128], VT[:, (b0 + j) * 128:(b0 + j + 1) * 128], self.ident_bf[:]))(), reads=[rVT, self.rident_bf], writes=[rps_tr])
                    P.op("act", (lambda b0=b0, nb=nb: lambda e: e.copy(out=Vh[:, b0:b0 + nb, :], in_=ps_tr[:, :nb * 128].rearrange("p (a b) -> p a b", b=128)))(), reads=[rps_tr], writes=[rVh])
                for (q0, qsz) in TT:
                    kbs = range(0, 2) if q0 < NCX else range(0, 34)
                    ons = []
                    for c in range(2):
                        pso, rpso = ps_o.get()
                        psr, rpsr = ps_r.get()
                        c0 = c * 64
                        nk = len(kbs)
                        pend = []

                        def emit_pv(item, pso=pso, rpso=rpso, psr=psr, rpsr=rpsr, nk=nk, qsz=qsz):
                            ik, kb, ptl, rptl = item
                            P.op("pe", (lambda: lambda e: e.matmul(pso[:, :qsz], lhsT=Vh[:, kb, :], rhs=ptl[:, :qsz], start=(ik == 0), stop=(ik == nk - 1)))(), reads=[rVh, rptl], writes=[rpso])
                            P.op("pe", (lambda: lambda e: e.matmul(psr[:, :qsz], lhsT=self.ones_bf[:], rhs=ptl[:, :qsz], start=(ik == 0), stop=(ik == nk - 1)))(), reads=[self.rones, rptl], writes=[rpsr])
                            if KEEP_WARM:
                                P.op("pe", (lambda: lambda e: e.matmul(ps_dum[:, :qsz], lhsT=self.ones_bf[:], rhs=ptl[:, :qsz], start=True, stop=True))(), reads=[self.rones, rptl], writes=[rps_dum])

                        for ik, kb in enumerate(kbs):
                            pss, rpss = ps_s.get()
                            P.op("pe", (lambda pss=pss, kb=kb, c=c, q0=q0, qsz=qsz: lambda e: e.matmul(pss[:, :qsz], lhsT=KR[:, kb * 128:(kb + 1) * 128], rhs=Qc[c][0][:, q0:q0 + qsz], start=True, stop=True))(), reads=[rKR, Qc[c][1]], writes=[rpss])
                            ptl, rptl = ptp.get()
                            P.op("act", (lambda ptl=ptl, pss=pss, qsz=qsz: lambda e: e.activation(out=ptl[:, :qsz], in_=pss[:, :qsz], func=AF.Exp, scale=0.125))(), reads=[rpss], writes=[rptl])
                            pend.append((ik, kb, ptl, rptl))
                            if len(pend) > 3:
                                emit_pv(pend.pop(0))
                        while pend:
                            emit_pv(pend.pop(0))
                        rc, rrc = rcp.get()
                        P.op("dve", (lambda rc=rc, psr=psr, qsz=qsz: lambda e: e.reciprocal(out=rc[:, :qsz], in_=psr[:, :qsz]))(), reads=[rpsr], writes=[rrc])
                        on, ron = onp.get()
                        P.op("dve", (lambda on=on, pso=pso, rc=rc, qsz=qsz: lambda e: e.tensor_tensor(out=on[:, :qsz], in0=pso[:, :qsz], in1=rc[:, :qsz], op=ALU.mult))(), reads=[rpso, rrc], writes=[ron])
                        ons.append((on, ron))
                    (on0, ron0), (on1, ron1) = ons
                    o, ro = o_p.get()
                    P.op("dve", (lambda o=o, on0=on0, on1=on1, qsz=qsz: lambda e: e.scalar_tensor_tensor(out=o[:, :qsz], in0=on1[:, :qsz], scalar=neglam[:, 0:1], in1=on0[:, :qsz], op0=ALU.mult, op1=ALU.add))(), reads=[ron0, ron1, rneglam], writes=[ro])
                    sq, rsq = sqp.get()
                    P.op("act", (lambda sq=sq, o=o, qsz=qsz: lambda e: e.activation(out=sq[:, :qsz], in_=o[:, :qsz], func=AF.Square))(), reads=[ro], writes=[rsq])
                    P.op("pe", (lambda sq=sq, qsz=qsz: lambda e: e.matmul(ps_m[:, :qsz], lhsT=self.ones_bf[:], rhs=sq[:, :qsz], start=True, stop=True))(), reads=[self.rones, rsq], writes=[rps_m])
                    rc, rrc = rcp.get()
                    P.op("act", (lambda rc=rc, qsz=qsz: lambda e: e.activation(out=rc[:, :qsz], in_=ps_m[:, :qsz], func=AF.Sqrt, bias=self.epsc[:, 0:1], scale=1.0 / 128))(), reads=[rps_m, self.repsc], writes=[rrc])
                    P.op("dve", (lambda rc=rc, qsz=qsz: lambda e: e.reciprocal(out=rc[:, :qsz], in_=rc[:, :qsz]))(), reads=[rrc], writes=[rrc])
                    P.op("dve", (lambda o=o, rc=rc, qsz=qsz: lambda e: e.tensor_tensor(out=o[:, :qsz], in0=o[:, :qsz], in1=rc[:, :qsz], op=ALU.mult))(), reads=[ro, rrc], writes=[ro])
                    ao, rao = aop.get()
                    P.op("act", (lambda ao=ao, o=o, qsz=qsz: lambda e: e.activation(out=ao[:, :qsz], in_=o[:, :qsz], func=AF.Copy, scale=gsub[:, 0:1]))(), reads=[ro, rgsub], writes=[rao])
                    P.dma("sp", (lambda ao=ao, h=h, q0=q0, qsz=qsz: lambda e: e.dma_start(out=self.dr["at"][h, :, q0:q0 + qsz], in_=ao[:, :qsz]))(), reads=[rao], writes=[self.drreg["at"]])

    def stage_lru(self):
        P = self.P
        pt, rpt = self.dr["pt"], self.drreg["pt"]
        SEG = [(0, NCX), (NCX, L)]
        with Stage(self) as S:
            xin, rxin = S.sb("lx", [128, T], BF16)
            ly, rly = S.sb("ly", [128, T], BF16)
            xc, rxc = S.sb("xc", [128, T], F32)
            xcb, rxcb = S.sb("xcb", [128, T], BF16)
            ra, rra = S.sb("ra", [128, T], F32)
            ib, rib = S.sb("ib", [128, T], F32)
            tmp, rtmp = S.sb("ltmp", [128, T], F32)
            hf, rhf = S.sb("hf", [128, T], F32)
            hb, rhb = S.sb("hb", [128, T], F32)
            ob, rob = S.sb("lout", [128, T], BF16)
            wab, rwab = S.sb("wab", [128, 2, 128], BF16)
            wib, rwib = S.sb("wib", [128, 2, 128], BF16)
            cl, rcl = S.sb("cl", [128, 4], F32)
            psp = S.pool("lps", 4, [128, 512], F32, psum=True)

            def rev(t, c0, n):
                a = t[:, c0:c0 + n]
                return bass.AP(a.tensor, a.offset + (n - 1), [[a.ap[0][0], 128], [-1, n]])

            for n in range(8):
                P.dma("sp", (lambda n=n: lambda e: e.dma_start(out=xin[:], in_=pt[C_LX // 128 + n]))(), reads=[rpt], writes=[rxin])
                P.dma("sp", (lambda n=n: lambda e: e.dma_start(out=ly[:], in_=pt[C_LY // 128 + n]))(), reads=[rpt], writes=[rly])
                P.dma("pool", (lambda n=n: lambda e: e.dma_start(out=wab[:], in_=self.dr["lru_wa"][:, n].rearrange("d i o -> i d o")))(), reads=[self.drreg["lru_wa"]], writes=[rwab])
                P.dma("pool", (lambda n=n: lambda e: e.dma_start(out=wib[:], in_=self.dr["lru_wi"][:, n].rearrange("d i o -> i d o")))(), reads=[self.drreg["lru_wi"]], writes=[rwib])
                w = lambda j, n=n: self.vcol("lru_cw", j * 8 + n)
                bcol = self.vcol("lru_cb", n)
                for (s0, ls) in SEG:
                    P.op("dve", (lambda s0=s0, ls=ls, n=n: lambda e: e.tensor_scalar(out=xc[:, s0:s0 + ls], in0=xin[:, s0:s0 + ls], scalar1=self.vcol("lru_cw", 2 * 8 + n), scalar2=self.vcol("lru_cb", n), op0=ALU.mult, op1=ALU.add))(),
                         reads=[rxin, self.rvp], writes=[rxc])
                    for j in (0, 1, 3):
                        d = j - 2
                        lo, hi = max(0, -d), ls - max(0, d)
                        P.op("dve", (lambda s0=s0, lo=lo, hi=hi, d=d, j=j, n=n: lambda e: e.scalar_tensor_tensor(out=xc[:, s0 + lo:s0 + hi], in0=xin[:, s0 + lo + d:s0 + hi + d], scalar=self.vcol("lru_cw", j * 8 + n), in1=xc[:, s0 + lo:s0 + hi], op0=ALU.mult, op1=ALU.add))(),
                             reads=[rxin, rxc, self.rvp], writes=[rxc])
                P.op("pool", lambda e: e.tensor_copy(out=xcb[:], in_=xc[:]), reads=[rxc], writes=[rxcb])
                for d in range(2):
                    hh, rhh = (hf, rhf) if d == 0 else (hb, rhb)
                    lamc = self.vcol("lru_lam", d * 8 + n)
                    P.op("act", (lambda lamc=lamc: lambda e: e.activation(out=cl[:, 0:1], in_=lamc, func=AF.Exp, scale=-1.0))(), reads=[self.rvp], writes=[rcl])
                    P.op("act", lambda e: e.activation(out=cl[:, 1:2], in_=cl[:, 0:1], func=AF.Ln, bias=self.epsc[:, 1:2], scale=1.0), reads=[rcl, self.repsc], writes=[rcl])
                    P.op("dve", lambda e: e.tensor_scalar(out=cl[:, 2:3], in0=cl[:, 1:2], scalar1=-8.0, scalar2=None, op0=ALU.mult), reads=[rcl], writes=[rcl])
                    for (n0, nsz) in TT:
                        ps, rps = psp.get()
                        P.op("pe", (lambda ps=ps, d=d, n0=n0, nsz=nsz: lambda e: e.matmul(ps[:, :nsz], lhsT=wab[:, d, :], rhs=xcb[:, n0:n0 + nsz], start=True, stop=True))(), reads=[rwab, rxcb], writes=[rps])
                        P.op("act", (lambda ps=ps, d=d, n=n, n0=n0, nsz=nsz: lambda e: e.activation(out=ra[:, n0:n0 + nsz], in_=ps[:, :nsz], func=AF.Sigmoid, bias=self.vcol("lru_ba", d * 8 + n), scale=1.0))(), reads=[rps, self.rvp], writes=[rra])
                        ps2, rps2 = psp.get()
                        P.op("pe", (lambda ps2=ps2, d=d, n0=n0, nsz=nsz: lambda e: e.matmul(ps2[:, :nsz], lhsT=wib[:, d, :], rhs=xcb[:, n0:n0 + nsz], start=True, stop=True))(), reads=[rwib, rxcb], writes=[rps2])
                        P.op("act", (lambda ps2=ps2, d=d, n=n, n0=n0, nsz=nsz: lambda e: e.activation(out=ib[:, n0:n0 + nsz], in_=ps2[:, :nsz], func=AF.Sigmoid, bias=self.vcol("lru_bi", d * 8 + n), scale=1.0))(), reads=[rps2, self.rvp], writes=[rib])
                    P.op("act", lambda e: e.activation(out=ra[:], in_=ra[:], func=AF.Exp, scale=cl[:, 2:3]), reads=[rra, rcl], writes=[rra])
                    P.op("pool", lambda e: e.tensor_tensor(out=tmp[:], in0=ra[:], in1=ra[:], op=ALU.mult), reads=[rra], writes=[rtmp])
                    P.op("act", lambda e: e.activation(out=tmp[:], in_=tmp[:], func=AF.Sqrt, bias=self.epsc[:, 1:2], scale=-1.0), reads=[rtmp, self.repsc], writes=[rtmp])
                    P.op("dve", lambda e: e.tensor_tensor(out=ib[:], in0=ib[:], in1=xc[:], op=ALU.mult), reads=[rib, rxc], writes=[rib])
                    P.op("dve", lambda e: e.tensor_tensor(out=ib[:], in0=ib[:], in1=tmp[:], op=ALU.mult), reads=[rib, rtmp], writes=[rib])
                    if d == 0:
                        P.op("dve", lambda e: e.tensor_tensor_scan(out=hf[:, 0:NCX], data0=ra[:, 0:NCX], data1=ib[:, 0:NCX], initial=0.0, op0=ALU.mult, op1=ALU.add), reads=[rra, rib], writes=[rhf])
                        P.op("dve", lambda e: e.tensor_tensor_scan(out=hf[:, NCX:T], data0=ra[:, NCX:T], data1=ib[:, NCX:T], initial=hf[:, NCX - 1:NCX], op0=ALU.mult, op1=ALU.add), reads=[rra, rib, rhf], writes=[rhf])
                    else:
                        P.op("dve", lambda e: e.tensor_tensor_scan(out=rev(hb, 0, NCX), data0=rev(ra, 0, NCX), data1=rev(ib, 0, NCX), initial=0.0, op0=ALU.mult, op1=ALU.add), reads=[rra, rib], writes=[rhb])
                        P.op("dve", lambda e: e.tensor_tensor_scan(out=rev(hb, NCX, L), data0=rev(ra, NCX, L), data1=rev(ib, NCX, L), initial=hb[:, 0:1], op0=ALU.mult, op1=ALU.add), reads=[rra, rib, rhb], writes=[rhb])
                P.op("pool", lambda e: e.tensor_tensor(out=hf[:], in0=hf[:], in1=hb[:], op=ALU.add), reads=[rhf, rhb], writes=[rhf])
                P.op("act", lambda e: e.activation(out=tmp[:], in_=ly[:], func=AF.Square), reads=[rly], writes=[rtmp])
                P.op("dve", lambda e: e.tensor_scalar(out=tmp[:], in0=tmp[:], scalar1=0.044715, scalar2=1.0, op0=ALU.mult, op1=ALU.add), reads=[rtmp], writes=[rtmp])
                P.op("dve", lambda e: e.tensor_tensor(out=tmp[:], in0=tmp[:], in1=ly[:], op=ALU.mult), reads=[rtmp, rly], writes=[rtmp])
                P.op("act", lambda e: e.activation(out=tmp[:], in_=tmp[:], func=AF.Sigmoid, scale=1.5957691216057308), reads=[rtmp], writes=[rtmp])
                P.op("dve", lambda e: e.tensor_tensor(out=tmp[:], in0=tmp[:], in1=ly[:], op=ALU.mult), reads=[rtmp, rly], writes=[rtmp])
                P.op("dve", lambda e: e.tensor_tensor(out=ob[:], in0=tmp[:], in1=hf[:], op=ALU.mult), reads=[rtmp, rhf], writes=[rob])
                P.dma("sp", (lambda n=n: lambda e: e.dma_start(out=self.dr["rt"][n], in_=ob[:]))(), reads=[rob], writes=[self.drreg["rt"]])

    def stage_merge(self):
        P = self.P
        with Stage(self) as S:
            wbr, rwbr = S.sb("wbr", [128, 3, 8, 1024], BF16)
            wo, rwo = S.sb("wo", [128, 8, 1024], BF16)
            for k in range(3):
                P.dma("pool", (lambda k=k: lambda e: e.dma_start(out=wbr[:, k], in_=self.dr["w_br"][k].rearrange("(c p) n -> p c n", p=128)))(), reads=[self.drreg["w_br"]], writes=[rwbr])
            P.dma("pool", lambda e: e.dma_start(out=wo[:], in_=self.dr["w_out"].rearrange("(c p) n -> p c n", p=128)), reads=[self.drreg["w_out"]], writes=[rwo])
            brp = [S.pool("br%d" % k, 1, [128, 8, 512], BF16) for k in range(3)]
            gp = S.pool("mg", 2, [128, 24, 512], BF16)
            xp = S.pool("mx", 2, [128, 8, 512], F32)
            mtp = S.pool("mt", 2, [128, 8, 512], BF16)
            macc = S.pool("macc", 2, [128, 512], F32)
            mtmp = S.pool("mtmp", 2, [128, 512], F32)
            psp = S.pool("mps", 4, [128, 512], F32, psum=True)
            names = ["at", "rt", "yt"]
            for (n0, nsz) in TT:
                w = 1 if n0 < NCX else 0
                brs = []
                for k in range(3):
                    bt, rbt = brp[k].get()
                    P.dma("sp", (lambda bt=bt, k=k, n0=n0, nsz=nsz: lambda e: e.dma_start(out=bt[:, :, :nsz], in_=self.dr[names[k]][:, :, n0:n0 + nsz].rearrange("c p n -> p c n")))(), reads=[self.drreg[names[k]]], writes=[rbt])
                    brs.append((bt, rbt))
                g, rg = gp.get()
                P.dma("sp", (lambda g=g, n0=n0, nsz=nsz: lambda e: e.dma_start(out=g[:, :, :nsz], in_=self.dr["pt"][C_G // 128:C_END // 128, :, n0:n0 + nsz].rearrange("c p n -> p c n")))(), reads=[self.drreg["pt"]], writes=[rg])
                xt_, rxt_ = xp.get()
                P.dma("sp", (lambda xt_=xt_, n0=n0, nsz=nsz: lambda e: e.dma_start(out=xt_[:, :, :nsz], in_=self.dr["xt"][:, :, n0:n0 + nsz]))(), reads=[self.drreg["xt"]], writes=[rxt_])
                sg, rsg = g, rg
                for c in range(24):
                    P.op("act", (lambda sg=sg, g=g, c=c, nsz=nsz: lambda e: e.activation(out=sg[:, c, :nsz], in_=g[:, c, :nsz], func=AF.Sigmoid, bias=self.vcol("b_gate", c), scale=1.0))(), reads=[rg, self.rvp], writes=[rsg])
                mt, rmt = mtp.get()
                for oc in range(8):
                    acc, racc = macc.get()
                    for k in range(3):
                        ps, rps = psp.get()
                        bt, rbt = brs[k]
                        for kc in range(8):
                            P.op("pe", (lambda ps=ps, bt=bt, k=k, kc=kc, oc=oc, nsz=nsz: lambda e: e.matmul(ps[:, :nsz], lhsT=wbr[:, k, kc, oc * 128:(oc + 1) * 128], rhs=bt[:, kc, :nsz], start=(kc == 0), stop=(kc == 7)))(), reads=[rwbr, rbt], writes=[rps])
                        if k == 0:
                            P.op("dve", (lambda acc=acc, ps=ps, sg=sg, oc=oc, nsz=nsz: lambda e: e.tensor_tensor(out=acc[:, :nsz], in0=ps[:, :nsz], in1=sg[:, oc, :nsz], op=ALU.mult))(), reads=[rps, rsg], writes=[racc])
                        else:
                            t_, rt_ = mtmp.get()
                            P.op("dve", (lambda t_=t_, ps=ps, sg=sg, k=k, oc=oc, nsz=nsz: lambda e: e.tensor_tensor(out=t_[:, :nsz], in0=ps[:, :nsz], in1=sg[:, k * 8 + oc, :nsz], op=ALU.mult))(), reads=[rps, rsg], writes=[rt_])
                            if k == 1:
                                P.op("pool", (lambda acc=acc, t_=t_, nsz=nsz: lambda e: e.tensor_tensor(out=acc[:, :nsz], in0=acc[:, :nsz], in1=t_[:, :nsz], op=ALU.add))(), reads=[racc, rt_], writes=[racc])
                            else:
                                P.op("pool", (lambda acc=acc, t_=t_, mt=mt, oc=oc, nsz=nsz: lambda e: e.tensor_tensor(out=mt[:, oc, :nsz], in0=acc[:, :nsz], in1=t_[:, :nsz], op=ALU.add))(), reads=[racc, rt_], writes=[rmt])
                xo, rxo = xt_, rxt_
                for oc in range(8):
                    ps, rps = psp.get()
                    for kc in range(8):
                        P.op("pe", (lambda ps=ps, mt=mt, kc=kc, oc=oc, nsz=nsz: lambda e: e.matmul(ps[:, :nsz], lhsT=wo[:, kc, oc * 128:(oc + 1) * 128], rhs=mt[:, kc, :nsz], start=(kc == 0), stop=(kc == 7)))(), reads=[rwo, rmt], writes=[rps])
                    P.op("dve", (lambda xo=xo, ps=ps, xt_=xt_, oc=oc, nsz=nsz, w=w: lambda e: e.scalar_tensor_tensor(out=xo[:, oc, :nsz], in0=ps[:, :nsz], scalar=self.modv[:, 16 + oc, w:w + 1], in1=xt_[:, oc, :nsz], op0=ALU.mult, op1=ALU.add))(), reads=[rps, rxt_, self.rmodv], writes=[rxo])
                P.dma("sp", (lambda xo=xo, n0=n0, nsz=nsz: lambda e: e.dma_start(out=self.dr["xt_mid"][:, :, n0:n0 + nsz], in_=xo[:, :, :nsz]))(), reads=[rxo], writes=[self.drreg["xt_mid"]])

    def stage_ffn(self):
        P = self.P
        moe = self.moe
        STS = [TT[0:3], TT[3:6], TT[6:9]]
        FG = [(0, 4), (4, 4), (8, 4), (12, 4), (16, 4), (20, 2)]
        if moe:
            self.dram("gate_s", [NE, T], F32, "Internal")
        with Stage(self) as S:
            fT, rfT = S.sb("fT", [128, 8, 1536], BF16)
            yacc, ryacc = S.sb("yacc", [128, 8, 1536], F32)
            wgp = S.pool("wg", 2, [128, 8, 512], BF16)
            wup = S.pool("wu", 2, [128, 8, 512], BF16)
            wdp = S.pool("wd", 2, [128, 4, 1024], BF16)
            if moe:
                wr, rwr = S.sb("wr", [128, 8, NE], F32)
                P.dma("sp", lambda e: e.dma_start(out=wr[:], in_=self.dr["router"].rearrange("(k p) n -> p k n", p=128)), reads=[self.drreg["router"]], writes=[rwr])
                gbc, rgbc = S.sb("gbc", [128, 1536], F32)
                identf = self.vp[:, VP["ident"][0]:VP["ident"][0] + 128]
            for st in STS:
                c0 = st[0][0]
                c1 = st[-1][0] + st[-1][1]
                with Stage(self) as S2:
                    router = None
                    if moe:
                        psr, rpsr = S2.ps("psr", [128, 4, NE], F32)
                        ps_t, rps_t = S2.ps("ps_t", [NE, 512], F32)
                        lg, rlg = S2.sb("lg", [128, 4, NE], F32)
                        l2, rl2 = S2.sb("l2", [128, 4, NE], F32)
                        m1, rm1 = S2.sb("m1", [128, 4], F32)
                        m2, rm2 = S2.sb("m2", [128, 4], F32)
                        gT, rgT = S2.sb("gT", [NE, 512], F32)
                        router = (wr, rwr, psr, rpsr)

                        def router_done(n0, nsz):
                            ns = nsz // 128
                            bc = lambda t: t[:, :ns].unsqueeze(2).to_broadcast([128, ns, NE])
                            P.op("dve", lambda e: e.tensor_copy(out=lg[:, :ns], in_=psr[:, :ns]), reads=[rpsr], writes=[rlg])
                            P.op("dve", lambda e: e.tensor_reduce(out=m1[:, :ns], in_=lg[:, :ns], axis=AX.X, op=ALU.max), reads=[rlg], writes=[rm1])
                            P.op("dve", lambda e: e.tensor_tensor(out=l2[:, :ns], in0=lg[:, :ns], in1=bc(m1), op=ALU.is_equal), reads=[rlg, rm1], writes=[rl2])
                            P.op("dve", lambda e: e.scalar_tensor_tensor(out=l2[:, :ns], in0=l2[:, :ns], scalar=-1e30, in1=lg[:, :ns], op0=ALU.mult, op1=ALU.add), reads=[rl2, rlg], writes=[rl2])
                            P.op("dve", lambda e: e.tensor_reduce(out=m2[:, :ns], in_=l2[:, :ns], axis=AX.X, op=ALU.max), reads=[rl2], writes=[rm2])
                            P.op("dve", lambda e: e.tensor_tensor(out=l2[:, :ns], in0=lg[:, :ns], in1=bc(m2), op=ALU.is_ge), reads=[rlg, rm2], writes=[rl2])
                            P.op("dve", lambda e: e.tensor_tensor(out=lg[:, :ns], in0=lg[:, :ns], in1=bc(m1), op=ALU.subtract), reads=[rlg, rm1], writes=[rlg])
                            P.op("act", lambda e: e.activation(out=lg[:, :ns], in_=lg[:, :ns], func=AF.Exp), reads=[rlg], writes=[rlg])
                            P.op("dve", lambda e: e.tensor_tensor(out=lg[:, :ns], in0=lg[:, :ns], in1=l2[:, :ns], op=ALU.mult), reads=[rlg, rl2], writes=[rlg])
                            P.op("dve", lambda e: e.tensor_reduce(out=m1[:, :ns], in_=lg[:, :ns], axis=AX.X, op=ALU.add), reads=[rlg], writes=[rm1])
                            P.op("dve", lambda e: e.reciprocal(out=m1[:, :ns], in_=m1[:, :ns]), reads=[rm1], writes=[rm1])
                            P.op("dve", lambda e: e.tensor_tensor(out=lg[:, :ns], in0=lg[:, :ns], in1=bc(m1), op=ALU.mult), reads=[rlg, rm1], writes=[rlg])
                            for sub in range(ns):
                                P.op("pe", (lambda sub=sub: lambda e: e.transpose(ps_t[:, sub * 128:(sub + 1) * 128], lg[:, sub, :], identf))(), reads=[rlg, self.rvp], writes=[rps_t])
                            P.op("dve", lambda e: e.tensor_copy(out=gT[:, :nsz], in_=ps_t[:, :nsz]), reads=[rps_t], writes=[rgT])
                            P.dma("sp", lambda e: e.dma_start(out=self.dr["gate_s"][:, n0:n0 + nsz], in_=gT[:, :nsz]), reads=[rgT], writes=[self.drreg["gate_s"]])

                        self._router_done = router_done
                    self.norm_mod(S2, "xt_mid", st, self.gsf, self.rgsf, 3, fT, rfT, c0, router=router)
                with Stage(self) as S3:
                    psg = S3.pool("psg", 2, [128, 512], F32, psum=True)
                    psu = S3.pool("psu", 2, [128, 512], F32, psum=True)
                    psd = S3.pool("psd", 3, [128, 512], F32, psum=True)
                    sgp = S3.pool("fsg", 2, [128, 512], F32)
                    actp = S3.pool("fact", 2, [128, 4, 512], BF16)
                    first = True
                    for ex in range(self.ne):
                        if moe:
                            P.dma("sp", (lambda ex=ex, c0=c0, c1=c1: lambda e: e.dma_start(out=gbc[:, :c1 - c0], in_=bcast_rows(self.dr["gate_s"][ex:ex + 1, c0:c1], c1 - c0)))(), reads=[self.drreg["gate_s"]], writes=[rgbc])
                        for (g0, gn) in FG:
                            wg, rwg = wgp.get()
                            wu, rwu = wup.get()
                            wd, rwd = wdp.get()
                            P.dma("pool", (lambda wg=wg, ex=ex, g0=g0, gn=gn: lambda e: e.dma_start(out=wg[:, :, :gn * 128], in_=self.dr["f_wg"][ex].rearrange("(k p) n -> p k n", p=128)[:, :, g0 * 128:(g0 + gn) * 128]))(), reads=[self.drreg["f_wg"]], writes=[rwg])
                            P.dma("pool", (lambda wu=wu, ex=ex, g0=g0, gn=gn: lambda e: e.dma_start(out=wu[:, :, :gn * 128], in_=self.dr["f_wu"][ex].rearrange("(k p) n -> p k n", p=128)[:, :, g0 * 128:(g0 + gn) * 128]))(), reads=[self.drreg["f_wu"]], writes=[rwu])
                            P.dma("pool", (lambda wd=wd, ex=ex, g0=g0, gn=gn: lambda e: e.dma_start(out=wd[:, :gn, :], in_=self.dr["f_wd"][ex].rearrange("(c p) n -> p c n", p=128)[:, g0:g0 + gn, :]))(), reads=[self.drreg["f_wd"]], writes=[rwd])
                            for (n0, nsz) in st:
                                o0 = n0 - c0
                                act, ract = actp.get()
                                for c in range(gn):
                                    pg, rpg = psg.get()
                                    pu, rpu = psu.get()
                                    for k in range(8):
                                        P.op("pe", (lambda pg=pg, wg=wg, k=k, c=c, o0=o0, nsz=nsz: lambda e: e.matmul(pg[:, :nsz], lhsT=wg[:, k, c * 128:(c + 1) * 128], rhs=fT[:, k, o0:o0 + nsz], start=(k == 0), stop=(k == 7)))(), reads=[rwg, rfT], writes=[rpg])
                                    for k in range(8):
                                        P.op("pe", (lambda pu=pu, wu=wu, k=k, c=c, o0=o0, nsz=nsz: lambda e: e.matmul(pu[:, :nsz], lhsT=wu[:, k, c * 128:(c + 1) * 128], rhs=fT[:, k, o0:o0 + nsz], start=(k == 0), stop=(k == 7)))(), reads=[rwu, rfT], writes=[rpu])
                                    sg, rsg = sgp.get()
                                    P.op("act", (lambda sg=sg, pg=pg, nsz=nsz: lambda e: e.activation(out=sg[:, :nsz], in_=pg[:, :nsz], func=AF.Silu))(), reads=[rpg], writes=[rsg])
                                    if moe:
                                        P.op("pool", (lambda sg=sg, o0=o0, nsz=nsz: lambda e: e.tensor_tensor(out=sg[:, :nsz], in0=sg[:, :nsz], in1=gbc[:, o0:o0 + nsz], op=ALU.mult))(), reads=[rsg, rgbc], writes=[rsg])
                                    P.op("dve", (lambda act=act, sg=sg, pu=pu, c=c, nsz=nsz: lambda e: e.tensor_tensor(out=act[:, c, :nsz], in0=pu[:, :nsz], in1=sg[:, :nsz], op=ALU.mult))(), reads=[rpu, rsg], writes=[ract])
                                for oc in range(8):
                                    pd, rpd = psd.get()
                                    for c in range(gn):
                                        P.op("pe", (lambda pd=pd, wd=wd, act=act, c=c, oc=oc, nsz=nsz, gn=gn: lambda e: e.matmul(pd[:, :nsz], lhsT=wd[:, c, oc * 128:(oc + 1) * 128], rhs=act[:, c, :nsz], start=(c == 0), stop=(c == gn - 1)))(), reads=[rwd, ract], writes=[rpd])
                                    if first:
                                        P.op("act", (lambda pd=pd, oc=oc, o0=o0, nsz=nsz: lambda e: e.copy(out=yacc[:, oc, o0:o0 + nsz], in_=pd[:, :nsz]))(), reads=[rpd], writes=[ryacc])
                                    else:
                                        P.op("dve", (lambda pd=pd, oc=oc, o0=o0, nsz=nsz: lambda e: e.tensor_tensor(out=yacc[:, oc, o0:o0 + nsz], in0=yacc[:, oc, o0:o0 + nsz], in1=pd[:, :nsz], op=ALU.add))(), reads=[rpd, ryacc], writes=[ryacc])
                            first = False
                    xp = S3.pool("fx", 2, [128, 8, 512], F32)
                    for (n0, nsz) in st:
                        o0 = n0 - c0
                        w = 1 if n0 < NCX else 0
                        xt_, rxt_ = xp.get()
                        P.dma("sp", (lambda xt_=xt_, n0=n0, nsz=nsz: lambda e: e.dma_start(out=xt_[:, :, :nsz], in_=self.dr["xt_mid"][:, :, n0:n0 + nsz]))(), reads=[self.drreg["xt_mid"]], writes=[rxt_])
                        for oc in range(8):
                            P.op("dve", (lambda xt_=xt_, oc=oc, o0=o0, nsz=nsz, w=w: lambda e: e.scalar_tensor_tensor(out=xt_[:, oc, :nsz], in0=yacc[:, oc, o0:o0 + nsz], scalar=self.modv[:, 40 + oc, w:w + 1], in1=xt_[:, oc, :nsz], op0=ALU.mult, op1=ALU.add))(), reads=[ryacc, rxt_, self.rmodv], writes=[rxt_])
                        P.dma("sp", (lambda xt_=xt_, n0=n0, nsz=nsz: lambda e: e.dma_start(out=self.dr["xt_out"][:, :, n0:n0 + nsz], in_=xt_[:, :, :nsz]))(), reads=[rxt_], writes=[self.drreg["xt_out"]])

    def stage_final(self):
        P = self.P
        with Stage(self) as S:
            gfin, rgfin = S.sb("gfin", [128, 8, 2], F32)
            zsh, rzsh = S.sb("zsh", [128, 8, 2], F32)
            o = VP["g_final"][0]
            for w in range(2):
                P.op("dve", (lambda w=w: lambda e: e.tensor_copy(out=gfin[:, :, w], in_=self.vp[:, o:o + 8]))(), reads=[self.rvp], writes=[rgfin])
            P.op("pool", lambda e: e.memset(zsh[:], 0.0), writes=[rzsh])
            for (n0, nsz) in TT[1:]:
                with Stage(self) as S2:
                    ot, rot = S2.sb("fin_o", [128, 8, 512], F32)
                    self.norm_mod(S2, "xt_out", [(n0, nsz)], gfin, rgfin, 0, ot, rot, n0, shift=(zsh, rzsh))
                    P.dma("sp", (lambda ot=ot, n0=n0: lambda e: e.dma_start(out=self.dr["y_out"][:, :, n0 - NCX:n0 - NCX + 512], in_=ot[:]))(), reads=[rot], writes=[self.drreg["y_out"]])

    def mod_reduce(self, S, t, rt, scr, rscr, M, shape_ap=None):
        P = self.P
        P.op("dve", lambda e: e.tensor_scalar(out=scr, in0=t, scalar1=1.0 / M, scalar2=MAGIC, op0=ALU.mult, op1=ALU.add), reads=[rt], writes=[rscr])
        P.op("dve", lambda e: e.tensor_scalar(out=scr, in0=scr, scalar1=-MAGIC, scalar2=None, op0=ALU.add), reads=[rscr], writes=[rscr])
        P.op("dve", lambda e: e.scalar_tensor_tensor(out=t, in0=scr, scalar=-float(M), in1=t, op0=ALU.mult, op1=ALU.add), reads=[rscr, rt], writes=[rt])

    def hy_dft_gen(self, Ls):
        P = self.P
        nt = Ls // 128
        N = 2 * Ls
        nm = "L%d" % Ls
        for k in ("CF", "SF", "CI", "SI"):
            self.dram(k + nm, [nt, 128, nt, 128], BF16, "Internal")
        with Stage(self) as S:
            q, rq = S.sb("q", [128, Ls], F32)
            x1, rx1 = S.sb("x1", [128, Ls], F32)
            scr, rscr = S.sb("scr", [128, Ls], F32)
            m, rm = S.sb("m", [128, Ls], F32)
            m2, rm2 = S.sb("m2", [128, Ls], F32)
            pc2, rpc2 = S.sb("pc2", [128, 1], F32)
            outp = S.pool("dfto", 2, [128, Ls], BF16)
            P.op("dve", lambda e: e.tensor_scalar(out=pc2[:], in0=self.vcol("pidx"), scalar1=2.0, scalar2=1.0, op0=ALU.mult, op1=ALU.add), reads=[self.rvp], writes=[rpc2])
            ptr = S.pool("dftptr", 2, [128, 512], BF16, psum=True)
            sI_p = S.pool("dftsI", 2, [128, nt, 128], BF16)
            P.op("pool", lambda e: e.iota(q[:], pattern=[[2, Ls]], base=1, channel_multiplier=0, allow_small_or_imprecise_dtypes=True), writes=[rq])
            M1, mult1, pcol = (2 * N) // 128, 128.0, self.vcol("pidx")
            for a in range(nt):
                P.op("dve", (lambda a=a: lambda e: e.tensor_scalar(out=x1[:], in0=q[:], scalar1=float(a), scalar2=None, op0=ALU.mult))(), reads=[rq], writes=[rx1])
                self.mod_reduce(S, x1[:], rx1, scr[:], rscr, M1)
                P.op("dve", lambda e: e.tensor_scalar(out=x1[:], in0=x1[:], scalar1=mult1, scalar2=None, op0=ALU.mult), reads=[rx1], writes=[rx1])
                P.op("dve", lambda e: e.scalar_tensor_tensor(out=m[:], in0=q[:], scalar=pcol, in1=x1[:], op0=ALU.mult, op1=ALU.add), reads=[rq, rx1, self.rvp], writes=[rm])
                P.op("pool", lambda e: e.tensor_scalar(out=m2[:], in0=m[:], scalar1=float(N // 2), scalar2=None, op0=ALU.add), reads=[rm], writes=[rm2])
                self.mod_reduce(S, m[:], rm, scr[:], rscr, 2 * N)
                self.mod_reduce(S, m2[:], rm2, scr[:], rscr, 2 * N)
                for (src, rsrc, cs) in ((m2, rm2, "C"), (m, rm, "S")):
                    ot, rot = outp.get()
                    P.op("act", (lambda ot=ot, src=src: lambda e: e.activation(out=ot[:], in_=src[:], func=AF.Sin, scale=3.1415925 / N))(), reads=[rsrc], writes=[rot])
                    P.dma("sp", (lambda ot=ot, cs=cs, a=a: lambda e: e.dma_start(out=self.dr[cs + "F" + nm][:, :, a, :].rearrange("c p j -> p c j"), in_=ot[:].rearrange("p (c j) -> p c j", j=128)))(), reads=[rot], writes=[self.drreg[cs + "F" + nm]])
                    sI, rsI = sI_p.get()
                    for c0 in range(0, nt, 4):
                        nb = min(4, nt - c0)
                        pt_, rpt_ = ptr.get()
                        for j in range(nb):
                            P.op("pe", (lambda pt_=pt_, ot=ot, c0=c0, j=j: lambda e: e.transpose(pt_[:, j * 128:(j + 1) * 128], ot[:, (c0 + j) * 128:(c0 + j + 1) * 128], self.ident_bf[:]))(), reads=[rot, self.rident_bf], writes=[rpt_])
                        P.op("act", (lambda pt_=pt_, sI=sI, c0=c0, nb=nb: lambda e: e.activation(out=sI[:, c0:c0 + nb, :], in_=pt_[:, :nb * 128].rearrange("p (a b) -> p a b", b=128), func=AF.Copy, scale=2.0 / N))(), reads=[rpt_], writes=[rsI])
                    P.dma("sp", (lambda sI=sI, cs=cs, a=a: lambda e: e.dma_start(out=self.dr[cs + "I" + nm][a], in_=sI[:]))(), reads=[rsI], writes=[self.drreg[cs + "I" + nm]])

    def hy_prep(self):
        P = self.P
        self.dram("utm", [3, 34, 128, 1024], BF16, "Internal")
        self.dram("z1", [34, 128, 1024], BF16, "Internal")
        self.dram("ytm", [34, 128, 1024], BF16, "Internal")
        SEG = [(0, NCX), (NCX, L)]
        with Stage(self) as S:
            xin_p = S.pool("hx", 2, [128, T], BF16)
            u, ru = S.sb("hu", [128, T], F32)
            ub, rub = S.sb("hub", [128, T], BF16)
            stg_p = S.pool("hstg", 2, [128, 34, 128], BF16)
            ptr = S.pool("hptr", 2, [128, 512], BF16, psum=True)
            for ch in range(24):
                xin, rxin = xin_p.get()
                P.dma("sp", (lambda xin=xin, ch=ch: lambda e: e.dma_start(out=xin[:], in_=self.dr["pt"][C_HY // 128 + ch]))(), reads=[self.drreg["pt"]], writes=[rxin])
                for (s0, ls) in SEG:
                    P.op("dve", (lambda xin=xin, s0=s0, ls=ls, ch=ch: lambda e: e.tensor_scalar(out=u[:, s0:s0 + ls], in0=xin[:, s0:s0 + ls], scalar1=self.vcol("hy_cw", 1 * 24 + ch), scalar2=self.vcol("hy_cb", ch), op0=ALU.mult, op1=ALU.add))(), reads=[rxin, self.rvp], writes=[ru])
                    for j in (0, 2):
                        d = j - 1
                        lo, hi = max(0, -d), ls - max(0, d)
                        eng = "dve"
                        P.op(eng, (lambda xin=xin, s0=s0, lo=lo, hi=hi, d=d, j=j, ch=ch: lambda e: e.scalar_tensor_tensor(out=u[:, s0 + lo:s0 + hi], in0=xin[:, s0 + lo + d:s0 + hi + d], scalar=self.vcol("hy_cw", j * 24 + ch), in1=u[:, s0 + lo:s0 + hi], op0=ALU.mult, op1=ALU.add))(), reads=[rxin, ru, self.rvp], writes=[ru])
                P.op("act", lambda e: e.copy(out=ub[:], in_=u[:]), reads=[ru], writes=[rub])
                stg, rstg = stg_p.get()
                for b0 in range(0, 34, 4):
                    nb = min(4, 34 - b0)
                    pt_, rpt_ = ptr.get()
                    for j in range(nb):
                        P.op("pe", (lambda pt_=pt_, b0=b0, j=j: lambda e: e.transpose(pt_[:, j * 128:(j + 1) * 128], ub[:, (b0 + j) * 128:(b0 + j + 1) * 128], self.ident_bf[:]))(), reads=[rub, self.rident_bf], writes=[rpt_])
                    eng = "act" if (b0 // 4) % 2 == 0 else "dve"
                    if eng == "act":
                        P.op("act", (lambda pt_=pt_, stg=stg, b0=b0, nb=nb: lambda e: e.copy(out=stg[:, b0:b0 + nb, :], in_=pt_[:, :nb * 128].rearrange("p (a b) -> p a b", b=128)))(), reads=[rpt_], writes=[rstg])
                    else:
                        P.op("dve", (lambda pt_=pt_, stg=stg, b0=b0, nb=nb: lambda e: e.tensor_copy(out=stg[:, b0:b0 + nb, :], in_=pt_[:, :nb * 128].rearrange("p (a b) -> p a b", b=128)))(), reads=[rpt_], writes=[rstg])
                wsel, cc = ch // 8, ch % 8
                P.dma("sp", (lambda stg=stg, wsel=wsel, cc=cc: lambda e: e.dma_start(out=self.dr["utm"][wsel][:, :, cc * 128:(cc + 1) * 128].rearrange("b p c -> p b c"), in_=stg[:]))(), reads=[rstg], writes=[self.drreg["utm"]])

    def hy_filters(self, Ls):
        P = self.P
        nt = Ls // 128
        nm = "L%d" % Ls
        self.dram("HS" + nm, [nt, 128, 2048], BF16, "Internal")
        self.dram("HD" + nm, [nt, 128, 2048], BF16, "Internal")
        self.dram("RN" + nm, [128, 2048], F32, "Internal")
        with Stage(self) as S:
            w1, rw1 = S.sb("hw1", [33, 64], F32)
            w2, rw2 = S.sb("hw2", [64, 64], F32)
            w3, rw3 = S.sb("hw3", [64, 4096], F32)
            dec, rdec = S.sb("hdec", [1, 2048], F32)
            P.dma("sp", lambda e: e.dma_start(out=w1[:], in_=self.dr["hy_w1"]), reads=[self.drreg["hy_w1"]], writes=[rw1])
            P.dma("sp", lambda e: e.dma_start(out=w2[:], in_=self.dr["hy_w2"]), reads=[self.drreg["hy_w2"]], writes=[rw2])
            P.dma("sp", lambda e: e.dma_start(out=w3[:], in_=self.dr["hy_w3"]), reads=[self.drreg["hy_w3"]], writes=[rw3])
            P.dma("sp", lambda e: e.dma_start(out=dec[:], in_=self.dr["hy_decay"]), reads=[self.drreg["hy_decay"]], writes=[rdec])
            P.op("act", lambda e: e.activation(out=dec[:], in_=dec[:], func=AF.Abs), reads=[rdec], writes=[rdec])
            nv, rnv = S.sb("hnv", [64, Ls], F32)
            z, rz = S.sb("hz", [64, Ls], F32)
            h1, rh1 = S.sb("hh1", [64, Ls], F32)
            h2, rh2 = S.sb("hh2", [64, Ls], F32)
            tv, rtv = S.sb("htv", [1, Ls], F32)
            P.op("pool", lambda e: e.iota(nv[:], pattern=[[1, Ls]], base=0, channel_multiplier=0, allow_small_or_imprecise_dtypes=True), writes=[rnv])
            P.op("dve", lambda e: e.tensor_scalar(out=h1[0:33, :], in0=nv[0:33, :], scalar1=2 * PI / Ls, scalar2=self.vp[0:33, VP["bands"][0]:VP["bands"][0] + 1], op0=ALU.mult, op1=ALU.mult), reads=[rnv, self.rvp], writes=[rh1])
            P.op("dve", lambda e: e.tensor_scalar(out=h1[0:33, :], in0=h1[0:33, :], scalar1=self.vp[0:33, VP["phase"][0]:VP["phase"][0] + 1], scalar2=None, op0=ALU.add), reads=[rh1, self.rvp], writes=[rh1])
            with Stage(self) as S2:
                self.range_sin(S2, z[0:33, :], rz, h1[0:33, :], rh1, [33, Ls], 0.0, whole=False)
            P.op("dve", lambda e: e.tensor_scalar(out=z[0:1, :], in0=nv[0:1, :], scalar1=1.0 / (Ls - 1), scalar2=None, op0=ALU.mult), reads=[rnv, rz], writes=[rz])
            P.op("dve", lambda e: e.tensor_scalar(out=tv[:], in0=nv[0:1, :], scalar1=1.0 / (Ls - 1), scalar2=None, op0=ALU.mult), reads=[rnv], writes=[rtv])
            psp = S.pool("hfps", 2, [128, 512], F32, psum=True)
            for (wt, rwt, kk, src, rsrc, dst, rdst, bname) in ((w1, rw1, 33, z, rz, h1, rh1, "hy_b1"), (w2, rw2, 64, h1, rh1, h2, rh2, "hy_b2")):
                pre, rpre = S.sb("hpre", [64, Ls], F32)
                for c0 in range(0, Ls, 512):
                    csz = min(512, Ls - c0)
                    ps, rps = psp.get()
                    P.op("pe", (lambda ps=ps, wt=wt, kk=kk, src=src, c0=c0, csz=csz: lambda e: e.matmul(ps[0:64, :csz], lhsT=wt[0:kk, :], rhs=src[0:kk, c0:c0 + csz], start=True, stop=True))(), reads=[rwt, rsrc], writes=[rps])
                    P.op("dve", (lambda ps=ps, pre=pre, c0=c0, bname=bname, csz=csz: lambda e: e.tensor_scalar(out=pre[:, c0:c0 + csz], in0=ps[0:64, :csz], scalar1=self.vp[0:64, VP[bname][0]:VP[bname][0] + 1], scalar2=self.vp[0:64, VP["hy_freq"][0]:VP["hy_freq"][0] + 1], op0=ALU.add, op1=ALU.mult))(), reads=[rps, self.rvp], writes=[rpre])
                with Stage(self) as S2:
                    self.range_sin(S2, dst[:, :], rdst, pre[:, :], rpre, [64, Ls], 0.0, whole=False)
            psn = [S.ps("hpsn", [128, 512], F32) for _ in range(4)]
            psw, rpsw = S.ps("hpsw", [128, 512], F32)
            winp = S.pool("hwin", 2, [128, 512], F32)
            fp_ = S.pool("hf", 4, [128, 512], F32)
            abp = S.pool("hab", 2, [128, 512], BF16)
            hsp = S.pool("hhs", 2, [128, 512], BF16)
            hdp = S.pool("hhd", 2, [128, 512], BF16)
            for a in range(nt):
                for o in range(2):
                    for ct in range(2):
                        P.op("pe", (lambda a=a, o=o, ct=ct: lambda e: e.matmul(psw[:], lhsT=tv[0:1, a * 128:(a + 1) * 128], rhs=dec[0:1, o * 1024 + ct * 512:o * 1024 + ct * 512 + 512], start=True, stop=True))(), reads=[rtv, rdec], writes=[rpsw])
                        win, rwin = winp.get()
                        P.op("act", (lambda win=win: lambda e: e.activation(out=win[:], in_=psw[:], func=AF.Exp, scale=-1.0))(), reads=[rpsw], writes=[rwin])
                        fs = []
                        for d in range(2):
                            ps, rps = psp.get()
                            col = o * 2048 + d * 1024 + ct * 512
                            P.op("pe", (lambda ps=ps, a=a, col=col: lambda e: e.matmul(ps[:], lhsT=h2[:, a * 128:(a + 1) * 128], rhs=w3[:, col:col + 512], start=True, stop=True))(), reads=[rh2, rw3], writes=[rps])
                            f, rf = fp_.get()
                            P.op("dve", (lambda f=f, ps=ps, win=win: lambda e: e.tensor_tensor(out=f[:], in0=ps[:], in1=win[:], op=ALU.mult))(), reads=[rps, rwin], writes=[rf])
                            ab, rab = abp.get()
                            P.op("dve", (lambda ab=ab, f=f: lambda e: e.scalar_tensor_tensor(out=ab[:], in0=f[:], scalar=-1.0, in1=f[:], op0=ALU.mult, op1=ALU.max))(), reads=[rf], writes=[rab])
                            pn, rpn = psn[o * 2 + ct]
                            P.op("pe", (lambda pn=pn, ab=ab, a=a, d=d: lambda e: e.matmul(pn[:], lhsT=self.ones_bf[:], rhs=ab[:], start=(a == 0 and d == 0), stop=(a == nt - 1 and d == 1)))(), reads=[self.rones, rab], writes=[rpn])
                            fs.append((f, rf))
                        (f0, rf0), (f1, rf1) = fs
                        hs, rhs_ = hsp.get()
                        hd, rhd = hdp.get()
                        P.op("pool", (lambda hs=hs, f0=f0, f1=f1: lambda e: e.tensor_tensor(out=hs[:], in0=f0[:], in1=f1[:], op=ALU.add))(), reads=[rf0, rf1], writes=[rhs_])
                        P.op("pool", (lambda hd=hd, f0=f0, f1=f1: lambda e: e.tensor_tensor(out=hd[:], in0=f0[:], in1=f1[:], op=ALU.subtract))(), reads=[rf0, rf1], writes=[rhd])
                        c2 = o * 1024 + ct * 512
                        P.dma("sp", (lambda hs=hs, a=a, c2=c2: lambda e: e.dma_start(out=self.dr["HS" + nm][a, :, c2:c2 + 512], in_=hs[:]))(), reads=[rhs_], writes=[self.drreg["HS" + nm]])
                        P.dma("sp", (lambda hd=hd, a=a, c2=c2: lambda e: e.dma_start(out=self.dr["HD" + nm][a, :, c2:c2 + 512], in_=hd[:]))(), reads=[rhd], writes=[self.drreg["HD" + nm]])
            rn, rrn = S.sb("hrn", [128, 2048], F32)
            for i in range(4):
                pn, rpn = psn[i]
                P.op("dve", (lambda pn=pn, i=i: lambda e: e.tensor_scalar(out=rn[:, i * 512:(i + 1) * 512], in0=pn[:], scalar1=EPS, scalar2=None, op0=ALU.add))(), reads=[rpn], writes=[rrn])
            P.op("dve", lambda e: e.reciprocal(out=rn[:], in_=rn[:]), reads=[rrn], writes=[rrn])
            P.dma("sp", lambda e: e.dma_start(out=self.dr["RN" + nm], in_=rn[:]), reads=[rrn], writes=[self.drreg["RN" + nm]])

    def hy_gemm(self, S, wnames, nm, xs, n_k, evac, nchunks):
        P = self.P
        wps = [S.pool("hgw%d" % i, 2, [128, n_k, 128], BF16) for i in range(len(wnames))]
        pps = [S.pool("hgp%d" % i, 2, [128, 512], F32, psum=True) for i in range(len(set(g for (_, g) in wnames)))]
        def load(oc):
            wts = []
            for i, (wn, g) in enumerate(wnames):
                wt, rwt = wps[i].get()
                P.dma("sp" if i % 2 == 0 else "act", (lambda wt=wt, wn=wn, oc=oc: lambda e: e.dma_start(out=wt[:], in_=self.dr[wn + nm][oc]))(), reads=[self.drreg[wn + nm]], writes=[rwt])
                wts.append((wt, rwt))
            return wts

        nxt = load(0)
        for oc in range(nchunks):
            wts = nxt
            if oc + 1 < nchunks:
                nxt = load(oc + 1)
            outs = {}
            groups = sorted(set(g for (_, g) in wnames))
            for g in groups:
                outs[g] = pps[g].get()
            cntg = {g: 0 for g in groups}
            totg = {g: sum(1 for (_, gg) in wnames if gg == g) * n_k for g in groups}
            for i, (wn, g) in enumerate(wnames):
                wt, rwt = wts[i]
                x, rx = xs[i]
                ps, rps = outs[g]
                for a in range(n_k):
                    first = cntg[g] == 0
                    cntg[g] += 1
                    last = cntg[g] == totg[g]
                    P.op("pe", (lambda ps=ps, wt=wt, x=x, a=a, first=first, last=last: lambda e: e.matmul(ps[:], lhsT=wt[:, a, :], rhs=x[:, a, :], start=first, stop=last))(), reads=[rwt, rx], writes=[rps])
            evac(oc, outs)

    def hy_spectra(self, Ls):
        P = self.P
        nt = Ls // 128
        nm = "L%d" % Ls
        self.dram("GR" + nm, [nt, 128, 2048], BF16, "Internal")
        self.dram("GQ" + nm, [nt, 128, 2048], BF16, "Internal")
        with Stage(self) as S:
            rn, rrn = S.sb("srn", [128, 2048], F32)
            P.dma("sp", lambda e: e.dma_start(out=rn[:], in_=self.dr["RN" + nm]), reads=[self.drreg["RN" + nm]], writes=[rrn])
            hs, rhs_ = S.sb("shs", [128, nt, 512], BF16)
            hd, rhd = S.sb("shd", [128, nt, 512], BF16)
            gop = S.pool("sgo", 4, [128, 512], BF16)
            for ct in range(4):
                P.dma("sp", (lambda ct=ct: lambda e: e.dma_start(out=hs[:], in_=self.dr["HS" + nm][:, :, ct * 512:(ct + 1) * 512].rearrange("a p c -> p a c")))(), reads=[self.drreg["HS" + nm]], writes=[rhs_])
                P.dma("sp", (lambda ct=ct: lambda e: e.dma_start(out=hd[:], in_=self.dr["HD" + nm][:, :, ct * 512:(ct + 1) * 512].rearrange("a p c -> p a c")))(), reads=[self.drreg["HD" + nm]], writes=[rhd])

                def evac(fc, outs, ct=ct):
                    for g, name in ((0, "GR"), (1, "GQ")):
                        ps, rps = outs[g]
                        go, rgo = gop.get()
                        P.op("dve", (lambda go=go, ps=ps: lambda e: e.tensor_tensor(out=go[:], in0=ps[:], in1=rn[:, ct * 512:(ct + 1) * 512], op=ALU.mult))(), reads=[rps, rrn], writes=[rgo])
                        P.dma("sp", (lambda go=go, name=name, fc=fc: lambda e: e.dma_start(out=self.dr[name + nm][fc, :, ct * 512:(ct + 1) * 512], in_=go[:]))(), reads=[rgo], writes=[self.drreg[name + nm]])

                with Stage(self) as S2:
                    self.hy_gemm(S2, [("CF", 0), ("SF", 1)], nm, [(hs, rhs_), (hd, rhd)], nt, evac, nt)

    def hy_conv(self, Ls, blk0):
        P = self.P
        nt = Ls // 128
        nm = "L%d" % Ls
        with Stage(self) as S:
            u, ru = S.sb("cu", [128, nt, 512], BF16)
            Yr, rYr = S.sb("cYr", [128, nt, 512], BF16)
            Yq, rYq = S.sb("cYq", [128, nt, 512], BF16)
            skb, rskb = S.sb("cskb", [128, 512], F32)
            grp = S.pool("cgr", 2, [128, 512], BF16)
            gqp = S.pool("cgq", 2, [128, 512], BF16)
            ap_ = S.pool("cA", 2, [128, 512], F32)
            bp_ = S.pool("cB", 2, [128, 512], F32)
            t1p = S.pool("ct1", 2, [128, 512], F32)
            t2p = S.pool("ct2", 2, [128, 512], F32)
            xgp = S.pool("cxg", 2, [128, 512], BF16)
            zop = S.pool("czo", 2, [128, 512], BF16)
            for o in range(2):
                for ct in range(2):
                    cs = slice(ct * 512, (ct + 1) * 512)
                    src = self.dr["utm"][0] if o == 0 else self.dr["z1"]
                    rsrc = self.drreg["utm"] if o == 0 else self.drreg["z1"]
                    P.dma("sp", (lambda src=src, cs=cs: lambda e: e.dma_start(out=u[:], in_=src[blk0:blk0 + nt, :, cs].rearrange("a p c -> p a c")))(), reads=[rsrc], writes=[ru])
                    sk0 = o * 1024 + ct * 512
                    P.dma("sp", (lambda sk0=sk0: lambda e: e.dma_start(out=skb[:], in_=bcast_rows(self.dr["hy_skip"][0:1, sk0:sk0 + 512], 512)))(), reads=[self.drreg["hy_skip"]], writes=[rskb])

                    def evac_f(fc, outs, o=o, ct=ct):
                        pa, rpa = outs[0]
                        pb, rpb = outs[1]
                        gr, rgr = grp.get()
                        gq, rgq = gqp.get()
                        gc0 = o * 1024 + ct * 512
                        P.dma("sp", (lambda gr=gr, fc=fc, gc0=gc0: lambda e: e.dma_start(out=gr[:], in_=self.dr["GR" + nm][fc, :, gc0:gc0 + 512]))(), reads=[self.drreg["GR" + nm]], writes=[rgr])
                        P.dma("sp", (lambda gq=gq, fc=fc, gc0=gc0: lambda e: e.dma_start(out=gq[:], in_=self.dr["GQ" + nm][fc, :, gc0:gc0 + 512]))(), reads=[self.drreg["GQ" + nm]], writes=[rgq])
                        A, rA = ap_.get()
                        Bq, rBq = bp_.get()
                        P.op("act", (lambda A=A, pa=pa: lambda e: e.copy(out=A[:], in_=pa[:]))(), reads=[rpa], writes=[rA])
                        P.op("act", (lambda Bq=Bq, pb=pb: lambda e: e.copy(out=Bq[:], in_=pb[:]))(), reads=[rpb], writes=[rBq])
                        t1, rt1 = t1p.get()
                        t2, rt2 = t2p.get()
                        P.op("dve", (lambda t1=t1, A=A, gr=gr: lambda e: e.tensor_tensor(out=t1[:], in0=A[:], in1=gr[:], op=ALU.mult))(), reads=[rA, rgr], writes=[rt1])
                        P.op("pool", (lambda t2=t2, Bq=Bq, gq=gq: lambda e: e.tensor_tensor(out=t2[:], in0=Bq[:], in1=gq[:], op=ALU.mult))(), reads=[rBq, rgq], writes=[rt2])
                        P.op("dve", (lambda t1=t1, t2=t2, fc=fc: lambda e: e.tensor_tensor(out=Yr[:, fc, :], in0=t1[:], in1=t2[:], op=ALU.subtract))(), reads=[rt1, rt2], writes=[rYr])
                        t3, rt3 = t1p.get()
                        t4, rt4 = t2p.get()
                        P.op("dve", (lambda t3=t3, A=A, gq=gq: lambda e: e.tensor_tensor(out=t3[:], in0=A[:], in1=gq[:], op=ALU.mult))(), reads=[rA, rgq], writes=[rt3])
                        P.op("pool", (lambda t4=t4, Bq=Bq, gr=gr: lambda e: e.tensor_tensor(out=t4[:], in0=Bq[:], in1=gr[:], op=ALU.mult))(), reads=[rBq, rgr], writes=[rt4])
                        P.op("pool", (lambda t3=t3, t4=t4, fc=fc: lambda e: e.tensor_tensor(out=Yq[:, fc, :], in0=t3[:], in1=t4[:], op=ALU.add))(), reads=[rt3, rt4], writes=[rYq])

                    with Stage(self) as S2:
                        self.hy_gemm(S2, [("CF", 0), ("SF", 1)], nm, [(u, ru), (u, ru)], nt, evac_f, nt)

                    def evac_i(tc, outs, o=o, ct=ct, cs=cs):
                        py, rpy = outs[0]
                        xg, rxg = xgp.get()
                        P.dma("sp", (lambda xg=xg, tc=tc: lambda e: e.dma_start(out=xg[:], in_=self.dr["utm"][1 + o][blk0 + tc, :, cs]))(), reads=[self.drreg["utm"]], writes=[rxg])
                        t1, rt1 = t1p.get()
                        P.op("pool", (lambda t1=t1, tc=tc: lambda e: e.tensor_tensor(out=t1[:], in0=u[:, tc, :], in1=skb[:], op=ALU.mult))(), reads=[ru, rskb], writes=[rt1])
                        P.op("dve", (lambda t1=t1, py=py: lambda e: e.tensor_tensor(out=t1[:], in0=py[:], in1=t1[:], op=ALU.add))(), reads=[rpy, rt1], writes=[rt1])
                        zo, rzo = zop.get()
                        P.op("dve", (lambda zo=zo, t1=t1, xg=xg: lambda e: e.tensor_tensor(out=zo[:], in0=t1[:], in1=xg[:], op=ALU.mult))(), reads=[rt1, rxg], writes=[rzo])
                        dst = "z1" if o == 0 else "ytm"
                        P.dma("sp", (lambda zo=zo, dst=dst, tc=tc: lambda e: e.dma_start(out=self.dr[dst][blk0 + tc, :, cs], in_=zo[:]))(), reads=[rzo], writes=[self.drreg[dst]])

                    with Stage(self) as S2:
                        self.hy_gemm(S2, [("CI", 0), ("SI", 0)], nm, [(Yr, rYr), (Yq, rYq)], nt, evac_i, nt)

    def hy_out(self):
        P = self.P
        with Stage(self) as S:
            yin_p = S.pool("yin", 2, [128, 1024], BF16)
            ptr = S.pool("yptr", 2, [128, 1024], BF16, psum=True)
            stg_p = S.pool("ystg", 2, [128, 8, 128], BF16)
            for blk in range(34):
                yin, ryin = yin_p.get()
                P.dma("sp", (lambda yin=yin, blk=blk: lambda e: e.dma_start(out=yin[:], in_=self.dr["ytm"][blk]))(), reads=[self.drreg["ytm"]], writes=[ryin])
                pt_, rpt_ = ptr.get()
                for c in range(8):
                    P.op("pe", (lambda pt_=pt_, yin=yin, c=c: lambda e: e.transpose(pt_[:, c * 128:(c + 1) * 128], yin[:, c * 128:(c + 1) * 128], self.ident_bf[:]))(), reads=[ryin, self.rident_bf], writes=[rpt_])
                stg, rstg = stg_p.get()
                P.op("act" if blk % 2 == 0 else "dve", (lambda pt_=pt_, stg=stg, blk=blk: (lambda e: e.copy(out=stg[:], in_=pt_[:].rearrange("p (a b) -> p a b", b=128))) if blk % 2 == 0 else (lambda e: e.tensor_copy(out=stg[:], in_=pt_[:].rearrange("p (a b) -> p a b", b=128))))(), reads=[rpt_], writes=[rstg])
                P.dma("sp", (lambda stg=stg, blk=blk: lambda e: e.dma_start(out=self.dr["yt"][:, :, blk * 128:(blk + 1) * 128].rearrange("c p n -> p c n"), in_=stg[:]))(), reads=[rstg], writes=[self.drreg["yt"]])

    def stage_hyena(self):
        self.hy_prep()
        for Ls, blk0 in ((NCX, 0), (L, 2)):
            if Ls not in self.dft_done:
                self.hy_dft_gen(Ls)
                self.dft_done.add(Ls)
            self.hy_filters(Ls)
            self.hy_spectra(Ls)
            self.hy_conv(Ls, blk0)
        self.hy_out()

    def build(self, stages=None):
        on = lambda s_: stages is None or s_ in stages
        for idx in range(len(self.layers)):
            self.set_layer(idx)
            with Stage(self) as S0:
                self.load_consts(S0)
                self.mod_vectors(S0)
                if on("inproj"):
                    self.stage_inproj()
                if on("attn"):
                    with Stage(self) as SA:
                        self.rope_tables(SA)
                        self.stage_attn()
                if on("lru"):
                    self.stage_lru()
                if on("hyena"):
                    self.stage_hyena()
                if on("merge"):
                    self.stage_merge()
                if on("ffn"):
                    self.stage_ffn()
                if idx == len(self.layers) - 1 and on("final"):
                    self.stage_final()
                self.P.flush()
        return self.nc


def make_xt(inp, b):
    tok = np.concatenate([inp["ctx"][b], inp["x"][b]], axis=0)
    return np.ascontiguousarray(tok.T.reshape(KC, 128, T).transpose(1, 0, 2))


def layer_inputs(inp, li, b):
    j = li // 2
    m = {"vp": make_vp(inp, li, b), "w_mod": inp["w_mod"][li], "w_in": inp["w_in"][li],
         "w_br": inp["w_br"][li], "w_out": inp["w_out"][li], "lru_wa": inp["lru_wa"][li], "lru_wi": inp["lru_wi"][li],
         "hy_w1": inp["hy_f_w1"][li], "hy_w2": inp["hy_f_w2"][li], "hy_w3": inp["hy_f_w3"][li],
         "hy_decay": inp["hy_decay"][li].reshape(1, 2048), "hy_skip": inp["hy_skip"][li].reshape(1, 2048),
         "da_lam": inp["da_lambda"][li].reshape(1, 256)}
    if li % 2 == 0:
        m["f_wg"] = inp["ffn_w_gate"][j][None]
        m["f_wu"] = inp["ffn_w_up"][j][None]
        m["f_wd"] = inp["ffn_w_down"][j][None]
    else:
        m["f_wg"] = inp["moe_w_gate"][j]
        m["f_wu"] = inp["moe_w_up"][j]
        m["f_wd"] = inp["moe_w_down"][j]
        m["router"] = inp["moe_router"][j]
    return {k + "_L%d" % li: np.ascontiguousarray(np.asarray(v, np.float32)) for k, v in m.items()}


def kernel(**inputs):
    inp = {k: np.asarray(v) for k, v in inputs.items()}
    nb = inp["x"].shape[0]
    layers = (0, 1, 2, 3)
    bld = Builder(layers)
    nc = bld.build()
    shared = {}
    in_maps = []
    for b in range(nb):
        m = {"x_ext": make_xt(inp, b)}
        for li in layers:
            li_in = layer_inputs(inp, li, b)
            for k, v in li_in.items():
                if k.startswith("vp"):
                    m[k] = v
                else:
                    m[k] = shared.setdefault(k, v)
        in_maps.append(m)
    res = run_bass_kernel_spmd(nc, in_maps, core_ids=list(range(nb)))
    out = np.empty((nb, L, D), np.float32)
    for b in range(nb):
        y = np.asarray(res.results[b]["y_out"], np.float32)
        out[b] = y.transpose(1, 0, 2).reshape(D, L).T
    return out
```

```python
import math
from contextlib import ExitStack
import numpy as np
import concourse.bass as bass
import concourse.mybir as mybir
from concourse.bass_utils import run_bass_kernel_spmd

F32 = mybir.dt.float32
BF16 = mybir.dt.bfloat16
ALU = mybir.AluOpType
AF = mybir.ActivationFunctionType
AX = mybir.AxisListType

D = 1024
L = 4096
NCX = 256
T = L + NCX
KC = 8
DFF = 2816
FC = DFF // 128
NE = 8
C_K, C_V, C_LX, C_Q, C_LY, C_HY, C_G, C_END = 0, 1024, 2048, 3072, 4096, 5120, 8192, 11264
EPS = 1e-6
TT = [(0, 256)] + [(256 + 512 * i, 512) for i in range(8)]
MAGIC = 12582912.0
PI = math.pi

ENGS = ("pe", "act", "dve", "pool", "sp")
SAME_ENGINE_SYNC = True
NDMASEM = 6
NPESEM = 6
KEEP_WARM = False


class Reg:
    __slots__ = ("w", "r", "name", "multi")

    def __init__(self, name="", multi=False):
        self.w = {}
        self.r = {}
        self.name = name
        self.multi = multi


class Prog:
    def __init__(self, nc):
        self.nc = nc
        self.q = {e: [] for e in ENGS}
        self.sems = {}
        self.cnt = {}
        self.key_eng = {}
        self.keys = {}
        for e in ENGS:
            nk = NPESEM if e == "pe" else 1
            self.keys[e] = []
            for i in range(nk):
                k = e if i == 0 else "%s%d" % (e, i)
                self.sems[k] = nc.alloc_semaphore(name="s_" + k)
                self.cnt[k] = 0
                self.key_eng[k] = e
                self.keys[e].append(k)
        self.cur = {e: 0 for e in ENGS}
        self.dma_next = {}
        for e in ("sp", "act", "pool"):
            for i in range(NDMASEM):
                k = "d_%s%d" % (e, i)
                self.sems[k] = nc.alloc_semaphore(name=k)
                self.cnt[k] = 0
            self.dma_next[e] = 0
        self.seen = {e: {} for e in ENGS}
        self.n_ins = 0
        self.n_wait = 0

    def _need(self, eng, tok, waits):
        k, v = tok
        if self.key_eng.get(k) == eng and (eng == "pe" or not SAME_ENGINE_SYNC):
            return
        if self.seen[eng].get(k, 0) >= v:
            return
        waits[k] = max(waits.get(k, 0), v)

    def _deps(self, eng, reads, writes):
        waits = {}
        for r in reads:
            for k, v in r.w.items():
                self._need(eng, (k, v), waits)
        for w in writes:
            if not w.multi:
                for k, v in w.w.items():
                    self._need(eng, (k, v), waits)
            for k, v in w.r.items():
                self._need(eng, (k, v), waits)
        for k, v in waits.items():
            self.seen[eng][k] = v
        return waits

    def _commit(self, tok, reads, writes):
        k, v = tok
        for r in reads:
            r.r[k] = max(r.r.get(k, 0), v)
        for w in writes:
            if w.multi:
                w.w[k] = max(w.w.get(k, 0), v)
            else:
                w.w = {k: v}
            w.r = {}

    def op(self, eng, fn, reads=(), writes=()):
        waits = self._deps(eng, reads, writes)
        key = self.keys[eng][self.cur[eng]]
        self.cnt[key] += 1
        tok = (key, self.cnt[key])
        self.q[eng].append((waits, fn, key, 1))
        self._commit(tok, reads, writes)
        self.n_ins += 1
        self.n_wait += len(waits)
        return tok

    def dma(self, eng, fn, reads=(), writes=()):
        i = self.dma_next[eng]
        self.dma_next[eng] = (i + 1) % NDMASEM
        k = "d_%s%d" % (eng, i)
        waits = self._deps(eng, reads, writes)
        prev = self.cnt[k]
        if prev > 0 and self.seen[eng].get(k, 0) < prev:
            waits[k] = max(waits.get(k, 0), prev)
            self.seen[eng][k] = prev
        self.cnt[k] += 16
        tok = (k, self.cnt[k])
        self.q[eng].append((waits, fn, k, 16))
        self._commit(tok, reads, writes)
        self.n_ins += 1
        self.n_wait += len(waits)
        return tok

    def barrier(self):
        for e in ENGS:
            waits = {}
            for k, v in self.cnt.items():
                if v > 0 and self.seen[e].get(k, 0) < v:
                    waits[k] = v
                    self.seen[e][k] = v
            if waits:
                self.q[e].append((waits, None, None, 0))
        self.cur["pe"] = (self.cur["pe"] + 1) % len(self.keys["pe"])

    def finish(self):
        self.flush()

    def flush(self):
        self.barrier()
        sems = self.sems
        q = self.q
        self.q = {e: [] for e in ENGS}

        def emit(e, lst):
            for waits, fn, k, inc in lst:
                for wk, wv in waits.items():
                    e.wait_ge(sems[wk], wv)
                if fn is not None:
                    fn(e).then_inc(sems[k], inc)

        with self.nc.Block() as block:
            @block.tensor
            def _(e):
                emit(e, q["pe"])

            @block.scalar
            def _(e):
                emit(e, q["act"])

            @block.vector
            def _(e):
                emit(e, q["dve"])

            @block.gpsimd
            def _(e):
                emit(e, q["pool"])

            @block.sync
            def _(e):
                emit(e, q["sp"])


def bcast_rows(a, n):
    return bass.AP(a.tensor, a.offset, [[0, 128], [1, n]])


_uid = [0]


def uname(s):
    _uid[0] += 1
    return "%s_%d" % (s, _uid[0])


class Stage:
    def __init__(self, B):
        self.B = B
        self.es = ExitStack()

    def sb(self, name, shape, dt):
        t = self.es.enter_context(self.B.nc.sbuf_tensor(uname(name), list(shape), dt))
        return t, Reg(name)

    def ps(self, name, shape, dt=F32):
        t = self.es.enter_context(self.B.nc.psum_tensor(uname(name), list(shape), dt))
        return t, Reg(name)

    def pool(self, name, n, shape, dt, psum=False):
        return RPool([(self.ps if psum else self.sb)(name, shape, dt) for _ in range(n)])

    def close(self):
        self.B.P.barrier()
        self.es.close()

    def __enter__(self):
        return self

    def __exit__(self, *a):
        if a[0] is None:
            self.close()
        return False


class RPool:
    def __init__(self, tiles):
        self.tiles = tiles
        self.i = 0

    def get(self):
        t = self.tiles[self.i]
        self.i = (self.i + 1) % len(self.tiles)
        return t


def _cols(v):
    v = np.asarray(v, np.float32).reshape(-1, 128)
    return np.ascontiguousarray(v.T)


VP = {}
_o = 0
for _n, _w in [("b_mod", 48), ("g_mix", 8), ("g_ffn", 8), ("b_gate", 24), ("lru_cw", 32), ("lru_cb", 8),
               ("lru_ba", 16), ("lru_bi", 16), ("lru_lam", 16), ("hy_cw", 72), ("hy_cb", 24), ("subln", 1),
               ("c", 8), ("c_ctx", 8), ("g_final", 8), ("hy_b1", 1), ("hy_b2", 1), ("hy_freq", 1),
               ("bands", 1), ("phase", 1), ("ropeinv", 1), ("pidx", 1), ("ident", 128), ("rmat", 128)]:
    VP[_n] = (_o, _w)
    _o += _w
NV = _o


def make_vp(inp, li, b):
    vp = np.zeros((128, NV), np.float32)

    def put(n, a):
        o, w = VP[n]
        a = np.asarray(a, np.float32)
        assert a.shape[1] == w, (n, a.shape, w)
        vp[:a.shape[0], o:o + w] = a

    put("b_mod", _cols(inp["b_mod"][li]))
    put("g_mix", _cols(inp["g_mix"][li]))
    put("g_ffn", _cols(inp["g_ffn"][li]))
    put("b_gate", _cols(inp["b_gate"][li]))
    put("lru_cw", _cols(inp["lru_conv_w"][li].reshape(-1)))
    put("lru_cb", _cols(inp["lru_conv_b"][li]))
    put("lru_ba", _cols(inp["lru_ba"][li].reshape(-1)))
    put("lru_bi", _cols(inp["lru_bi"][li].reshape(-1)))
    put("lru_lam", _cols(inp["lru_lambda"][li].reshape(-1)))
    put("hy_cw", _cols(inp["hy_conv_w"][li].reshape(-1)))
    put("hy_cb", _cols(inp["hy_conv_b"][li]))
    put("subln", _cols(inp["da_subln_g"][li]))
    put("c", _cols(inp["c"][b]))
    put("c_ctx", _cols(inp["c_ctx"]))
    put("g_final", _cols(inp["g_final"]))
    put("hy_b1", inp["hy_f_b1"][li].reshape(64, 1))
    put("hy_b2", inp["hy_f_b2"][li].reshape(64, 1))
    put("hy_freq", inp["hy_f_freq"][li].reshape(64, 1))
    bands = np.linspace(1e-4, 15.0, 16, dtype=np.float32)
    bcol = np.zeros((33, 1), np.float32)
    bcol[1:17, 0] = bands
    bcol[17:33, 0] = bands
    pcol = np.zeros((33, 1), np.float32)
    pcol[1:17, 0] = np.float32(PI / 2)
    pcol[17:33, 0] = np.float32(PI)
    put("bands", bcol)
    put("phase", pcol)
    inv = (10000.0 ** (-(np.arange(128) % 16).astype(np.float32) / 16.0)).astype(np.float32)
    put("ropeinv", inv.reshape(128, 1))
    put("pidx", np.arange(128, dtype=np.float32).reshape(128, 1))
    put("ident", np.eye(128, dtype=np.float32))
    rm = np.zeros((128, 128), np.float32)
    for m in range(128):
        if m % 32 < 16:
            rm[m + 16, m] = -1.0
        else:
            rm[m - 16, m] = 1.0
    put("rmat", rm)
    return vp


class Builder:
    LAYER_INPUTS = [("vp", [128, NV]), ("w_mod", [D, 6 * D]), ("w_in", [D, C_END]), ("w_br", [3, D, D]), ("w_out", [D, D]),
                    ("lru_wa", [2, 8, 128, 128]), ("lru_wi", [2, 8, 128, 128]), ("hy_w1", [33, 64]), ("hy_w2", [64, 64]),
                    ("hy_w3", [64, 4096]), ("hy_decay", [1, 2048]), ("hy_skip", [1, 2048]), ("da_lam", [1, 256])]

    def __init__(self, layers=(0, 1, 2, 3), debug=False):
        self.layers = list(layers)
        self.debug = debug
        nc = bass.Bass("TRN2", target_bir_lowering=False)
        self.nc = nc
        self.P = Prog(nc)
        self.dr = {}
        self.drreg = {}
        self.dft_done = set()
        I = "ExternalInput"
        self.dram("x_ext", [128, KC, T], F32, I)
        for li in self.layers:
            sfx = "_L%d" % li
            for nm, shp in self.LAYER_INPUTS:
                self.dram(nm + sfx, shp, F32, I)
            ne = NE if li % 2 == 1 else 1
            self.dram("f_wg" + sfx, [ne, D, DFF], F32, I)
            self.dram("f_wu" + sfx, [ne, D, DFF], F32, I)
            self.dram("f_wd" + sfx, [ne, DFF, D], F32, I)
            if li % 2 == 1:
                self.dram("router" + sfx, [D, NE], F32, I)
        dk = "Internal"
        self.dram("pt", [88, 128, T], BF16, dk)
        self.dram("at", [8, 128, T], BF16, dk)
        self.dram("rt", [8, 128, T], BF16, dk)
        self.dram("yt", [8, 128, T], BF16, dk)
        dk2 = "ExternalOutput" if debug else "Internal"
        self.dram("xt_mid", [128, KC, T], F32, dk2)
        self.dram("xr0", [128, KC, T], F32, dk2)
        self.dram("xr1", [128, KC, T], F32, dk2)
        self.dram("y_out", [128, KC, L], F32, "ExternalOutput")

    def set_layer(self, idx):
        li = self.layers[idx]
        self.li = li
        self.moe = (li % 2 == 1)
        self.ne = NE if self.moe else 1
        self.lam_init = 0.8 - 0.6 * math.exp(-0.3 * li)
        sfx = "_L%d" % li
        names = [n for n, _ in self.LAYER_INPUTS] + ["f_wg", "f_wu", "f_wd"] + (["router"] if self.moe else [])
        for n in names:
            self.dr[n] = self.dr[n + sfx]
            self.drreg[n] = self.drreg[n + sfx]
        xin = "x_ext" if idx == 0 else "xr%d" % ((idx - 1) % 2)
        xout = "xr%d" % (idx % 2)
        for alias, real in (("xt", xin), ("xt_out", xout)):
            self.dr[alias] = self.dr[real]
            self.drreg[alias] = self.drreg[real]

    def dram(self, name, shape, dt, kind):
        if name in self.dr:
            return self.dr[name]
        self.dr[name] = self.nc.dram_tensor(name, list(shape), dt, kind=kind).ap()
        self.drreg[name] = Reg(name, multi=True)
        return self.dr[name]

    def load_consts(self, S):
        P, nc = self.P, self.nc
        self.vp, self.rvp = S.sb("vp", [128, NV], F32)
        vp, rvp = self.vp, self.rvp
        P.dma("sp", lambda e: e.dma_start(out=vp[:], in_=self.dr["vp"]), reads=[self.drreg["vp"]], writes=[rvp])
        self.ones_bf, self.rones = S.sb("ones", [128, 128], BF16)
        P.op("pool", lambda e: e.memset(self.ones_bf[:], 1.0), writes=[self.rones])
        self.epsc, self.repsc = S.sb("epsc", [128, 4], F32)
        P.op("pool", lambda e: e.memset(self.epsc[:, 0:1], EPS), writes=[self.repsc])
        P.op("pool", lambda e: e.memset(self.epsc[:, 1:2], 1.0), writes=[self.repsc])
        P.op("pool", lambda e: e.memset(self.epsc[:, 2:3], 0.0), writes=[self.repsc])
        self.ident_bf, self.rident_bf = S.sb("identbf", [128, 128], BF16)
        o = VP["ident"][0]
        P.op("dve", lambda e: e.tensor_copy(out=self.ident_bf[:], in_=vp[:, o:o + 128]), reads=[rvp], writes=[self.rident_bf])
        self.rmat_bf, self.rrmat = S.sb("rmatbf", [128, 128], BF16)
        o2 = VP["rmat"][0]
        P.op("dve", lambda e: e.tensor_copy(out=self.rmat_bf[:], in_=vp[:, o2:o2 + 128]), reads=[rvp], writes=[self.rrmat])

    def vcol(self, name, j=0, w=1):
        o = VP[name][0]
        return self.vp[:, o + j:o + j + w]

    def mod_vectors(self, S):
        P = self.P
        self.modv, self.rmodv = S.sb("modv", [128, 48, 2], F32)
        self.gsm, self.rgsm = S.sb("gsm", [128, 8, 2], F32)
        self.gsf, self.rgsf = S.sb("gsf", [128, 8, 2], F32)
        modv, rmodv = self.modv, self.rmodv
        with Stage(self) as S2:
            cs, rcs = S2.sb("cs", [128, 8, 2], F32)
            oc_, occ = VP["c"][0], VP["c_ctx"][0]
            vp = self.vp
            P.op("act", lambda e: e.activation(out=cs[:, :, 0], in_=vp[:, oc_:oc_ + 8], func=AF.Silu), reads=[self.rvp], writes=[rcs])
            P.op("act", lambda e: e.activation(out=cs[:, :, 1], in_=vp[:, occ:occ + 8], func=AF.Silu), reads=[self.rvp], writes=[rcs])
            wpool = S2.pool("wmod", 2, [128, 8, 768], F32)
            pspool = S2.pool("psmod", 2, [128, 2], F32, psum=True)
            wsrc = self.dr["w_mod"].rearrange("(k p) n -> p k n", p=128)
            ob = VP["b_mod"][0]
            for og in range(8):
                wt, rwt = wpool.get()
                P.dma("sp", (lambda wt=wt, og=og: lambda e: e.dma_start(out=wt[:], in_=wsrc[:, :, og * 768:(og + 1) * 768]))(),
                      reads=[self.drreg["w_mod"]], writes=[rwt])
                for oc in range(6):
                    ps, rps = pspool.get()
                    for k in range(8):
                        P.op("pe", (lambda ps=ps, wt=wt, k=k, oc=oc: lambda e: e.matmul(ps[:], lhsT=wt[:, k, oc * 128:(oc + 1) * 128], rhs=cs[:, k, :], start=(k == 0), stop=(k == 7)))(),
                             reads=[rwt, rcs], writes=[rps])
                    j = og * 6 + oc
                    P.op("dve", (lambda ps=ps, j=j: lambda e: e.tensor_scalar(out=modv[:, j, :], in0=ps[:], scalar1=vp[:, ob + j:ob + j + 1], scalar2=None, op0=ALU.add))(),
                         reads=[rps, self.rvp], writes=[rmodv])
            for (gs, rgs, gname, j0) in ((self.gsm, self.rgsm, "g_mix", 8), (self.gsf, self.rgsf, "g_ffn", 32)):
                og_ = VP[gname][0]
                for w in range(2):
                    P.op("dve", (lambda gs=gs, j0=j0, w=w: lambda e: e.tensor_scalar(out=gs[:, :, w], in0=modv[:, j0:j0 + 8, w], scalar1=1.0, scalar2=None, op0=ALU.add))(),
                         reads=[rmodv], writes=[rgs])
                    P.op("dve", (lambda gs=gs, og_=og_, w=w: lambda e: e.tensor_tensor(out=gs[:, :, w], in0=gs[:, :, w], in1=vp[:, og_:og_ + 8], op=ALU.mult))(),
                         reads=[rgs, self.rvp], writes=[rgs])

    def norm_mod(self, S, src, tiles, gs, rgs, shift_j, out, rout, col0, shift=None, router=None):
        P = self.P
        xin_p = S.pool("nm_x", 1, [128, 8, 512], F32)
        sq_p = S.pool("nm_sq", 1, [128, 8, 512], BF16)
        rs_p = S.pool("nm_rs", 2, [128, 512], F32)
        tmp_p = S.pool("nm_t", 2, [128, 512], F32)
        ps_p = S.pool("nm_ps", 2, [128, 512], F32, psum=True)
        modv, rmodv = (self.modv, self.rmodv) if shift is None else shift
        if router is not None:
            f32_all = S.sb("nm_f32", [128, 8, 512], F32)
            f32, rf32 = f32_all
        for (n0, nsz) in tiles:
            w = 1 if n0 < NCX else 0
            xin, rxin = xin_p.get()
            P.dma("sp", (lambda xin=xin, n0=n0, nsz=nsz: lambda e: e.dma_start(out=xin[:, :, :nsz], in_=self.dr[src][:, :, n0:n0 + nsz]))(),
                  reads=[self.drreg[src]], writes=[rxin])
            sq, rsq = sq_p.get()
            P.op("act", (lambda sq=sq, xin=xin, nsz=nsz: lambda e: e.activation(out=sq[:, :, :nsz], in_=xin[:, :, :nsz], func=AF.Square))(),
                 reads=[rxin], writes=[rsq])
            ps, rps = ps_p.get()
            for k in range(8):
                P.op("pe", (lambda ps=ps, sq=sq, k=k, nsz=nsz: lambda e: e.matmul(ps[:, :nsz], lhsT=self.ones_bf[:], rhs=sq[:, k, :nsz], start=(k == 0), stop=(k == 7)))(),
                     reads=[rsq, self.rones], writes=[rps])
            rs, rrs = rs_p.get()
            P.op("act", (lambda rs=rs, ps=ps, nsz=nsz: lambda e: e.activation(out=rs[:, :nsz], in_=ps[:, :nsz], func=AF.Sqrt, bias=self.epsc[:, 0:1], scale=1.0 / D))(),
                 reads=[rps, self.repsc], writes=[rrs])
            P.op("dve", (lambda rs=rs, nsz=nsz: lambda e: e.reciprocal(out=rs[:, :nsz], in_=rs[:, :nsz]))(), reads=[rrs], writes=[rrs])
            for k in range(8):
                tmp, rtmp = tmp_p.get()
                P.op("dve", (lambda tmp=tmp, xin=xin, rs=rs, k=k, nsz=nsz: lambda e: e.tensor_tensor(out=tmp[:, :nsz], in0=xin[:, k, :nsz], in1=rs[:, :nsz], op=ALU.mult))(),
                     reads=[rxin, rrs], writes=[rtmp])
                P.op("act", (lambda tmp=tmp, k=k, nsz=nsz, n0=n0, w=w: lambda e: e.activation(out=out[:, k, n0 - col0:n0 - col0 + nsz], in_=tmp[:, :nsz], func=AF.Identity,
                                                                                         bias=modv[:, shift_j * 8 + k, w:w + 1], scale=gs[:, k, w:w + 1]))(),
                     reads=[rtmp, rmodv, rgs], writes=[rout])
                if router is not None:
                    f32, rf32 = f32_all
                    P.op("act", (lambda tmp=tmp, k=k, nsz=nsz, w=w: lambda e: e.activation(out=f32[:, k, :nsz], in_=tmp[:, :nsz], func=AF.Identity,
                                                                                bias=modv[:, shift_j * 8 + k, w:w + 1], scale=gs[:, k, w:w + 1]))(),
                         reads=[rtmp, rmodv, rgs], writes=[rf32])
            if router is not None:
                wr, rwr, psr, rpsr = router
                for sub in range(nsz // 128):
                    for k in range(8):
                        P.op("pe", (lambda sub=sub, k=k: lambda e: e.matmul(psr[:, sub, :], lhsT=f32[:, k, sub * 128:(sub + 1) * 128], rhs=wr[:, k, :], start=(k == 0), stop=(k == 7)))(),
                             reads=[rf32, rwr], writes=[rpsr])
            if router is not None:
                router_done = getattr(self, "_router_done")
                router_done(n0, nsz)

    def gemm(self, S, wsrc, rw, n_k, m_total, xT, rxT, tiles, col0, evac, mg=512, skip_chunks=()):
        P = self.P
        wp = S.pool("g_w", 2, [128, n_k, mg], BF16)
        psp = S.pool("g_ps", 4, [128, 512], F32, psum=True)
        for g0 in range(0, m_total, mg):
            gsz = min(mg, m_total - g0)
            chunks = [c for c in range(g0 // 128, (g0 + gsz) // 128) if c not in skip_chunks]
            if not chunks:
                continue
            wt, rwt = wp.get()
            P.dma("pool", (lambda wt=wt, g0=g0, gsz=gsz: lambda e: e.dma_start(out=wt[:, :, :gsz], in_=wsrc[:, :, g0:g0 + gsz]))(),
                  reads=[rw], writes=[rwt])
            for ti, (n0, nsz) in enumerate(tiles):
                for mc in chunks:
                    ps, rps = psp.get()
                    mo = mc * 128 - g0
                    for k in range(n_k):
                        P.op("pe", (lambda ps=ps, wt=wt, k=k, mo=mo, n0=n0, nsz=nsz: lambda e: e.matmul(ps[:, :nsz], lhsT=wt[:, k, mo:mo + 128], rhs=xT[:, k, n0 - col0:n0 - col0 + nsz], start=(k == 0), stop=(k == n_k - 1)))(),
                             reads=[rwt, rxT], writes=[rps])
                    evac(mc, ti, n0, nsz, ps, rps)

    def stage_inproj(self):
        P = self.P
        with Stage(self) as S:
            hT, rhT = S.sb("hT", [128, 8, T], BF16)
            with Stage(self) as S2:
                self.norm_mod(S2, "xt", TT, self.gsm, self.rgsm, 0, hT, rhT, 0)
            op_ = S.pool("ip_o", 4, [128, 512], BF16)
            cnt = [0]

            def evac(mc, ti, n0, nsz, ps, rps):
                ot, rot = op_.get()
                if cnt[0] % 2 == 0:
                    P.op("act", lambda e: e.copy(out=ot[:, :nsz], in_=ps[:, :nsz]), reads=[rps], writes=[rot])
                else:
                    P.op("dve", lambda e: e.tensor_copy(out=ot[:, :nsz], in_=ps[:, :nsz]), reads=[rps], writes=[rot])
                cnt[0] += 1
                P.dma("sp", lambda e: e.dma_start(out=self.dr["pt"][mc, :, n0:n0 + nsz], in_=ot[:, :nsz]), reads=[rot], writes=[self.drreg["pt"]])

            wsrc = self.dr["w_in"].rearrange("(k p) n -> p k n", p=128)
            self.gemm(S, wsrc, self.drreg["w_in"], 8, C_END, hT, rhT, TT, 0, evac)

    def range_sin(self, S, out, rout, src, rsrc, shape, shift=0.0, whole=True):
        P = self.P
        t1, rt1 = S.sb("rs_t1", shape, F32)
        t2, rt2 = S.sb("rs_t2", shape, F32)
        inv2pi = 1.0 / (2 * PI)
        if shift != 0.0:
            t0, rt0 = S.sb("rs_t0", shape, F32)
            P.op("dve", (lambda s0=src: lambda e: e.tensor_scalar(out=t0[:], in0=s0[:], scalar1=shift, scalar2=None, op0=ALU.add))(), reads=[rsrc], writes=[rt0])
            src, rsrc = t0, rt0
        P.op("dve", lambda e: e.tensor_scalar(out=t1[:], in0=src[:], scalar1=inv2pi, scalar2=MAGIC, op0=ALU.mult, op1=ALU.add), reads=[rsrc], writes=[rt1])
        P.op("dve", lambda e: e.tensor_scalar(out=t1[:], in0=t1[:], scalar1=-MAGIC, scalar2=None, op0=ALU.add), reads=[rt1], writes=[rt1])
        P.op("dve", lambda e: e.scalar_tensor_tensor(out=t2[:], in0=t1[:], scalar=-2 * PI, in1=src[:], op0=ALU.mult, op1=ALU.add), reads=[rt1, rsrc], writes=[rt2])
        P.op("dve", lambda e: e.tensor_scalar(out=t2[:], in0=t2[:], scalar1=PI, scalar2=-PI, op0=ALU.min, op1=ALU.max), reads=[rt2], writes=[rt2])
        P.op("act", lambda e: e.activation(out=out[:], in_=t2[:], func=AF.Sin), reads=[rt2], writes=[rout])

    def rope_tables(self, S):
        P = self.P
        self.cosT, self.rcosT = S.sb("cosT", [128, L], F32)
        self.sinT, self.rsinT = S.sb("sinT", [128, L], F32)
        with Stage(self) as S2:
            pos, rpos = S2.sb("pos", [128, L], F32)
            for p0 in (0, 32, 64, 96):
                pat = [[1, 64], [0, 64]] if (p0 % 64) == 0 else [[0, 64], [1, 64]]
                P.op("pool", (lambda p0=p0, pat=pat: lambda e: e.iota(pos[p0:p0 + 32, :].rearrange("p (a b) -> p a b", a=64), pattern=pat, base=0, channel_multiplier=0, allow_small_or_imprecise_dtypes=True))(), writes=[rpos])
            P.op("dve", lambda e: e.tensor_scalar(out=pos[:], in0=pos[:], scalar1=self.vcol("ropeinv"), scalar2=None, op0=ALU.mult), reads=[rpos, self.rvp], writes=[rpos])
            self.range_sin(S2, self.sinT, self.rsinT, pos, rpos, [128, L], 0.0)
            self.range_sin(S2, self.cosT, self.rcosT, pos, rpos, [128, L], PI / 2)

    def stage_attn(self):
        P = self.P
        pt, rpt = self.dr["pt"], self.drreg["pt"]
        with Stage(self) as S:
            neglam, rneglam = S.sb("neglam", [128, 1], F32)
            gsub, rgsub = S.sb("gsub", [128, 1], F32)
            lamrow, rlamrow = S.sb("lamrow", [1, 256], F32)
            lamw, rlamw = S.sb("lamw", [1, 8], F32)
            onesf, ronesf = S.sb("onesf", [1, 128], F32)
            ps_m, rps_m = S.ps("ps_m", [128, 512], F32)
            P.op("pool", lambda e: e.memset(onesf[:], 1.0), writes=[ronesf])
            P.dma("sp", lambda e: e.dma_start(out=lamrow[:], in_=self.dr["da_lam"]), reads=[self.drreg["da_lam"]], writes=[rlamrow])
            P.op("dve", lambda e: e.tensor_tensor(out=lamrow[:, 0:64], in0=lamrow[:, 0:64], in1=lamrow[:, 64:128], op=ALU.mult), reads=[rlamrow], writes=[rlamrow])
            P.op("dve", lambda e: e.tensor_tensor(out=lamrow[:, 128:192], in0=lamrow[:, 128:192], in1=lamrow[:, 192:256], op=ALU.mult), reads=[rlamrow], writes=[rlamrow])
            P.op("dve", lambda e: e.reduce_sum(out=lamw[:, 0:1], in_=lamrow[:, 0:64], axis=AX.X), reads=[rlamrow], writes=[rlamw])
            P.op("dve", lambda e: e.reduce_sum(out=lamw[:, 1:2], in_=lamrow[:, 128:192], axis=AX.X), reads=[rlamrow], writes=[rlamw])
            P.op("act", lambda e: e.activation(out=lamw[:, 2:4], in_=lamw[:, 0:2], func=AF.Exp), reads=[rlamw], writes=[rlamw])
            P.op("dve", lambda e: e.tensor_tensor(out=lamw[:, 4:5], in0=lamw[:, 3:4], in1=lamw[:, 2:3], op=ALU.subtract), reads=[rlamw], writes=[rlamw])
            P.op("dve", lambda e: e.tensor_scalar(out=lamw[:, 5:6], in0=lamw[:, 4:5], scalar1=-self.lam_init, scalar2=None, op0=ALU.add), reads=[rlamw], writes=[rlamw])
            P.op("pe", lambda e: e.matmul(ps_m[:, 0:1], lhsT=onesf[:], rhs=lamw[:, 5:6], start=True, stop=True), reads=[ronesf, rlamw], writes=[rps_m])
            P.op("dve", lambda e: e.tensor_copy(out=neglam[:], in_=ps_m[:, 0:1]), reads=[rps_m], writes=[rneglam])
            P.op("dve", lambda e: e.tensor_scalar(out=gsub[:], in0=self.vcol("subln"), scalar1=1.0 - self.lam_init, scalar2=None, op0=ALU.mult), reads=[self.rvp], writes=[rgsub])

            KT, rKT = S.sb("KT", [128, T], BF16)
            QT, rQT = S.sb("QT", [128, T], BF16)
            VT, rVT = S.sb("VT", [128, T], BF16)
            KR, rKR = S.sb("KR", [128, T], BF16)
            QR, rQR = S.sb("QR", [128, T], BF16)
            Vh, rVh = S.sb("Vh", [128, 34, 128], BF16)
            Qc = [S.sb("Qc%d" % c, [128, T], BF16) for c in range(2)]
            for c in range(2):
                P.op("pool", (lambda c=c: lambda e: e.memset(Qc[c][0][:], 0.0))(), writes=[Qc[c][1]])
            ps_tr, rps_tr = S.ps("ps_tr", [128, 512], BF16)
            ps_s = S.pool("ps_s", 4, [128, 512], F32, psum=True)
            ps_dum, rps_dum = None, None
            ps_o = S.pool("ps_o", 1, [128, 512], F32, psum=True)
            ps_r = S.pool("ps_r", 1, [128, 512], F32, psum=True)
            ptp = S.pool("ptile", 6, [128, 512], BF16)
            t1p = S.pool("at1", 2, [128, 512], F32)
            t2p = S.pool("at2", 2, [128, 512], F32)
            onp = S.pool("on", 4, [128, 512], F32)
            rcp = S.pool("rc", 2, [128, 512], F32)
            o_p = S.pool("o", 2, [128, 512], F32)
            sqp = S.pool("asq", 2, [128, 512], BF16)
            aop = S.pool("aout", 2, [128, 512], BF16)
            for h in range(8):
                P.dma("sp", (lambda h=h: lambda e: e.dma_start(out=KT[:], in_=pt[C_K // 128 + h]))(), reads=[rpt], writes=[rKT])
                P.dma("sp", (lambda h=h: lambda e: e.dma_start(out=QT[:], in_=pt[C_Q // 128 + h]))(), reads=[rpt], writes=[rQT])
                P.dma("sp", (lambda h=h: lambda e: e.dma_start(out=VT[:], in_=pt[C_V // 128 + h]))(), reads=[rpt], writes=[rVT])
                P.op("pool", lambda e: e.tensor_copy(out=KR[:, 0:NCX], in_=KT[:, 0:NCX]), reads=[rKT], writes=[rKR])
                P.op("pool", lambda e: e.tensor_copy(out=QR[:, 0:NCX], in_=QT[:, 0:NCX]), reads=[rQT], writes=[rQR])
                for (src, rsrc, dst, rdst) in ((KT, rKT, KR, rKR), (QT, rQT, QR, rQR)):
                    for (n0, nsz) in TT[1:]:
                        P.op("pe", (lambda src=src, n0=n0: lambda e: e.matmul(ps_m[:], lhsT=self.rmat_bf[:], rhs=src[:, n0:n0 + 512], start=True, stop=True))(), reads=[self.rrmat, rsrc], writes=[rps_m])
                        t1, rt1 = t1p.get()
                        t2, rt2 = t2p.get()
                        P.op("dve", (lambda t1=t1, src=src, n0=n0: lambda e: e.tensor_tensor(out=t1[:], in0=src[:, n0:n0 + 512], in1=self.cosT[:, n0 - NCX:n0 - NCX + 512], op=ALU.mult))(), reads=[rsrc, self.rcosT], writes=[rt1])
                        P.op("dve", (lambda t2=t2, n0=n0: lambda e: e.tensor_tensor(out=t2[:], in0=ps_m[:], in1=self.sinT[:, n0 - NCX:n0 - NCX + 512], op=ALU.mult))(), reads=[rps_m, self.rsinT], writes=[rt2])
                        P.op("dve", (lambda t1=t1, t2=t2, dst=dst, n0=n0: lambda e: e.tensor_tensor(out=dst[:, n0:n0 + 512], in0=t1[:], in1=t2[:], op=ALU.add))(), reads=[rt1, rt2], writes=[rdst])
                for c in range(2):
                    P.op("pool", (lambda c=c: lambda e: e.tensor_copy(out=Qc[c][0][c * 64:(c + 1) * 64, :], in_=QR[c * 64:(c + 1) * 64, :]))(), reads=[rQR], writes=[Qc[c][1]])
                for b0 in range(0, 34, 4):
                    nb = min(4, 34 - b0)
                    for j in range(nb):
                        P.op("pe", (lambda b0=b0, j=j: lambda e: e.transpose(ps_tr[:, j * 128:(j + 1) * 128], VT[:, (b0 + j) * 128:(b0 + j + 1) * 128], self.ident_bf[:]))(), reads=[rVT, self.rident_bf], writes=[rps_tr])
                    P.op("act", (lambda b0=b0, nb=nb: lambda e: e.copy(out=Vh[:, b0:b0 + nb, :], in_=ps_tr[:, :nb * 128].rearrange("p (a b) -> p a b", b=128)))(), reads=[rps_tr], writes=[rVh])
                for (q0, qsz) in TT:
                    kbs = range(0, 2) if q0 < NCX else range(0, 34)
                    ons = []
                    for c in range(2):
                        pso, rpso = ps_o.get()
                        psr, rpsr = ps_r.get()
                        c0 = c * 64
                        nk = len(kbs)
                        pend = []

                        def emit_pv(item, pso=pso, rpso=rpso, psr=psr, rpsr=rpsr, nk=nk, qsz=qsz):
                            ik, kb, ptl, rptl = item
                            P.op("pe", (lambda: lambda e: e.matmul(pso[:, :qsz], lhsT=Vh[:, kb, :], rhs=ptl[:, :qsz], start=(ik == 0), stop=(ik == nk - 1)))(), reads=[rVh, rptl], writes=[rpso])
                            P.op("pe", (lambda: lambda e: e.matmul(psr[:, :qsz], lhsT=self.ones_bf[:], rhs=ptl[:, :qsz], start=(ik == 0), stop=(ik == nk - 1)))(), reads=[self.rones, rptl], writes=[rpsr])
                            if KEEP_WARM:
                                P.op("pe", (lambda: lambda e: e.matmul(ps_dum[:, :qsz], lhsT=self.ones_bf[:], rhs=ptl[:, :qsz], start=True, stop=True))(), reads=[self.rones, rptl], writes=[rps_dum])

                        for ik, kb in enumerate(kbs):
                            pss, rpss = ps_s.get()
                            P.op("pe", (lambda pss=pss, kb=kb, c=c, q0=q0, qsz=qsz: lambda e: e.matmul(pss[:, :qsz], lhsT=KR[:, kb * 128:(kb + 1) * 128], rhs=Qc[c][0][:, q0:q0 + qsz], start=True, stop=True))(), reads=[rKR, Qc[c][1]], writes=[rpss])
                            ptl, rptl = ptp.get()
                            P.op("act", (lambda ptl=ptl, pss=pss, qsz=qsz: lambda e: e.activation(out=ptl[:, :qsz], in_=pss[:, :qsz], func=AF.Exp, scale=0.125))(), reads=[rpss], writes=[rptl])
                            pend.append((ik, kb, ptl, rptl))
                            if len(pend) > 3:
                                emit_pv(pend.pop(0))
                        while pend:
                            emit_pv(pend.pop(0))
                        rc, rrc = rcp.get()
                        P.op("dve", (lambda rc=rc, psr=psr, qsz=qsz: lambda e: e.reciprocal(out=rc[:, :qsz], in_=psr[:, :qsz]))(), reads=[rpsr], writes=[rrc])
                        on, ron = onp.get()
                        P.op("dve", (lambda on=on, pso=pso, rc=rc, qsz=qsz: lambda e: e.tensor_tensor(out=on[:, :qsz], in0=pso[:, :qsz], in1=rc[:, :qsz], op=ALU.mult))(), reads=[rpso, rrc], writes=[ron])
                        ons.append((on, ron))
                    (on0, ron0), (on1, ron1) = ons
                    o, ro = o_p.get()
                    P.op("dve", (lambda o=o, on0=on0, on1=on1, qsz=qsz: lambda e: e.scalar_tensor_tensor(out=o[:, :qsz], in0=on1[:, :qsz], scalar=neglam[:, 0:1], in1=on0[:, :qsz], op0=ALU.mult, op1=ALU.add))(), reads=[ron0, ron1, rneglam], writes=[ro])
                    sq, rsq = sqp.get()
                    P.op("act", (lambda sq=sq, o=o, qsz=qsz: lambda e: e.activation(out=sq[:, :qsz], in_=o[:, :qsz], func=AF.Square))(), reads=[ro], writes=[rsq])
                    P.op("pe", (lambda sq=sq, qsz=qsz: lambda e: e.matmul(ps_m[:, :qsz], lhsT=self.ones_bf[:], rhs=sq[:, :qsz], start=True, stop=True))(), reads=[self.rones, rsq], writes=[rps_m])
                    rc, rrc = rcp.get()
                    P.op("act", (lambda rc=rc, qsz=qsz: lambda e: e.activation(out=rc[:, :qsz], in_=ps_m[:, :qsz], func=AF.Sqrt, bias=self.epsc[:, 0:1], scale=1.0 / 128))(), reads=[rps_m, self.repsc], writes=[rrc])
                    P.op("dve", (lambda rc=rc, qsz=qsz: lambda e: e.reciprocal(out=rc[:, :qsz], in_=rc[:, :qsz]))(), reads=[rrc], writes=[rrc])
                    P.op("dve", (lambda o=o, rc=rc, qsz=qsz: lambda e: e.tensor_tensor(out=o[:, :qsz], in0=o[:, :qsz], in1=rc[:, :qsz], op=ALU.mult))(), reads=[ro, rrc], writes=[ro])
                    ao, rao = aop.get()
                    P.op("act", (lambda ao=ao, o=o, qsz=qsz: lambda e: e.activation(out=ao[:, :qsz], in_=o[:, :qsz], func=AF.Copy, scale=gsub[:, 0:1]))(), reads=[ro, rgsub], writes=[rao])
                    P.dma("sp", (lambda ao=ao, h=h, q0=q0, qsz=qsz: lambda e: e.dma_start(out=self.dr["at"][h, :, q0:q0 + qsz], in_=ao[:, :qsz]))(), reads=[rao], writes=[self.drreg["at"]])

    def stage_lru(self):
        P = self.P
        pt, rpt = self.dr["pt"], self.drreg["pt"]
        SEG = [(0, NCX), (NCX, L)]
        with Stage(self) as S:
            xin, rxin = S.sb("lx", [128, T], BF16)
            ly, rly = S.sb("ly", [128, T], BF16)
            xc, rxc = S.sb("xc", [128, T], F32)
            xcb, rxcb = S.sb("xcb", [128, T], BF16)
            ra, rra = S.sb("ra", [128, T], F32)
            ib, rib = S.sb("ib", [128, T], F32)
            tmp, rtmp = S.sb("ltmp", [128, T], F32)
            hf, rhf = S.sb("hf", [128, T], F32)
            hb, rhb = S.sb("hb", [128, T], F32)
            ob, rob = S.sb("lout", [128, T], BF16)
            wab, rwab = S.sb("wab", [128, 2, 128], BF16)
            wib, rwib = S.sb("wib", [128, 2, 128], BF16)
            cl, rcl = S.sb("cl", [128, 4], F32)
            psp = S.pool("lps", 4, [128, 512], F32, psum=True)

            def rev(t, c0, n):
                a = t[:, c0:c0 + n]
                return bass.AP(a.tensor, a.offset + (n - 1), [[a.ap[0][0], 128], [-1, n]])

            for n in range(8):
                P.dma("sp", (lambda n=n: lambda e: e.dma_start(out=xin[:], in_=pt[C_LX // 128 + n]))(), reads=[rpt], writes=[rxin])
                P.dma("sp", (lambda n=n: lambda e: e.dma_start(out=ly[:], in_=pt[C_LY // 128 + n]))(), reads=[rpt], writes=[rly])
                P.dma("pool", (lambda n=n: lambda e: e.dma_start(out=wab[:], in_=self.dr["lru_wa"][:, n].rearrange("d i o -> i d o")))(), reads=[self.drreg["lru_wa"]], writes=[rwab])
                P.dma("pool", (lambda n=n: lambda e: e.dma_start(out=wib[:], in_=self.dr["lru_wi"][:, n].rearrange("d i o -> i d o")))(), reads=[self.drreg["lru_wi"]], writes=[rwib])
                w = lambda j, n=n: self.vcol("lru_cw", j * 8 + n)
                bcol = self.vcol("lru_cb", n)
                for (s0, ls) in SEG:
                    P.op("dve", (lambda s0=s0, ls=ls, n=n: lambda e: e.tensor_scalar(out=xc[:, s0:s0 + ls], in0=xin[:, s0:s0 + ls], scalar1=self.vcol("lru_cw", 2 * 8 + n), scalar2=self.vcol("lru_cb", n), op0=ALU.mult, op1=ALU.add))(),
                         reads=[rxin, self.rvp], writes=[rxc])
                    for j in (0, 1, 3):
                        d = j - 2
                        lo, hi = max(0, -d), ls - max(0, d)
                        P.op("dve", (lambda s0=s0, lo=lo, hi=hi, d=d, j=j, n=n: lambda e: e.scalar_tensor_tensor(out=xc[:, s0 + lo:s0 + hi], in0=xin[:, s0 + lo + d:s0 + hi + d], scalar=self.vcol("lru_cw", j * 8 + n), in1=xc[:, s0 + lo:s0 + hi], op0=ALU.mult, op1=ALU.add))(),
                             reads=[rxin, rxc, self.rvp], writes=[rxc])
                P.op("pool", lambda e: e.tensor_copy(out=xcb[:], in_=xc[:]), reads=[rxc], writes=[rxcb])
                for d in range(2):
                    hh, rhh = (hf, rhf) if d == 0 else (hb, rhb)
                    lamc = self.vcol("lru_lam", d * 8 + n)
                    P.op("act", (lambda lamc=lamc: lambda e: e.activation(out=cl[:, 0:1], in_=lamc, func=AF.Exp, scale=-1.0))(), reads=[self.rvp], writes=[rcl])
                    P.op("act", lambda e: e.activation(out=cl[:, 1:2], in_=cl[:, 0:1], func=AF.Ln, bias=self.epsc[:, 1:2], scale=1.0), reads=[rcl, self.repsc], writes=[rcl])
                    P.op("dve", lambda e: e.tensor_scalar(out=cl[:, 2:3], in0=cl[:, 1:2], scalar1=-8.0, scalar2=None, op0=ALU.mult), reads=[rcl], writes=[rcl])
                    for (n0, nsz) in TT:
                        ps, rps = psp.get()
                        P.op("pe", (lambda ps=ps, d=d, n0=n0, nsz=nsz: lambda e: e.matmul(ps[:, :nsz], lhsT=wab[:, d, :], rhs=xcb[:, n0:n0 + nsz], start=True, stop=True))(), reads=[rwab, rxcb], writes=[rps])
                        P.op("act", (lambda ps=ps, d=d, n=n, n0=n0, nsz=nsz: lambda e: e.activation(out=ra[:, n0:n0 + nsz], in_=ps[:, :nsz], func=AF.Sigmoid, bias=self.vcol("lru_ba", d * 8 + n), scale=1.0))(), reads=[rps, self.rvp], writes=[rra])
                        ps2, rps2 = psp.get()
                        P.op("pe", (lambda ps2=ps2, d=d, n0=n0, nsz=nsz: lambda e: e.matmul(ps2[:, :nsz], lhsT=wib[:, d, :], rhs=xcb[:, n0:n0 + nsz], start=True, stop=True))(), reads=[rwib, rxcb], writes=[rps2])
                        P.op("act", (lambda ps2=ps2, d=d, n=n, n0=n0, nsz=nsz: lambda e: e.activation(out=ib[:, n0:n0 + nsz], in_=ps2[:, :nsz], func=AF.Sigmoid, bias=self.vcol("lru_bi", d * 8 + n), scale=1.0))(), reads=[rps2, self.rvp], writes=[rib])
                    P.op("act", lambda e: e.activation(out=ra[:], in_=ra[:], func=AF.Exp, scale=cl[:, 2:3]), reads=[rra, rcl], writes=[rra])
                    P.op("pool", lambda e: e.tensor_tensor(out=tmp[:], in0=ra[:], in1=ra[:], op=ALU.mult), reads=[rra], writes=[rtmp])
                    P.op("act", lambda e: e.activation(out=tmp[:], in_=tmp[:], func=AF.Sqrt, bias=self.epsc[:, 1:2], scale=-1.0), reads=[rtmp, self.repsc], writes=[rtmp])
                    P.op("dve", lambda e: e.tensor_tensor(out=ib[:], in0=ib[:], in1=xc[:], op=ALU.mult), reads=[rib, rxc], writes=[rib])
                    P.op("dve", lambda e: e.tensor_tensor(out=ib[:], in0=ib[:], in1=tmp[:], op=ALU.mult), reads=[rib, rtmp], writes=[rib])
                    if d == 0:
                        P.op("dve", lambda e: e.tensor_tensor_scan(out=hf[:, 0:NCX], data0=ra[:, 0:NCX], data1=ib[:, 0:NCX], initial=0.0, op0=ALU.mult, op1=ALU.add), reads=[rra, rib], writes=[rhf])
                        P.op("dve", lambda e: e.tensor_tensor_scan(out=hf[:, NCX:T], data0=ra[:, NCX:T], data1=ib[:, NCX:T], initial=hf[:, NCX - 1:NCX], op0=ALU.mult, op1=ALU.add), reads=[rra, rib, rhf], writes=[rhf])
                    else:
                        P.op("dve", lambda e: e.tensor_tensor_scan(out=rev(hb, 0, NCX), data0=rev(ra, 0, NCX), data1=rev(ib, 0, NCX), initial=0.0, op0=ALU.mult, op1=ALU.add), reads=[rra, rib], writes=[rhb])
                        P.op("dve", lambda e: e.tensor_tensor_scan(out=rev(hb, NCX, L), data0=rev(ra, NCX, L), data1=rev(ib, NCX, L), initial=hb[:, 0:1], op0=ALU.mult, op1=ALU.add), reads=[rra, rib, rhb], writes=[rhb])
                P.op("pool", lambda e: e.tensor_tensor(out=hf[:], in0=hf[:], in1=hb[:], op=ALU.add), reads=[rhf, rhb], writes=[rhf])
                P.op("act", lambda e: e.activation(out=tmp[:], in_=ly[:], func=AF.Square), reads=[rly], writes=[rtmp])
                P.op("dve", lambda e: e.tensor_scalar(out=tmp[:], in0=tmp[:], scalar1=0.044715, scalar2=1.0, op0=ALU.mult, op1=ALU.add), reads=[rtmp], writes=[rtmp])
                P.op("dve", lambda e: e.tensor_tensor(out=tmp[:], in0=tmp[:], in1=ly[:], op=ALU.mult), reads=[rtmp, rly], writes=[rtmp])
                P.op("act", lambda e: e.activation(out=tmp[:], in_=tmp[:], func=AF.Sigmoid, scale=1.5957691216057308), reads=[rtmp], writes=[rtmp])
                P.op("dve", lambda e: e.tensor_tensor(out=tmp[:], in0=tmp[:], in1=ly[:], op=ALU.mult), reads=[rtmp, rly], writes=[rtmp])
                P.op("dve", lambda e: e.tensor_tensor(out=ob[:], in0=tmp[:], in1=hf[:], op=ALU.mult), reads=[rtmp, rhf], writes=[rob])
                P.dma("sp", (lambda n=n: lambda e: e.dma_start(out=self.dr["rt"][n], in_=ob[:]))(), reads=[rob], writes=[self.drreg["rt"]])

    def stage_merge(self):
        P = self.P
        with Stage(self) as S:
            wbr, rwbr = S.sb("wbr", [128, 3, 8, 1024], BF16)
            wo, rwo = S.sb("wo", [128, 8, 1024], BF16)
            for k in range(3):
                P.dma("pool", (lambda k=k: lambda e: e.dma_start(out=wbr[:, k], in_=self.dr["w_br"][k].rearrange("(c p) n -> p c n", p=128)))(), reads=[self.drreg["w_br"]], writes=[rwbr])
            P.dma("pool", lambda e: e.dma_start(out=wo[:], in_=self.dr["w_out"].rearrange("(c p) n -> p c n", p=128)), reads=[self.drreg["w_out"]], writes=[rwo])
            brp = [S.pool("br%d" % k, 1, [128, 8, 512], BF16) for k in range(3)]
            gp = S.pool("mg", 2, [128, 24, 512], BF16)
            xp = S.pool("mx", 2, [128, 8, 512], F32)
            mtp = S.pool("mt", 2, [128, 8, 512], BF16)
            macc = S.pool("macc", 2, [128, 512], F32)
            mtmp = S.pool("mtmp", 2, [128, 512], F32)
            psp = S.pool("mps", 4, [128, 512], F32, psum=True)
            names = ["at", "rt", "yt"]
            for (n0, nsz) in TT:
                w = 1 if n0 < NCX else 0
                brs = []
                for k in range(3):
                    bt, rbt = brp[k].get()
                    P.dma("sp", (lambda bt=bt, k=k, n0=n0, nsz=nsz: lambda e: e.dma_start(out=bt[:, :, :nsz], in_=self.dr[names[k]][:, :, n0:n0 + nsz].rearrange("c p n -> p c n")))(), reads=[self.drreg[names[k]]], writes=[rbt])
                    brs.append((bt, rbt))
                g, rg = gp.get()
                P.dma("sp", (lambda g=g, n0=n0, nsz=nsz: lambda e: e.dma_start(out=g[:, :, :nsz], in_=self.dr["pt"][C_G // 128:C_END // 128, :, n0:n0 + nsz].rearrange("c p n -> p c n")))(), reads=[self.drreg["pt"]], writes=[rg])
                xt_, rxt_ = xp.get()
                P.dma("sp", (lambda xt_=xt_, n0=n0, nsz=nsz: lambda e: e.dma_start(out=xt_[:, :, :nsz], in_=self.dr["xt"][:, :, n0:n0 + nsz]))(), reads=[self.drreg["xt"]], writes=[rxt_])
                sg, rsg = g, rg
                for c in range(24):
                    P.op("act", (lambda sg=sg, g=g, c=c, nsz=nsz: lambda e: e.activation(out=sg[:, c, :nsz], in_=g[:, c, :nsz], func=AF.Sigmoid, bias=self.vcol("b_gate", c), scale=1.0))(), reads=[rg, self.rvp], writes=[rsg])
                mt, rmt = mtp.get()
                for oc in range(8):
                    acc, racc = macc.get()
                    for k in range(3):
                        ps, rps = psp.get()
                        bt, rbt = brs[k]
                        for kc in range(8):
                            P.op("pe", (lambda ps=ps, bt=bt, k=k, kc=kc, oc=oc, nsz=nsz: lambda e: e.matmul(ps[:, :nsz], lhsT=wbr[:, k, kc, oc * 128:(oc + 1) * 128], rhs=bt[:, kc, :nsz], start=(kc == 0), stop=(kc == 7)))(), reads=[rwbr, rbt], writes=[rps])
                        if k == 0:
                            P.op("dve", (lambda acc=acc, ps=ps, sg=sg, oc=oc, nsz=nsz: lambda e: e.tensor_tensor(out=acc[:, :nsz], in0=ps[:, :nsz], in1=sg[:, oc, :nsz], op=ALU.mult))(), reads=[rps, rsg], writes=[racc])
                        else:
                            t_, rt_ = mtmp.get()
                            P.op("dve", (lambda t_=t_, ps=ps, sg=sg, k=k, oc=oc, nsz=nsz: lambda e: e.tensor_tensor(out=t_[:, :nsz], in0=ps[:, :nsz], in1=sg[:, k * 8 + oc, :nsz], op=ALU.mult))(), reads=[rps, rsg], writes=[rt_])
                            if k == 1:
                                P.op("pool", (lambda acc=acc, t_=t_, nsz=nsz: lambda e: e.tensor_tensor(out=acc[:, :nsz], in0=acc[:, :nsz], in1=t_[:, :nsz], op=ALU.add))(), reads=[racc, rt_], writes=[racc])
                            else:
                                P.op("pool", (lambda acc=acc, t_=t_, mt=mt, oc=oc, nsz=nsz: lambda e: e.tensor_tensor(out=mt[:, oc, :nsz], in0=acc[:, :nsz], in1=t_[:, :nsz], op=ALU.add))(), reads=[racc, rt_], writes=[rmt])
                xo, rxo = xt_, rxt_
                for oc in range(8):
                    ps, rps = psp.get()
                    for kc in range(8):
                        P.op("pe", (lambda ps=ps, mt=mt, kc=kc, oc=oc, nsz=nsz: lambda e: e.matmul(ps[:, :nsz], lhsT=wo[:, kc, oc * 128:(oc + 1) * 128], rhs=mt[:, kc, :nsz], start=(kc == 0), stop=(kc == 7)))(), reads=[rwo, rmt], writes=[rps])
                    P.op("dve", (lambda xo=xo, ps=ps, xt_=xt_, oc=oc, nsz=nsz, w=w: lambda e: e.scalar_tensor_tensor(out=xo[:, oc, :nsz], in0=ps[:, :nsz], scalar=self.modv[:, 16 + oc, w:w + 1], in1=xt_[:, oc, :nsz], op0=ALU.mult, op1=ALU.add))(), reads=[rps, rxt_, self.rmodv], writes=[rxo])
                P.dma("sp", (lambda xo=xo, n0=n0, nsz=nsz: lambda e: e.dma_start(out=self.dr["xt_mid"][:, :, n0:n0 + nsz], in_=xo[:, :, :nsz]))(), reads=[rxo], writes=[self.drreg["xt_mid"]])

    def stage_ffn(self):
        P = self.P
        moe = self.moe
        STS = [TT[0:3], TT[3:6], TT[6:9]]
        FG = [(0, 4), (4, 4), (8, 4), (12, 4), (16, 4), (20, 2)]
        if moe:
            self.dram("gate_s", [NE, T], F32, "Internal")
        with Stage(self) as S:
            fT, rfT = S.sb("fT", [128, 8, 1536], BF16)
            yacc, ryacc = S.sb("yacc", [128, 8, 1536], F32)
            wgp = S.pool("wg", 2, [128, 8, 512], BF16)
            wup = S.pool("wu", 2, [128, 8, 512], BF16)
            wdp = S.pool("wd", 2, [128, 4, 1024], BF16)
            if moe:
                wr, rwr = S.sb("wr", [128, 8, NE], F32)
                P.dma("sp", lambda e: e.dma_start(out=wr[:], in_=self.dr["router"].rearrange("(k p) n -> p k n", p=128)), reads=[self.drreg["router"]], writes=[rwr])
                gbc, rgbc = S.sb("gbc", [128, 1536], F32)
                identf = self.vp[:, VP["ident"][0]:VP["ident"][0] + 128]
            for st in STS:
                c0 = st[0][0]
                c1 = st[-1][0] + st[-1][1]
                with Stage(self) as S2:
                    router = None
                    if moe:
                        psr, rpsr = S2.ps("psr", [128, 4, NE], F32)
                        ps_t, rps_t = S2.ps("ps_t", [NE, 512], F32)
                        lg, rlg = S2.sb("lg", [128, 4, NE], F32)
                        l2, rl2 = S2.sb("l2", [128, 4, NE], F32)
                        m1, rm1 = S2.sb("m1", [128, 4], F32)
                        m2, rm2 = S2.sb("m2", [128, 4], F32)
                        gT, rgT = S2.sb("gT", [NE, 512], F32)
                        router = (wr, rwr, psr, rpsr)

                        def router_done(n0, nsz):
                            ns = nsz // 128
                            bc = lambda t: t[:, :ns].unsqueeze(2).to_broadcast([128, ns, NE])
                            P.op("dve", lambda e: e.tensor_copy(out=lg[:, :ns], in_=psr[:, :ns]), reads=[rpsr], writes=[rlg])
                            P.op("dve", lambda e: e.tensor_reduce(out=m1[:, :ns], in_=lg[:, :ns], axis=AX.X, op=ALU.max), reads=[rlg], writes=[rm1])
                            P.op("dve", lambda e: e.tensor_tensor(out=l2[:, :ns], in0=lg[:, :ns], in1=bc(m1), op=ALU.is_equal), reads=[rlg, rm1], writes=[rl2])
                            P.op("dve", lambda e: e.scalar_tensor_tensor(out=l2[:, :ns], in0=l2[:, :ns], scalar=-1e30, in1=lg[:, :ns], op0=ALU.mult, op1=ALU.add), reads=[rl2, rlg], writes=[rl2])
                            P.op("dve", lambda e: e.tensor_reduce(out=m2[:, :ns], in_=l2[:, :ns], axis=AX.X, op=ALU.max), reads=[rl2], writes=[rm2])
                            P.op("dve", lambda e: e.tensor_tensor(out=l2[:, :ns], in0=lg[:, :ns], in1=bc(m2), op=ALU.is_ge), reads=[rlg, rm2], writes=[rl2])
                            P.op("dve", lambda e: e.tensor_tensor(out=lg[:, :ns], in0=lg[:, :ns], in1=bc(m1), op=ALU.subtract), reads=[rlg, rm1], writes=[rlg])
                            P.op("act", lambda e: e.activation(out=lg[:, :ns], in_=lg[:, :ns], func=AF.Exp), reads=[rlg], writes=[rlg])
                            P.op("dve", lambda e: e.tensor_tensor(out=lg[:, :ns], in0=lg[:, :ns], in1=l2[:, :ns], op=ALU.mult), reads=[rlg, rl2], writes=[rlg])
                            P.op("dve", lambda e: e.tensor_reduce(out=m1[:, :ns], in_=lg[:, :ns], axis=AX.X, op=ALU.add), reads=[rlg], writes=[rm1])
                            P.op("dve", lambda e: e.reciprocal(out=m1[:, :ns], in_=m1[:, :ns]), reads=[rm1], writes=[rm1])
                            P.op("dve", lambda e: e.tensor_tensor(out=lg[:, :ns], in0=lg[:, :ns], in1=bc(m1), op=ALU.mult), reads=[rlg, rm1], writes=[rlg])
                            for sub in range(ns):
                                P.op("pe", (lambda sub=sub: lambda e: e.transpose(ps_t[:, sub * 128:(sub + 1) * 128], lg[:, sub, :], identf))(), reads=[rlg, self.rvp], writes=[rps_t])
                            P.op("dve", lambda e: e.tensor_copy(out=gT[:, :nsz], in_=ps_t[:, :nsz]), reads=[rps_t], writes=[rgT])
                            P.dma("sp", lambda e: e.dma_start(out=self.dr["gate_s"][:, n0:n0 + nsz], in_=gT[:, :nsz]), reads=[rgT], writes=[self.drreg["gate_s"]])

                        self._router_done = router_done
                    self.norm_mod(S2, "xt_mid", st, self.gsf, self.rgsf, 3, fT, rfT, c0, router=router)
                with Stage(self) as S3:
                    psg = S3.pool("psg", 2, [128, 512], F32, psum=True)
                    psu = S3.pool("psu", 2, [128, 512], F32, psum=True)
                    psd = S3.pool("psd", 3, [128, 512], F32, psum=True)
                    sgp = S3.pool("fsg", 2, [128, 512], F32)
                    actp = S3.pool("fact", 2, [128, 4, 512], BF16)
                    first = True
                    pend_down = [None]
                    for ex in range(self.ne):
                        if moe:
                            P.dma("sp", (lambda ex=ex, c0=c0, c1=c1: lambda e: e.dma_start(out=gbc[:, :c1 - c0], in_=bcast_rows(self.dr["gate_s"][ex:ex + 1, c0:c1], c1 - c0)))(), reads=[self.drreg["gate_s"]], writes=[rgbc])
                        for (g0, gn) in FG:
                            wg, rwg = wgp.get()
                            wu, rwu = wup.get()
                            wd, rwd = wdp.get()
                            P.dma("pool", (lambda wg=wg, ex=ex, g0=g0, gn=gn: lambda e: e.dma_start(out=wg[:, :, :gn * 128], in_=self.dr["f_wg"][ex].rearrange("(k p) n -> p k n", p=128)[:, :, g0 * 128:(g0 + gn) * 128]))(), reads=[self.drreg["f_wg"]], writes=[rwg])
                            P.dma("pool", (lambda wu=wu, ex=ex, g0=g0, gn=gn: lambda e: e.dma_start(out=wu[:, :, :gn * 128], in_=self.dr["f_wu"][ex].rearrange("(k p) n -> p k n", p=128)[:, :, g0 * 128:(g0 + gn) * 128]))(), reads=[self.drreg["f_wu"]], writes=[rwu])
                            P.dma("pool", (lambda wd=wd, ex=ex, g0=g0, gn=gn: lambda e: e.dma_start(out=wd[:, :gn, :], in_=self.dr["f_wd"][ex].rearrange("(c p) n -> p c n", p=128)[:, g0:g0 + gn, :]))(), reads=[self.drreg["f_wd"]], writes=[rwd])
                            for (n0, nsz) in st:
                                o0 = n0 - c0
                                act, ract = actp.get()
                                for c in range(gn):
                                    pg, rpg = psg.get()
                                    pu, rpu = psu.get()
                                    for k in range(8):
                                        P.op("pe", (lambda pg=pg, wg=wg, k=k, c=c, o0=o0, nsz=nsz: lambda e: e.matmul(pg[:, :nsz], lhsT=wg[:, k, c * 128:(c + 1) * 128], rhs=fT[:, k, o0:o0 + nsz], start=(k == 0), stop=(k == 7)))(), reads=[rwg, rfT], writes=[rpg])
                                    for k in range(8):
                                        P.op("pe", (lambda pu=pu, wu=wu, k=k, c=c, o0=o0, nsz=nsz: lambda e: e.matmul(pu[:, :nsz], lhsT=wu[:, k, c * 128:(c + 1) * 128], rhs=fT[:, k, o0:o0 + nsz], start=(k == 0), stop=(k == 7)))(), reads=[rwu, rfT], writes=[rpu])
                                    sg, rsg = sgp.get()
                                    P.op("act", (lambda sg=sg, pg=pg, nsz=nsz: lambda e: e.activation(out=sg[:, :nsz], in_=pg[:, :nsz], func=AF.Silu))(), reads=[rpg], writes=[rsg])
                                    if moe:
                                        P.op("pool", (lambda sg=sg, o0=o0, nsz=nsz: lambda e: e.tensor_tensor(out=sg[:, :nsz], in0=sg[:, :nsz], in1=gbc[:, o0:o0 + nsz], op=ALU.mult))(), reads=[rsg, rgbc], writes=[rsg])
                                    P.op("dve", (lambda act=act, sg=sg, pu=pu, c=c, nsz=nsz: lambda e: e.tensor_tensor(out=act[:, c, :nsz], in0=pu[:, :nsz], in1=sg[:, :nsz], op=ALU.mult))(), reads=[rpu, rsg], writes=[ract])
                                def emit_down(act=act, ract=ract, wd=wd, rwd=rwd, gn=gn, o0=o0, nsz=nsz, first=first):
                                    for oc in range(8):
                                        pd, rpd = psd.get()
                                        for c in range(gn):
                                            P.op("pe", (lambda pd=pd, c=c, oc=oc: lambda e: e.matmul(pd[:, :nsz], lhsT=wd[:, c, oc * 128:(oc + 1) * 128], rhs=act[:, c, :nsz], start=(c == 0), stop=(c == gn - 1)))(), reads=[rwd, ract], writes=[rpd])
                                        if first:
                                            P.op("act", (lambda pd=pd, oc=oc: lambda e: e.copy(out=yacc[:, oc, o0:o0 + nsz], in_=pd[:, :nsz]))(), reads=[rpd], writes=[ryacc])
                                        else:
                                            P.op("dve", (lambda pd=pd, oc=oc: lambda e: e.tensor_tensor(out=yacc[:, oc, o0:o0 + nsz], in0=yacc[:, oc, o0:o0 + nsz], in1=pd[:, :nsz], op=ALU.add))(), reads=[rpd, ryacc], writes=[ryacc])

                                if pend_down[0] is not None:
                                    pend_down[0]()
                                pend_down[0] = emit_down
                            first = False
                    if pend_down[0] is not None:
                        pend_down[0]()
                        pend_down[0] = None
                    xp = S3.pool("fx", 2, [128, 8, 512], F32)
                    for (n0, nsz) in st:
                        o0 = n0 - c0
                        w = 1 if n0 < NCX else 0
                        xt_, rxt_ = xp.get()
                        P.dma("sp", (lambda xt_=xt_, n0=n0, nsz=nsz: lambda e: e.dma_start(out=xt_[:, :, :nsz], in_=self.dr["xt_mid"][:, :, n0:n0 + nsz]))(), reads=[self.drreg["xt_mid"]], writes=[rxt_])
                        for oc in range(8):
                            P.op("dve", (lambda xt_=xt_, oc=oc, o0=o0, nsz=nsz, w=w: lambda e: e.scalar_tensor_tensor(out=xt_[:, oc, :nsz], in0=yacc[:, oc, o0:o0 + nsz], scalar=self.modv[:, 40 + oc, w:w + 1], in1=xt_[:, oc, :nsz], op0=ALU.mult, op1=ALU.add))(), reads=[ryacc, rxt_, self.rmodv], writes=[rxt_])
                        P.dma("sp", (lambda xt_=xt_, n0=n0, nsz=nsz: lambda e: e.dma_start(out=self.dr["xt_out"][:, :, n0:n0 + nsz], in_=xt_[:, :, :nsz]))(), reads=[rxt_], writes=[self.drreg["xt_out"]])

    def stage_final(self):
        P = self.P
        with Stage(self) as S:
            gfin, rgfin = S.sb("gfin", [128, 8, 2], F32)
            zsh, rzsh = S.sb("zsh", [128, 8, 2], F32)
            o = VP["g_final"][0]
            for w in range(2):
                P.op("dve", (lambda w=w: lambda e: e.tensor_copy(out=gfin[:, :, w], in_=self.vp[:, o:o + 8]))(), reads=[self.rvp], writes=[rgfin])
            P.op("pool", lambda e: e.memset(zsh[:], 0.0), writes=[rzsh])
            for (n0, nsz) in TT[1:]:
                with Stage(self) as S2:
                    ot, rot = S2.sb("fin_o", [128, 8, 512], F32)
                    self.norm_mod(S2, "xt_out", [(n0, nsz)], gfin, rgfin, 0, ot, rot, n0, shift=(zsh, rzsh))
                    P.dma("sp", (lambda ot=ot, n0=n0: lambda e: e.dma_start(out=self.dr["y_out"][:, :, n0 - NCX:n0 - NCX + 512], in_=ot[:]))(), reads=[rot], writes=[self.drreg["y_out"]])

    def mod_reduce(self, S, t, rt, scr, rscr, M, shape_ap=None):
        P = self.P
        P.op("dve", lambda e: e.tensor_scalar(out=scr, in0=t, scalar1=1.0 / M, scalar2=MAGIC, op0=ALU.mult, op1=ALU.add), reads=[rt], writes=[rscr])
        P.op("dve", lambda e: e.tensor_scalar(out=scr, in0=scr, scalar1=-MAGIC, scalar2=None, op0=ALU.add), reads=[rscr], writes=[rscr])
        P.op("dve", lambda e: e.scalar_tensor_tensor(out=t, in0=scr, scalar=-float(M), in1=t, op0=ALU.mult, op1=ALU.add), reads=[rscr, rt], writes=[rt])

    def hy_dft_gen(self, Ls):
        P = self.P
        nt = Ls // 128
        N = 2 * Ls
        nm = "L%d" % Ls
        for k in ("CF", "SF", "CI", "SI"):
            self.dram(k + nm, [nt, 128, nt, 128], BF16, "Internal")
        with Stage(self) as S:
            q, rq = S.sb("q", [128, Ls], F32)
            x1, rx1 = S.sb("x1", [128, Ls], F32)
            scr, rscr = S.sb("scr", [128, Ls], F32)
            m, rm = S.sb("m", [128, Ls], F32)
            m2, rm2 = S.sb("m2", [128, Ls], F32)
            pc2, rpc2 = S.sb("pc2", [128, 1], F32)
            outp = S.pool("dfto", 2, [128, Ls], BF16)
            P.op("dve", lambda e: e.tensor_scalar(out=pc2[:], in0=self.vcol("pidx"), scalar1=2.0, scalar2=1.0, op0=ALU.mult, op1=ALU.add), reads=[self.rvp], writes=[rpc2])
            ptr = S.pool("dftptr", 2, [128, 512], BF16, psum=True)
            sI_p = S.pool("dftsI", 2, [128, nt, 128], BF16)
            P.op("pool", lambda e: e.iota(q[:], pattern=[[2, Ls]], base=1, channel_multiplier=0, allow_small_or_imprecise_dtypes=True), writes=[rq])
            M1, mult1, pcol = (2 * N) // 128, 128.0, self.vcol("pidx")
            for a in range(nt):
                P.op("dve", (lambda a=a: lambda e: e.tensor_scalar(out=x1[:], in0=q[:], scalar1=float(a), scalar2=None, op0=ALU.mult))(), reads=[rq], writes=[rx1])
                self.mod_reduce(S, x1[:], rx1, scr[:], rscr, M1)
                P.op("dve", lambda e: e.tensor_scalar(out=x1[:], in0=x1[:], scalar1=mult1, scalar2=None, op0=ALU.mult), reads=[rx1], writes=[rx1])
                P.op("dve", lambda e: e.scalar_tensor_tensor(out=m[:], in0=q[:], scalar=pcol, in1=x1[:], op0=ALU.mult, op1=ALU.add), reads=[rq, rx1, self.rvp], writes=[rm])
                P.op("pool", lambda e: e.tensor_scalar(out=m2[:], in0=m[:], scalar1=float(N // 2), scalar2=None, op0=ALU.add), reads=[rm], writes=[rm2])
                self.mod_reduce(S, m[:], rm, scr[:], rscr, 2 * N)
                self.mod_reduce(S, m2[:], rm2, scr[:], rscr, 2 * N)
                for (src, rsrc, cs) in ((m2, rm2, "C"), (m, rm, "S")):
                    ot, rot = outp.get()
                    P.op("act", (lambda ot=ot, src=src: lambda e: e.activation(out=ot[:], in_=src[:], func=AF.Sin, scale=3.1415925 / N))(), reads=[rsrc], writes=[rot])
                    P.dma("sp", (lambda ot=ot, cs=cs, a=a: lambda e: e.dma_start(out=self.dr[cs + "F" + nm][:, :, a, :].rearrange("c p j -> p c j"), in_=ot[:].rearrange("p (c j) -> p c j", j=128)))(), reads=[rot], writes=[self.drreg[cs + "F" + nm]])
                    sI, rsI = sI_p.get()
                    for c0 in range(0, nt, 4):
                        nb = min(4, nt - c0)
                        pt_, rpt_ = ptr.get()
                        for j in range(nb):
                            P.op("pe", (lambda pt_=pt_, ot=ot, c0=c0, j=j: lambda e: e.transpose(pt_[:, j * 128:(j + 1) * 128], ot[:, (c0 + j) * 128:(c0 + j + 1) * 128], self.ident_bf[:]))(), reads=[rot, self.rident_bf], writes=[rpt_])
                        P.op("act", (lambda pt_=pt_, sI=sI, c0=c0, nb=nb: lambda e: e.activation(out=sI[:, c0:c0 + nb, :], in_=pt_[:, :nb * 128].rearrange("p (a b) -> p a b", b=128), func=AF.Copy, scale=2.0 / N))(), reads=[rpt_], writes=[rsI])
                    P.dma("sp", (lambda sI=sI, cs=cs, a=a: lambda e: e.dma_start(out=self.dr[cs + "I" + nm][a], in_=sI[:]))(), reads=[rsI], writes=[self.drreg[cs + "I" + nm]])

    def hy_prep(self):
        P = self.P
        self.dram("utm", [3, 34, 128, 1024], BF16, "Internal")
        self.dram("z1", [34, 128, 1024], BF16, "Internal")
        self.dram("ytm", [34, 128, 1024], BF16, "Internal")
        SEG = [(0, NCX), (NCX, L)]
        with Stage(self) as S:
            xin_p = S.pool("hx", 2, [128, T], BF16)
            u, ru = S.sb("hu", [128, T], F32)
            ub, rub = S.sb("hub", [128, T], BF16)
            stg_p = S.pool("hstg", 2, [128, 34, 128], BF16)
            ptr = S.pool("hptr", 2, [128, 512], BF16, psum=True)
            for ch in range(24):
                xin, rxin = xin_p.get()
                P.dma("sp", (lambda xin=xin, ch=ch: lambda e: e.dma_start(out=xin[:], in_=self.dr["pt"][C_HY // 128 + ch]))(), reads=[self.drreg["pt"]], writes=[rxin])
                for (s0, ls) in SEG:
                    P.op("dve", (lambda xin=xin, s0=s0, ls=ls, ch=ch: lambda e: e.tensor_scalar(out=u[:, s0:s0 + ls], in0=xin[:, s0:s0 + ls], scalar1=self.vcol("hy_cw", 1 * 24 + ch), scalar2=self.vcol("hy_cb", ch), op0=ALU.mult, op1=ALU.add))(), reads=[rxin, self.rvp], writes=[ru])
                    for j in (0, 2):
                        d = j - 1
                        lo, hi = max(0, -d), ls - max(0, d)
                        eng = "dve"
                        P.op(eng, (lambda xin=xin, s0=s0, lo=lo, hi=hi, d=d, j=j, ch=ch: lambda e: e.scalar_tensor_tensor(out=u[:, s0 + lo:s0 + hi], in0=xin[:, s0 + lo + d:s0 + hi + d], scalar=self.vcol("hy_cw", j * 24 + ch), in1=u[:, s0 + lo:s0 + hi], op0=ALU.mult, op1=ALU.add))(), reads=[rxin, ru, self.rvp], writes=[ru])
                P.op("act", lambda e: e.copy(out=ub[:], in_=u[:]), reads=[ru], writes=[rub])
                stg, rstg = stg_p.get()
                for b0 in range(0, 34, 4):
                    nb = min(4, 34 - b0)
                    pt_, rpt_ = ptr.get()
                    for j in range(nb):
                        P.op("pe", (lambda pt_=pt_, b0=b0, j=j: lambda e: e.transpose(pt_[:, j * 128:(j + 1) * 128], ub[:, (b0 + j) * 128:(b0 + j + 1) * 128], self.ident_bf[:]))(), reads=[rub, self.rident_bf], writes=[rpt_])
                    eng = "act" if (b0 // 4) % 2 == 0 else "dve"
                    if eng == "act":
                        P.op("act", (lambda pt_=pt_, stg=stg, b0=b0, nb=nb: lambda e: e.copy(out=stg[:, b0:b0 + nb, :], in_=pt_[:, :nb * 128].rearrange("p (a b) -> p a b", b=128)))(), reads=[rpt_], writes=[rstg])
                    else:
                        P.op("dve", (lambda pt_=pt_, stg=stg, b0=b0, nb=nb: lambda e: e.tensor_copy(out=stg[:, b0:b0 + nb, :], in_=pt_[:, :nb * 128].rearrange("p (a b) -> p a b", b=128)))(), reads=[rpt_], writes=[rstg])
                wsel, cc = ch // 8, ch % 8
                P.dma("sp", (lambda stg=stg, wsel=wsel, cc=cc: lambda e: e.dma_start(out=self.dr["utm"][wsel][:, :, cc * 128:(cc + 1) * 128].rearrange("b p c -> p b c"), in_=stg[:]))(), reads=[rstg], writes=[self.drreg["utm"]])

    def hy_filters(self, Ls):
        P = self.P
        nt = Ls // 128
        nm = "L%d" % Ls
        self.dram("HS" + nm, [nt, 128, 2048], BF16, "Internal")
        self.dram("HD" + nm, [nt, 128, 2048], BF16, "Internal")
        self.dram("RN" + nm, [128, 2048], F32, "Internal")
        with Stage(self) as S:
            w1, rw1 = S.sb("hw1", [33, 64], F32)
            w2, rw2 = S.sb("hw2", [64, 64], F32)
            w3, rw3 = S.sb("hw3", [64, 4096], F32)
            dec, rdec = S.sb("hdec", [1, 2048], F32)
            P.dma("sp", lambda e: e.dma_start(out=w1[:], in_=self.dr["hy_w1"]), reads=[self.drreg["hy_w1"]], writes=[rw1])
            P.dma("sp", lambda e: e.dma_start(out=w2[:], in_=self.dr["hy_w2"]), reads=[self.drreg["hy_w2"]], writes=[rw2])
            P.dma("sp", lambda e: e.dma_start(out=w3[:], in_=self.dr["hy_w3"]), reads=[self.drreg["hy_w3"]], writes=[rw3])
            P.dma("sp", lambda e: e.dma_start(out=dec[:], in_=self.dr["hy_decay"]), reads=[self.drreg["hy_decay"]], writes=[rdec])
            P.op("act", lambda e: e.activation(out=dec[:], in_=dec[:], func=AF.Abs), reads=[rdec], writes=[rdec])
            nv, rnv = S.sb("hnv", [64, Ls], F32)
            z, rz = S.sb("hz", [64, Ls], F32)
            h1, rh1 = S.sb("hh1", [64, Ls], F32)
            h2, rh2 = S.sb("hh2", [64, Ls], F32)
            tv, rtv = S.sb("htv", [1, Ls], F32)
            P.op("pool", lambda e: e.iota(nv[:], pattern=[[1, Ls]], base=0, channel_multiplier=0, allow_small_or_imprecise_dtypes=True), writes=[rnv])
            P.op("dve", lambda e: e.tensor_scalar(out=h1[0:33, :], in0=nv[0:33, :], scalar1=2 * PI / Ls, scalar2=self.vp[0:33, VP["bands"][0]:VP["bands"][0] + 1], op0=ALU.mult, op1=ALU.mult), reads=[rnv, self.rvp], writes=[rh1])
            P.op("dve", lambda e: e.tensor_scalar(out=h1[0:33, :], in0=h1[0:33, :], scalar1=self.vp[0:33, VP["phase"][0]:VP["phase"][0] + 1], scalar2=None, op0=ALU.add), reads=[rh1, self.rvp], writes=[rh1])
            with Stage(self) as S2:
                self.range_sin(S2, z[0:33, :], rz, h1[0:33, :], rh1, [33, Ls], 0.0, whole=False)
            P.op("dve", lambda e: e.tensor_scalar(out=z[0:1, :], in0=nv[0:1, :], scalar1=1.0 / (Ls - 1), scalar2=None, op0=ALU.mult), reads=[rnv, rz], writes=[rz])
            P.op("dve", lambda e: e.tensor_scalar(out=tv[:], in0=nv[0:1, :], scalar1=1.0 / (Ls - 1), scalar2=None, op0=ALU.mult), reads=[rnv], writes=[rtv])
            psp = S.pool("hfps", 2, [128, 512], F32, psum=True)
            for (wt, rwt, kk, src, rsrc, dst, rdst, bname) in ((w1, rw1, 33, z, rz, h1, rh1, "hy_b1"), (w2, rw2, 64, h1, rh1, h2, rh2, "hy_b2")):
                pre, rpre = S.sb("hpre", [64, Ls], F32)
                for c0 in range(0, Ls, 512):
                    csz = min(512, Ls - c0)
                    ps, rps = psp.get()
                    P.op("pe", (lambda ps=ps, wt=wt, kk=kk, src=src, c0=c0, csz=csz: lambda e: e.matmul(ps[0:64, :csz], lhsT=wt[0:kk, :], rhs=src[0:kk, c0:c0 + csz], start=True, stop=True))(), reads=[rwt, rsrc], writes=[rps])
                    P.op("dve", (lambda ps=ps, pre=pre, c0=c0, bname=bname, csz=csz: lambda e: e.tensor_scalar(out=pre[:, c0:c0 + csz], in0=ps[0:64, :csz], scalar1=self.vp[0:64, VP[bname][0]:VP[bname][0] + 1], scalar2=self.vp[0:64, VP["hy_freq"][0]:VP["hy_freq"][0] + 1], op0=ALU.add, op1=ALU.mult))(), reads=[rps, self.rvp], writes=[rpre])
                with Stage(self) as S2:
                    self.range_sin(S2, dst[:, :], rdst, pre[:, :], rpre, [64, Ls], 0.0, whole=False)
            psn = [S.ps("hpsn", [128, 512], F32) for _ in range(4)]
            psw, rpsw = S.ps("hpsw", [128, 512], F32)
            winp = S.pool("hwin", 2, [128, 512], F32)
            fp_ = S.pool("hf", 4, [128, 512], F32)
            abp = S.pool("hab", 2, [128, 512], BF16)
            hsp = S.pool("hhs", 2, [128, 512], BF16)
            hdp = S.pool("hhd", 2, [128, 512], BF16)
            for a in range(nt):
                for o in range(2):
                    for ct in range(2):
                        P.op("pe", (lambda a=a, o=o, ct=ct: lambda e: e.matmul(psw[:], lhsT=tv[0:1, a * 128:(a + 1) * 128], rhs=dec[0:1, o * 1024 + ct * 512:o * 1024 + ct * 512 + 512], start=True, stop=True))(), reads=[rtv, rdec], writes=[rpsw])
                        win, rwin = winp.get()
                        P.op("act", (lambda win=win: lambda e: e.activation(out=win[:], in_=psw[:], func=AF.Exp, scale=-1.0))(), reads=[rpsw], writes=[rwin])
                        fs = []
                        for d in range(2):
                            ps, rps = psp.get()
                            col = o * 2048 + d * 1024 + ct * 512
                            P.op("pe", (lambda ps=ps, a=a, col=col: lambda e: e.matmul(ps[:], lhsT=h2[:, a * 128:(a + 1) * 128], rhs=w3[:, col:col + 512], start=True, stop=True))(), reads=[rh2, rw3], writes=[rps])
                            f, rf = fp_.get()
                            P.op("dve", (lambda f=f, ps=ps, win=win: lambda e: e.tensor_tensor(out=f[:], in0=ps[:], in1=win[:], op=ALU.mult))(), reads=[rps, rwin], writes=[rf])
                            ab, rab = abp.get()
                            P.op("dve", (lambda ab=ab, f=f: lambda e: e.scalar_tensor_tensor(out=ab[:], in0=f[:], scalar=-1.0, in1=f[:], op0=ALU.mult, op1=ALU.max))(), reads=[rf], writes=[rab])
                            pn, rpn = psn[o * 2 + ct]
                            P.op("pe", (lambda pn=pn, ab=ab, a=a, d=d: lambda e: e.matmul(pn[:], lhsT=self.ones_bf[:], rhs=ab[:], start=(a == 0 and d == 0), stop=(a == nt - 1 and d == 1)))(), reads=[self.rones, rab], writes=[rpn])
                            fs.append((f, rf))
                        (f0, rf0), (f1, rf1) = fs
                        hs, rhs_ = hsp.get()
                        hd, rhd = hdp.get()
                        P.op("pool", (lambda hs=hs, f0=f0, f1=f1: lambda e: e.tensor_tensor(out=hs[:], in0=f0[:], in1=f1[:], op=ALU.add))(), reads=[rf0, rf1], writes=[rhs_])
                        P.op("pool", (lambda hd=hd, f0=f0, f1=f1: lambda e: e.tensor_tensor(out=hd[:], in0=f0[:], in1=f1[:], op=ALU.subtract))(), reads=[rf0, rf1], writes=[rhd])
                        c2 = o * 1024 + ct * 512
                        P.dma("sp", (lambda hs=hs, a=a, c2=c2: lambda e: e.dma_start(out=self.dr["HS" + nm][a, :, c2:c2 + 512], in_=hs[:]))(), reads=[rhs_], writes=[self.drreg["HS" + nm]])
                        P.dma("sp", (lambda hd=hd, a=a, c2=c2: lambda e: e.dma_start(out=self.dr["HD" + nm][a, :, c2:c2 + 512], in_=hd[:]))(), reads=[rhd], writes=[self.drreg["HD" + nm]])
            rn, rrn = S.sb("hrn", [128, 2048], F32)
            for i in range(4):
                pn, rpn = psn[i]
                P.op("dve", (lambda pn=pn, i=i: lambda e: e.tensor_scalar(out=rn[:, i * 512:(i + 1) * 512], in0=pn[:], scalar1=EPS, scalar2=None, op0=ALU.add))(), reads=[rpn], writes=[rrn])
            P.op("dve", lambda e: e.reciprocal(out=rn[:], in_=rn[:]), reads=[rrn], writes=[rrn])
            P.dma("sp", lambda e: e.dma_start(out=self.dr["RN" + nm], in_=rn[:]), reads=[rrn], writes=[self.drreg["RN" + nm]])

    def hy_gemm(self, S, wnames, nm, xs, n_k, evac, nchunks):
        P = self.P
        wps = [S.pool("hgw%d" % i, 2, [128, n_k, 128], BF16) for i in range(len(wnames))]
        pps = [S.pool("hgp%d" % i, 2, [128, 512], F32, psum=True) for i in range(len(set(g for (_, g) in wnames)))]
        def load(oc):
            wts = []
            for i, (wn, g) in enumerate(wnames):
                wt, rwt = wps[i].get()
                P.dma("sp" if i % 2 == 0 else "act", (lambda wt=wt, wn=wn, oc=oc: lambda e: e.dma_start(out=wt[:], in_=self.dr[wn + nm][oc]))(), reads=[self.drreg[wn + nm]], writes=[rwt])
                wts.append((wt, rwt))
            return wts

        nxt = load(0)
        for oc in range(nchunks):
            wts = nxt
            if oc + 1 < nchunks:
                nxt = load(oc + 1)
            outs = {}
            groups = sorted(set(g for (_, g) in wnames))
            for g in groups:
                outs[g] = pps[g].get()
            cntg = {g: 0 for g in groups}
            totg = {g: sum(1 for (_, gg) in wnames if gg == g) * n_k for g in groups}
            for i, (wn, g) in enumerate(wnames):
                wt, rwt = wts[i]
                x, rx = xs[i]
                ps, rps = outs[g]
                for a in range(n_k):
                    first = cntg[g] == 0
                    cntg[g] += 1
                    last = cntg[g] == totg[g]
                    P.op("pe", (lambda ps=ps, wt=wt, x=x, a=a, first=first, last=last: lambda e: e.matmul(ps[:], lhsT=wt[:, a, :], rhs=x[:, a, :], start=first, stop=last))(), reads=[rwt, rx], writes=[rps])
            evac(oc, outs)

    def hy_spectra(self, Ls):
        P = self.P
        nt = Ls // 128
        nm = "L%d" % Ls
        self.dram("GR" + nm, [nt, 128, 2048], BF16, "Internal")
        self.dram("GQ" + nm, [nt, 128, 2048], BF16, "Internal")
        with Stage(self) as S:
            rn, rrn = S.sb("srn", [128, 2048], F32)
            P.dma("sp", lambda e: e.dma_start(out=rn[:], in_=self.dr["RN" + nm]), reads=[self.drreg["RN" + nm]], writes=[rrn])
            hs, rhs_ = S.sb("shs", [128, nt, 512], BF16)
            hd, rhd = S.sb("shd", [128, nt, 512], BF16)
            gop = S.pool("sgo", 4, [128, 512], BF16)
            for ct in range(4):
                P.dma("sp", (lambda ct=ct: lambda e: e.dma_start(out=hs[:], in_=self.dr["HS" + nm][:, :, ct * 512:(ct + 1) * 512].rearrange("a p c -> p a c")))(), reads=[self.drreg["HS" + nm]], writes=[rhs_])
                P.dma("sp", (lambda ct=ct: lambda e: e.dma_start(out=hd[:], in_=self.dr["HD" + nm][:, :, ct * 512:(ct + 1) * 512].rearrange("a p c -> p a c")))(), reads=[self.drreg["HD" + nm]], writes=[rhd])

                def evac(fc, outs, ct=ct):
                    for g, name in ((0, "GR"), (1, "GQ")):
                        ps, rps = outs[g]
                        go, rgo = gop.get()
                        P.op("dve", (lambda go=go, ps=ps: lambda e: e.tensor_tensor(out=go[:], in0=ps[:], in1=rn[:, ct * 512:(ct + 1) * 512], op=ALU.mult))(), reads=[rps, rrn], writes=[rgo])
                        P.dma("sp", (lambda go=go, name=name, fc=fc: lambda e: e.dma_start(out=self.dr[name + nm][fc, :, ct * 512:(ct + 1) * 512], in_=go[:]))(), reads=[rgo], writes=[self.drreg[name + nm]])

                with Stage(self) as S2:
                    self.hy_gemm(S2, [("CF", 0), ("SF", 1)], nm, [(hs, rhs_), (hd, rhd)], nt, evac, nt)

    def hy_conv(self, Ls, blk0):
        P = self.P
        nt = Ls // 128
        nm = "L%d" % Ls
        with Stage(self) as S:
            u, ru = S.sb("cu", [128, nt, 512], BF16)
            Yr, rYr = S.sb("cYr", [128, nt, 512], BF16)
            Yq, rYq = S.sb("cYq", [128, nt, 512], BF16)
            skb, rskb = S.sb("cskb", [128, 512], F32)
            grp = S.pool("cgr", 2, [128, 512], BF16)
            gqp = S.pool("cgq", 2, [128, 512], BF16)
            ap_ = S.pool("cA", 2, [128, 512], F32)
            bp_ = S.pool("cB", 2, [128, 512], F32)
            t1p = S.pool("ct1", 2, [128, 512], F32)
            t2p = S.pool("ct2", 2, [128, 512], F32)
            xgp = S.pool("cxg", 2, [128, 512], BF16)
            zop = S.pool("czo", 2, [128, 512], BF16)
            for o in range(2):
                for ct in range(2):
                    cs = slice(ct * 512, (ct + 1) * 512)
                    src = self.dr["utm"][0] if o == 0 else self.dr["z1"]
                    rsrc = self.drreg["utm"] if o == 0 else self.drreg["z1"]
                    P.dma("sp", (lambda src=src, cs=cs: lambda e: e.dma_start(out=u[:], in_=src[blk0:blk0 + nt, :, cs].rearrange("a p c -> p a c")))(), reads=[rsrc], writes=[ru])
                    sk0 = o * 1024 + ct * 512
                    P.dma("sp", (lambda sk0=sk0: lambda e: e.dma_start(out=skb[:], in_=bcast_rows(self.dr["hy_skip"][0:1, sk0:sk0 + 512], 512)))(), reads=[self.drreg["hy_skip"]], writes=[rskb])

                    def evac_f(fc, outs, o=o, ct=ct):
                        pa, rpa = outs[0]
                        pb, rpb = outs[1]
                        gr, rgr = grp.get()
                        gq, rgq = gqp.get()
                        gc0 = o * 1024 + ct * 512
                        P.dma("sp", (lambda gr=gr, fc=fc, gc0=gc0: lambda e: e.dma_start(out=gr[:], in_=self.dr["GR" + nm][fc, :, gc0:gc0 + 512]))(), reads=[self.drreg["GR" + nm]], writes=[rgr])
                        P.dma("sp", (lambda gq=gq, fc=fc, gc0=gc0: lambda e: e.dma_start(out=gq[:], in_=self.dr["GQ" + nm][fc, :, gc0:gc0 + 512]))(), reads=[self.drreg["GQ" + nm]], writes=[rgq])
                        A, rA = ap_.get()
                        Bq, rBq = bp_.get()
                        P.op("act", (lambda A=A, pa=pa: lambda e: e.copy(out=A[:], in_=pa[:]))(), reads=[rpa], writes=[rA])
                        P.op("act", (lambda Bq=Bq, pb=pb: lambda e: e.copy(out=Bq[:], in_=pb[:]))(), reads=[rpb], writes=[rBq])
                        t1, rt1 = t1p.get()
                        t2, rt2 = t2p.get()
                        P.op("dve", (lambda t1=t1, A=A, gr=gr: lambda e: e.tensor_tensor(out=t1[:], in0=A[:], in1=gr[:], op=ALU.mult))(), reads=[rA, rgr], writes=[rt1])
                        P.op("pool", (lambda t2=t2, Bq=Bq, gq=gq: lambda e: e.tensor_tensor(out=t2[:], in0=Bq[:], in1=gq[:], op=ALU.mult))(), reads=[rBq, rgq], writes=[rt2])
                        P.op("dve", (lambda t1=t1, t2=t2, fc=fc: lambda e: e.tensor_tensor(out=Yr[:, fc, :], in0=t1[:], in1=t2[:], op=ALU.subtract))(), reads=[rt1, rt2], writes=[rYr])
                        t3, rt3 = t1p.get()
                        t4, rt4 = t2p.get()
                        P.op("dve", (lambda t3=t3, A=A, gq=gq: lambda e: e.tensor_tensor(out=t3[:], in0=A[:], in1=gq[:], op=ALU.mult))(), reads=[rA, rgq], writes=[rt3])
                        P.op("pool", (lambda t4=t4, Bq=Bq, gr=gr: lambda e: e.tensor_tensor(out=t4[:], in0=Bq[:], in1=gr[:], op=ALU.mult))(), reads=[rBq, rgr], writes=[rt4])
                        P.op("pool", (lambda t3=t3, t4=t4, fc=fc: lambda e: e.tensor_tensor(out=Yq[:, fc, :], in0=t3[:], in1=t4[:], op=ALU.add))(), reads=[rt3, rt4], writes=[rYq])

                    with Stage(self) as S2:
                        self.hy_gemm(S2, [("CF", 0), ("SF", 1)], nm, [(u, ru), (u, ru)], nt, evac_f, nt)

                    def evac_i(tc, outs, o=o, ct=ct, cs=cs):
                        py, rpy = outs[0]
                        xg, rxg = xgp.get()
                        P.dma("sp", (lambda xg=xg, tc=tc: lambda e: e.dma_start(out=xg[:], in_=self.dr["utm"][1 + o][blk0 + tc, :, cs]))(), reads=[self.drreg["utm"]], writes=[rxg])
                        t1, rt1 = t1p.get()
                        P.op("pool", (lambda t1=t1, tc=tc: lambda e: e.tensor_tensor(out=t1[:], in0=u[:, tc, :], in1=skb[:], op=ALU.mult))(), reads=[ru, rskb], writes=[rt1])
                        P.op("dve", (lambda t1=t1, py=py: lambda e: e.tensor_tensor(out=t1[:], in0=py[:], in1=t1[:], op=ALU.add))(), reads=[rpy, rt1], writes=[rt1])
                        zo, rzo = zop.get()
                        P.op("dve", (lambda zo=zo, t1=t1, xg=xg: lambda e: e.tensor_tensor(out=zo[:], in0=t1[:], in1=xg[:], op=ALU.mult))(), reads=[rt1, rxg], writes=[rzo])
                        dst = "z1" if o == 0 else "ytm"
                        P.dma("sp", (lambda zo=zo, dst=dst, tc=tc: lambda e: e.dma_start(out=self.dr[dst][blk0 + tc, :, cs], in_=zo[:]))(), reads=[rzo], writes=[self.drreg[dst]])

                    with Stage(self) as S2:
                        self.hy_gemm(S2, [("CI", 0), ("SI", 0)], nm, [(Yr, rYr), (Yq, rYq)], nt, evac_i, nt)

    def hy_out(self):
        P = self.P
        with Stage(self) as S:
            yin_p = S.pool("yin", 2, [128, 1024], BF16)
            ptr = S.pool("yptr", 2, [128, 1024], BF16, psum=True)
            stg_p = S.pool("ystg", 2, [128, 8, 128], BF16)
            for blk in range(34):
                yin, ryin = yin_p.get()
                P.dma("sp", (lambda yin=yin, blk=blk: lambda e: e.dma_start(out=yin[:], in_=self.dr["ytm"][blk]))(), reads=[self.drreg["ytm"]], writes=[ryin])
                pt_, rpt_ = ptr.get()
                for c in range(8):
                    P.op("pe", (lambda pt_=pt_, yin=yin, c=c: lambda e: e.transpose(pt_[:, c * 128:(c + 1) * 128], yin[:, c * 128:(c + 1) * 128], self.ident_bf[:]))(), reads=[ryin, self.rident_bf], writes=[rpt_])
                stg, rstg = stg_p.get()
                P.op("act" if blk % 2 == 0 else "dve", (lambda pt_=pt_, stg=stg, blk=blk: (lambda e: e.copy(out=stg[:], in_=pt_[:].rearrange("p (a b) -> p a b", b=128))) if blk % 2 == 0 else (lambda e: e.tensor_copy(out=stg[:], in_=pt_[:].rearrange("p (a b) -> p a b", b=128))))(), reads=[rpt_], writes=[rstg])
                P.dma("sp", (lambda stg=stg, blk=blk: lambda e: e.dma_start(out=self.dr["yt"][:, :, blk * 128:(blk + 1) * 128].rearrange("c p n -> p c n"), in_=stg[:]))(), reads=[rstg], writes=[self.drreg["yt"]])

    def stage_hyena(self):
        self.hy_prep()
        for Ls, blk0 in ((NCX, 0), (L, 2)):
            if Ls not in self.dft_done:
                self.hy_dft_gen(Ls)
                self.dft_done.add(Ls)
            self.hy_filters(Ls)
            self.hy_spectra(Ls)
            self.hy_conv(Ls, blk0)
        self.hy_out()

    def build(self, stages=None):
        on = lambda s_: stages is None or s_ in stages
        for idx in range(len(self.layers)):
            self.set_layer(idx)
            with Stage(self) as S0:
                self.load_consts(S0)
                self.mod_vectors(S0)
                if on("inproj"):
                    self.stage_inproj()
                if on("attn"):
                    with Stage(self) as SA:
                        self.rope_tables(SA)
                        self.stage_attn()
                if on("lru"):
                    self.stage_lru()
                if on("hyena"):
                    self.stage_hyena()
                if on("merge"):
                    self.stage_merge()
                if on("ffn"):
                    self.stage_ffn()
                if idx == len(self.layers) - 1 and on("final"):
                    self.stage_final()
                self.P.flush()
        return self.nc


def make_xt(inp, b):
    tok = np.concatenate([inp["ctx"][b], inp["x"][b]], axis=0)
    return np.ascontiguousarray(tok.T.reshape(KC, 128, T).transpose(1, 0, 2))


def layer_inputs(inp, li, b):
    j = li // 2
    m = {"vp": make_vp(inp, li, b), "w_mod": inp["w_mod"][li], "w_in": inp["w_in"][li],
         "w_br": inp["w_br"][li], "w_out": inp["w_out"][li], "lru_wa": inp["lru_wa"][li], "lru_wi": inp["lru_wi"][li],
         "hy_w1": inp["hy_f_w1"][li], "hy_w2": inp["hy_f_w2"][li], "hy_w3": inp["hy_f_w3"][li],
         "hy_decay": inp["hy_decay"][li].reshape(1, 2048), "hy_skip": inp["hy_skip"][li].reshape(1, 2048),
         "da_lam": inp["da_lambda"][li].reshape(1, 256)}
    if li % 2 == 0:
        m["f_wg"] = inp["ffn_w_gate"][j][None]
        m["f_wu"] = inp["ffn_w_up"][j][None]
        m["f_wd"] = inp["ffn_w_down"][j][None]
    else:
        m["f_wg"] = inp["moe_w_gate"][j]
        m["f_wu"] = inp["moe_w_up"][j]
        m["f_wd"] = inp["moe_w_down"][j]
        m["router"] = inp["moe_router"][j]
    return {k + "_L%d" % li: np.ascontiguousarray(np.asarray(v, np.float32)) for k, v in m.items()}


def kernel(**inputs):
    inp = {k: np.asarray(v) for k, v in inputs.items()}
    nb = inp["x"].shape[0]
    layers = (0, 1, 2, 3)
    bld = Builder(layers)
    nc = bld.build()
    shared = {}
    in_maps = []
    for b in range(nb):
        m = {"x_ext": make_xt(inp, b)}
        for li in layers:
            li_in = layer_inputs(inp, li, b)
            for k, v in li_in.items():
                if k.startswith("vp"):
                    m[k] = v
                else:
                    m[k] = shared.setdefault(k, v)
        in_maps.append(m)
    res = run_bass_kernel_spmd(nc, in_maps, core_ids=list(range(nb)))
    out = np.empty((nb, L, D), np.float32)
    for b in range(nb):
        y = np.asarray(res.results[b]["y_out"], np.float32)
        out[b] = y.transpose(1, 0, 2).reshape(D, L).T
    return out
```
